# Optimizing a Trainium2 kernel written in Bass

```python
import math
import jax, jax.numpy as jnp
from jax import lax
import numpy as np

D_MODEL = 1024
BATCH = 16
SEQ = 2048
DEPTH = 4

CHUNK = 64
Q_BLOCK = 128
MLA_WIDTH = D_MODEL // 2
MLA_V_DIM = 64
MLA_HEADS = MLA_WIDTH // MLA_V_DIM
MLA_QK_NOPE = 64
MLA_QK_ROPE = 32
MLA_Q_RANK = D_MODEL // 4
MLA_KV_RANK = D_MODEL // 8
ROPE_THETA = 10000.0
GDN_WIDTH = D_MODEL // 4
GDN_HEAD_DIM = 64
GDN_HEADS = GDN_WIDTH // GDN_HEAD_DIM
GDN_CONV = 4
POOL_WIDTH = D_MODEL - MLA_WIDTH - GDN_WIDTH
POOL_WINDOWS = (2, 4, 8, 16)
POOL_GROUPS = 4
POOL_GROUP_DIM = POOL_WIDTH // POOL_GROUPS
MIX_WIDTH = MLA_WIDTH + GDN_WIDTH + POOL_WIDTH
IN_SIZES = (MLA_Q_RANK, MLA_KV_RANK, MLA_QK_ROPE, GDN_WIDTH, GDN_WIDTH, GDN_WIDTH, GDN_WIDTH, GDN_HEADS, GDN_HEADS, POOL_WIDTH)
IN_PROJ_DIM = sum(IN_SIZES)
N_GROUPS = 4
EXPERTS_PER_GROUP = 8
N_EXPERTS = N_GROUPS * EXPERTS_PER_GROUP
TOP_K = 2
EXPERT_FF = D_MODEL // 4
MOE_BLOCK = 128
DEEPNORM_ALPHA = (2 * DEPTH) ** 0.25
DEEPNORM_BETA = (8 * DEPTH) ** -0.25
LN_EPS = 1e-5
RMS_EPS = 1e-6

kernel_name = 'hybrid_mla_gdn_pool_hmoe_deepnorm'

F32 = jnp.float32


def layer_norm(x, g, b):
    xf = x.astype(F32)
    mu = jnp.mean(xf, axis=-1, keepdims=True)
    var = jnp.mean(jnp.square(xf - mu), axis=-1, keepdims=True)
    return ((xf - mu) * lax.rsqrt(var + LN_EPS)).astype(x.dtype) * g + b


def rms_norm(x, g):
    xf = x.astype(F32)
    return (xf * lax.rsqrt(jnp.mean(xf * xf, axis=-1, keepdims=True) + RMS_EPS)).astype(x.dtype) * g


def l2_norm(x):
    xf = x.astype(F32)
    return xf * lax.rsqrt(jnp.sum(xf * xf, axis=-1, keepdims=True) + RMS_EPS)


def split_sizes(x, sizes):
    offs, acc = [], 0
    for s in sizes[:-1]:
        acc += s
        offs.append(acc)
    return jnp.split(x, offs, axis=-1)


def rope_tables(seq):
    inv_freq = jnp.power(ROPE_THETA, -jnp.arange(0, MLA_QK_ROPE, 2, dtype=F32) / MLA_QK_ROPE)
    ang = jnp.arange(seq, dtype=F32)[:, None] * inv_freq[None, :]
    return jnp.cos(ang), jnp.sin(ang)


def apply_rope(x, cos, sin):
    xf = x.astype(F32)
    x1, x2 = jnp.split(xf, 2, axis=-1)
    return jnp.concatenate([x1 * cos - x2 * sin, x2 * cos + x1 * sin], axis=-1).astype(x.dtype)


def mla_attention(c_q, c_kv, k_rope, q_norm_g, kv_norm_g, w_uq, w_ukv, cos, sin):
    B, S, _ = c_q.shape
    q = (rms_norm(c_q, q_norm_g) @ w_uq).reshape(B, S, MLA_HEADS, MLA_QK_NOPE + MLA_QK_ROPE)
    q_nope, q_rope = q[..., :MLA_QK_NOPE], q[..., MLA_QK_NOPE:]
    q_rope = apply_rope(q_rope, cos[:, None, :], sin[:, None, :])
    kv = (rms_norm(c_kv, kv_norm_g) @ w_ukv).reshape(B, S, MLA_HEADS, MLA_QK_NOPE + MLA_V_DIM)
    k_nope, v = kv[..., :MLA_QK_NOPE], kv[..., MLA_QK_NOPE:]
    k_rope = apply_rope(k_rope, cos, sin)
    scale = (MLA_QK_NOPE + MLA_QK_ROPE) ** -0.5
    outs = []
    for qb in range(S // Q_BLOCK):
        q0, kend = qb * Q_BLOCK, (qb + 1) * Q_BLOCK
        s = (jnp.einsum('bqhd,bkhd->bhqk', q_nope[:, q0:kend], k_nope[:, :kend])
             + jnp.einsum('bqhd,bkd->bhqk', q_rope[:, q0:kend], k_rope[:, :kend]))
        s = s.astype(F32) * scale
        q_chunk = jnp.arange(q0, kend) // CHUNK
        k_chunk = jnp.arange(kend) // CHUNK
        allowed = k_chunk[None, :] <= q_chunk[:, None]
        p = jax.nn.softmax(jnp.where(allowed, s, -jnp.inf), axis=-1).astype(v.dtype)
        outs.append(jnp.einsum('bhqk,bkhd->bqhd', p, v[:, :kend]))
    return jnp.concatenate(outs, axis=1).reshape(B, S, MLA_WIDTH)


def causal_depthwise_conv(x, w):
    C = x.shape[-1]
    return lax.conv_general_dilated(x, w[:, None, :].astype(x.dtype), window_strides=(1,),
                                    padding=[(GDN_CONV - 1, 0)],
                                    dimension_numbers=('NWC', 'WIO', 'NWC'),
                                    feature_group_count=C)


def gated_delta_rule(q, k, v, g, beta):
    B, S, H, dk = q.shape
    dv = v.shape[-1]
    nc = S // CHUNK

    def to_chunks(t):
        t = t.astype(F32).reshape((B, nc, CHUNK, H) + t.shape[3:])
        return jnp.moveaxis(t, 3, 1)

    qc = to_chunks(q) * dk ** -0.5
    kc, vc = to_chunks(k), to_chunks(v)
    gc = jnp.cumsum(to_chunks(g), axis=-1)
    bc = to_chunks(beta)
    idx = jnp.arange(CHUNK)
    incl = idx[:, None] >= idx[None, :]
    strict = idx[:, None] > idx[None, :]
    diff = gc[..., :, None] - gc[..., None, :]
    decay = jnp.where(incl, jnp.exp(jnp.where(incl, diff, 0.0)), 0.0)
    kb = kc * bc[..., None]
    a_mat = jnp.where(strict, jnp.einsum('bhnid,bhnjd->bhnij', kb, kc) * decay, 0.0)
    lower = a_mat + jnp.eye(CHUNK, dtype=F32)
    rhs = jnp.concatenate([vc * bc[..., None], kb * jnp.exp(gc)[..., None]], axis=-1)
    sol = lax.linalg.triangular_solve(lower, rhs, left_side=True, lower=True, unit_diagonal=True)
    u, w = sol[..., :dv], sol[..., dv:]
    qk = jnp.where(incl, jnp.einsum('bhnid,bhnjd->bhnij', qc, kc) * decay, 0.0)
    q_dec = qc * jnp.exp(gc)[..., None]
    k_dec = kc * jnp.exp(gc[..., -1:] - gc)[..., None]
    g_last = jnp.exp(gc[..., -1])

    def step(state, inp):
        q_i, k_i, u_i, w_i, qk_i, gl_i = inp
        v_new = u_i - jnp.einsum('bhck,bhkv->bhcv', w_i, state)
        o_i = jnp.einsum('bhck,bhkv->bhcv', q_i, state) + jnp.einsum('bhij,bhjv->bhiv', qk_i, v_new)
        state = state * gl_i[..., None, None] + jnp.einsum('bhck,bhcv->bhkv', k_i, v_new)
        return state, o_i

    xs = tuple(jnp.moveaxis(t, 2, 0) for t in (q_dec, k_dec, u, w, qk, g_last))
    _, o = lax.scan(step, jnp.zeros((B, H, dk, dv), F32), xs)
    o = jnp.moveaxis(o, 0, 2).reshape(B, H, S, dv)
    return jnp.transpose(o, (0, 2, 1, 3))


def gdn_mixer(q, k, v, z, a, b, conv_w, a_log, dt_bias, out_g):
    B, S, _ = q.shape
    qkv = jax.nn.silu(causal_depthwise_conv(jnp.concatenate([q, k, v], axis=-1), conv_w))
    q, k, v = jnp.split(qkv, 3, axis=-1)
    shp = (B, S, GDN_HEADS, GDN_HEAD_DIM)
    q, k, v = l2_norm(q.reshape(shp)), l2_norm(k.reshape(shp)), v.reshape(shp)
    g = -jnp.exp(a_log.astype(F32)) * jax.nn.softplus(a.astype(F32) + dt_bias.astype(F32))
    beta = jax.nn.sigmoid(b.astype(F32))
    o = gated_delta_rule(q, k, v, g, beta).astype(z.dtype)
    o = rms_norm(o, out_g) * jax.nn.silu(z.reshape(shp))
    return o.reshape(B, S, GDN_WIDTH)


def pool_mixer(p, pool_w, pool_scale):
    B, S, _ = p.shape
    pg = p.reshape(B, S, POOL_GROUPS, POOL_GROUP_DIM).astype(F32)
    cs = jnp.cumsum(pg, axis=1)
    cs = jnp.concatenate([jnp.zeros_like(cs[:, :1]), cs], axis=1)
    t = jnp.arange(S)
    means = []
    for gi, win in enumerate(POOL_WINDOWS):
        start = jnp.maximum(t + 1 - win, 0)
        cnt = (t + 1 - start).astype(F32)
        cs_g = cs[:, :, gi, :]
        means.append((cs_g[:, 1:] - cs_g[:, start]) / cnt[None, :, None])
    delta = (jnp.stack(means, axis=2) - pg).astype(p.dtype)
    mixed = jnp.einsum('bsgc,gcd->bsgd', delta, pool_w)
    return mixed.reshape(B, S, POOL_WIDTH) * pool_scale


def hybrid_mixer(h, w_in, q_norm_g, kv_norm_g, w_uq, w_ukv, conv_w, a_log, dt_bias, gdn_g,
                 pool_w, pool_scale, w_out, cos, sin):
    proj = h @ w_in
    c_q, c_kv, k_rope, gq, gk, gv, gz, ga, gb, p = split_sizes(proj, IN_SIZES)
    y_mla = mla_attention(c_q, c_kv, k_rope, q_norm_g, kv_norm_g, w_uq, w_ukv, cos, sin)
    y_gdn = gdn_mixer(gq, gk, gv, gz, ga, gb, conv_w, a_log, dt_bias, gdn_g)
    y_pool = pool_mixer(p, pool_w, pool_scale)
    return jnp.concatenate([y_mla, y_gdn, y_pool], axis=-1) @ w_out


def grouped_expert_ffn(hf, expert, weights, w_gate, w_up, w_down):
    T, D = hf.shape
    A = T * TOP_K
    flat_e = expert.reshape(A)
    flat_tok = jnp.arange(A, dtype=jnp.int32) // TOP_K
    flat_w = weights.reshape(A).astype(hf.dtype)
    order = jnp.argsort(flat_e)
    se = flat_e[order]
    counts = jax.ops.segment_sum(jnp.ones((A,), jnp.int32), flat_e, num_segments=N_EXPERTS)
    padded = (counts + MOE_BLOCK - 1) // MOE_BLOCK * MOE_BLOCK
    pad_end = jnp.cumsum(padded)
    pad_start = pad_end - padded
    start = jnp.cumsum(counts) - counts
    dest = pad_start[se] + (jnp.arange(A, dtype=jnp.int32) - start[se])
    nb = (A + N_EXPERTS * (MOE_BLOCK - 1) + MOE_BLOCK - 1) // MOE_BLOCK
    P = nb * MOE_BLOCK
    buf_tok = jnp.full((P,), T, jnp.int32).at[dest].set(flat_tok[order])
    buf_w = jnp.zeros((P,), hf.dtype).at[dest].set(flat_w[order])
    block_expert = jnp.clip(jnp.searchsorted(pad_end, jnp.arange(nb) * MOE_BLOCK, side='right'), 0, N_EXPERTS - 1)
    xpad = jnp.concatenate([hf, jnp.zeros((1, D), hf.dtype)], axis=0)
    xb = xpad[buf_tok].reshape(nb, MOE_BLOCK, D)

    def block_ffn(args):
        xb_i, e = args
        return (jax.nn.silu(xb_i @ w_gate[e]) * (xb_i @ w_up[e])) @ w_down[e]

    yb = lax.map(block_ffn, (xb, block_expert)).reshape(P, D)
    out = jnp.zeros((T + 1, D), yb.dtype).at[buf_tok].add(yb * buf_w[:, None])
    return out[:T]


def hierarchical_moe(h, w_rg, b_rg, w_re, b_re, w_gate, w_up, w_down):
    B, S, D = h.shape
    hf = h.reshape(B * S, D)
    g_prob = jax.nn.softmax((hf @ w_rg + b_rg).astype(F32), axis=-1)
    g_p, g_sel = lax.top_k(g_prob, 1)
    e_logit = (hf @ w_re + b_re).astype(F32).reshape(-1, N_GROUPS, EXPERTS_PER_GROUP)
    e_logit = jnp.take_along_axis(e_logit, g_sel[:, :, None], axis=1)[:, 0]
    e_p, e_idx = lax.top_k(jax.nn.softmax(e_logit, axis=-1), TOP_K)
    weights = g_p * e_p / jnp.sum(e_p, axis=-1, keepdims=True)
    expert = g_sel * EXPERTS_PER_GROUP + e_idx
    return grouped_expert_ffn(hf, expert, weights, w_gate, w_up, w_down).reshape(B, S, D)


def setup_inputs(seed: int = 0) -> dict:
    key = jax.random.key(seed)
    ks = jax.random.split(key, 32)
    L, D = DEPTH, D_MODEL

    def nrm(k, shape, scale):
        return jax.random.normal(k, shape, F32) * scale

    dt = jnp.exp(jax.random.uniform(ks[9], (L, GDN_HEADS), F32, math.log(1e-3), math.log(1e-1)))
    return {
        'x': nrm(ks[0], (BATCH, SEQ, D), 1.0),
        'c': nrm(ks[1], (BATCH, D), 1.0),
        'w_in': nrm(ks[2], (L, D, IN_PROJ_DIM), D ** -0.5),
        'mla_q_norm': 1.0 + nrm(ks[3], (L, MLA_Q_RANK), 0.05),
        'mla_kv_norm': 1.0 + nrm(ks[4], (L, MLA_KV_RANK), 0.05),
        'mla_w_uq': nrm(ks[5], (L, MLA_Q_RANK, MLA_HEADS * (MLA_QK_NOPE + MLA_QK_ROPE)), MLA_Q_RANK ** -0.5),
        'mla_w_ukv': nrm(ks[6], (L, MLA_KV_RANK, MLA_HEADS * (MLA_QK_NOPE + MLA_V_DIM)), MLA_KV_RANK ** -0.5),
        'gdn_conv': nrm(ks[7], (L, GDN_CONV, 3 * GDN_WIDTH), GDN_CONV ** -0.5),
        'gdn_a_log': jnp.log(jax.random.uniform(ks[8], (L, GDN_HEADS), F32, 1.0, 16.0)),
        'gdn_dt_bias': dt + jnp.log(-jnp.expm1(-dt)),
        'gdn_out_norm': 1.0 + nrm(ks[10], (L, GDN_HEAD_DIM), 0.05),
        'pool_w': nrm(ks[11], (L, POOL_GROUPS, POOL_GROUP_DIM, POOL_GROUP_DIM), POOL_GROUP_DIM ** -0.5),
        'pool_scale': 1.0 + nrm(ks[12], (L, POOL_WIDTH), 0.05),
        'w_out': nrm(ks[13], (L, MIX_WIDTH, D), MIX_WIDTH ** -0.5 * DEEPNORM_BETA),
        'w_mod': nrm(ks[14], (L, D, 6 * D), 0.2 * D ** -0.5),
        'b_mod': nrm(ks[15], (L, 6 * D), 0.02),
        'ln1_g': 1.0 + nrm(ks[16], (L, D), 0.05),
        'ln1_b': nrm(ks[17], (L, D), 0.02),
        'ln2_g': 1.0 + nrm(ks[18], (L, D), 0.05),
        'ln2_b': nrm(ks[19], (L, D), 0.02),
        'router_w_group': nrm(ks[20], (L, D, N_GROUPS), D ** -0.5),
        'router_b_group': nrm(ks[21], (L, N_GROUPS), 0.01),
        'router_w_expert': nrm(ks[22], (L, D, N_EXPERTS), D ** -0.5),
        'router_b_expert': nrm(ks[23], (L, N_EXPERTS), 0.01),
        'moe_w_gate': nrm(ks[24], (L, N_EXPERTS, D, EXPERT_FF), D ** -0.5),
        'moe_w_up': nrm(ks[25], (L, N_EXPERTS, D, EXPERT_FF), D ** -0.5),
        'moe_w_down': nrm(ks[26], (L, N_EXPERTS, EXPERT_FF, D), EXPERT_FF ** -0.5 * DEEPNORM_BETA),
    }


def reference(x, c, w_in, mla_q_norm, mla_kv_norm, mla_w_uq, mla_w_ukv, gdn_conv, gdn_a_log,
              gdn_dt_bias, gdn_out_norm, pool_w, pool_scale, w_out, w_mod, b_mod, ln1_g, ln1_b,
              ln2_g, ln2_b, router_w_group, router_b_group, router_w_expert, router_b_expert,
              moe_w_gate, moe_w_up, moe_w_down):
    cos, sin = rope_tables(x.shape[1])
    c_act = jax.nn.silu(c)
    for l in range(DEPTH):
        mod = c_act @ w_mod[l] + b_mod[l]
        sh1, sc1, gt1, sh2, sc2, gt2 = [m[:, None, :] for m in jnp.split(mod, 6, axis=-1)]
        h = x * (1.0 + sc1) + sh1
        y = hybrid_mixer(h, w_in[l], mla_q_norm[l], mla_kv_norm[l], mla_w_uq[l], mla_w_ukv[l],
                         gdn_conv[l], gdn_a_log[l], gdn_dt_bias[l], gdn_out_norm[l],
                         pool_w[l], pool_scale[l], w_out[l], cos, sin)
        x = layer_norm(DEEPNORM_ALPHA * x + (1.0 + gt1) * y, ln1_g[l], ln1_b[l])
        h = x * (1.0 + sc2) + sh2
        y = hierarchical_moe(h, router_w_group[l], router_b_group[l], router_w_expert[l],
                             router_b_expert[l], moe_w_gate[l], moe_w_up[l], moe_w_down[l])
        x = layer_norm(DEEPNORM_ALPHA * x + (1.0 + gt2) * y, ln2_g[l], ln2_b[l])
    return x
```

```python
import contextlib
import numpy as np
import concourse.bass as bass
import concourse.mybir as mybir
from concourse.bass_utils import run_bass_kernel_spmd

F32 = mybir.dt.float32
BF16 = mybir.dt.bfloat16
AF = mybir.ActivationFunctionType
ALU = mybir.AluOpType
AX = mybir.AxisListType

SEG = 28000
ENGS = ("pe", "dve", "act", "pool", "sp")
NDMA = 8

D = 1024
NEXP = 32
ALPHA = 8.0 ** 0.25
LN_EPS = 1e-5
RMS_EPS = 1e-6
NEG = -30000.0

C_ID, C_ONE, C_U, C_MLO, C_MUP, C_INV16, C_BD, NCON = 0, 128, 256, 320, 384, 448, 480, 608
K_QN, K_KVN, K_LN1G, K_LN1B, K_LN2G, K_LN2B, K_PS, K_GO, K_CONV, K_BMOD, NCOLS = 0, 2, 3, 11, 19, 27, 35, 37, 38, 62, 110


class Buf:
    __slots__ = ("name", "lw", "rd", "excl")

    def __init__(self, name="", excl=False):
        self.name = name
        self.lw = None
        self.rd = {}
        self.excl = excl


def PB():
    return Buf("psum", True)


class Prog:
    def __init__(self, nc, same_engine_sync=True):
        self.nc = nc
        self.es = contextlib.ExitStack()
        self.cnt = {e: 0 for e in ENGS}
        self.seen = {e: {} for e in ENGS}
        self.sems = {}
        self.dma_i = {e: 0 for e in ENGS}
        self.same = same_engine_sync
        self.E = {"pe": nc.tensor, "dve": nc.vector, "act": nc.scalar, "pool": nc.gpsimd, "sp": nc.sync}
        self.last_dma = {}

    def sem(self, key):
        if key not in self.sems:
            nm = "s_" + "_".join(str(k) for k in key)
            self.sems[key] = self.es.enter_context(self.nc.semaphore(nm))
        return self.sems[key]

    def _wait(self, eng, tok):
        key, val = tok
        if self.seen[eng].get(key, 0) >= val:
            return
        if key[0] == eng and (eng == "pe" or not self.same):
            return
        self.seen[eng][key] = val
        self.E[eng].wait_ge(self.sem(key), val)

    def _deps(self, eng, reads, writes):
        for b in reads:
            if b.lw is not None:
                self._wait(eng, b.lw)
        for b in writes:
            if b.lw is not None:
                self._wait(eng, b.lw)
            for k, v in b.rd.items():
                self._wait(eng, (k, v))

    def _mark(self, tok, reads, writes):
        k, v = tok
        for b in reads:
            if b.rd.get(k, 0) < v:
                b.rd[k] = v
        for b in writes:
            b.lw = tok
            b.rd = {}

    def op(self, eng, fn, reads=(), writes=()):
        if any(b.excl for b in reads):
            writes = list(writes) + [b for b in reads if b.excl]
            reads = [b for b in reads if not b.excl]
        self._deps(eng, reads, writes)
        n = self.cnt[eng]
        key = (eng, n // SEG)
        val = n % SEG + 1
        fn(self.E[eng]).then_inc(self.sem(key), 1)
        self.cnt[eng] = n + 1
        self._mark((key, val), reads, writes)

    def dma(self, eng, out, in_, reads=(), writes=(), **kw):
        i = self.dma_i[eng]
        self.dma_i[eng] = i + 1
        key = ("d" + eng, i % NDMA)
        val = (i // NDMA + 1) * 16
        if val > 16:
            self._wait(eng, (key, val - 16))
        self._deps(eng, reads, writes)
        self.E[eng].dma_start(out=out, in_=in_, **kw).then_inc(self.sem(key), 16)
        self.last_dma[key] = val
        self._mark((key, val), reads, writes)

    def barrier(self):
        toks = []
        for e in ENGS:
            n = self.cnt[e]
            if n:
                toks.append(((e, (n - 1) // SEG), (n - 1) % SEG + 1))
        toks += list(self.last_dma.items())
        for e in ENGS:
            for t in toks:
                self._wait(e, t)


def build(T, DEPTH, NSEQ, dbg=None):
    nc = bass.Bass("TRN2", target_bir_lowering=False)
    P = Prog(nc)
    NB = T // 512
    NT = T // 128
    NCH = T // 64
    dr = {}

    def din(name, shape):
        dr[name] = nc.dram_tensor(name, list(shape), F32, kind="ExternalInput").ap()

    din("xT", [NSEQ, D, T]); din("cT", [D, NSEQ]); din("consts", [128, NCON]); din("rope", [32, 2, T])
    din("cols", [4, 128, NCOLS]); din("w_in", [4, D, 1704]); din("mla_w_uq", [4, 256, 768])
    din("mla_w_ukv", [4, 128, 1024]); din("gdn_a_log", [4, 4]); din("gdn_dt_bias", [4, 4])
    din("pool_w", [4, 4, 64, 64]); din("w_out", [4, D, D]); din("w_mod", [4, D, 6 * D])
    din("w_r", [4, D, 36]); din("b_r", [4, 36])
    din("moe_w_gate", [4, NEXP, D, 256]); din("moe_w_up", [4, NEXP, D, 256]); din("moe_w_down", [4, NEXP, 256, D])
    yT = nc.dram_tensor("yT", [NSEQ, D, T], F32, kind="ExternalOutput").ap()
    scr = nc.dram_tensor("scr_wt", [NEXP, T], F32).ap()
    bscr = Buf("scr")
    if dbg:
        dbg_out = nc.dram_tensor("dbg", [D, T], F32, kind="ExternalOutput").ap()

    uid = [0]

    def sb(name, shape, dt=F32):
        uid[0] += 1
        return nc.sbuf_tensor("%s_%d" % (name, uid[0]), list(shape), dt)

    @contextlib.contextmanager
    def ps(name, shape, dt=F32):
        uid[0] += 1
        isz = 4 if dt == F32 else 2
        per = 2048 // isz
        n = 1
        for d_ in shape[1:]:
            n *= d_
        nb = (n + per - 1) // per
        with nc.psum_tensor("%s_%d" % (name, uid[0]), [128, nb * per], dt) as t:
            v = t[0:shape[0], 0:n]
            if len(shape) == 3:
                v = v.rearrange("p (a b) -> p a b", a=shape[1])
            elif len(shape) == 4:
                v = v.rearrange("p (a b c) -> p a b c", a=shape[1], b=shape[2])
            yield v

    es = contextlib.ExitStack()
    with es:
        cst = es.enter_context(sb("cst", [128, NCON])); bcst = Buf()
        idb = es.enter_context(sb("idb", [128, 128], BF16))
        oneb = es.enter_context(sb("oneb", [128, 128], BF16))
        colsT = es.enter_context(sb("colsT", [128, DEPTH, NCOLS])); bcols = Buf()
        modT = es.enter_context(sb("modT", [128, DEPTH, 48, NSEQ])); bmod = Buf()
        xT = es.enter_context(sb("xT_sb", [128, 8, T])); bx = [Buf() for _ in range(8)]
        hT = es.enter_context(sb("hT_sb", [128, 8, T], BF16)); bh = [Buf() for _ in range(8)]
        mixT = es.enter_context(sb("mixT_sb", [128, 8, T], BF16)); bmix = [Buf() for _ in range(8)]

        P.dma("sp", cst[:], dr["consts"][:, :], writes=[bcst])
        for l in range(DEPTH):
            P.dma("sp", colsT[:, l, :], dr["cols"][l, :, :], writes=[bcols])
        P.op("dve", lambda E: E.tensor_copy(idb[:], cst[:, C_ID:C_ID + 128]), reads=[bcst], writes=[bcst])
        P.op("dve", lambda E: E.tensor_copy(oneb[:], cst[:, C_ONE:C_ONE + 128]), reads=[bcst], writes=[bcst])
        ident = cst[:, C_ID:C_ID + 128]
        ones = cst[:, C_ONE:C_ONE + 128]

        def col(l, k):
            return colsT[:, l, k:k + 1]

        with contextlib.ExitStack() as _S:
            scT = _S.enter_context(sb("scT", [128, 8, NSEQ]))
            sgT = _S.enter_context(sb("sgT", [128, 8, NSEQ]))
            wm0 = _S.enter_context(sb("wm0", [128, 8, 768]))
            wm1 = _S.enter_context(sb("wm1", [128, 8, 768]))
            modps = _S.enter_context(ps("modps", [128, 48, NSEQ]))
            bsc = Buf(); bwm = [Buf(), Buf()]; bmp = PB()
            wms = [wm0, wm1]
            P.dma("sp", scT[:], dr["cT"].rearrange("(k p) s -> p k s", p=128), writes=[bsc])
            P.op("act", lambda E: E.activation(sgT[:], scT[:], AF.Sigmoid), reads=[bsc], writes=[bsc])
            P.op("dve", lambda E: E.tensor_tensor(scT[:], scT[:], sgT[:], ALU.mult), reads=[bsc], writes=[bsc])
            ci = 0
            for l in range(DEPTH):
                for c in range(8):
                    wm = wms[ci % 2]; bw = bwm[ci % 2]; ci += 1
                    P.dma("sp", wm[:], dr["w_mod"][l, :, c * 768:(c + 1) * 768].rearrange("(k p) n -> p k n", p=128),
                          writes=[bw])
                    for mm in range(6):
                        m = c * 6 + mm
                        for kt in range(8):
                            P.op("pe", lambda E: E.matmul(modps[:, m, :], wm[:, kt, mm * 128:(mm + 1) * 128],
                                                          scT[:, kt, :], start=(kt == 0), stop=(kt == 7)),
                                 reads=[bw, bsc], writes=[bmp])
                P.op("dve", lambda E: E.tensor_tensor(
                    modT[:, l, :, :], modps[:],
                    colsT[:, l, K_BMOD:K_BMOD + 48].unsqueeze(2).to_broadcast([128, 48, NSEQ]), ALU.add),
                    reads=[bmp, bcols], writes=[bmod])
                for a in (8, 32):
                    P.op("dve", lambda E: E.tensor_scalar_add(modT[:, l, a:a + 16, :], modT[:, l, a:a + 16, :], 1.0),
                         reads=[bmod], writes=[bmod])
        P.barrier()

        def modc(l, chunk, kt, s):
            return modT[:, l, chunk * 8 + kt, s:s + 1]

        def layer_norm(l, kg, kb):
            with contextlib.ExitStack() as _S:
                p_s = _S.enter_context(ps("ln_s", [128, 512]))
                p_q = _S.enter_context(ps("ln_q", [128, 512]))
                sq = _S.enter_context(sb("ln_sq", [128, 2, 512]))
                mean = _S.enter_context(sb("ln_mean", [128, 512]))
                rstd = _S.enter_context(sb("ln_rstd", [128, 512]))
                tt = _S.enter_context(sb("ln_t", [128, 2, 512]))
                bps, bpq, bsq, bmean, brstd, btt = PB(), PB(), [Buf(), Buf()], Buf(), Buf(), [Buf(), Buf()]
                for b in range(NB):
                    sl = slice(b * 512, (b + 1) * 512)
                    for kt in range(8):
                        P.op("pe", lambda E: E.matmul(p_s[:], ones, xT[:, kt, sl], start=(kt == 0), stop=(kt == 7)),
                             reads=[bx[kt], bcst], writes=[bps])
                    for kt in range(8):
                        j = kt % 2
                        P.op("act", lambda E: E.activation(sq[:, j, :], xT[:, kt, sl], AF.Square),
                             reads=[bx[kt]], writes=[bsq[j]])
                        P.op("pe", lambda E: E.matmul(p_q[:], ones, sq[:, j, :], start=(kt == 0), stop=(kt == 7)),
                             reads=[bsq[j], bcst], writes=[bpq])
                    P.op("act", lambda E: E.mul(mean[:], p_s[:], 1.0 / D), reads=[bps], writes=[bmean])
                    P.op("dve", lambda E: E.tensor_tensor(rstd[:], mean[:], mean[:], ALU.mult),
                         reads=[bmean], writes=[brstd])
                    P.op("dve", lambda E: E.scalar_tensor_tensor(rstd[:], p_q[:], 1.0 / D, rstd[:], ALU.mult,
                                                                 ALU.subtract), reads=[bpq, brstd], writes=[brstd])
                    P.op("act", lambda E: E.activation(rstd[:], rstd[:], AF.Sqrt, bias=LN_EPS, scale=1.0),
                         reads=[brstd], writes=[brstd])
                    P.op("dve", lambda E: E.reciprocal(rstd[:], rstd[:]), reads=[brstd], writes=[brstd])
                    for kt in range(8):
                        j = kt % 2
                        P.op("pool", lambda E: E.tensor_tensor(tt[:, j, :], xT[:, kt, sl], mean[:], ALU.subtract),
                             reads=[bx[kt], bmean], writes=[btt[j]])
                        P.op("dve", lambda E: E.tensor_tensor(tt[:, j, :], tt[:, j, :], rstd[:], ALU.mult),
                             reads=[btt[j], brstd], writes=[btt[j]])
                        P.op("dve", lambda E: E.tensor_scalar(xT[:, kt, sl], tt[:, j, :], col(l, kg + kt),
                                                              col(l, kb + kt), ALU.mult, ALU.add),
                             reads=[btt[j], bcols], writes=[bx[kt]])
            P.barrier()

        def modulate(l, s, sh_chunk, sc_chunk):
            for kt in range(8):
                P.op("act", lambda E: E.activation(hT[:, kt, :], xT[:, kt, :], AF.Identity,
                                                   bias=modc(l, sh_chunk, kt, s), scale=modc(l, sc_chunk, kt, s)),
                     reads=[bx[kt], bmod], writes=[bh[kt]])

        def load_w_in(wt, bw, l, c0, c1):
            for kt in range(8):
                P.dma("pool", wt[:, kt, :], dr["w_in"][l, kt * 128:(kt + 1) * 128, c0:c1], writes=[bw])

        def pool_stage(l, s):
            with contextlib.ExitStack() as _S:
                pw = _S.enter_context(sb("pw", [128, 8, 256], BF16))
                pbd = _S.enter_context(sb("pbd", [128, 2, 128], BF16))
                px = _S.enter_context(sb("px", [128, 16 + T]))
                pa = _S.enter_context(sb("pa", [128, 16 + T]))
                pb = _S.enter_context(sb("pb", [128, 16 + T]))
                pdl = _S.enter_context(sb("pdl", [128, T], BF16))
                ptmp = _S.enter_context(sb("ptmp", [128, 16]))
                pps0 = _S.enter_context(ps("pps0", [128, 512]))
                pps1 = _S.enter_context(ps("pps1", [128, 512]))
                bpw, bpbd, bpx, bpa, bpb, bpdl, bptmp = Buf(), Buf(), Buf(), Buf(), Buf(), Buf(), Buf()
                pps = [pps0, pps1]; bpps = [PB(), PB()]
                load_w_in(pw, bpw, l, 1448, 1704)
                P.op("pool", lambda E: E.memset(pbd[:], 0.0), writes=[bpbd])
                for g in range(4):
                    j, r = g // 2, g % 2
                    P.dma("pool", pbd[64 * r:64 * r + 64, j, 64 * r:64 * r + 64], dr["pool_w"][l, g, :, :],
                          writes=[bpbd])
                for t in (px, pa, pb):
                    P.op("pool", lambda E: E.memset(t[:, 0:16], 0.0), writes=[bpx, bpa, bpb])
                k = 0
                for j in range(2):
                    for b in range(NB):
                        pp = pps[k % 2]; bpp = bpps[k % 2]; k += 1
                        sl = slice(b * 512, (b + 1) * 512)
                        for kt in range(8):
                            P.op("pe", lambda E: E.matmul(pp[:], pw[:, kt, j * 128:(j + 1) * 128], hT[:, kt, sl],
                                                          start=(kt == 0), stop=(kt == 7)),
                                 reads=[bpw, bh[kt]], writes=[bpp])
                        P.op("act", lambda E: E.copy(px[:, 16 + b * 512:16 + (b + 1) * 512], pp[:]),
                             reads=[bpp], writes=[bpx])

                    def shift_add(dst, src, sh, bd, bs):
                        P.op("dve", lambda E: E.tensor_tensor(dst[:, 16:16 + T], src[:, 16:16 + T],
                                                              src[:, 16 - sh:16 - sh + T], ALU.add),
                             reads=[bs], writes=[bd])

                    def delta(src, bs, r, w):
                        rows = slice(64 * r, 64 * r + 64)
                        P.op("dve", lambda E: E.scalar_tensor_tensor(pdl[rows, :], src[rows, 16:16 + T], 1.0 / w,
                                                                     px[rows, 16:16 + T], ALU.mult, ALU.subtract),
                             reads=[bs, bpx], writes=[bpdl])
                        P.op("dve", lambda E: E.tensor_tensor(ptmp[rows, :], src[rows, 16:32],
                                                              cst[rows, C_INV16 + 16 * j:C_INV16 + 16 * j + 16],
                                                              ALU.mult), reads=[bs, bcst], writes=[bptmp])
                        P.op("dve", lambda E: E.tensor_tensor(pdl[rows, 0:16], ptmp[rows, :], px[rows, 16:32],
                                                              ALU.subtract), reads=[bptmp, bpx, bpdl], writes=[bpdl])

                    shift_add(pa, px, 1, bpa, bpx)
                    if j == 0:
                        delta(pa, bpa, 0, 2)
                    shift_add(pb, pa, 2, bpb, bpa)
                    if j == 0:
                        delta(pb, bpb, 1, 4)
                    else:
                        shift_add(pa, pb, 4, bpa, bpb)
                        delta(pa, bpa, 0, 8)
                        shift_add(pb, pa, 8, bpb, bpa)
                        delta(pb, bpb, 1, 16)
                    for b in range(NB):
                        pp = pps[k % 2]; bpp = bpps[k % 2]; k += 1
                        sl = slice(b * 512, (b + 1) * 512)
                        P.op("pe", lambda E: E.matmul(pp[:], pbd[:, j, :], pdl[:, sl], start=True, stop=True),
                             reads=[bpbd, bpdl], writes=[bpp])
                        P.op("act", lambda E: E.activation(mixT[:, 6 + j, sl], pp[:], AF.Identity, scale=col(l, K_PS + j)),
                             reads=[bpp, bcols], writes=[bmix[6 + j]])
            P.barrier()

        def mla_stage(l, s):
            with contextlib.ExitStack() as _S:
                wA = _S.enter_context(sb("wA", [128, 8, 416], BF16))
                wuq = _S.enter_context(sb("wuq", [128, 2, 768], BF16))
                wuqr = _S.enter_context(sb("wuqr", [128, 2, 8, 32], BF16))
                wukv = _S.enter_context(sb("wukv", [128, 1024], BF16))
                wkr = _S.enter_context(sb("wkr", [128, 8, 32], BF16))
                cqn = _S.enter_context(sb("cqn", [128, 2, T], BF16))
                ckvn = _S.enter_context(sb("ckvn", [128, T], BF16))
                krope = _S.enter_context(sb("krope", [32, T], BF16))
                rp0 = _S.enter_context(sb("rp0", [32, 2, 512]))
                rp1 = _S.enter_context(sb("rp1", [32, 2, 512]))
                QT = _S.enter_context(sb("QT", [64, T], BF16))
                qrp = _S.enter_context(sb("qr", [32, T], BF16))
                KT = _S.enter_context(sb("KT", [64, T], BF16))
                Vh = _S.enter_context(sb("Vh", [128, NT, 64], BF16))
                csb = _S.enter_context(sb("csb", [128, 3, 512]))
                sq = _S.enter_context(sb("sq", [128, 3, 512]))
                rs = _S.enter_context(sb("rs", [128, 2, 512]))
                rt = _S.enter_context(sb("rt", [32, 2, 512]))
                pt0 = _S.enter_context(sb("pt0", [128, 128], BF16))
                pt1 = _S.enter_context(sb("pt1", [128, 128], BF16))
                pt2 = _S.enter_context(sb("pt2", [128, 128], BF16))
                rden = _S.enter_context(sb("rden", [64, 128]))
                bwA, bwuq, bwuqr, bwukv, bwkr, bcqn, bckvn, bkrope, brope = [Buf() for _ in range(9)]
                bQT, bqr, bKT, bVh, bcsb, bsq, brs, brt, brden = [Buf() for _ in range(9)]
                pts = [pt0, pt1, pt2]; bpts = [Buf(), Buf(), Buf()]
                load_w_in(wA, bwA, l, 0, 416)
                P.dma("pool", wuq[:], dr["mla_w_uq"][l, :, :].rearrange("(j p) n -> p j n", p=128), writes=[bwuq])
                P.dma("pool", wukv[:], dr["mla_w_ukv"][l, :, :], writes=[bwukv])
                rps = [rp0, rp1]; brps = [Buf(), Buf()]; rpi = [0]

                def load_rope(sl):
                    i = rpi[0] % 2; rpi[0] += 1
                    P.dma("sp", rps[i][:], dr["rope"][:, :, sl], writes=[brps[i]])
                    return rps[i], brps[i]
                P.op("pool", lambda E: E.tensor_scalar_mul(wkr[:, :, 0:16], wA[:, :, 400:416], -1.0),
                     reads=[bwA], writes=[bwkr])
                P.op("pool", lambda E: E.tensor_copy(wkr[:, :, 16:32], wA[:, :, 384:400]), reads=[bwA], writes=[bwkr])
                wq4 = wuq[:].rearrange("p j (h d) -> p j h d", d=96)
                P.op("pool", lambda E: E.tensor_scalar_mul(wuqr[:, :, :, 0:16], wq4[:, :, :, 80:96], -1.0),
                     reads=[bwuq], writes=[bwuqr])
                P.op("pool", lambda E: E.tensor_copy(wuqr[:, :, :, 16:32], wq4[:, :, :, 64:80]),
                     reads=[bwuq], writes=[bwuqr])
                scale = 96.0 ** -0.5
                with contextlib.ExitStack() as _S:
                    m_a = _S.enter_context(ps("m_a", [128, 3, 512]))
                    m_s = _S.enter_context(ps("m_s", [128, 2, 512]))
                    m_r = _S.enter_context(ps("m_r", [32, 2, 512]))
                    bma, bms, bmr = PB(), PB(), PB()
                    for b in range(NB):
                        sl = slice(b * 512, (b + 1) * 512)
                        for j in range(3):
                            for kt in range(8):
                                P.op("pe", lambda E: E.matmul(m_a[:, j, :], wA[:, kt, j * 128:(j + 1) * 128],
                                                              hT[:, kt, sl], start=(kt == 0), stop=(kt == 7)),
                                     reads=[bwA, bh[kt]], writes=[bma])
                        P.op("act", lambda E: E.copy(csb[:], m_a[:]), reads=[bma], writes=[bcsb])
                        P.op("act", lambda E: E.activation(sq[:], m_a[:], AF.Square), reads=[bma], writes=[bsq])
                        P.op("pe", lambda E: E.matmul(m_s[:, 0, :], ones, sq[:, 0, :], start=True, stop=False),
                             reads=[bsq, bcst], writes=[bms])
                        P.op("pe", lambda E: E.matmul(m_s[:, 0, :], ones, sq[:, 1, :], start=False, stop=True),
                             reads=[bsq, bcst], writes=[bms])
                        P.op("pe", lambda E: E.matmul(m_s[:, 1, :], ones, sq[:, 2, :], start=True, stop=True),
                             reads=[bsq, bcst], writes=[bms])
                        P.op("act", lambda E: E.activation(rs[:, 0, :], m_s[:, 0, :], AF.Sqrt, bias=RMS_EPS,
                                                           scale=1.0 / 256), reads=[bms], writes=[brs])
                        P.op("act", lambda E: E.activation(rs[:, 1, :], m_s[:, 1, :], AF.Sqrt, bias=RMS_EPS,
                                                           scale=1.0 / 128), reads=[bms], writes=[brs])
                        P.op("dve", lambda E: E.reciprocal(rs[:], rs[:]), reads=[brs], writes=[brs])
                        for j in range(2):
                            P.op("dve", lambda E: E.scalar_tensor_tensor(cqn[:, j, sl], csb[:, j, :],
                                                                         col(l, K_QN + j), rs[:, 0, :], ALU.mult,
                                                                         ALU.mult),
                                 reads=[bcsb, brs, bcols], writes=[bcqn])
                        P.op("dve", lambda E: E.scalar_tensor_tensor(ckvn[:, sl], csb[:, 2, :], col(l, K_KVN),
                                                                     rs[:, 1, :], ALU.mult, ALU.mult),
                             reads=[bcsb, brs, bcols], writes=[bckvn])
                        for kt in range(8):
                            P.op("pe", lambda E: E.matmul(m_r[:, 0, :], wA[:, kt, 384:416], hT[:, kt, sl],
                                                          start=(kt == 0), stop=(kt == 7)),
                                 reads=[bwA, bh[kt]], writes=[bmr])
                        for kt in range(8):
                            P.op("pe", lambda E: E.matmul(m_r[:, 1, :], wkr[:, kt, :], hT[:, kt, sl],
                                                          start=(kt == 0), stop=(kt == 7)),
                                 reads=[bwkr, bh[kt]], writes=[bmr])
                        rpt, brp = load_rope(sl)
                        P.op("dve", lambda E: E.tensor_tensor(rt[:], m_r[:], rpt[:], ALU.mult),
                             reads=[bmr, brp], writes=[brt])
                        P.op("dve", lambda E: E.tensor_tensor(krope[:, sl], rt[:, 0, :], rt[:, 1, :], ALU.add),
                             reads=[brt], writes=[bkrope])
                    P.barrier()
                with contextlib.ExitStack() as _S:
                    h_q = _S.enter_context(ps("h_q", [64, 512]))
                    h_r = _S.enter_context(ps("h_r", [32, 2, 512]))
                    h_v = _S.enter_context(ps("h_v", [128, 4, 128]))
                    a_s0 = _S.enter_context(ps("a_s0", [128, 128]))
                    a_s1 = _S.enter_context(ps("a_s1", [128, 128]))
                    a_o = _S.enter_context(ps("a_ob", [64, 128]))
                    a_d = _S.enter_context(ps("a_db", [64, 128]))
                    h_k = h_q
                    bhq, bhr, bhv, bao, bad = [PB() for _ in range(5)]
                    bhk = bhq
                    a_s = [a_s0, a_s1]; bas = [PB(), PB()]
                    pi = 0
                    for h in range(8):
                        for b in range(NB):
                            sl = slice(b * 512, (b + 1) * 512)
                            for j in range(2):
                                P.op("pe", lambda E: E.matmul(h_q[:], wuq[:, j, 96 * h:96 * h + 64], cqn[:, j, sl],
                                                              start=(j == 0), stop=(j == 1)),
                                     reads=[bwuq, bcqn], writes=[bhq])
                            P.op("act", lambda E: E.copy(QT[:, sl], h_q[:]), reads=[bhq], writes=[bQT])
                            for j in range(2):
                                P.op("pe", lambda E: E.matmul(h_r[:, 0, :], wuq[:, j, 96 * h + 64:96 * h + 96],
                                                              cqn[:, j, sl], start=(j == 0), stop=(j == 1)),
                                     reads=[bwuq, bcqn], writes=[bhr])
                            for j in range(2):
                                P.op("pe", lambda E: E.matmul(h_r[:, 1, :], wuqr[:, j, h, :], cqn[:, j, sl],
                                                              start=(j == 0), stop=(j == 1)),
                                     reads=[bwuqr, bcqn], writes=[bhr])
                            rpt, brp = load_rope(sl)
                            P.op("dve", lambda E: E.tensor_tensor(rt[:], h_r[:], rpt[:], ALU.mult),
                                 reads=[bhr, brp], writes=[brt])
                            P.op("dve", lambda E: E.tensor_tensor(qrp[:, sl], rt[:, 0, :], rt[:, 1, :], ALU.add),
                                 reads=[brt], writes=[bqr])
                            P.op("pe", lambda E: E.matmul(h_k[:], wukv[:, 128 * h:128 * h + 64], ckvn[:, sl],
                                                          start=True, stop=True), reads=[bwukv, bckvn], writes=[bhk])
                            P.op("act", lambda E: E.copy(KT[:, sl], h_k[:]), reads=[bhk], writes=[bKT])
                            for tq in range(4):
                                tt_ = b * 4 + tq
                                P.op("pe", lambda E: E.matmul(h_v[:, tq, :], ckvn[:, tt_ * 128:(tt_ + 1) * 128],
                                                              wukv[:, 128 * h:128 * h + 128], start=True, stop=True),
                                     reads=[bwukv, bckvn], writes=[bhv])
                            P.op("act", lambda E: E.copy(Vh[:, b * 4:(b + 1) * 4, :], h_v[:, :, 64:128]),
                                 reads=[bhv], writes=[bVh])
                        r = h % 2
                        for qb in range(NT):
                            qs = slice(qb * 128, (qb + 1) * 128)
                            for kb in range(qb + 1):
                                ks = slice(kb * 128, (kb + 1) * 128)
                                sps = a_s[pi % 2]; bsp = bas[pi % 2]
                                pt = pts[pi % 3]; bpt = bpts[pi % 3]; pi += 1
                                P.op("pe", lambda E: E.matmul(sps, KT[:, ks], QT[:, qs], start=True, stop=False),
                                     reads=[bKT, bQT], writes=[bsp])
                                P.op("pe", lambda E: E.matmul(sps, krope[:, ks], qrp[:, qs], start=False,
                                                              stop=True), reads=[bkrope, bqr], writes=[bsp])
                                P.op("act", lambda E: E.activation(pt[:], sps, AF.Exp, scale=scale), reads=[bsp], writes=[bpt])
                                if kb == qb:
                                    P.op("pool", lambda E: E.memset(pt[64:128, 0:64], 0.0), writes=[bpt])
                                P.op("pe", lambda E: E.matmul(a_o, Vh[:, kb, :], pt[:], start=(kb == 0),
                                                              stop=(kb == qb)), reads=[bVh, bpt], writes=[bao])
                                P.op("pe", lambda E: E.matmul(a_d, oneb[:, 0:64], pt[:], start=(kb == 0),
                                                              stop=(kb == qb)), reads=[bpt, bcst], writes=[bad])
                            P.op("dve", lambda E: E.reciprocal(rden[:], a_d), reads=[bad], writes=[brden])
                            P.op("dve", lambda E: E.tensor_tensor(mixT[64 * r:64 * r + 64, h // 2, qs], a_o,
                                                                  rden[:], ALU.mult),
                                 reads=[bao, brden], writes=[bmix[h // 2]])
            P.barrier()

        def gdn_stage(l, s):
            with contextlib.ExitStack() as _S:
                gw = _S.enter_context(sb("gw", [128, 8, 256], BF16))
                gwab = _S.enter_context(sb("gwab", [128, 8, 8], BF16))
                gqT = _S.enter_context(sb("gqT", [128, 2, T], BF16))
                gkT = _S.enter_context(sb("gkT", [128, 2, T], BF16))
                gvT = _S.enter_context(sb("gvT", [128, 2, T], BF16))
                gzT = _S.enter_context(sb("gzT", [128, 2, T], BF16))
                gpre = _S.enter_context(sb("gpre", [128, 3 + T]))
                gacc = _S.enter_context(sb("gacc", [128, 512]))
                gsig = _S.enter_context(sb("gsig", [128, 512]))
                grn = _S.enter_context(sb("grn", [128, 512]))
                gpar = _S.enter_context(sb("gpar", [64, 12]))
                bgw, bgwab, bgq, bgk, bgv, bgz, bgpre, bgacc, bgsig, bgrn, bgpar = [Buf() for _ in range(11)]
                load_w_in(gwab, bgwab, l, 1440, 1448)
                P.dma("sp", gpar[:, 0:4], dr["gdn_a_log"][l:l + 1, :].partition_broadcast(64), writes=[bgpar])
                P.dma("sp", gpar[:, 4:8], dr["gdn_dt_bias"][l:l + 1, :].partition_broadcast(64), writes=[bgpar])
                P.op("act", lambda E: E.activation(gpar[:, 8:12], gpar[:, 0:4], AF.Exp), reads=[bgpar], writes=[bgpar])
                P.op("dve", lambda E: E.tensor_scalar_mul(gpar[:, 8:12], gpar[:, 8:12], -1.0), reads=[bgpar],
                     writes=[bgpar])
                P.op("pool", lambda E: E.memset(gpre[:, 0:3], 0.0), writes=[bgpre])
                bd = cst[:, C_BD:C_BD + 128]
                with contextlib.ExitStack() as _S:
                    g_p0 = _S.enter_context(ps("g_p0", [128, 512]))
                    g_p1 = _S.enter_context(ps("g_p1", [128, 512]))
                    g_ss = _S.enter_context(ps("g_ss", [128, 512]))
                    gps_ = [g_p0, g_p1]; bgps = [PB(), PB()]; bgss = PB()
                    k = 0
                    for gi, (c0, dst, bdst) in enumerate(((416, gqT, bgq), (672, gkT, bgk), (928, gvT, bgv),
                                                           (1184, gzT, bgz))):
                        load_w_in(gw, bgw, l, c0, c0 + 256)
                        for j in range(2):
                            if gi == 3:
                                for b in range(NB):
                                    sl = slice(b * 512, (b + 1) * 512)
                                    pp = gps_[k % 2]; bpp = bgps[k % 2]; k += 1
                                    for kt in range(8):
                                        P.op("pe", lambda E: E.matmul(pp[:], gw[:, kt, j * 128:(j + 1) * 128],
                                                                      hT[:, kt, sl], start=(kt == 0), stop=(kt == 7)),
                                             reads=[bgw, bh[kt]], writes=[bpp])
                                    P.op("act", lambda E: E.activation(gsig[:], pp[:], AF.Sigmoid), reads=[bpp],
                                         writes=[bgsig])
                                    P.op("dve", lambda E: E.tensor_tensor(dst[:, j, sl], pp[:], gsig[:], ALU.mult),
                                         reads=[bpp, bgsig], writes=[bdst])
                                continue
                            for b in range(NB):
                                sl = slice(b * 512, (b + 1) * 512)
                                pp = gps_[k % 2]; bpp = bgps[k % 2]; k += 1
                                for kt in range(8):
                                    P.op("pe", lambda E: E.matmul(pp[:], gw[:, kt, j * 128:(j + 1) * 128],
                                                                  hT[:, kt, sl], start=(kt == 0), stop=(kt == 7)),
                                         reads=[bgw, bh[kt]], writes=[bpp])
                                P.op("act", lambda E: E.copy(gpre[:, 3 + b * 512:3 + (b + 1) * 512], pp[:]),
                                     reads=[bpp], writes=[bgpre])
                            ct = gi * 2 + j
                            for b in range(NB):
                                sl = slice(b * 512, (b + 1) * 512)
                                P.op("dve", lambda E: E.tensor_scalar(gacc[:], gpre[:, b * 512:b * 512 + 512],
                                                                      col(l, K_CONV + ct * 4), None, ALU.mult),
                                     reads=[bgpre, bcols], writes=[bgacc])
                                for tap in range(1, 4):
                                    P.op("dve", lambda E: E.scalar_tensor_tensor(
                                        gacc[:], gpre[:, b * 512 + tap:b * 512 + tap + 512],
                                        col(l, K_CONV + ct * 4 + tap), gacc[:], ALU.mult, ALU.add),
                                        reads=[bgpre, bcols, bgacc], writes=[bgacc])
                                P.op("act", lambda E: E.activation(gsig[:], gacc[:], AF.Sigmoid), reads=[bgacc],
                                     writes=[bgsig])
                                if gi == 2:
                                    P.op("dve", lambda E: E.tensor_tensor(dst[:, j, sl], gacc[:], gsig[:], ALU.mult),
                                         reads=[bgacc, bgsig], writes=[bdst])
                                    continue
                                P.op("dve", lambda E: E.tensor_tensor(gacc[:], gacc[:], gsig[:], ALU.mult),
                                     reads=[bgacc, bgsig], writes=[bgacc])
                                P.op("act", lambda E: E.activation(gsig[:], gacc[:], AF.Square), reads=[bgacc],
                                     writes=[bgsig])
                                P.op("pe", lambda E: E.matmul(g_ss[:], bd, gsig[:], start=True, stop=True),
                                     reads=[bgsig, bcst], writes=[bgss])
                                P.op("act", lambda E: E.activation(grn[:], g_ss[:], AF.Sqrt, bias=RMS_EPS, scale=1.0),
                                     reads=[bgss], writes=[bgrn])
                                P.op("dve", lambda E: E.reciprocal(grn[:], grn[:]), reads=[bgrn], writes=[bgrn])
                                P.op("dve", lambda E: E.scalar_tensor_tensor(dst[:, j, sl], gacc[:],
                                                                             0.125 if gi == 0 else 1.0, grn[:],
                                                                             ALU.mult, ALU.mult),
                                     reads=[bgacc, bgrn], writes=[bdst])
                P.barrier()
                U = cst[0:64, C_U:C_U + 64]
                MLO = cst[0:64, C_MLO:C_MLO + 64]
                MUP = cst[0:64, C_MUP:C_MUP + 64]
                with contextlib.ExitStack() as _S:
                    sm = _S.enter_context(sb("c_sm", [64, 64]))
                    Gd = _S.enter_context(sb("c_Gd", [64, 4, 64]))
                    Dd = _S.enter_context(sb("c_D", [64, 4, 64]))
                    Ee = _S.enter_context(sb("c_E", [64, 4, 64]))
                    Et = _S.enter_context(sb("c_Et", [64, 4, 64]))
                    EGB = _S.enter_context(sb("c_EGB", [128, 256]))
                    tA = _S.enter_context(sb("c_tA", [64, 4, 64]))
                    A0 = _S.enter_context(sb("c_A0", [64, 4, 64], BF16))
                    A1 = _S.enter_context(sb("c_A1", [64, 4, 64], BF16))
                    B0 = _S.enter_context(sb("c_B0", [64, 4, 64], BF16))
                    B1 = _S.enter_context(sb("c_B1", [64, 4, 64], BF16))
                    Pm = _S.enter_context(sb("c_P", [64, 4, 64], BF16))
                    qkT = _S.enter_context(sb("c_qk", [64, 4, 64], BF16))
                    vtm = _S.enter_context(sb("c_vtm", [64, 4, 64]))
                    KDP = _S.enter_context(sb("c_KDP", [64, 4, 128], BF16))
                    VNP = _S.enter_context(sb("c_VNP", [64, 4, 128], BF16))
                    Rb = _S.enter_context(sb("c_R", [64, 4, 64], BF16))
                    t1 = _S.enter_context(sb("c_t1", [64, 4, 64]))
                    qdT = _S.enter_context(sb("c_qd", [128, 2, 64], BF16))
                    SP = _S.enter_context(sb("c_SP", [128, 2, 128]))
                    SPb = _S.enter_context(sb("c_SPb", [128, 2, 128], BF16))
                    glc = _S.enter_context(sb("c_gl", [128, 2]))
                    osq = _S.enter_context(sb("c_osq", [128, 2, 64]))
                    ors = _S.enter_context(sb("c_ors", [128, 2, 64]))
                    otm = _S.enter_context(sb("c_otm", [128, 2, 64]))
                    kmisc = _S.enter_context(ps("k_misc", [128, 512]))
                    kgcb = _S.enter_context(ps("k_gcb", [128, 256]))
                    kgk = _S.enter_context(ps("k_gk", [64, 2, 4, 64]))
                    ksq = _S.enter_context(ps("k_sq", [64, 2, 4, 64]))
                    kpv = _S.enter_context(ps("k_pv", [64, 2, 4, 64]))
                    ktr = _S.enter_context(ps("k_tr", [64, 3, 256], BF16))
                    kps1 = _S.enter_context(ps("k_ps1", [64, 4, 64]))
                    kU = _S.enter_context(ps("k_U", [128, 2, 128]))
                    (bsm, bGd, bD, bE, bEt, bEGB, btA, bP, bqk, bvtm, bKDP, bVNP, bR, bt1, bqd, bSP, bSPb, bgl, bosq,
                     bors, botm) = [Buf() for _ in range(21)]
                    bA = [Buf(), Buf()]; bB = [Buf(), Buf()]
                    b_ab = PB(); b_gcc = b_ab; b_ss = b_ab; b_o = b_ab
                    b_gcb = PB(); b_G = PB(); b_KQ = b_G; b_sq = PB(); b_pu = PB(); b_vn = b_pu
                    b_trB = PB(); b_trv = b_trB; b_trk = b_trB; b_ps1 = PB(); b_U = PB()
                    As = [A0, A1]; Bs = [B0, B1]
                    ab_ps = kmisc[0:64, 0:8]; gcc_ps = kmisc[0:64, 8:12]
                    ss_ps = kmisc[:, 64:192].rearrange("p (j c) -> p j c", j=2)
                    o_ps = kmisc[:, 256:384].rearrange("p (j c) -> p j c", j=2)
                    idb64 = idb[0:64, 0:64]
                    kz = _S.enter_context(sb("c_kz", [128, 2, 4, 64], BF16)); bkz = Buf()
                    P.op("pool", lambda E: E.memset(kz[:], 0.0), writes=[bkz])
                    P.op("pool", lambda E: E.memset(KDP[:], 0.0), writes=[bKDP])
                    P.op("pool", lambda E: E.memset(VNP[:], 0.0), writes=[bVNP])
                    P.op("pool", lambda E: E.memset(SP[:], 0.0), writes=[bSP])
                    P.op("pool", lambda E: E.memset(SPb[:], 0.0), writes=[bSPb])
                    g4, bt4, gcc, egc, ekd, tm4, sp4 = [sm[:, 4 * i:4 * i + 4] for i in range(7)]

                    def bc_h(ap4):
                        return ap4.unsqueeze(2).to_broadcast([64, 4, 64])

                    def bc_m(ap):
                        return ap.unsqueeze(1).to_broadcast([64, 4, 64])

                    opc = [0]
                    maxops = dbg.get("maxops", 10 ** 9) if dbg else 10 ** 9

                    def GOP(eng, fn, rd, wrt):
                        opc[0] += 1
                        if opc[0] <= maxops:
                            P.op(eng, fn, reads=rd, writes=wrt)

                    V = lambda fn, rd, wrt: GOP("dve", fn, rd, wrt)
                    AC = lambda fn, rd, wrt: GOP("act", fn, rd, wrt)
                    PE = lambda fn, rd, wrt: GOP("pe", fn, rd, wrt)
                    for c in range(NCH if not (dbg and 'nch' in dbg) else dbg['nch']):
                        cs_ = slice(c * 64, (c + 1) * 64)
                        for kt in range(8):
                            PE(lambda E: E.matmul(ab_ps, hT[:, kt, cs_], gwab[:, kt, :], start=(kt == 0),
                                                  stop=(kt == 7)), [bh[kt], bgwab], [b_ab])
                        V(lambda E: E.tensor_tensor(sp4, ab_ps[:, 0:4], gpar[:, 4:8], ALU.add), [b_ab, bgpar], [bsm])
                        AC(lambda E: E.activation(sp4, sp4, AF.Exp), [bsm], [bsm])
                        AC(lambda E: E.activation(sp4, sp4, AF.Ln, bias=1.0, scale=1.0), [bsm], [bsm])
                        V(lambda E: E.tensor_tensor(g4, sp4, gpar[:, 8:12], ALU.mult), [bsm, bgpar], [bsm])
                        AC(lambda E: E.activation(bt4, ab_ps[:, 4:8], AF.Exp, scale=-1.0), [b_ab], [bsm])
                        V(lambda E: E.tensor_scalar_add(bt4, bt4, 1.0), [bsm], [bsm])
                        V(lambda E: E.reciprocal(bt4, bt4), [bsm], [bsm])
                        PE(lambda E: E.matmul(gcc_ps, U, g4, start=True, stop=True), [bsm, bcst], [b_gcc])
                        V(lambda E: E.tensor_copy(gcc, gcc_ps), [b_gcc], [bsm])
                        V(lambda E: E.tensor_tensor(Gd[:], bc_m(U), bc_h(g4), ALU.mult), [bsm, bcst], [bGd])
                        PE(lambda E: E.matmul(kgcb[:], cst[0:64, C_ONE:C_ONE + 128],
                                              Gd[:].rearrange("p h j -> p (h j)"), start=True, stop=True),
                           [bGd, bcst], [b_gcb])
                        gcb3 = kgcb[0:64, :].rearrange("p (h j) -> p h j", h=4)
                        V(lambda E: E.tensor_tensor(Dd[:], bc_h(gcc), gcb3, ALU.subtract), [bsm, b_gcb], [bD])
                        V(lambda E: E.tensor_tensor(Ee[:], Dd[:], bc_m(MLO), ALU.add), [bD, bcst], [bE])
                        V(lambda E: E.tensor_tensor(Et[:], bc_m(MUP), Dd[:], ALU.subtract), [bD, bcst], [bEt])
                        AC(lambda E: E.activation(Ee[:], Ee[:], AF.Exp), [bE], [bE])
                        AC(lambda E: E.activation(Et[:], Et[:], AF.Exp), [bEt], [bEt])
                        AC(lambda E: E.activation(EGB[:], kgcb[:], AF.Exp), [b_gcb], [bEGB])
                        AC(lambda E: E.activation(egc, gcc, AF.Exp), [bsm], [bsm])
                        V(lambda E: E.tensor_tensor(tm4, gcb3[:, :, 63], gcc, ALU.subtract), [b_gcb, bsm], [bsm])
                        AC(lambda E: E.activation(ekd, tm4, AF.Exp), [bsm], [bsm])
                        EG4 = EGB[:].rearrange("p (j r c) -> p j r c", j=2, r=2)
                        V(lambda E: E.tensor_copy(glc[0:64, :], EG4[0:64, :, 0, 63]), [bEGB], [bgl])
                        V(lambda E: E.tensor_copy(glc[64:128, :], EG4[64:128, :, 1, 63]), [bEGB], [bgl])
                        for h in range(4):
                            j, r = h // 2, h % 2
                            rows = slice(64 * r, 64 * r + 64)
                            GOP("pool", lambda E: E.tensor_copy(kz[rows, 0, h, :], gkT[rows, j, cs_]), [bgk], [bkz])
                            GOP("pool", lambda E: E.tensor_copy(kz[rows, 1, h, :], gqT[rows, j, cs_]), [bgq], [bkz])
                        for h in range(4):
                            j, r = h // 2, h % 2
                            PE(lambda E: E.matmul(kgk[:, 0, h, :], gkT[:, j, cs_], kz[:, 0, h, :], start=True,
                                                  stop=True), [bgk, bkz], [b_G])
                            PE(lambda E: E.matmul(kgk[:, 1, h, :], gkT[:, j, cs_], kz[:, 1, h, :], start=True,
                                                  stop=True), [bgk, bkz], [b_KQ])
                        a = 0
                        V(lambda E: E.tensor_tensor(tA[:], kgk[:, 0, :, :], Ee[:], ALU.mult), [b_G, bE], [btA])
                        V(lambda E: E.tensor_tensor(As[a][:], tA[:], bc_h(bt4), ALU.mult), [btA, bsm], [bA[a]])
                        V(lambda E: E.tensor_tensor(qkT[:], kgk[:, 1, :, :], Et[:], ALU.mult), [b_KQ, bEt], [bqk])
                        trB = ktr[:, 0, :].rearrange("p (h i) -> p h i", h=4)
                        for h in range(4):
                            PE(lambda E: E.transpose(trB[:, h, :], As[a][:, h, :], idb64), [bA[a], bcst], [b_trB])
                        V(lambda E: E.tensor_copy(Bs[a][:], trB), [b_trB], [bB[a]])
                        V(lambda E: E.tensor_tensor(Pm[:], bc_m(idb64), Bs[a][:], ALU.subtract), [bB[a], bcst], [bP])
                        for lev in range(5):
                            n = 1 - a
                            for h in range(4):
                                PE(lambda E: E.matmul(ksq[:, 0, h, :], Bs[a][:, h, :], As[a][:, h, :], start=True,
                                                      stop=True), [bA[a], bB[a]], [b_sq])
                                PE(lambda E: E.matmul(ksq[:, 1, h, :], As[a][:, h, :], Bs[a][:, h, :], start=True,
                                                      stop=True), [bA[a], bB[a]], [b_sq])
                            AC(lambda E: E.copy(As[n][:], ksq[:, 0, :, :]), [b_sq], [bA[n]])
                            V(lambda E: E.tensor_copy(Bs[n][:], ksq[:, 1, :, :]), [b_sq], [bB[n]])
                            a = n
                            for h in range(4):
                                PE(lambda E: E.matmul(kpv[:, 0, h, :], As[a][:, h, :], Pm[:, h, :], start=True,
                                                      stop=True), [bA[a], bP], [b_pu])
                            V(lambda E: E.tensor_tensor(Pm[:], Pm[:], kpv[:, 0, :, :], ALU.add), [bP, b_pu], [bP])
                        for j in range(2):
                            PE(lambda E: E.transpose(ktr[:, 1, j * 128:(j + 1) * 128], gvT[:, j, cs_], idb[:]),
                               [bgv, bcst], [b_trv])
                            PE(lambda E: E.transpose(ktr[:, 2, j * 128:(j + 1) * 128], gkT[:, j, cs_], idb[:]),
                               [bgk, bcst], [b_trk])
                        AC(lambda E: E.copy(vtm[:].rearrange("p h d -> p (h d)"), ktr[:, 1, :]), [b_trv], [bvtm])
                        k4 = ktr[:, 2, :].rearrange("p (j r d) -> p j r d", j=2, r=2)
                        KD5 = KDP[:].rearrange("p (j r) m -> p j r m", j=2)
                        ekd3 = ekd.rearrange("p (j r) -> p j r", j=2)
                        for r in range(2):
                            V(lambda E: E.tensor_tensor(KD5[:, :, r, 64 * r:64 * r + 64], k4[:, :, r, :],
                                                        ekd3[:, :, r].unsqueeze(2).to_broadcast([64, 2, 64]),
                                                        ALU.mult), [b_trk, bsm], [bKDP])
                        for h in range(4):
                            j, r = h // 2, h % 2
                            rows = slice(64 * r, 64 * r + 64)
                            GOP("pool", lambda E: E.tensor_tensor(qdT[rows, j, :], gqT[rows, j, cs_],
                                                                  EGB[rows, h * 64:(h + 1) * 64], ALU.mult),
                                [bgq, bEGB], [bqd])
                        for j in range(2):
                            PE(lambda E: E.matmul(kps1[:, 2 * j:2 * j + 2, :].rearrange("p a b -> p (a b)"),
                                                  gkT[:, j, cs_], SPb[:, j, :], start=True, stop=True),
                               [bgk, bSPb], [b_ps1])
                        V(lambda E: E.tensor_tensor(t1[:], kps1[:], bc_h(egc), ALU.mult), [b_ps1, bsm], [bt1])
                        V(lambda E: E.tensor_tensor(t1[:], vtm[:], t1[:], ALU.subtract), [bvtm, bt1], [bt1])
                        V(lambda E: E.tensor_tensor(Rb[:], t1[:], bc_h(bt4), ALU.mult), [bt1, bsm], [bR])
                        for h in range(4):
                            PE(lambda E: E.matmul(kpv[:, 1, h, :], Pm[:, h, :], Rb[:, h, :], start=True, stop=True),
                               [bP, bR], [b_vn])
                        VN5 = VNP[:].rearrange("p (j r) m -> p j r m", j=2)
                        vn4 = kpv[:, 1, :, :].rearrange("p (j r) d -> p j r d", j=2)
                        for r in range(2):
                            AC(lambda E: E.copy(VN5[:, :, r, 64 * r:64 * r + 64], vn4[:, :, r, :]), [b_vn], [bVNP])
                        for j in range(2):
                            PE(lambda E: E.matmul(o_ps[:, j, :], SPb[:, j, :], qdT[:, j, :], start=True, stop=False),
                               [bSPb, bqd], [b_o])
                            PE(lambda E: E.matmul(o_ps[:, j, :], VNP[:, 2 * j, :], qkT[:, 2 * j, :], start=False,
                                                  stop=False), [bVNP, bqk], [b_o])
                            PE(lambda E: E.matmul(o_ps[:, j, :], VNP[:, 2 * j + 1, :], qkT[:, 2 * j + 1, :],
                                                  start=False, stop=True), [bVNP, bqk], [b_o])
                        AC(lambda E: E.activation(osq[:], o_ps, AF.Square), [b_o], [bosq])
                        PE(lambda E: E.matmul(ss_ps.rearrange("p j c -> p (j c)"), bd,
                                              osq[:].rearrange("p j c -> p (j c)"), start=True, stop=True),
                           [bosq, bcst], [b_ss])
                        AC(lambda E: E.activation(ors[:], ss_ps, AF.Sqrt, bias=RMS_EPS, scale=1.0 / 64), [b_ss], [bors])
                        V(lambda E: E.reciprocal(ors[:], ors[:]), [bors], [bors])
                        V(lambda E: E.scalar_tensor_tensor(otm[:], o_ps, col(l, K_GO), ors[:], ALU.mult, ALU.mult),
                          [b_o, bors, bcols], [botm])
                        for j in range(2):
                            V(lambda E: E.tensor_tensor(mixT[:, 4 + j, cs_], otm[:, j, :], gzT[:, j, cs_], ALU.mult),
                              [botm, bgz], [bmix[4 + j]])
                        for j in range(2):
                            PE(lambda E: E.matmul(kU[:, j, :], KDP[:, 2 * j, :], VNP[:, 2 * j, :], start=True,
                                                  stop=False), [bKDP, bVNP], [b_U])
                            PE(lambda E: E.matmul(kU[:, j, :], KDP[:, 2 * j + 1, :], VNP[:, 2 * j + 1, :],
                                                  start=False, stop=True), [bKDP, bVNP], [b_U])
                        for j in range(2):
                            V(lambda E: E.scalar_tensor_tensor(SP[:, j, :], SP[:, j, :], glc[:, j:j + 1],
                                                               kU[:, j, :], ALU.mult, ALU.add),
                              [bSP, bgl, b_U], [bSP])
                        AC(lambda E: E.copy(SPb[:], SP[:]), [bSP], [bSPb])
            P.barrier()

        def out_proj(l, s):
            with contextlib.ExitStack() as _S:
                wo = _S.enter_context(sb("wo", [128, 8, D], BF16))
                o0 = _S.enter_context(ps("ops0", [128, 512]))
                o1 = _S.enter_context(ps("ops1", [128, 512]))
                bwo = Buf(); ops_ = [o0, o1]; bops = [PB(), PB()]
                for kt in range(8):
                    P.dma("pool", wo[:, kt, :], dr["w_out"][l, kt * 128:(kt + 1) * 128, :], writes=[bwo])
                for kt in range(8):
                    P.op("act", lambda E: E.mul(xT[:, kt, :], xT[:, kt, :], ALPHA), reads=[bx[kt]], writes=[bx[kt]])
                k = 0
                for b in range(NB):
                    sl = slice(b * 512, (b + 1) * 512)
                    for m in range(8):
                        pp = ops_[k % 2]; bpp = bops[k % 2]; k += 1
                        for kt in range(8):
                            P.op("pe", lambda E: E.matmul(pp[:], wo[:, kt, m * 128:(m + 1) * 128], mixT[:, kt, sl],
                                                          start=(kt == 0), stop=(kt == 7)),
                                 reads=[bwo, bmix[kt]], writes=[bpp])
                        P.op("dve", lambda E: E.scalar_tensor_tensor(xT[:, m, sl], pp[:], modc(l, 2, m, s),
                                                                     xT[:, m, sl], ALU.mult, ALU.add),
                             reads=[bpp, bmod, bx[m]], writes=[bx[m]])
            P.barrier()

        def moe_stage(l, s):
            with contextlib.ExitStack() as _S:
                wr = _S.enter_context(sb("wr", [128, 8, 36]))
                brb = _S.enter_context(sb("brb", [128, 36]))
                h2f = _S.enter_context(sb("h2f", [128, 8, 512]))
                Lg = _S.enter_context(sb("Lg", [128, 36]))
                sm = _S.enter_context(sb("sm", [128, 16]))
                msk = _S.enter_context(sb("msk", [128, 3, 32]))
                Wt = _S.enter_context(sb("Wt", [128, 32]))
                WtT = _S.enter_context(sb("WtT", [32, T]))
                r_l = _S.enter_context(ps("r_l", [128, 36]))
                r_t = _S.enter_context(ps("r_t", [32, 128]))
                bwr, bbrb, bh2f, bLg, bsm, bmsk, bWt, bWtT = [Buf() for _ in range(8)]
                brl, brt_ = PB(), PB()
                P.dma("sp", wr[:], dr["w_r"][l, :, :].rearrange("(k p) n -> p k n", p=128), writes=[bwr])
                P.dma("sp", brb[:], dr["b_r"][l:l + 1, :].partition_broadcast(128), writes=[bbrb])
                for b in range(NB):
                    sl = slice(b * 512, (b + 1) * 512)
                    for kt in range(8):
                        P.op("act", lambda E: E.activation(h2f[:, kt, :], xT[:, kt, sl], AF.Identity,
                                                           bias=modc(l, 3, kt, s), scale=modc(l, 4, kt, s)),
                             reads=[bx[kt], bmod], writes=[bh2f])
                        P.op("pool", lambda E: E.tensor_copy(hT[:, kt, sl], h2f[:, kt, :]), reads=[bh2f],
                             writes=[bh[kt]])
                    for tq in range(4):
                        t0 = b * 512 + tq * 128
                        for kt in range(8):
                            P.op("pe", lambda E: E.matmul(r_l[:], h2f[:, kt, tq * 128:(tq + 1) * 128], wr[:, kt, :],
                                                          start=(kt == 0), stop=(kt == 7)),
                                 reads=[bh2f, bwr], writes=[brl])
                        V = lambda fn, rd, wrt: P.op("dve", fn, reads=rd, writes=wrt)
                        V(lambda E: E.tensor_tensor(Lg[:], r_l[:], brb[:], ALU.add), [brl, bbrb], [bLg])
                        V(lambda E: E.reduce_max(sm[:, 0:1], Lg[:, 0:4], AX.X), [bLg], [bsm])
                        V(lambda E: E.tensor_scalar_mul(sm[:, 1:2], sm[:, 0:1], -1.0), [bsm], [bsm])
                        V(lambda E: E.tensor_scalar(msk[:, 0, 0:4], Lg[:, 0:4], sm[:, 0:1], None, ALU.is_equal),
                          [bLg, bsm], [bmsk])
                        P.op("act", lambda E: E.activation(msk[:, 0, 8:12], Lg[:, 0:4], AF.Exp, bias=sm[:, 1:2],
                                                           scale=1.0, accum_out=sm[:, 2:3]),
                             reads=[bLg, bsm], writes=[bmsk, bsm])
                        V(lambda E: E.reciprocal(sm[:, 3:4], sm[:, 2:3]), [bsm], [bsm])
                        V(lambda E: E.tensor_scalar(msk[:, 0, 4:8], msk[:, 0, 0:4], -NEG, NEG, ALU.mult, ALU.add),
                          [bmsk], [bmsk])
                        V(lambda E: E.tensor_tensor(msk[:, 1, :].rearrange("p (g e) -> p g e", g=4),
                                                    Lg[:, 4:36].rearrange("p (g e) -> p g e", g=4),
                                                    msk[:, 0, 4:8].unsqueeze(2).to_broadcast([128, 4, 8]), ALU.add),
                          [bLg, bmsk], [bmsk])
                        V(lambda E: E.reduce_max(sm[:, 4:5], msk[:, 1, :], AX.X), [bmsk], [bsm])
                        V(lambda E: E.tensor_scalar(msk[:, 2, :], msk[:, 1, :], sm[:, 4:5], None, ALU.is_equal),
                          [bmsk, bsm], [bmsk])
                        V(lambda E: E.scalar_tensor_tensor(msk[:, 1, :], msk[:, 2, :], NEG, msk[:, 1, :], ALU.mult,
                                                           ALU.add), [bmsk], [bmsk])
                        V(lambda E: E.reduce_max(sm[:, 5:6], msk[:, 1, :], AX.X), [bmsk], [bsm])
                        V(lambda E: E.tensor_scalar(msk[:, 1, :], msk[:, 1, :], sm[:, 5:6], None, ALU.is_equal),
                          [bmsk, bsm], [bmsk])
                        V(lambda E: E.tensor_tensor(sm[:, 6:7], sm[:, 5:6], sm[:, 4:5], ALU.subtract), [bsm], [bsm])
                        P.op("act", lambda E: E.activation(sm[:, 7:8], sm[:, 6:7], AF.Exp), reads=[bsm], writes=[bsm])
                        V(lambda E: E.tensor_scalar_add(sm[:, 8:9], sm[:, 7:8], 1.0), [bsm], [bsm])
                        V(lambda E: E.reciprocal(sm[:, 8:9], sm[:, 8:9]), [bsm], [bsm])
                        V(lambda E: E.tensor_tensor(sm[:, 9:10], sm[:, 3:4], sm[:, 8:9], ALU.mult), [bsm], [bsm])
                        V(lambda E: E.tensor_tensor(sm[:, 10:11], sm[:, 9:10], sm[:, 7:8], ALU.mult), [bsm], [bsm])
                        V(lambda E: E.tensor_scalar(Wt[:], msk[:, 2, :], sm[:, 9:10], None, ALU.mult),
                          [bmsk, bsm], [bWt])
                        V(lambda E: E.scalar_tensor_tensor(Wt[:], msk[:, 1, :], sm[:, 10:11], Wt[:], ALU.mult,
                                                           ALU.add), [bmsk, bsm, bWt], [bWt])
                        P.op("pe", lambda E: E.transpose(r_t[:], Wt[:], ident), reads=[bWt, bcst], writes=[brt_])
                        P.op("act", lambda E: E.copy(WtT[:, t0:t0 + 128], r_t[:]), reads=[brt_], writes=[bWtT])
                P.dma("sp", scr[:, :], WtT[:], reads=[bWtT], writes=[bscr])
            P.barrier()
            for kt in range(8):
                P.op("act", lambda E: E.mul(xT[:, kt, :], xT[:, kt, :], ALPHA), reads=[bx[kt]], writes=[bx[kt]])
            TB = 256
            NTB = T // TB
            with contextlib.ExitStack() as _S:
                wg0 = _S.enter_context(sb("wg0", [128, 8, 256], BF16))
                wg1 = _S.enter_context(sb("wg1", [128, 8, 256], BF16))
                wu0 = _S.enter_context(sb("wu0", [128, 8, 256], BF16))
                wu1 = _S.enter_context(sb("wu1", [128, 8, 256], BF16))
                wd0 = _S.enter_context(sb("wd0", [128, 2, D], BF16))
                wd1 = _S.enter_context(sb("wd1", [128, 2, D], BF16))
                wb0 = _S.enter_context(sb("wb0", [128, T]))
                wb1 = _S.enter_context(sb("wb1", [128, T]))
                ea0 = _S.enter_context(sb("ea0", [128, 2, TB]))
                ea1 = _S.enter_context(sb("ea1", [128, 2, TB]))
                eb0 = _S.enter_context(sb("eb0", [128, 2, TB], BF16))
                eb1 = _S.enter_context(sb("eb1", [128, 2, TB], BF16))
                eg0 = _S.enter_context(ps("eg0", [128, 2, TB]))
                eg1 = _S.enter_context(ps("eg1", [128, 2, TB]))
                eu0 = _S.enter_context(ps("eu0", [128, 2, TB]))
                eu1 = _S.enter_context(ps("eu1", [128, 2, TB]))
                ey = _S.enter_context(ps("ey", [128, 8, TB]))
                wg, wu, wd, wb = [wg0, wg1], [wu0, wu1], [wd0, wd1], [wb0, wb1]
                bwg, bwu, bwd, bwb = [[Buf(), Buf()] for _ in range(4)]
                ea, eb, eg, eu = [ea0, ea1], [eb0, eb1], [eg0, eg1], [eu0, eu1]
                bea, beb = [[Buf(), Buf()] for _ in range(2)]
                beg, beu = [[PB(), PB()] for _ in range(2)]
                beyb = [PB() for _ in range(4)]
                k = 0
                for e in range(NEXP if not (dbg and 'nexp' in dbg) else dbg['nexp']):
                    w = e % 2
                    for kt in range(8):
                        P.dma("pool", wg[w][:, kt, :], dr["moe_w_gate"][l, e, kt * 128:(kt + 1) * 128, :],
                              writes=[bwg[w]])
                    for kt in range(8):
                        P.dma("pool", wu[w][:, kt, :], dr["moe_w_up"][l, e, kt * 128:(kt + 1) * 128, :],
                              writes=[bwu[w]])
                    for f in range(2):
                        P.dma("pool", wd[w][:, f, :], dr["moe_w_down"][l, e, f * 128:(f + 1) * 128, :],
                              writes=[bwd[w]])
                    P.dma("sp", wb[w][:], scr[e:e + 1, :].partition_broadcast(128), reads=[bscr], writes=[bwb[w]])
                    for tb in range(NTB):
                        sl = slice(tb * TB, (tb + 1) * TB)
                        q = k % 2; k += 1
                        for f in range(2):
                            for kt in range(8):
                                P.op("pe", lambda E: E.matmul(eg[q][:, f, :], wg[w][:, kt, f * 128:(f + 1) * 128],
                                                              hT[:, kt, sl], start=(kt == 0), stop=(kt == 7)),
                                     reads=[bwg[w], bh[kt]], writes=[beg[q]])
                        for f in range(2):
                            for kt in range(8):
                                P.op("pe", lambda E: E.matmul(eu[q][:, f, :], wu[w][:, kt, f * 128:(f + 1) * 128],
                                                              hT[:, kt, sl], start=(kt == 0), stop=(kt == 7)),
                                     reads=[bwu[w], bh[kt]], writes=[beu[q]])
                        P.op("act", lambda E: E.activation(ea[q][:], eg[q][:], AF.Silu), reads=[beg[q]],
                             writes=[bea[q]])
                        P.op("dve", lambda E: E.tensor_tensor(ea[q][:], ea[q][:], eu[q][:], ALU.mult),
                             reads=[bea[q], beu[q]], writes=[bea[q]])
                        P.op("pool", lambda E: E.tensor_tensor(eb[q][:], ea[q][:],
                                                               wb[w][:, sl].unsqueeze(1).to_broadcast([128, 2, TB]),
                                                               ALU.mult), reads=[bea[q], bwb[w]], writes=[beb[q]])
                        for m in range(8):
                            for f in range(2):
                                P.op("pe", lambda E: E.matmul(ey[:, m, :], wd[w][:, f, m * 128:(m + 1) * 128],
                                                              eb[q][:, f, :], start=(f == 0), stop=(f == 1)),
                                     reads=[bwd[w], beb[q]], writes=[beyb[m // 2]])
                        for m in range(8):
                            P.op("dve", lambda E: E.scalar_tensor_tensor(xT[:, m, sl], ey[:, m, :], modc(l, 5, m, s),
                                                                         xT[:, m, sl], ALU.mult, ALU.add),
                                 reads=[beyb[m // 2], bmod, bx[m]], writes=[bx[m]])
            P.barrier()

        STAGES = dbg.get("stages", "mgpoLME") if dbg else "mgpoLME"
        for s in range(NSEQ):
            for kt in range(8):
                P.dma("sp", xT[:, kt, :], dr["xT"][s, kt * 128:(kt + 1) * 128, :], writes=[bx[kt]])
            for l in range(DEPTH):
                modulate(l, s, 0, 1)
                if "z" in STAGES:
                    for kt in range(8):
                        P.op("pool", lambda E: E.memset(mixT[:, kt, :], 0.0), writes=[bmix[kt]])
                if "m" in STAGES:
                    mla_stage(l, s)
                if "g" in STAGES:
                    gdn_stage(l, s)
                if "p" in STAGES:
                    pool_stage(l, s)
                if dbg and dbg.get("dump") == "mix" and s == dbg.get("s", 0) and l == dbg.get("l", 0):
                    with contextlib.ExitStack() as _S:
                        dmp = _S.enter_context(sb("dmp", [128, 8, T]))
                        bd = Buf()
                        for kt in range(8):
                            P.op("dve", lambda E: E.tensor_copy(dmp[:, kt, :], (hT if dbg.get("src") == "h" else mixT)[:, kt, :]), reads=[bmix[kt], bh[kt]],
                                 writes=[bd])
                        P.dma("sp", dbg_out.rearrange("(k p) t -> p k t", p=128), dmp[:], reads=[bd])
                        P.barrier()
                if "o" in STAGES:
                    out_proj(l, s)
                if "L" in STAGES:
                    layer_norm(l, K_LN1G, K_LN1B)
                if "M" in STAGES:
                    moe_stage(l, s)
                if "E" in STAGES:
                    layer_norm(l, K_LN2G, K_LN2B)
            for kt in range(8):
                P.dma("sp", yT[s, kt * 128:(kt + 1) * 128, :], xT[:, kt, :], reads=[bx[kt]])
        P.barrier()
    P.es.close()
    return nc


def make_consts():
    c = np.zeros((128, NCON), np.float32)
    c[:, C_ID:C_ID + 128] = np.eye(128, dtype=np.float32)
    c[:, C_ONE:C_ONE + 128] = 1.0
    i = np.arange(64)
    c[:64, C_U:C_U + 64] = (i[:, None] <= i[None, :]).astype(np.float32)
    c[:64, C_MLO:C_MLO + 64] = np.where(i[:, None] > i[None, :], 0.0, NEG)
    c[:64, C_MUP:C_MUP + 64] = np.where(i[None, :] >= i[:, None], 0.0, NEG)
    t = np.arange(16)
    for j in range(2):
        for p in range(128):
            w = 2 ** (2 * j + p // 64 + 1)
            c[p, C_INV16 + 16 * j:C_INV16 + 16 * j + 16] = 1.0 / np.minimum(t + 1, w)
    p = np.arange(128)
    c[:, C_BD:C_BD + 128] = (p[:, None] // 64 == p[None, :] // 64).astype(np.float32)
    return c


def make_rope(T):
    inv_freq = np.power(np.float32(10000.0), -np.arange(0, 32, 2, dtype=np.float32) / np.float32(32)).astype(np.float32)
    ang = (np.arange(T, dtype=np.float32)[:, None] * inv_freq[None, :]).astype(np.float32)
    cos, sin = np.cos(ang).astype(np.float32).T, np.sin(ang).astype(np.float32).T
    r = np.zeros((32, 2, T), np.float32)
    r[:16, 0], r[16:, 0], r[:16, 1], r[16:, 1] = cos, cos, sin, sin
    return r


def make_cols(inp):
    L = inp["w_in"].shape[0]
    cols = np.zeros((L, 128, NCOLS), np.float32)

    def put(k, vec, n):
        cols[:, :, k:k + n] = vec.reshape(L, n, 128).transpose(0, 2, 1)

    put(K_QN, inp["mla_q_norm"], 2); put(K_KVN, inp["mla_kv_norm"], 1)
    put(K_LN1G, inp["ln1_g"], 8); put(K_LN1B, inp["ln1_b"], 8); put(K_LN2G, inp["ln2_g"], 8); put(K_LN2B, inp["ln2_b"], 8)
    put(K_PS, inp["pool_scale"], 2)
    cols[:, :, K_GO] = np.concatenate([inp["gdn_out_norm"], inp["gdn_out_norm"]], axis=1)
    cv = inp["gdn_conv"].reshape(L, 4, 6, 128)
    cols[:, :, K_CONV:K_CONV + 24] = cv.transpose(0, 3, 2, 1).reshape(L, 128, 24)
    put(K_BMOD, inp["b_mod"], 48)
    return cols


_NC_CACHE = {}


def run(inp, T, DEPTH, NSEQ, ncores, dbg=None):
    key = (T, DEPTH, NSEQ, repr(dbg))
    if key not in _NC_CACHE:
        _NC_CACHE[key] = build(T, DEPTH, NSEQ, dbg)
    nc = _NC_CACHE[key]
    f = lambda a: np.ascontiguousarray(np.asarray(a, dtype=np.float32))
    shared = {
        "consts": make_consts(), "rope": make_rope(T), "cols": f(make_cols(inp)),
        "w_in": f(inp["w_in"]), "mla_w_uq": f(inp["mla_w_uq"]), "mla_w_ukv": f(inp["mla_w_ukv"]),
        "gdn_a_log": f(inp["gdn_a_log"]), "gdn_dt_bias": f(inp["gdn_dt_bias"]), "pool_w": f(inp["pool_w"]),
        "w_out": f(inp["w_out"]), "w_mod": f(inp["w_mod"]),
        "w_r": f(np.concatenate([inp["router_w_group"], inp["router_w_expert"]], axis=2)),
        "b_r": f(np.concatenate([inp["router_b_group"], inp["router_b_expert"]], axis=1)),
        "moe_w_gate": f(inp["moe_w_gate"]), "moe_w_up": f(inp["moe_w_up"]), "moe_w_down": f(inp["moe_w_down"]),
    }
    x = np.asarray(inp["x"], np.float32); c = np.asarray(inp["c"], np.float32)
    in_maps = []
    for i in range(ncores):
        m = dict(shared)
        m["xT"] = f(x[i * NSEQ:(i + 1) * NSEQ].transpose(0, 2, 1))
        m["cT"] = f(c[i * NSEQ:(i + 1) * NSEQ].T)
        in_maps.append(m)
    res = run_bass_kernel_spmd(nc, in_maps, core_ids=list(range(ncores)))
    out = np.concatenate([r["yT"].transpose(0, 2, 1) for r in res.results], axis=0)
    return out, res


def kernel(**inputs):
    out, _ = run(inputs, 2048, 4, 2, 8)
    return out.astype(np.float32)
```

```python
import contextlib
import numpy as np
import concourse.bass as bass
import concourse.mybir as mybir
from concourse.bass_utils import run_bass_kernel_spmd

F32 = mybir.dt.float32
BF16 = mybir.dt.bfloat16
AF = mybir.ActivationFunctionType
ALU = mybir.AluOpType
AX = mybir.AxisListType

SEG = 28000
ENGS = ("pe", "dve", "act", "pool", "sp")
NDMA = 8

D = 1024
NEXP = 32
ALPHA = 8.0 ** 0.25
LN_EPS = 1e-5
RMS_EPS = 1e-6
NEG = -30000.0

C_ID, C_ONE, C_U, C_MLO, C_MUP, C_INV16, C_BD, NCON = 0, 128, 256, 320, 384, 448, 480, 608
K_QN, K_KVN, K_LN1G, K_LN1B, K_LN2G, K_LN2B, K_PS, K_GO, K_CONV, K_BMOD, NCOLS = 0, 2, 3, 11, 19, 27, 35, 37, 38, 62, 110


class Buf:
    __slots__ = ("name", "lw", "rd", "excl")

    def __init__(self, name="", excl=False):
        self.name = name
        self.lw = None
        self.rd = {}
        self.excl = excl


def PB():
    return Buf("psum", True)


class Prog:
    def __init__(self, nc, same_engine_sync=True):
        self.nc = nc
        self.es = contextlib.ExitStack()
        self.cnt = {e: 0 for e in ENGS}
        self.seen = {e: {} for e in ENGS}
        self.sems = {}
        self.dma_i = {e: 0 for e in ENGS}
        self.same = same_engine_sync
        self.E = {"pe": nc.tensor, "dve": nc.vector, "act": nc.scalar, "pool": nc.gpsimd, "sp": nc.sync}
        self.last_dma = {}

    def sem(self, key):
        if key not in self.sems:
            nm = "s_" + "_".join(str(k) for k in key)
            self.sems[key] = self.es.enter_context(self.nc.semaphore(nm))
        return self.sems[key]

    def _wait(self, eng, tok):
        key, val = tok
        if self.seen[eng].get(key, 0) >= val:
            return
        if key[0] == eng and (eng == "pe" or not self.same):
            return
        self.seen[eng][key] = val
        self.E[eng].wait_ge(self.sem(key), val)

    def _deps(self, eng, reads, writes):
        for b in reads:
            if b.lw is not None:
                self._wait(eng, b.lw)
        for b in writes:
            if b.excl:
                if b.lw is not None and b.lw[0][0] != eng:
                    self._wait(eng, b.lw)
                for k, v in b.rd.items():
                    if k[0] != eng:
                        self._wait(eng, (k, v))
                continue
            if b.lw is not None:
                self._wait(eng, b.lw)
            for k, v in b.rd.items():
                if k[0] != eng:
                    self._wait(eng, (k, v))

    def _mark(self, tok, reads, writes):
        k, v = tok
        for b in reads:
            if b.rd.get(k, 0) < v:
                b.rd[k] = v
        for b in writes:
            if b.excl:
                if b.lw is not None and b.lw[0][0] != k[0]:
                    pk, pv = b.lw
                    if b.rd.get(pk, 0) < pv:
                        b.rd[pk] = pv
                b.rd = {kk: vv for kk, vv in b.rd.items() if kk[0] != k[0]}
                b.lw = tok
                continue
            b.lw = tok
            b.rd = {}

    def op(self, eng, fn, reads=(), writes=()):
        if any(b.excl for b in reads):
            writes = list(writes) + [b for b in reads if b.excl]
            reads = [b for b in reads if not b.excl]
        self._deps(eng, reads, writes)
        n = self.cnt[eng]
        key = (eng, n // SEG)
        val = n % SEG + 1
        fn(self.E[eng]).then_inc(self.sem(key), 1)
        self.cnt[eng] = n + 1
        self._mark((key, val), reads, writes)

    def dma(self, eng, out, in_, reads=(), writes=(), **kw):
        i = self.dma_i[eng]
        self.dma_i[eng] = i + 1
        key = ("d" + eng, i % NDMA)
        val = (i // NDMA + 1) * 16
        if val > 16:
            self._wait(eng, (key, val - 16))
        self._deps(eng, reads, writes)
        self.E[eng].dma_start(out=out, in_=in_, **kw).then_inc(self.sem(key), 16)
        self.last_dma[key] = val
        self._mark((key, val), reads, writes)

    def barrier(self):
        toks = []
        for e in ENGS:
            n = self.cnt[e]
            if n:
                toks.append(((e, (n - 1) // SEG), (n - 1) % SEG + 1))
        toks += list(self.last_dma.items())
        for e in ENGS:
            for t in toks:
                self._wait(e, t)


def build(T, DEPTH, NSEQ, dbg=None):
    nc = bass.Bass("TRN2", target_bir_lowering=False)
    P = Prog(nc)
    NB = T // 512
    NT = T // 128
    NCH = T // 64
    dr = {}

    def din(name, shape):
        dr[name] = nc.dram_tensor(name, list(shape), F32, kind="ExternalInput").ap()

    din("xT", [NSEQ, D, T]); din("cT", [D, NSEQ]); din("consts", [128, NCON]); din("rope", [32, 2, T])
    din("cols", [4, 128, NCOLS]); din("w_in", [4, D, 1704]); din("mla_w_uq", [4, 256, 768])
    din("mla_w_ukv", [4, 128, 1024]); din("gdn_a_log", [4, 4]); din("gdn_dt_bias", [4, 4])
    din("pool_w", [4, 4, 64, 64]); din("w_out", [4, D, D]); din("w_mod", [4, D, 6 * D])
    din("w_r", [4, D, 36]); din("b_r", [4, 36])
    din("moe_w_gate", [4, NEXP, D, 256]); din("moe_w_up", [4, NEXP, D, 256]); din("moe_w_down", [4, NEXP, 256, D])
    yT = nc.dram_tensor("yT", [NSEQ, D, T], F32, kind="ExternalOutput").ap()
    scr = nc.dram_tensor("scr_wt", [NEXP, T], F32).ap()
    bscr = Buf("scr")
    if dbg:
        dbg_out = nc.dram_tensor("dbg", [D, T], F32, kind="ExternalOutput").ap()

    uid = [0]

    def sb(name, shape, dt=F32):
        uid[0] += 1
        return nc.sbuf_tensor("%s_%d" % (name, uid[0]), list(shape), dt)

    @contextlib.contextmanager
    def ps(name, shape, dt=F32):
        uid[0] += 1
        isz = 4 if dt == F32 else 2
        per = 2048 // isz
        n = 1
        for d_ in shape[1:]:
            n *= d_
        nb = (n + per - 1) // per
        with nc.psum_tensor("%s_%d" % (name, uid[0]), [128, nb * per], dt) as t:
            v = t[0:shape[0], 0:n]
            if len(shape) == 3:
                v = v.rearrange("p (a b) -> p a b", a=shape[1])
            elif len(shape) == 4:
                v = v.rearrange("p (a b c) -> p a b c", a=shape[1], b=shape[2])
            yield v

    es = contextlib.ExitStack()
    with es:
        cst = es.enter_context(sb("cst", [128, NCON])); bcst = Buf()
        idb = es.enter_context(sb("idb", [128, 128], BF16))
        oneb = es.enter_context(sb("oneb", [128, 128], BF16))
        colsT = es.enter_context(sb("colsT", [128, DEPTH, NCOLS])); bcols = Buf()
        modT = es.enter_context(sb("modT", [128, DEPTH, 48, NSEQ])); bmod = Buf()
        xT = es.enter_context(sb("xT_sb", [128, 8, T])); bx = [Buf() for _ in range(8)]
        hT = es.enter_context(sb("hT_sb", [128, 8, T], BF16)); bh = [Buf() for _ in range(8)]
        mixT = es.enter_context(sb("mixT_sb", [128, 8, T], BF16)); bmix = [Buf() for _ in range(8)]

        P.dma("sp", cst[:], dr["consts"][:, :], writes=[bcst])
        for l in range(DEPTH):
            P.dma("sp", colsT[:, l, :], dr["cols"][l, :, :], writes=[bcols])
        P.op("dve", lambda E: E.tensor_copy(idb[:], cst[:, C_ID:C_ID + 128]), reads=[bcst], writes=[bcst])
        P.op("dve", lambda E: E.tensor_copy(oneb[:], cst[:, C_ONE:C_ONE + 128]), reads=[bcst], writes=[bcst])
        ident = cst[:, C_ID:C_ID + 128]
        ones = cst[:, C_ONE:C_ONE + 128]

        def col(l, k):
            return colsT[:, l, k:k + 1]

        with contextlib.ExitStack() as _S:
            scT = _S.enter_context(sb("scT", [128, 8, NSEQ]))
            sgT = _S.enter_context(sb("sgT", [128, 8, NSEQ]))
            wm0 = _S.enter_context(sb("wm0", [128, 8, 768]))
            wm1 = _S.enter_context(sb("wm1", [128, 8, 768]))
            modps = _S.enter_context(ps("modps", [128, 48, NSEQ]))
            bsc = Buf(); bwm = [Buf(), Buf()]; bmp = PB()
            wms = [wm0, wm1]
            P.dma("sp", scT[:], dr["cT"].rearrange("(k p) s -> p k s", p=128), writes=[bsc])
            P.op("act", lambda E: E.activation(sgT[:], scT[:], AF.Sigmoid), reads=[bsc], writes=[bsc])
            P.op("dve", lambda E: E.tensor_tensor(scT[:], scT[:], sgT[:], ALU.mult), reads=[bsc], writes=[bsc])
            ci = 0
            for l in range(DEPTH):
                for c in range(8):
                    wm = wms[ci % 2]; bw = bwm[ci % 2]; ci += 1
                    P.dma("sp", wm[:], dr["w_mod"][l, :, c * 768:(c + 1) * 768].rearrange("(k p) n -> p k n", p=128),
                          writes=[bw])
                    for mm in range(6):
                        m = c * 6 + mm
                        for kt in range(8):
                            P.op("pe", lambda E: E.matmul(modps[:, m, :], wm[:, kt, mm * 128:(mm + 1) * 128],
                                                          scT[:, kt, :], start=(kt == 0), stop=(kt == 7)),
                                 reads=[bw, bsc], writes=[bmp])
                P.op("dve", lambda E: E.tensor_tensor(
                    modT[:, l, :, :], modps[:],
                    colsT[:, l, K_BMOD:K_BMOD + 48].unsqueeze(2).to_broadcast([128, 48, NSEQ]), ALU.add),
                    reads=[bmp, bcols], writes=[bmod])
                for a in (8, 32):
                    P.op("dve", lambda E: E.tensor_scalar_add(modT[:, l, a:a + 16, :], modT[:, l, a:a + 16, :], 1.0),
                         reads=[bmod], writes=[bmod])
        P.barrier()

        def modc(l, chunk, kt, s):
            return modT[:, l, chunk * 8 + kt, s:s + 1]

        def layer_norm(l, kg, kb):
            with contextlib.ExitStack() as _S:
                p_s = _S.enter_context(ps("ln_s", [128, 512]))
                p_q = _S.enter_context(ps("ln_q", [128, 512]))
                sq = _S.enter_context(sb("ln_sq", [128, 2, 512]))
                mean = _S.enter_context(sb("ln_mean", [128, 512]))
                rstd = _S.enter_context(sb("ln_rstd", [128, 512]))
                tt = _S.enter_context(sb("ln_t", [128, 2, 512]))
                bps, bpq, bsq, bmean, brstd, btt = PB(), PB(), [Buf(), Buf()], Buf(), Buf(), [Buf(), Buf()]
                for b in range(NB):
                    sl = slice(b * 512, (b + 1) * 512)
                    for kt in range(8):
                        P.op("pe", lambda E: E.matmul(p_s[:], ones, xT[:, kt, sl], start=(kt == 0), stop=(kt == 7)),
                             reads=[bx[kt], bcst], writes=[bps])
                    for kt in range(8):
                        j = kt % 2
                        P.op("act", lambda E: E.activation(sq[:, j, :], xT[:, kt, sl], AF.Square),
                             reads=[bx[kt]], writes=[bsq[j]])
                        P.op("pe", lambda E: E.matmul(p_q[:], ones, sq[:, j, :], start=(kt == 0), stop=(kt == 7)),
                             reads=[bsq[j], bcst], writes=[bpq])
                    P.op("act", lambda E: E.mul(mean[:], p_s[:], 1.0 / D), reads=[bps], writes=[bmean])
                    P.op("dve", lambda E: E.tensor_tensor(rstd[:], mean[:], mean[:], ALU.mult),
                         reads=[bmean], writes=[brstd])
                    P.op("dve", lambda E: E.scalar_tensor_tensor(rstd[:], p_q[:], 1.0 / D, rstd[:], ALU.mult,
                                                                 ALU.subtract), reads=[bpq, brstd], writes=[brstd])
                    P.op("act", lambda E: E.activation(rstd[:], rstd[:], AF.Sqrt, bias=LN_EPS, scale=1.0),
                         reads=[brstd], writes=[brstd])
                    P.op("dve", lambda E: E.reciprocal(rstd[:], rstd[:]), reads=[brstd], writes=[brstd])
                    for kt in range(8):
                        j = kt % 2
                        P.op("pool", lambda E: E.tensor_tensor(tt[:, j, :], xT[:, kt, sl], mean[:], ALU.subtract),
                             reads=[bx[kt], bmean], writes=[btt[j]])
                        P.op("dve", lambda E: E.tensor_tensor(tt[:, j, :], tt[:, j, :], rstd[:], ALU.mult),
                             reads=[btt[j], brstd], writes=[btt[j]])
                        P.op("dve", lambda E: E.tensor_scalar(xT[:, kt, sl], tt[:, j, :], col(l, kg + kt),
                                                              col(l, kb + kt), ALU.mult, ALU.add),
                             reads=[btt[j], bcols], writes=[bx[kt]])
            P.barrier()

        def modulate(l, s, sh_chunk, sc_chunk):
            for kt in range(8):
                P.op("act", lambda E: E.activation(hT[:, kt, :], xT[:, kt, :], AF.Identity,
                                                   bias=modc(l, sh_chunk, kt, s), scale=modc(l, sc_chunk, kt, s)),
                     reads=[bx[kt], bmod], writes=[bh[kt]])

        def load_w_in(wt, bw, l, c0, c1):
            for kt in range(8):
                P.dma("pool", wt[:, kt, :], dr["w_in"][l, kt * 128:(kt + 1) * 128, c0:c1], writes=[bw])

        def pool_stage(l, s):
            with contextlib.ExitStack() as _S:
                pw = _S.enter_context(sb("pw", [128, 8, 256], BF16))
                pbd = _S.enter_context(sb("pbd", [128, 2, 128], BF16))
                px = _S.enter_context(sb("px", [128, 16 + T]))
                pa = _S.enter_context(sb("pa", [128, 16 + T]))
                pb = _S.enter_context(sb("pb", [128, 16 + T]))
                pdl = _S.enter_context(sb("pdl", [128, T], BF16))
                ptmp = _S.enter_context(sb("ptmp", [128, 16]))
                pps0 = _S.enter_context(ps("pps0", [128, 512]))
                pps1 = _S.enter_context(ps("pps1", [128, 512]))
                bpw, bpbd, bpx, bpa, bpb, bpdl, bptmp = Buf(), Buf(), Buf(), Buf(), Buf(), Buf(), Buf()
                pps = [pps0, pps1]; bpps = [PB(), PB()]
                load_w_in(pw, bpw, l, 1448, 1704)
                P.op("pool", lambda E: E.memset(pbd[:], 0.0), writes=[bpbd])
                for g in range(4):
                    j, r = g // 2, g % 2
                    P.dma("pool", pbd[64 * r:64 * r + 64, j, 64 * r:64 * r + 64], dr["pool_w"][l, g, :, :],
                          writes=[bpbd])
                for t in (px, pa, pb):
                    P.op("pool", lambda E: E.memset(t[:, 0:16], 0.0), writes=[bpx, bpa, bpb])
                k = 0
                for j in range(2):
                    for b in range(NB):
                        pp = pps[k % 2]; bpp = bpps[k % 2]; k += 1
                        sl = slice(b * 512, (b + 1) * 512)
                        for kt in range(8):
                            P.op("pe", lambda E: E.matmul(pp[:], pw[:, kt, j * 128:(j + 1) * 128], hT[:, kt, sl],
                                                          start=(kt == 0), stop=(kt == 7)),
                                 reads=[bpw, bh[kt]], writes=[bpp])
                        P.op("act", lambda E: E.copy(px[:, 16 + b * 512:16 + (b + 1) * 512], pp[:]),
                             reads=[bpp], writes=[bpx])

                    def shift_add(dst, src, sh, bd, bs):
                        P.op("dve", lambda E: E.tensor_tensor(dst[:, 16:16 + T], src[:, 16:16 + T],
                                                              src[:, 16 - sh:16 - sh + T], ALU.add),
                             reads=[bs], writes=[bd])

                    def delta(src, bs, r, w):
                        rows = slice(64 * r, 64 * r + 64)
                        P.op("dve", lambda E: E.scalar_tensor_tensor(pdl[rows, :], src[rows, 16:16 + T], 1.0 / w,
                                                                     px[rows, 16:16 + T], ALU.mult, ALU.subtract),
                             reads=[bs, bpx], writes=[bpdl])
                        P.op("dve", lambda E: E.tensor_tensor(ptmp[rows, :], src[rows, 16:32],
                                                              cst[rows, C_INV16 + 16 * j:C_INV16 + 16 * j + 16],
                                                              ALU.mult), reads=[bs, bcst], writes=[bptmp])
                        P.op("dve", lambda E: E.tensor_tensor(pdl[rows, 0:16], ptmp[rows, :], px[rows, 16:32],
                                                              ALU.subtract), reads=[bptmp, bpx, bpdl], writes=[bpdl])

                    shift_add(pa, px, 1, bpa, bpx)
                    if j == 0:
                        delta(pa, bpa, 0, 2)
                    shift_add(pb, pa, 2, bpb, bpa)
                    if j == 0:
                        delta(pb, bpb, 1, 4)
                    else:
                        shift_add(pa, pb, 4, bpa, bpb)
                        delta(pa, bpa, 0, 8)
                        shift_add(pb, pa, 8, bpb, bpa)
                        delta(pb, bpb, 1, 16)
                    for b in range(NB):
                        pp = pps[k % 2]; bpp = bpps[k % 2]; k += 1
                        sl = slice(b * 512, (b + 1) * 512)
                        P.op("pe", lambda E: E.matmul(pp[:], pbd[:, j, :], pdl[:, sl], start=True, stop=True),
                             reads=[bpbd, bpdl], writes=[bpp])
                        P.op("act", lambda E: E.activation(mixT[:, 6 + j, sl], pp[:], AF.Identity, scale=col(l, K_PS + j)),
                             reads=[bpp, bcols], writes=[bmix[6 + j]])
            P.barrier()

        def mla_stage(l, s):
            with contextlib.ExitStack() as _S:
                wA = _S.enter_context(sb("wA", [128, 8, 416], BF16))
                wuq = _S.enter_context(sb("wuq", [128, 2, 768], BF16))
                wuqr = _S.enter_context(sb("wuqr", [128, 2, 8, 32], BF16))
                wukv = _S.enter_context(sb("wukv", [128, 1024], BF16))
                wkr = _S.enter_context(sb("wkr", [128, 8, 32], BF16))
                cqn = _S.enter_context(sb("cqn", [128, 2, T], BF16))
                ckvn = _S.enter_context(sb("ckvn", [128, T], BF16))
                krope = _S.enter_context(sb("krope", [32, T], BF16))
                rp0 = _S.enter_context(sb("rp0", [32, 2, 512]))
                rp1 = _S.enter_context(sb("rp1", [32, 2, 512]))
                QT = _S.enter_context(sb("QT", [64, T], BF16))
                qrp = _S.enter_context(sb("qr", [32, T], BF16))
                KT = _S.enter_context(sb("KT", [64, T], BF16))
                Vh = _S.enter_context(sb("Vh", [128, NT, 64], BF16))
                csb = _S.enter_context(sb("csb", [128, 3, 512]))
                sq = _S.enter_context(sb("sq", [128, 3, 512], BF16))
                rs = _S.enter_context(sb("rs", [128, 2, 512]))
                rt = _S.enter_context(sb("rt", [32, 2, 512]))
                pt0 = _S.enter_context(sb("pt0", [128, 512], BF16))
                pt1 = _S.enter_context(sb("pt1", [128, 512], BF16))
                pt2 = _S.enter_context(sb("pt2", [128, 512], BF16))
                rden = _S.enter_context(sb("rden", [64, 512]))
                bwA, bwuq, bwuqr, bwukv, bwkr, bcqn, bckvn, bkrope, brope = [Buf() for _ in range(9)]
                bQT, bqr, bKT, bVh, bcsb, bsq, brs, brt, brden = [Buf() for _ in range(9)]
                pts = [pt0, pt1, pt2]; bpts = [Buf(), Buf(), Buf()]
                load_w_in(wA, bwA, l, 0, 416)
                P.dma("pool", wuq[:], dr["mla_w_uq"][l, :, :].rearrange("(j p) n -> p j n", p=128), writes=[bwuq])
                P.dma("pool", wukv[:], dr["mla_w_ukv"][l, :, :], writes=[bwukv])
                rps = [rp0, rp1]; brps = [Buf(), Buf()]; rpi = [0]

                def load_rope(sl):
                    i = rpi[0] % 2; rpi[0] += 1
                    P.dma("sp", rps[i][:], dr["rope"][:, :, sl], writes=[brps[i]])
                    return rps[i], brps[i]
                P.op("pool", lambda E: E.tensor_scalar_mul(wkr[:, :, 0:16], wA[:, :, 400:416], -1.0),
                     reads=[bwA], writes=[bwkr])
                P.op("pool", lambda E: E.tensor_copy(wkr[:, :, 16:32], wA[:, :, 384:400]), reads=[bwA], writes=[bwkr])
                wq4 = wuq[:].rearrange("p j (h d) -> p j h d", d=96)
                P.op("pool", lambda E: E.tensor_scalar_mul(wuqr[:, :, :, 0:16], wq4[:, :, :, 80:96], -1.0),
                     reads=[bwuq], writes=[bwuqr])
                P.op("pool", lambda E: E.tensor_copy(wuqr[:, :, :, 16:32], wq4[:, :, :, 64:80]),
                     reads=[bwuq], writes=[bwuqr])
                scale = 96.0 ** -0.5
                with contextlib.ExitStack() as _S:
                    m_a = _S.enter_context(ps("m_a", [128, 3, 512]))
                    m_s = _S.enter_context(ps("m_s", [128, 2, 512]))
                    m_r = _S.enter_context(ps("m_r", [32, 2, 512]))
                    bma, bms, bmr = PB(), PB(), PB()
                    for b in range(NB):
                        sl = slice(b * 512, (b + 1) * 512)
                        for j in range(3):
                            for kt in range(8):
                                P.op("pe", lambda E: E.matmul(m_a[:, j, :], wA[:, kt, j * 128:(j + 1) * 128],
                                                              hT[:, kt, sl], start=(kt == 0), stop=(kt == 7)),
                                     reads=[bwA, bh[kt]], writes=[bma])
                        P.op("act", lambda E: E.copy(csb[:], m_a[:]), reads=[bma], writes=[bcsb])
                        P.op("act", lambda E: E.activation(sq[:], m_a[:], AF.Square), reads=[bma], writes=[bsq])
                        P.op("pe", lambda E: E.matmul(m_s[:, 0, :], oneb[:], sq[:, 0, :], start=True, stop=False),
                             reads=[bsq, bcst], writes=[bms])
                        P.op("pe", lambda E: E.matmul(m_s[:, 0, :], oneb[:], sq[:, 1, :], start=False, stop=True),
                             reads=[bsq, bcst], writes=[bms])
                        P.op("pe", lambda E: E.matmul(m_s[:, 1, :], oneb[:], sq[:, 2, :], start=True, stop=True),
                             reads=[bsq, bcst], writes=[bms])
                        P.op("act", lambda E: E.activation(rs[:, 0, :], m_s[:, 0, :], AF.Sqrt, bias=RMS_EPS,
                                                           scale=1.0 / 256), reads=[bms], writes=[brs])
                        P.op("act", lambda E: E.activation(rs[:, 1, :], m_s[:, 1, :], AF.Sqrt, bias=RMS_EPS,
                                                           scale=1.0 / 128), reads=[bms], writes=[brs])
                        P.op("dve", lambda E: E.reciprocal(rs[:], rs[:]), reads=[brs], writes=[brs])
                        for j in range(2):
                            P.op("dve", lambda E: E.scalar_tensor_tensor(cqn[:, j, sl], csb[:, j, :],
                                                                         col(l, K_QN + j), rs[:, 0, :], ALU.mult,
                                                                         ALU.mult),
                                 reads=[bcsb, brs, bcols], writes=[bcqn])
                        P.op("dve", lambda E: E.scalar_tensor_tensor(ckvn[:, sl], csb[:, 2, :], col(l, K_KVN),
                                                                     rs[:, 1, :], ALU.mult, ALU.mult),
                             reads=[bcsb, brs, bcols], writes=[bckvn])
                        for kt in range(8):
                            P.op("pe", lambda E: E.matmul(m_r[:, 0, :], wA[:, kt, 384:416], hT[:, kt, sl],
                                                          start=(kt == 0), stop=(kt == 7)),
                                 reads=[bwA, bh[kt]], writes=[bmr])
                        for kt in range(8):
                            P.op("pe", lambda E: E.matmul(m_r[:, 1, :], wkr[:, kt, :], hT[:, kt, sl],
                                                          start=(kt == 0), stop=(kt == 7)),
                                 reads=[bwkr, bh[kt]], writes=[bmr])
                        rpt, brp = load_rope(sl)
                        P.op("dve", lambda E: E.tensor_tensor(rt[:], m_r[:], rpt[:], ALU.mult),
                             reads=[bmr, brp], writes=[brt])
                        P.op("dve", lambda E: E.tensor_tensor(krope[:, sl], rt[:, 0, :], rt[:, 1, :], ALU.add),
                             reads=[brt], writes=[bkrope])
                    P.barrier()
                with contextlib.ExitStack() as _S:
                    h_q = _S.enter_context(ps("h_q", [64, 512]))
                    h_r = _S.enter_context(ps("h_r", [32, 2, 512]))
                    h_v = _S.enter_context(ps("h_v", [128, 4, 128]))
                    a_s0 = _S.enter_context(ps("a_s0", [128, 512]))
                    a_s1 = _S.enter_context(ps("a_s1", [128, 512]))
                    a_o = _S.enter_context(ps("a_ob", [64, 512]))
                    a_d = _S.enter_context(ps("a_db", [64, 512]))
                    h_k = h_q
                    bhq, bhr, bhv, bao, bad = [PB() for _ in range(5)]
                    bhk = bhq
                    a_s = [a_s0, a_s1]; bas = [PB(), PB()]
                    pi = 0
                    for h in range(8):
                        for b in range(NB):
                            sl = slice(b * 512, (b + 1) * 512)
                            for j in range(2):
                                P.op("pe", lambda E: E.matmul(h_q[:], wuq[:, j, 96 * h:96 * h + 64], cqn[:, j, sl],
                                                              start=(j == 0), stop=(j == 1)),
                                     reads=[bwuq, bcqn], writes=[bhq])
                            P.op("act", lambda E: E.copy(QT[:, sl], h_q[:]), reads=[bhq], writes=[bQT])
                            for j in range(2):
                                P.op("pe", lambda E: E.matmul(h_r[:, 0, :], wuq[:, j, 96 * h + 64:96 * h + 96],
                                                              cqn[:, j, sl], start=(j == 0), stop=(j == 1)),
                                     reads=[bwuq, bcqn], writes=[bhr])
                            for j in range(2):
                                P.op("pe", lambda E: E.matmul(h_r[:, 1, :], wuqr[:, j, h, :], cqn[:, j, sl],
                                                              start=(j == 0), stop=(j == 1)),
                                     reads=[bwuqr, bcqn], writes=[bhr])
                            rpt, brp = load_rope(sl)
                            P.op("dve", lambda E: E.tensor_tensor(rt[:], h_r[:], rpt[:], ALU.mult),
                                 reads=[bhr, brp], writes=[brt])
                            P.op("dve", lambda E: E.tensor_tensor(qrp[:, sl], rt[:, 0, :], rt[:, 1, :], ALU.add),
                                 reads=[brt], writes=[bqr])
                            P.op("pe", lambda E: E.matmul(h_k[:], wukv[:, 128 * h:128 * h + 64], ckvn[:, sl],
                                                          start=True, stop=True), reads=[bwukv, bckvn], writes=[bhk])
                            P.op("act", lambda E: E.copy(KT[:, sl], h_k[:]), reads=[bhk], writes=[bKT])
                            for tq in range(4):
                                tt_ = b * 4 + tq
                                P.op("pe", lambda E: E.matmul(h_v[:, tq, :], ckvn[:, tt_ * 128:(tt_ + 1) * 128],
                                                              wukv[:, 128 * h:128 * h + 128], start=True, stop=True),
                                     reads=[bwukv, bckvn], writes=[bhv])
                            P.op("act", lambda E: E.copy(Vh[:, b * 4:(b + 1) * 4, :], h_v[:, :, 64:128]),
                                 reads=[bhv], writes=[bVh])
                        r = h % 2
                        steps = []
                        for g in range(NB):
                            for kb in range(4 * g + 4):
                                steps.append((g, kb, kb == 0, kb == 4 * g + 3))

                        def issue_S(i):
                            g, kb, first, last = steps[i]
                            sps = a_s[i % 2]; bsp = bas[i % 2]
                            qs = slice(g * 512, (g + 1) * 512); ks = slice(kb * 128, (kb + 1) * 128)
                            P.op("pe", lambda E: E.matmul(sps, KT[:, ks], QT[:, qs], start=True, stop=False),
                                 reads=[bKT, bQT], writes=[bsp])
                            P.op("pe", lambda E: E.matmul(sps, krope[:, ks], qrp[:, qs], start=False, stop=True),
                                 reads=[bkrope, bqr], writes=[bsp])

                        def issue_rest(i):
                            g, kb, first, last = steps[i]
                            sps = a_s[i % 2]; bsp = bas[i % 2]
                            pt = pts[i % 3]; bpt = bpts[i % 3]
                            qs = slice(g * 512, (g + 1) * 512)
                            P.op("act", lambda E: E.activation(pt[:], sps, AF.Exp, scale=scale), reads=[bsp],
                                 writes=[bpt])
                            ii = kb - 4 * g
                            if ii >= 0:
                                if ii > 0:
                                    P.op("pool", lambda E: E.memset(pt[:, 0:ii * 128], 0.0), writes=[bpt])
                                P.op("pool", lambda E: E.memset(pt[64:128, ii * 128:ii * 128 + 64], 0.0),
                                     writes=[bpt])
                            P.op("pe", lambda E: E.matmul(a_o, Vh[:, kb, :], pt[:], start=first, stop=last),
                                 reads=[bVh, bpt], writes=[bao])
                            P.op("pe", lambda E: E.matmul(a_d, oneb[:, 0:64], pt[:], start=first, stop=last),
                                 reads=[bpt, bcst], writes=[bad])
                            if last:
                                P.op("dve", lambda E: E.reciprocal(rden[:], a_d), reads=[bad], writes=[brden])
                                P.op("dve", lambda E: E.tensor_tensor(mixT[64 * r:64 * r + 64, h // 2, qs], a_o,
                                                                      rden[:], ALU.mult),
                                     reads=[bao, brden], writes=[bmix[h // 2]])

                        issue_S(0)
                        for i in range(len(steps)):
                            if i + 1 < len(steps):
                                issue_S(i + 1)
                            issue_rest(i)
            P.barrier()

        def gdn_stage(l, s):
            with contextlib.ExitStack() as _S:
                gw = _S.enter_context(sb("gw", [128, 8, 256], BF16))
                gwab = _S.enter_context(sb("gwab", [128, 8, 8], BF16))
                gqT = _S.enter_context(sb("gqT", [128, 2, T], BF16))
                gkT = _S.enter_context(sb("gkT", [128, 2, T], BF16))
                gvT = _S.enter_context(sb("gvT", [128, 2, T], BF16))
                gzT = _S.enter_context(sb("gzT", [128, 2, T], BF16))
                gpre = _S.enter_context(sb("gpre", [128, 3 + T]))
                gacc = _S.enter_context(sb("gacc", [128, 512]))
                gsig = _S.enter_context(sb("gsig", [128, 512]))
                grn = _S.enter_context(sb("grn", [128, 512]))
                gpar = _S.enter_context(sb("gpar", [64, 12]))
                bgw, bgwab, bgq, bgk, bgv, bgz, bgpre, bgacc, bgsig, bgrn, bgpar = [Buf() for _ in range(11)]
                load_w_in(gwab, bgwab, l, 1440, 1448)
                P.dma("sp", gpar[:, 0:4], dr["gdn_a_log"][l:l + 1, :].partition_broadcast(64), writes=[bgpar])
                P.dma("sp", gpar[:, 4:8], dr["gdn_dt_bias"][l:l + 1, :].partition_broadcast(64), writes=[bgpar])
                P.op("act", lambda E: E.activation(gpar[:, 8:12], gpar[:, 0:4], AF.Exp), reads=[bgpar], writes=[bgpar])
                P.op("dve", lambda E: E.tensor_scalar_mul(gpar[:, 8:12], gpar[:, 8:12], -1.0), reads=[bgpar],
                     writes=[bgpar])
                P.op("pool", lambda E: E.memset(gpre[:, 0:3], 0.0), writes=[bgpre])
                bd = cst[:, C_BD:C_BD + 128]
                with contextlib.ExitStack() as _S:
                    g_p0 = _S.enter_context(ps("g_p0", [128, 512]))
                    g_p1 = _S.enter_context(ps("g_p1", [128, 512]))
                    g_ss = _S.enter_context(ps("g_ss", [128, 512]))
                    gps_ = [g_p0, g_p1]; bgps = [PB(), PB()]; bgss = PB()
                    k = 0
                    for gi, (c0, dst, bdst) in enumerate(((416, gqT, bgq), (672, gkT, bgk), (928, gvT, bgv),
                                                           (1184, gzT, bgz))):
                        load_w_in(gw, bgw, l, c0, c0 + 256)
                        for j in range(2):
                            if gi == 3:
                                for b in range(NB):
                                    sl = slice(b * 512, (b + 1) * 512)
                                    pp = gps_[k % 2]; bpp = bgps[k % 2]; k += 1
                                    for kt in range(8):
                                        P.op("pe", lambda E: E.matmul(pp[:], gw[:, kt, j * 128:(j + 1) * 128],
                                                                      hT[:, kt, sl], start=(kt == 0), stop=(kt == 7)),
                                             reads=[bgw, bh[kt]], writes=[bpp])
                                    P.op("act", lambda E: E.activation(gsig[:], pp[:], AF.Sigmoid), reads=[bpp],
                                         writes=[bgsig])
                                    P.op("dve", lambda E: E.tensor_tensor(dst[:, j, sl], pp[:], gsig[:], ALU.mult),
                                         reads=[bpp, bgsig], writes=[bdst])
                                continue
                            for b in range(NB):
                                sl = slice(b * 512, (b + 1) * 512)
                                pp = gps_[k % 2]; bpp = bgps[k % 2]; k += 1
                                for kt in range(8):
                                    P.op("pe", lambda E: E.matmul(pp[:], gw[:, kt, j * 128:(j + 1) * 128],
                                                                  hT[:, kt, sl], start=(kt == 0), stop=(kt == 7)),
                                         reads=[bgw, bh[kt]], writes=[bpp])
                                P.op("act", lambda E: E.copy(gpre[:, 3 + b * 512:3 + (b + 1) * 512], pp[:]),
                                     reads=[bpp], writes=[bgpre])
                            ct = gi * 2 + j
                            for b in range(NB):
                                sl = slice(b * 512, (b + 1) * 512)
                                P.op("dve", lambda E: E.tensor_scalar(gacc[:], gpre[:, b * 512:b * 512 + 512],
                                                                      col(l, K_CONV + ct * 4), None, ALU.mult),
                                     reads=[bgpre, bcols], writes=[bgacc])
                                for tap in range(1, 4):
                                    P.op("dve", lambda E: E.scalar_tensor_tensor(
                                        gacc[:], gpre[:, b * 512 + tap:b * 512 + tap + 512],
                                        col(l, K_CONV + ct * 4 + tap), gacc[:], ALU.mult, ALU.add),
                                        reads=[bgpre, bcols, bgacc], writes=[bgacc])
                                P.op("act", lambda E: E.activation(gsig[:], gacc[:], AF.Sigmoid), reads=[bgacc],
                                     writes=[bgsig])
                                if gi == 2:
                                    P.op("dve", lambda E: E.tensor_tensor(dst[:, j, sl], gacc[:], gsig[:], ALU.mult),
                                         reads=[bgacc, bgsig], writes=[bdst])
                                    continue
                                P.op("dve", lambda E: E.tensor_tensor(gacc[:], gacc[:], gsig[:], ALU.mult),
                                     reads=[bgacc, bgsig], writes=[bgacc])
                                P.op("act", lambda E: E.activation(gsig[:], gacc[:], AF.Square), reads=[bgacc],
                                     writes=[bgsig])
                                P.op("pe", lambda E: E.matmul(g_ss[:], bd, gsig[:], start=True, stop=True),
                                     reads=[bgsig, bcst], writes=[bgss])
                                P.op("act", lambda E: E.activation(grn[:], g_ss[:], AF.Sqrt, bias=RMS_EPS, scale=1.0),
                                     reads=[bgss], writes=[bgrn])
                                P.op("dve", lambda E: E.reciprocal(grn[:], grn[:]), reads=[bgrn], writes=[bgrn])
                                P.op("dve", lambda E: E.scalar_tensor_tensor(dst[:, j, sl], gacc[:],
                                                                             0.125 if gi == 0 else 1.0, grn[:],
                                                                             ALU.mult, ALU.mult),
                                     reads=[bgacc, bgrn], writes=[bdst])
                P.barrier()
                U = cst[0:64, C_U:C_U + 64]
                MLO = cst[0:64, C_MLO:C_MLO + 64]
                MUP = cst[0:64, C_MUP:C_MUP + 64]
                with contextlib.ExitStack() as _S:
                    sm = _S.enter_context(sb("c_sm", [64, 64]))
                    Gd = _S.enter_context(sb("c_Gd", [64, 4, 64]))
                    Dd = _S.enter_context(sb("c_D", [64, 4, 64]))
                    Ee = _S.enter_context(sb("c_E", [64, 4, 64]))
                    Et = _S.enter_context(sb("c_Et", [64, 4, 64]))
                    EGB = _S.enter_context(sb("c_EGB", [128, 256]))
                    tA = _S.enter_context(sb("c_tA", [64, 4, 64]))
                    A0 = _S.enter_context(sb("c_A0", [64, 4, 64], BF16))
                    A1 = _S.enter_context(sb("c_A1", [64, 4, 64], BF16))
                    B0 = _S.enter_context(sb("c_B0", [64, 4, 64], BF16))
                    B1 = _S.enter_context(sb("c_B1", [64, 4, 64], BF16))
                    Pm = _S.enter_context(sb("c_P", [64, 4, 64], BF16))
                    qkT = _S.enter_context(sb("c_qk", [64, 4, 64], BF16))
                    vtm = _S.enter_context(sb("c_vtm", [64, 4, 64]))
                    KDP = _S.enter_context(sb("c_KDP", [64, 4, 128], BF16))
                    VNP = _S.enter_context(sb("c_VNP", [64, 4, 128], BF16))
                    Rb = _S.enter_context(sb("c_R", [64, 4, 64], BF16))
                    t1 = _S.enter_context(sb("c_t1", [64, 4, 64]))
                    qdT = _S.enter_context(sb("c_qd", [128, 2, 64], BF16))
                    SP = _S.enter_context(sb("c_SP", [128, 2, 128]))
                    SPb = _S.enter_context(sb("c_SPb", [128, 2, 128], BF16))
                    glc = _S.enter_context(sb("c_gl", [128, 2]))
                    osq = _S.enter_context(sb("c_osq", [128, 2, 64]))
                    ors = _S.enter_context(sb("c_ors", [128, 2, 64]))
                    otm = _S.enter_context(sb("c_otm", [128, 2, 64]))
                    kmisc = _S.enter_context(ps("k_misc", [128, 512]))
                    kgcb = _S.enter_context(ps("k_gcb", [128, 256]))
                    kgk = _S.enter_context(ps("k_gk", [64, 2, 4, 64]))
                    ksq = _S.enter_context(ps("k_sq", [64, 2, 4, 64]))
                    kpv = _S.enter_context(ps("k_pv", [64, 2, 4, 64]))
                    ktr = _S.enter_context(ps("k_tr", [64, 3, 256], BF16))
                    kps1 = _S.enter_context(ps("k_ps1", [64, 4, 64]))
                    kU = _S.enter_context(ps("k_U", [128, 2, 128]))
                    (bsm, bGd, bD, bE, bEt, bEGB, btA, bP, bqk, bvtm, bKDP, bVNP, bR, bt1, bqd, bSP, bSPb, bgl, bosq,
                     bors, botm) = [Buf() for _ in range(21)]
                    bA = [Buf(), Buf()]; bB = [Buf(), Buf()]
                    b_ab = PB(); b_gcc = b_ab; b_ss = b_ab; b_o = b_ab
                    b_gcb = PB(); b_G = PB(); b_KQ = b_G; b_sq = PB(); b_pu = PB(); b_vn = b_pu
                    b_trB = PB(); b_trv = b_trB; b_trk = b_trB; b_ps1 = PB(); b_U = PB()
                    As = [A0, A1]; Bs = [B0, B1]
                    ab_ps = kmisc[0:64, 0:8]; gcc_ps = kmisc[0:64, 8:12]
                    ss_ps = kmisc[:, 64:192].rearrange("p (j c) -> p j c", j=2)
                    o_ps = kmisc[:, 256:384].rearrange("p (j c) -> p j c", j=2)
                    idb64 = idb[0:64, 0:64]
                    kz = _S.enter_context(sb("c_kz", [128, 2, 4, 64], BF16)); bkz = Buf()
                    P.op("pool", lambda E: E.memset(kz[:], 0.0), writes=[bkz])
                    P.op("pool", lambda E: E.memset(KDP[:], 0.0), writes=[bKDP])
                    P.op("pool", lambda E: E.memset(VNP[:], 0.0), writes=[bVNP])
                    P.op("pool", lambda E: E.memset(SP[:], 0.0), writes=[bSP])
                    P.op("pool", lambda E: E.memset(SPb[:], 0.0), writes=[bSPb])
                    g4, bt4, gcc, egc, ekd, tm4, sp4 = [sm[:, 4 * i:4 * i + 4] for i in range(7)]

                    def bc_h(ap4):
                        return ap4.unsqueeze(2).to_broadcast([64, 4, 64])

                    def bc_m(ap):
                        return ap.unsqueeze(1).to_broadcast([64, 4, 64])

                    opc = [0]
                    maxops = dbg.get("maxops", 10 ** 9) if dbg else 10 ** 9

                    def GOP(eng, fn, rd, wrt):
                        opc[0] += 1
                        if opc[0] <= maxops:
                            P.op(eng, fn, reads=rd, writes=wrt)

                    V = lambda fn, rd, wrt: GOP("dve", fn, rd, wrt)
                    AC = lambda fn, rd, wrt: GOP("act", fn, rd, wrt)
                    PE = lambda fn, rd, wrt: GOP("pe", fn, rd, wrt)
                    for c in range(NCH if not (dbg and 'nch' in dbg) else dbg['nch']):
                        cs_ = slice(c * 64, (c + 1) * 64)
                        for kt in range(8):
                            PE(lambda E: E.matmul(ab_ps, hT[:, kt, cs_], gwab[:, kt, :], start=(kt == 0),
                                                  stop=(kt == 7)), [bh[kt], bgwab], [b_ab])
                        V(lambda E: E.tensor_tensor(sp4, ab_ps[:, 0:4], gpar[:, 4:8], ALU.add), [b_ab, bgpar], [bsm])
                        AC(lambda E: E.activation(sp4, sp4, AF.Exp), [bsm], [bsm])
                        AC(lambda E: E.activation(sp4, sp4, AF.Ln, bias=1.0, scale=1.0), [bsm], [bsm])
                        V(lambda E: E.tensor_tensor(g4, sp4, gpar[:, 8:12], ALU.mult), [bsm, bgpar], [bsm])
                        AC(lambda E: E.activation(bt4, ab_ps[:, 4:8], AF.Exp, scale=-1.0), [b_ab], [bsm])
                        V(lambda E: E.tensor_scalar_add(bt4, bt4, 1.0), [bsm], [bsm])
                        V(lambda E: E.reciprocal(bt4, bt4), [bsm], [bsm])
                        PE(lambda E: E.matmul(gcc_ps, U, g4, start=True, stop=True), [bsm, bcst], [b_gcc])
                        V(lambda E: E.tensor_copy(gcc, gcc_ps), [b_gcc], [bsm])
                        V(lambda E: E.tensor_tensor(Gd[:], bc_m(U), bc_h(g4), ALU.mult), [bsm, bcst], [bGd])
                        PE(lambda E: E.matmul(kgcb[:], cst[0:64, C_ONE:C_ONE + 128],
                                              Gd[:].rearrange("p h j -> p (h j)"), start=True, stop=True),
                           [bGd, bcst], [b_gcb])
                        gcb3 = kgcb[0:64, :].rearrange("p (h j) -> p h j", h=4)
                        V(lambda E: E.tensor_tensor(Dd[:], bc_h(gcc), gcb3, ALU.subtract), [bsm, b_gcb], [bD])
                        V(lambda E: E.tensor_tensor(Ee[:], Dd[:], bc_m(MLO), ALU.add), [bD, bcst], [bE])
                        V(lambda E: E.tensor_tensor(Et[:], bc_m(MUP), Dd[:], ALU.subtract), [bD, bcst], [bEt])
                        AC(lambda E: E.activation(Ee[:], Ee[:], AF.Exp), [bE], [bE])
                        AC(lambda E: E.activation(Et[:], Et[:], AF.Exp), [bEt], [bEt])
                        AC(lambda E: E.activation(EGB[:], kgcb[:], AF.Exp), [b_gcb], [bEGB])
                        AC(lambda E: E.activation(egc, gcc, AF.Exp), [bsm], [bsm])
                        V(lambda E: E.tensor_tensor(tm4, gcb3[:, :, 63], gcc, ALU.subtract), [b_gcb, bsm], [bsm])
                        AC(lambda E: E.activation(ekd, tm4, AF.Exp), [bsm], [bsm])
                        EG4 = EGB[:].rearrange("p (j r c) -> p j r c", j=2, r=2)
                        V(lambda E: E.tensor_copy(glc[0:64, :], EG4[0:64, :, 0, 63]), [bEGB], [bgl])
                        V(lambda E: E.tensor_copy(glc[64:128, :], EG4[64:128, :, 1, 63]), [bEGB], [bgl])
                        for h in range(4):
                            j, r = h // 2, h % 2
                            rows = slice(64 * r, 64 * r + 64)
                            GOP("pool", lambda E: E.tensor_copy(kz[rows, 0, h, :], gkT[rows, j, cs_]), [bgk], [bkz])
                            GOP("pool", lambda E: E.tensor_copy(kz[rows, 1, h, :], gqT[rows, j, cs_]), [bgq], [bkz])
                        for h in range(4):
                            j, r = h // 2, h % 2
                            PE(lambda E: E.matmul(kgk[:, 0, h, :], gkT[:, j, cs_], kz[:, 0, h, :], start=True,
                                                  stop=True), [bgk, bkz], [b_G])
                            PE(lambda E: E.matmul(kgk[:, 1, h, :], gkT[:, j, cs_], kz[:, 1, h, :], start=True,
                                                  stop=True), [bgk, bkz], [b_KQ])
                        a = 0
                        V(lambda E: E.tensor_tensor(tA[:], kgk[:, 0, :, :], Ee[:], ALU.mult), [b_G, bE], [btA])
                        V(lambda E: E.tensor_tensor(As[a][:], tA[:], bc_h(bt4), ALU.mult), [btA, bsm], [bA[a]])
                        V(lambda E: E.tensor_tensor(qkT[:], kgk[:, 1, :, :], Et[:], ALU.mult), [b_KQ, bEt], [bqk])
                        trB = ktr[:, 0, :].rearrange("p (h i) -> p h i", h=4)
                        for h in range(4):
                            PE(lambda E: E.transpose(trB[:, h, :], As[a][:, h, :], idb64), [bA[a], bcst], [b_trB])
                        V(lambda E: E.tensor_copy(Bs[a][:], trB), [b_trB], [bB[a]])
                        V(lambda E: E.tensor_tensor(Pm[:], bc_m(idb64), Bs[a][:], ALU.subtract), [bB[a], bcst], [bP])
                        for lev in range(5):
                            n = 1 - a
                            for h in range(4):
                                PE(lambda E: E.matmul(ksq[:, 0, h, :], Bs[a][:, h, :], As[a][:, h, :], start=True,
                                                      stop=True), [bA[a], bB[a]], [b_sq])
                                PE(lambda E: E.matmul(ksq[:, 1, h, :], As[a][:, h, :], Bs[a][:, h, :], start=True,
                                                      stop=True), [bA[a], bB[a]], [b_sq])
                            AC(lambda E: E.copy(As[n][:], ksq[:, 0, :, :]), [b_sq], [bA[n]])
                            V(lambda E: E.tensor_copy(Bs[n][:], ksq[:, 1, :, :]), [b_sq], [bB[n]])
                            a = n
                            for h in range(4):
                                PE(lambda E: E.matmul(kpv[:, 0, h, :], As[a][:, h, :], Pm[:, h, :], start=True,
                                                      stop=True), [bA[a], bP], [b_pu])
                            V(lambda E: E.tensor_tensor(Pm[:], Pm[:], kpv[:, 0, :, :], ALU.add), [bP, b_pu], [bP])
                        for j in range(2):
                            PE(lambda E: E.transpose(ktr[:, 1, j * 128:(j + 1) * 128], gvT[:, j, cs_], idb[:]),
                               [bgv, bcst], [b_trv])
                            PE(lambda E: E.transpose(ktr[:, 2, j * 128:(j + 1) * 128], gkT[:, j, cs_], idb[:]),
                               [bgk, bcst], [b_trk])
                        AC(lambda E: E.copy(vtm[:].rearrange("p h d -> p (h d)"), ktr[:, 1, :]), [b_trv], [bvtm])
                        k4 = ktr[:, 2, :].rearrange("p (j r d) -> p j r d", j=2, r=2)
                        KD5 = KDP[:].rearrange("p (j r) m -> p j r m", j=2)
                        ekd3 = ekd.rearrange("p (j r) -> p j r", j=2)
                        for r in range(2):
                            V(lambda E: E.tensor_tensor(KD5[:, :, r, 64 * r:64 * r + 64], k4[:, :, r, :],
                                                        ekd3[:, :, r].unsqueeze(2).to_broadcast([64, 2, 64]),
                                                        ALU.mult), [b_trk, bsm], [bKDP])
                        for h in range(4):
                            j, r = h // 2, h % 2
                            rows = slice(64 * r, 64 * r + 64)
                            GOP("pool", lambda E: E.tensor_tensor(qdT[rows, j, :], gqT[rows, j, cs_],
                                                                  EGB[rows, h * 64:(h + 1) * 64], ALU.mult),
                                [bgq, bEGB], [bqd])
                        for j in range(2):
                            PE(lambda E: E.matmul(kps1[:, 2 * j:2 * j + 2, :].rearrange("p a b -> p (a b)"),
                                                  gkT[:, j, cs_], SPb[:, j, :], start=True, stop=True),
                               [bgk, bSPb], [b_ps1])
                        V(lambda E: E.tensor_tensor(t1[:], kps1[:], bc_h(egc), ALU.mult), [b_ps1, bsm], [bt1])
                        V(lambda E: E.tensor_tensor(t1[:], vtm[:], t1[:], ALU.subtract), [bvtm, bt1], [bt1])
                        V(lambda E: E.tensor_tensor(Rb[:], t1[:], bc_h(bt4), ALU.mult), [bt1, bsm], [bR])
                        for h in range(4):
                            PE(lambda E: E.matmul(kpv[:, 1, h, :], Pm[:, h, :], Rb[:, h, :], start=True, stop=True),
                               [bP, bR], [b_vn])
                        VN5 = VNP[:].rearrange("p (j r) m -> p j r m", j=2)
                        vn4 = kpv[:, 1, :, :].rearrange("p (j r) d -> p j r d", j=2)
                        for r in range(2):
                            AC(lambda E: E.copy(VN5[:, :, r, 64 * r:64 * r + 64], vn4[:, :, r, :]), [b_vn], [bVNP])
                        for j in range(2):
                            PE(lambda E: E.matmul(o_ps[:, j, :], SPb[:, j, :], qdT[:, j, :], start=True, stop=False),
                               [bSPb, bqd], [b_o])
                            PE(lambda E: E.matmul(o_ps[:, j, :], VNP[:, 2 * j, :], qkT[:, 2 * j, :], start=False,
                                                  stop=False), [bVNP, bqk], [b_o])
                            PE(lambda E: E.matmul(o_ps[:, j, :], VNP[:, 2 * j + 1, :], qkT[:, 2 * j + 1, :],
                                                  start=False, stop=True), [bVNP, bqk], [b_o])
                        AC(lambda E: E.activation(osq[:], o_ps, AF.Square), [b_o], [bosq])
                        PE(lambda E: E.matmul(ss_ps.rearrange("p j c -> p (j c)"), bd,
                                              osq[:].rearrange("p j c -> p (j c)"), start=True, stop=True),
                           [bosq, bcst], [b_ss])
                        AC(lambda E: E.activation(ors[:], ss_ps, AF.Sqrt, bias=RMS_EPS, scale=1.0 / 64), [b_ss], [bors])
                        V(lambda E: E.reciprocal(ors[:], ors[:]), [bors], [bors])
                        V(lambda E: E.scalar_tensor_tensor(otm[:], o_ps, col(l, K_GO), ors[:], ALU.mult, ALU.mult),
                          [b_o, bors, bcols], [botm])
                        for j in range(2):
                            V(lambda E: E.tensor_tensor(mixT[:, 4 + j, cs_], otm[:, j, :], gzT[:, j, cs_], ALU.mult),
                              [botm, bgz], [bmix[4 + j]])
                        for j in range(2):
                            PE(lambda E: E.matmul(kU[:, j, :], KDP[:, 2 * j, :], VNP[:, 2 * j, :], start=True,
                                                  stop=False), [bKDP, bVNP], [b_U])
                            PE(lambda E: E.matmul(kU[:, j, :], KDP[:, 2 * j + 1, :], VNP[:, 2 * j + 1, :],
                                                  start=False, stop=True), [bKDP, bVNP], [b_U])
                        for j in range(2):
                            V(lambda E: E.scalar_tensor_tensor(SP[:, j, :], SP[:, j, :], glc[:, j:j + 1],
                                                               kU[:, j, :], ALU.mult, ALU.add),
                              [bSP, bgl, b_U], [bSP])
                        AC(lambda E: E.copy(SPb[:], SP[:]), [bSP], [bSPb])
            P.barrier()

        def out_proj(l, s):
            with contextlib.ExitStack() as _S:
                wo = _S.enter_context(sb("wo", [128, 8, D], BF16))
                o0 = _S.enter_context(ps("ops0", [128, 512]))
                o1 = _S.enter_context(ps("ops1", [128, 512]))
                bwo = Buf(); ops_ = [o0, o1]; bops = [PB(), PB()]
                for kt in range(8):
                    P.dma("pool", wo[:, kt, :], dr["w_out"][l, kt * 128:(kt + 1) * 128, :], writes=[bwo])
                for kt in range(8):
                    P.op("act", lambda E: E.mul(xT[:, kt, :], xT[:, kt, :], ALPHA), reads=[bx[kt]], writes=[bx[kt]])
                k = 0
                for b in range(NB):
                    sl = slice(b * 512, (b + 1) * 512)
                    for m in range(8):
                        pp = ops_[k % 2]; bpp = bops[k % 2]; k += 1
                        for kt in range(8):
                            P.op("pe", lambda E: E.matmul(pp[:], wo[:, kt, m * 128:(m + 1) * 128], mixT[:, kt, sl],
                                                          start=(kt == 0), stop=(kt == 7)),
                                 reads=[bwo, bmix[kt]], writes=[bpp])
                        P.op("dve", lambda E: E.scalar_tensor_tensor(xT[:, m, sl], pp[:], modc(l, 2, m, s),
                                                                     xT[:, m, sl], ALU.mult, ALU.add),
                             reads=[bpp, bmod, bx[m]], writes=[bx[m]])
            P.barrier()

        def moe_stage(l, s):
            with contextlib.ExitStack() as _S:
                wr = _S.enter_context(sb("wr", [128, 8, 36]))
                brb = _S.enter_context(sb("brb", [128, 36]))
                h2f = _S.enter_context(sb("h2f", [128, 8, 512]))
                Lg = _S.enter_context(sb("Lg", [128, 36]))
                sm = _S.enter_context(sb("sm", [128, 16]))
                msk = _S.enter_context(sb("msk", [128, 3, 32]))
                Wt = _S.enter_context(sb("Wt", [128, 32]))
                WtT = _S.enter_context(sb("WtT", [32, T]))
                r_l = _S.enter_context(ps("r_l", [128, 36]))
                r_t = _S.enter_context(ps("r_t", [32, 128]))
                bwr, bbrb, bh2f, bLg, bsm, bmsk, bWt, bWtT = [Buf() for _ in range(8)]
                brl, brt_ = PB(), PB()
                P.dma("sp", wr[:], dr["w_r"][l, :, :].rearrange("(k p) n -> p k n", p=128), writes=[bwr])
                P.dma("sp", brb[:], dr["b_r"][l:l + 1, :].partition_broadcast(128), writes=[bbrb])
                for b in range(NB):
                    sl = slice(b * 512, (b + 1) * 512)
                    for kt in range(8):
                        P.op("act", lambda E: E.activation(h2f[:, kt, :], xT[:, kt, sl], AF.Identity,
                                                           bias=modc(l, 3, kt, s), scale=modc(l, 4, kt, s)),
                             reads=[bx[kt], bmod], writes=[bh2f])
                        P.op("pool", lambda E: E.tensor_copy(hT[:, kt, sl], h2f[:, kt, :]), reads=[bh2f],
                             writes=[bh[kt]])
                    for tq in range(4):
                        t0 = b * 512 + tq * 128
                        for kt in range(8):
                            P.op("pe", lambda E: E.matmul(r_l[:], h2f[:, kt, tq * 128:(tq + 1) * 128], wr[:, kt, :],
                                                          start=(kt == 0), stop=(kt == 7)),
                                 reads=[bh2f, bwr], writes=[brl])
                        V = lambda fn, rd, wrt: P.op("dve", fn, reads=rd, writes=wrt)
                        V(lambda E: E.tensor_tensor(Lg[:], r_l[:], brb[:], ALU.add), [brl, bbrb], [bLg])
                        V(lambda E: E.reduce_max(sm[:, 0:1], Lg[:, 0:4], AX.X), [bLg], [bsm])
                        V(lambda E: E.tensor_scalar_mul(sm[:, 1:2], sm[:, 0:1], -1.0), [bsm], [bsm])
                        V(lambda E: E.tensor_scalar(msk[:, 0, 0:4], Lg[:, 0:4], sm[:, 0:1], None, ALU.is_equal),
                          [bLg, bsm], [bmsk])
                        P.op("act", lambda E: E.activation(msk[:, 0, 8:12], Lg[:, 0:4], AF.Exp, bias=sm[:, 1:2],
                                                           scale=1.0, accum_out=sm[:, 2:3]),
                             reads=[bLg, bsm], writes=[bmsk, bsm])
                        V(lambda E: E.reciprocal(sm[:, 3:4], sm[:, 2:3]), [bsm], [bsm])
                        V(lambda E: E.tensor_scalar(msk[:, 0, 4:8], msk[:, 0, 0:4], -NEG, NEG, ALU.mult, ALU.add),
                          [bmsk], [bmsk])
                        V(lambda E: E.tensor_tensor(msk[:, 1, :].rearrange("p (g e) -> p g e", g=4),
                                                    Lg[:, 4:36].rearrange("p (g e) -> p g e", g=4),
                                                    msk[:, 0, 4:8].unsqueeze(2).to_broadcast([128, 4, 8]), ALU.add),
                          [bLg, bmsk], [bmsk])
                        V(lambda E: E.reduce_max(sm[:, 4:5], msk[:, 1, :], AX.X), [bmsk], [bsm])
                        V(lambda E: E.tensor_scalar(msk[:, 2, :], msk[:, 1, :], sm[:, 4:5], None, ALU.is_equal),
                          [bmsk, bsm], [bmsk])
                        V(lambda E: E.scalar_tensor_tensor(msk[:, 1, :], msk[:, 2, :], NEG, msk[:, 1, :], ALU.mult,
                                                           ALU.add), [bmsk], [bmsk])
                        V(lambda E: E.reduce_max(sm[:, 5:6], msk[:, 1, :], AX.X), [bmsk], [bsm])
                        V(lambda E: E.tensor_scalar(msk[:, 1, :], msk[:, 1, :], sm[:, 5:6], None, ALU.is_equal),
                          [bmsk, bsm], [bmsk])
                        V(lambda E: E.tensor_tensor(sm[:, 6:7], sm[:, 5:6], sm[:, 4:5], ALU.subtract), [bsm], [bsm])
                        P.op("act", lambda E: E.activation(sm[:, 7:8], sm[:, 6:7], AF.Exp), reads=[bsm], writes=[bsm])
                        V(lambda E: E.tensor_scalar_add(sm[:, 8:9], sm[:, 7:8], 1.0), [bsm], [bsm])
                        V(lambda E: E.reciprocal(sm[:, 8:9], sm[:, 8:9]), [bsm], [bsm])
                        V(lambda E: E.tensor_tensor(sm[:, 9:10], sm[:, 3:4], sm[:, 8:9], ALU.mult), [bsm], [bsm])
                        V(lambda E: E.tensor_tensor(sm[:, 10:11], sm[:, 9:10], sm[:, 7:8], ALU.mult), [bsm], [bsm])
                        V(lambda E: E.tensor_scalar(Wt[:], msk[:, 2, :], sm[:, 9:10], None, ALU.mult),
                          [bmsk, bsm], [bWt])
                        V(lambda E: E.scalar_tensor_tensor(Wt[:], msk[:, 1, :], sm[:, 10:11], Wt[:], ALU.mult,
                                                           ALU.add), [bmsk, bsm, bWt], [bWt])
                        P.op("pe", lambda E: E.transpose(r_t[:], Wt[:], ident), reads=[bWt, bcst], writes=[brt_])
                        P.op("act", lambda E: E.copy(WtT[:, t0:t0 + 128], r_t[:]), reads=[brt_], writes=[bWtT])
                P.dma("sp", scr[:, :], WtT[:], reads=[bWtT], writes=[bscr])
            P.barrier()
            for kt in range(8):
                P.op("act", lambda E: E.mul(xT[:, kt, :], xT[:, kt, :], ALPHA), reads=[bx[kt]], writes=[bx[kt]])
            TB = 256
            NTB = T // TB
            with contextlib.ExitStack() as _S:
                A_ = lambda nm, shp, dt=F32: _S.enter_context(sb(nm, shp, dt))
                stg_g, stg_u, stg_d = A_("stg_g", [128, 8, 256]), A_("stg_u", [128, 8, 256]), A_("stg_d", [128, 2, D])
                wg = [A_("wg0", [128, 8, 256], BF16), A_("wg1", [128, 8, 256], BF16)]
                wu = [A_("wu0", [128, 8, 256], BF16), A_("wu1", [128, 8, 256], BF16)]
                wd = [A_("wd0", [128, 2, D], BF16), A_("wd1", [128, 2, D], BF16)]
                wb = [A_("wb0", [128, T]), A_("wb1", [128, T])]
                ea = [A_("ea0", [128, 2, TB]), A_("ea1", [128, 2, TB])]
                eb = [A_("eb0", [128, 2, TB], BF16), A_("eb1", [128, 2, TB], BF16)]
                eg = [_S.enter_context(ps("eg0", [128, 2, TB])), _S.enter_context(ps("eg1", [128, 2, TB]))]
                eu = [_S.enter_context(ps("eu0", [128, 2, TB])), _S.enter_context(ps("eu1", [128, 2, TB]))]
                ey = _S.enter_context(ps("ey", [128, 8, TB]))
                bsg, bsu, bsd = Buf(), Buf(), Buf()
                bwg, bwu, bwd, bwb, bea, beb = [[Buf(), Buf()] for _ in range(6)]
                beg, beu = [[PB(), PB()] for _ in range(2)]
                beyb = [PB() for _ in range(4)]
                NE = NEXP if not (dbg and 'nexp' in dbg) else dbg['nexp']

                def dma_expert(e):
                    if e >= NE:
                        return
                    P.dma("sp", stg_g[:], dr["moe_w_gate"][l, e, :, :].rearrange("(k p) n -> p k n", p=128),
                          writes=[bsg])
                    P.dma("sp", stg_u[:], dr["moe_w_up"][l, e, :, :].rearrange("(k p) n -> p k n", p=128),
                          writes=[bsu])
                    P.dma("sp", stg_d[:], dr["moe_w_down"][l, e, :, :].rearrange("(f p) n -> p f n", p=128),
                          writes=[bsd])

                def cast_expert(e):
                    if e >= NE:
                        return
                    w = e % 2
                    P.op("act", lambda E: E.copy(wg[w][:], stg_g[:]), reads=[bsg], writes=[bwg[w]])
                    P.op("pool", lambda E: E.tensor_copy(wu[w][:], stg_u[:]), reads=[bsu], writes=[bwu[w]])
                    P.op("pool", lambda E: E.tensor_copy(wd[w][:], stg_d[:]), reads=[bsd], writes=[bwd[w]])
                    P.dma("sp", wb[w][:], scr[e:e + 1, :].partition_broadcast(128), reads=[bscr], writes=[bwb[w]])

                units = [(e, tb) for e in range(NE) for tb in range(NTB)]

                def gu(i):
                    e, tb = units[i]
                    w = e % 2; q = i % 2
                    sl = slice(tb * TB, (tb + 1) * TB)
                    for f in range(2):
                        for kt in range(8):
                            P.op("pe", lambda E: E.matmul(eg[q][:, f, :], wg[w][:, kt, f * 128:(f + 1) * 128],
                                                          hT[:, kt, sl], start=(kt == 0), stop=(kt == 7)),
                                 reads=[bwg[w], bh[kt]], writes=[beg[q]])
                    for f in range(2):
                        for kt in range(8):
                            P.op("pe", lambda E: E.matmul(eu[q][:, f, :], wu[w][:, kt, f * 128:(f + 1) * 128],
                                                          hT[:, kt, sl], start=(kt == 0), stop=(kt == 7)),
                                 reads=[bwu[w], bh[kt]], writes=[beu[q]])

                def rest(i):
                    e, tb = units[i]
                    w = e % 2; q = i % 2
                    sl = slice(tb * TB, (tb + 1) * TB)
                    P.op("act", lambda E: E.activation(ea[q][:], eg[q][:], AF.Silu), reads=[beg[q]], writes=[bea[q]])
                    P.op("dve", lambda E: E.tensor_tensor(ea[q][:], ea[q][:], eu[q][:], ALU.mult),
                         reads=[bea[q], beu[q]], writes=[bea[q]])
                    P.op("pool", lambda E: E.tensor_tensor(eb[q][:], ea[q][:],
                                                           wb[w][:, sl].unsqueeze(1).to_broadcast([128, 2, TB]),
                                                           ALU.mult), reads=[bea[q], bwb[w]], writes=[beb[q]])
                    for m in range(8):
                        for f in range(2):
                            P.op("pe", lambda E: E.matmul(ey[:, m, :], wd[w][:, f, m * 128:(m + 1) * 128],
                                                          eb[q][:, f, :], start=(f == 0), stop=(f == 1)),
                                 reads=[bwd[w], beb[q]], writes=[beyb[m // 2]])
                    for m in range(8):
                        P.op("dve", lambda E: E.scalar_tensor_tensor(xT[:, m, sl], ey[:, m, :], modc(l, 5, m, s),
                                                                     xT[:, m, sl], ALU.mult, ALU.add),
                             reads=[beyb[m // 2], bmod, bx[m]], writes=[bx[m]])

                dma_expert(0); cast_expert(0); dma_expert(1); cast_expert(1); dma_expert(2)
                if units:
                    gu(0)
                for i in range(len(units)):
                    if i + 1 < len(units):
                        gu(i + 1)
                    rest(i)
                    e, tb = units[i]
                    if tb == NTB - 1:
                        cast_expert(e + 2)
                        dma_expert(e + 3)
            P.barrier()

        STAGES = dbg.get("stages", "mgpoLME") if dbg else "mgpoLME"
        for s in range(NSEQ):
            for kt in range(8):
                P.dma("sp", xT[:, kt, :], dr["xT"][s, kt * 128:(kt + 1) * 128, :], writes=[bx[kt]])
            for l in range(DEPTH):
                modulate(l, s, 0, 1)
                if "z" in STAGES:
                    for kt in range(8):
                        P.op("pool", lambda E: E.memset(mixT[:, kt, :], 0.0), writes=[bmix[kt]])
                if "m" in STAGES:
                    mla_stage(l, s)
                if "g" in STAGES:
                    gdn_stage(l, s)
                if "p" in STAGES:
                    pool_stage(l, s)
                if dbg and dbg.get("dump") == "mix" and s == dbg.get("s", 0) and l == dbg.get("l", 0):
                    with contextlib.ExitStack() as _S:
                        dmp = _S.enter_context(sb("dmp", [128, 8, T]))
                        bd = Buf()
                        for kt in range(8):
                            P.op("dve", lambda E: E.tensor_copy(dmp[:, kt, :], (hT if dbg.get("src") == "h" else mixT)[:, kt, :]), reads=[bmix[kt], bh[kt]],
                                 writes=[bd])
                        P.dma("sp", dbg_out.rearrange("(k p) t -> p k t", p=128), dmp[:], reads=[bd])
                        P.barrier()
                if "o" in STAGES:
                    out_proj(l, s)
                if "L" in STAGES:
                    layer_norm(l, K_LN1G, K_LN1B)
                if "M" in STAGES:
                    moe_stage(l, s)
                if "E" in STAGES:
                    layer_norm(l, K_LN2G, K_LN2B)
            for kt in range(8):
                P.dma("sp", yT[s, kt * 128:(kt + 1) * 128, :], xT[:, kt, :], reads=[bx[kt]])
        P.barrier()
    P.es.close()
    return nc


def make_consts():
    c = np.zeros((128, NCON), np.float32)
    c[:, C_ID:C_ID + 128] = np.eye(128, dtype=np.float32)
    c[:, C_ONE:C_ONE + 128] = 1.0
    i = np.arange(64)
    c[:64, C_U:C_U + 64] = (i[:, None] <= i[None, :]).astype(np.float32)
    c[:64, C_MLO:C_MLO + 64] = np.where(i[:, None] > i[None, :], 0.0, NEG)
    c[:64, C_MUP:C_MUP + 64] = np.where(i[None, :] >= i[:, None], 0.0, NEG)
    t = np.arange(16)
    for j in range(2):
        for p in range(128):
            w = 2 ** (2 * j + p // 64 + 1)
            c[p, C_INV16 + 16 * j:C_INV16 + 16 * j + 16] = 1.0 / np.minimum(t + 1, w)
    p = np.arange(128)
    c[:, C_BD:C_BD + 128] = (p[:, None] // 64 == p[None, :] // 64).astype(np.float32)
    return c


def make_rope(T):
    inv_freq = np.power(np.float32(10000.0), -np.arange(0, 32, 2, dtype=np.float32) / np.float32(32)).astype(np.float32)
    ang = (np.arange(T, dtype=np.float32)[:, None] * inv_freq[None, :]).astype(np.float32)
    cos, sin = np.cos(ang).astype(np.float32).T, np.sin(ang).astype(np.float32).T
    r = np.zeros((32, 2, T), np.float32)
    r[:16, 0], r[16:, 0], r[:16, 1], r[16:, 1] = cos, cos, sin, sin
    return r


def make_cols(inp):
    L = inp["w_in"].shape[0]
    cols = np.zeros((L, 128, NCOLS), np.float32)

    def put(k, vec, n):
        cols[:, :, k:k + n] = vec.reshape(L, n, 128).transpose(0, 2, 1)

    put(K_QN, inp["mla_q_norm"], 2); put(K_KVN, inp["mla_kv_norm"], 1)
    put(K_LN1G, inp["ln1_g"], 8); put(K_LN1B, inp["ln1_b"], 8); put(K_LN2G, inp["ln2_g"], 8); put(K_LN2B, inp["ln2_b"], 8)
    put(K_PS, inp["pool_scale"], 2)
    cols[:, :, K_GO] = np.concatenate([inp["gdn_out_norm"], inp["gdn_out_norm"]], axis=1)
    cv = inp["gdn_conv"].reshape(L, 4, 6, 128)
    cols[:, :, K_CONV:K_CONV + 24] = cv.transpose(0, 3, 2, 1).reshape(L, 128, 24)
    put(K_BMOD, inp["b_mod"], 48)
    return cols


_NC_CACHE = {}


def run(inp, T, DEPTH, NSEQ, ncores, dbg=None):
    key = (T, DEPTH, NSEQ, repr(dbg))
    if key not in _NC_CACHE:
        _NC_CACHE[key] = build(T, DEPTH, NSEQ, dbg)
    nc = _NC_CACHE[key]
    f = lambda a: np.ascontiguousarray(np.asarray(a, dtype=np.float32))
    shared = {
        "consts": make_consts(), "rope": make_rope(T), "cols": f(make_cols(inp)),
        "w_in": f(inp["w_in"]), "mla_w_uq": f(inp["mla_w_uq"]), "mla_w_ukv": f(inp["mla_w_ukv"]),
        "gdn_a_log": f(inp["gdn_a_log"]), "gdn_dt_bias": f(inp["gdn_dt_bias"]), "pool_w": f(inp["pool_w"]),
        "w_out": f(inp["w_out"]), "w_mod": f(inp["w_mod"]),
        "w_r": f(np.concatenate([inp["router_w_group"], inp["router_w_expert"]], axis=2)),
        "b_r": f(np.concatenate([inp["router_b_group"], inp["router_b_expert"]], axis=1)),
        "moe_w_gate": f(inp["moe_w_gate"]), "moe_w_up": f(inp["moe_w_up"]), "moe_w_down": f(inp["moe_w_down"]),
    }
    x = np.asarray(inp["x"], np.float32); c = np.asarray(inp["c"], np.float32)
    in_maps = []
    for i in range(ncores):
        m = dict(shared)
        m["xT"] = f(x[i * NSEQ:(i + 1) * NSEQ].transpose(0, 2, 1))
        m["cT"] = f(c[i * NSEQ:(i + 1) * NSEQ].T)
        in_maps.append(m)
    res = run_bass_kernel_spmd(nc, in_maps, core_ids=list(range(ncores)))
    out = np.concatenate([r["yT"].transpose(0, 2, 1) for r in res.results], axis=0)
    return out, res


def kernel(**inputs):
    out, _ = run(inputs, 2048, 4, 2, 8)
    return out.astype(np.float32)
```

```python
import contextlib
import numpy as np
import concourse.bass as bass
import concourse.mybir as mybir
from concourse.bass_utils import run_bass_kernel_spmd

F32 = mybir.dt.float32
BF16 = mybir.dt.bfloat16
AF = mybir.ActivationFunctionType
ALU = mybir.AluOpType
AX = mybir.AxisListType

SEG = 28000
ENGS = ("pe", "dve", "act", "pool", "sp")
NDMA = 8

D = 1024
NEXP = 32
ALPHA = 8.0 ** 0.25
LN_EPS = 1e-5
RMS_EPS = 1e-6
NEG = -30000.0

C_ID, C_ONE, C_U, C_MLO, C_MUP, C_INV16, C_BD, NCON = 0, 128, 256, 320, 384, 448, 480, 608
K_QN, K_KVN, K_LN1G, K_LN1B, K_LN2G, K_LN2B, K_PS, K_GO, K_CONV, K_BMOD, NCOLS = 0, 2, 3, 11, 19, 27, 35, 37, 38, 62, 110


class Buf:
    __slots__ = ("name", "lw", "rd", "excl")

    def __init__(self, name="", excl=False):
        self.name = name
        self.lw = None
        self.rd = {}
        self.excl = excl


def PB():
    return Buf("psum", True)


class Prog:
    def __init__(self, nc, same_engine_sync=True):
        self.nc = nc
        self.es = contextlib.ExitStack()
        self.cnt = {e: 0 for e in ENGS}
        self.seen = {e: {} for e in ENGS}
        self.sems = {}
        self.dma_i = {e: 0 for e in ENGS}
        self.same = same_engine_sync
        self.E = {"pe": nc.tensor, "dve": nc.vector, "act": nc.scalar, "pool": nc.gpsimd, "sp": nc.sync}
        self.last_dma = {}

    def sem(self, key):
        if key not in self.sems:
            nm = "s_" + "_".join(str(k) for k in key)
            self.sems[key] = self.es.enter_context(self.nc.semaphore(nm))
        return self.sems[key]

    def _wait(self, eng, tok):
        key, val = tok
        if self.seen[eng].get(key, 0) >= val:
            return
        if key[0] == eng and (eng == "pe" or not self.same):
            return
        self.seen[eng][key] = val
        self.E[eng].wait_ge(self.sem(key), val)

    def _deps(self, eng, reads, writes):
        for b in reads:
            if b.lw is not None:
                self._wait(eng, b.lw)
        for b in writes:
            if b.excl:
                if b.lw is not None and b.lw[0][0] != eng:
                    self._wait(eng, b.lw)
                for k, v in b.rd.items():
                    if k[0] != eng:
                        self._wait(eng, (k, v))
                continue
            if b.lw is not None:
                self._wait(eng, b.lw)
            for k, v in b.rd.items():
                if k[0] != eng:
                    self._wait(eng, (k, v))

    def _mark(self, tok, reads, writes):
        k, v = tok
        for b in reads:
            if b.rd.get(k, 0) < v:
                b.rd[k] = v
        for b in writes:
            if b.excl:
                if b.lw is not None and b.lw[0][0] != k[0]:
                    pk, pv = b.lw
                    if b.rd.get(pk, 0) < pv:
                        b.rd[pk] = pv
                b.rd = {kk: vv for kk, vv in b.rd.items() if kk[0] != k[0]}
                b.lw = tok
                continue
            b.lw = tok
            b.rd = {}

    def op(self, eng, fn, reads=(), writes=()):
        if any(b.excl for b in reads):
            writes = list(writes) + [b for b in reads if b.excl]
            reads = [b for b in reads if not b.excl]
        self._deps(eng, reads, writes)
        n = self.cnt[eng]
        key = (eng, n // SEG)
        val = n % SEG + 1
        fn(self.E[eng]).then_inc(self.sem(key), 1)
        self.cnt[eng] = n + 1
        self._mark((key, val), reads, writes)

    def dma(self, eng, out, in_, reads=(), writes=(), **kw):
        i = self.dma_i[eng]
        self.dma_i[eng] = i + 1
        key = ("d" + eng, i % NDMA)
        val = (i // NDMA + 1) * 16
        if val > 16:
            self._wait(eng, (key, val - 16))
        self._deps(eng, reads, writes)
        self.E[eng].dma_start(out=out, in_=in_, **kw).then_inc(self.sem(key), 16)
        self.last_dma[key] = val
        self._mark((key, val), reads, writes)

    def barrier(self):
        toks = []
        for e in ENGS:
            n = self.cnt[e]
            if n:
                toks.append(((e, (n - 1) // SEG), (n - 1) % SEG + 1))
        toks += list(self.last_dma.items())
        for e in ENGS:
            for t in toks:
                self._wait(e, t)


def build(T, DEPTH, NSEQ, dbg=None):
    nc = bass.Bass("TRN2", target_bir_lowering=False)
    P = Prog(nc)
    NB = T // 512
    NT = T // 128
    NCH = T // 64
    dr = {}

    def din(name, shape):
        dr[name] = nc.dram_tensor(name, list(shape), F32, kind="ExternalInput").ap()

    din("xT", [NSEQ, D, T]); din("cT", [D, NSEQ]); din("consts", [128, NCON]); din("rope", [32, 2, T])
    din("cols", [4, 128, NCOLS]); din("w_in", [4, D, 1704]); din("mla_w_uq", [4, 256, 768])
    din("mla_w_ukv", [4, 128, 1024]); din("gdn_a_log", [4, 4]); din("gdn_dt_bias", [4, 4])
    din("pool_w", [4, 4, 64, 64]); din("w_out", [4, D, D]); din("w_mod", [4, D, 6 * D])
    din("w_r", [4, D, 36]); din("b_r", [4, 36])
    din("moe_w_gate", [4, NEXP, D, 256]); din("moe_w_up", [4, NEXP, D, 256]); din("moe_w_down", [4, NEXP, 256, D])
    yT = nc.dram_tensor("yT", [NSEQ, D, T], F32, kind="ExternalOutput").ap()
    scr = nc.dram_tensor("scr_wt", [NEXP, T], F32).ap()
    bscr = Buf("scr")
    if dbg:
        dbg_out = nc.dram_tensor("dbg", [D, T], F32, kind="ExternalOutput").ap()

    uid = [0]

    def sb(name, shape, dt=F32):
        uid[0] += 1
        return nc.sbuf_tensor("%s_%d" % (name, uid[0]), list(shape), dt)

    @contextlib.contextmanager
    def ps(name, shape, dt=F32):
        uid[0] += 1
        isz = 4 if dt == F32 else 2
        per = 2048 // isz
        n = 1
        for d_ in shape[1:]:
            n *= d_
        nb = (n + per - 1) // per
        with nc.psum_tensor("%s_%d" % (name, uid[0]), [128, nb * per], dt) as t:
            v = t[0:shape[0], 0:n]
            if len(shape) == 3:
                v = v.rearrange("p (a b) -> p a b", a=shape[1])
            elif len(shape) == 4:
                v = v.rearrange("p (a b c) -> p a b c", a=shape[1], b=shape[2])
            yield v

    es = contextlib.ExitStack()
    with es:
        cst = es.enter_context(sb("cst", [128, NCON])); bcst = Buf()
        idb = es.enter_context(sb("idb", [128, 128], BF16))
        oneb = es.enter_context(sb("oneb", [128, 128], BF16))
        colsT = es.enter_context(sb("colsT", [128, DEPTH, NCOLS])); bcols = Buf()
        modT = es.enter_context(sb("modT", [128, DEPTH, 48, NSEQ])); bmod = Buf()
        xT = es.enter_context(sb("xT_sb", [128, 8, T])); bx = [Buf() for _ in range(8)]
        hT = es.enter_context(sb("hT_sb", [128, 8, T], BF16)); bh = [Buf() for _ in range(8)]
        mixT = es.enter_context(sb("mixT_sb", [128, 8, T], BF16)); bmix = [Buf() for _ in range(8)]

        P.dma("sp", cst[:], dr["consts"][:, :], writes=[bcst])
        for l in range(DEPTH):
            P.dma("sp", colsT[:, l, :], dr["cols"][l, :, :], writes=[bcols])
        P.op("dve", lambda E: E.tensor_copy(idb[:], cst[:, C_ID:C_ID + 128]), reads=[bcst], writes=[bcst])
        P.op("dve", lambda E: E.tensor_copy(oneb[:], cst[:, C_ONE:C_ONE + 128]), reads=[bcst], writes=[bcst])
        ident = cst[:, C_ID:C_ID + 128]
        ones = cst[:, C_ONE:C_ONE + 128]

        def col(l, k):
            return colsT[:, l, k:k + 1]

        with contextlib.ExitStack() as _S:
            scT = _S.enter_context(sb("scT", [128, 8, NSEQ]))
            sgT = _S.enter_context(sb("sgT", [128, 8, NSEQ]))
            wm0 = _S.enter_context(sb("wm0", [128, 8, 768]))
            wm1 = _S.enter_context(sb("wm1", [128, 8, 768]))
            modps = _S.enter_context(ps("modps", [128, 48, NSEQ]))
            bsc = Buf(); bwm = [Buf(), Buf()]; bmp = PB()
            wms = [wm0, wm1]
            P.dma("sp", scT[:], dr["cT"].rearrange("(k p) s -> p k s", p=128), writes=[bsc])
            P.op("act", lambda E: E.activation(sgT[:], scT[:], AF.Sigmoid), reads=[bsc], writes=[bsc])
            P.op("dve", lambda E: E.tensor_tensor(scT[:], scT[:], sgT[:], ALU.mult), reads=[bsc], writes=[bsc])
            ci = 0
            for l in range(DEPTH):
                for c in range(8):
                    wm = wms[ci % 2]; bw = bwm[ci % 2]; ci += 1
                    P.dma("sp", wm[:], dr["w_mod"][l, :, c * 768:(c + 1) * 768].rearrange("(k p) n -> p k n", p=128),
                          writes=[bw])
                    for mm in range(6):
                        m = c * 6 + mm
                        for kt in range(8):
                            P.op("pe", lambda E: E.matmul(modps[:, m, :], wm[:, kt, mm * 128:(mm + 1) * 128],
                                                          scT[:, kt, :], start=(kt == 0), stop=(kt == 7)),
                                 reads=[bw, bsc], writes=[bmp])
                P.op("dve", lambda E: E.tensor_tensor(
                    modT[:, l, :, :], modps[:],
                    colsT[:, l, K_BMOD:K_BMOD + 48].unsqueeze(2).to_broadcast([128, 48, NSEQ]), ALU.add),
                    reads=[bmp, bcols], writes=[bmod])
                for a in (8, 32):
                    P.op("dve", lambda E: E.tensor_scalar_add(modT[:, l, a:a + 16, :], modT[:, l, a:a + 16, :], 1.0),
                         reads=[bmod], writes=[bmod])
        P.barrier()

        def modc(l, chunk, kt, s):
            return modT[:, l, chunk * 8 + kt, s:s + 1]

        def layer_norm(l, kg, kb):
            with contextlib.ExitStack() as _S:
                p_s = _S.enter_context(ps("ln_s", [128, 512]))
                p_q = _S.enter_context(ps("ln_q", [128, 512]))
                sq = _S.enter_context(sb("ln_sq", [128, 2, 512]))
                mean = _S.enter_context(sb("ln_mean", [128, 512]))
                rstd = _S.enter_context(sb("ln_rstd", [128, 512]))
                tt = _S.enter_context(sb("ln_t", [128, 2, 512]))
                bps, bpq, bsq, bmean, brstd, btt = PB(), PB(), [Buf(), Buf()], Buf(), Buf(), [Buf(), Buf()]
                for b in range(NB):
                    sl = slice(b * 512, (b + 1) * 512)
                    for kt in range(8):
                        P.op("pe", lambda E: E.matmul(p_s[:], ones, xT[:, kt, sl], start=(kt == 0), stop=(kt == 7)),
                             reads=[bx[kt], bcst], writes=[bps])
                    for kt in range(8):
                        j = kt % 2
                        P.op("act", lambda E: E.activation(sq[:, j, :], xT[:, kt, sl], AF.Square),
                             reads=[bx[kt]], writes=[bsq[j]])
                        P.op("pe", lambda E: E.matmul(p_q[:], ones, sq[:, j, :], start=(kt == 0), stop=(kt == 7)),
                             reads=[bsq[j], bcst], writes=[bpq])
                    P.op("act", lambda E: E.mul(mean[:], p_s[:], 1.0 / D), reads=[bps], writes=[bmean])
                    P.op("dve", lambda E: E.tensor_tensor(rstd[:], mean[:], mean[:], ALU.mult),
                         reads=[bmean], writes=[brstd])
                    P.op("dve", lambda E: E.scalar_tensor_tensor(rstd[:], p_q[:], 1.0 / D, rstd[:], ALU.mult,
                                                                 ALU.subtract), reads=[bpq, brstd], writes=[brstd])
                    P.op("act", lambda E: E.activation(rstd[:], rstd[:], AF.Sqrt, bias=LN_EPS, scale=1.0),
                         reads=[brstd], writes=[brstd])
                    P.op("dve", lambda E: E.reciprocal(rstd[:], rstd[:]), reads=[brstd], writes=[brstd])
                    for kt in range(8):
                        j = kt % 2
                        P.op("pool", lambda E: E.tensor_tensor(tt[:, j, :], xT[:, kt, sl], mean[:], ALU.subtract),
                             reads=[bx[kt], bmean], writes=[btt[j]])
                        P.op("dve", lambda E: E.tensor_tensor(tt[:, j, :], tt[:, j, :], rstd[:], ALU.mult),
                             reads=[btt[j], brstd], writes=[btt[j]])
                        P.op("dve", lambda E: E.tensor_scalar(xT[:, kt, sl], tt[:, j, :], col(l, kg + kt),
                                                              col(l, kb + kt), ALU.mult, ALU.add),
                             reads=[btt[j], bcols], writes=[bx[kt]])
            P.barrier()

        def modulate(l, s, sh_chunk, sc_chunk):
            for kt in range(8):
                P.op("act", lambda E: E.activation(hT[:, kt, :], xT[:, kt, :], AF.Identity,
                                                   bias=modc(l, sh_chunk, kt, s), scale=modc(l, sc_chunk, kt, s)),
                     reads=[bx[kt], bmod], writes=[bh[kt]])

        def load_w_in(wt, bw, l, c0, c1):
            for kt in range(8):
                P.dma("pool", wt[:, kt, :], dr["w_in"][l, kt * 128:(kt + 1) * 128, c0:c1], writes=[bw])

        def pool_stage(l, s):
            with contextlib.ExitStack() as _S:
                pw = _S.enter_context(sb("pw", [128, 8, 256], BF16))
                pbd = _S.enter_context(sb("pbd", [128, 2, 128], BF16))
                px = _S.enter_context(sb("px", [128, 16 + T]))
                pa = _S.enter_context(sb("pa", [128, 16 + T]))
                pb = _S.enter_context(sb("pb", [128, 16 + T]))
                pdl = _S.enter_context(sb("pdl", [128, T], BF16))
                ptmp = _S.enter_context(sb("ptmp", [128, 16]))
                pps0 = _S.enter_context(ps("pps0", [128, 512]))
                pps1 = _S.enter_context(ps("pps1", [128, 512]))
                bpw, bpbd, bpx, bpa, bpb, bpdl, bptmp = Buf(), Buf(), Buf(), Buf(), Buf(), Buf(), Buf()
                pps = [pps0, pps1]; bpps = [PB(), PB()]
                load_w_in(pw, bpw, l, 1448, 1704)
                P.op("pool", lambda E: E.memset(pbd[:], 0.0), writes=[bpbd])
                for g in range(4):
                    j, r = g // 2, g % 2
                    P.dma("pool", pbd[64 * r:64 * r + 64, j, 64 * r:64 * r + 64], dr["pool_w"][l, g, :, :],
                          writes=[bpbd])
                for t in (px, pa, pb):
                    P.op("pool", lambda E: E.memset(t[:, 0:16], 0.0), writes=[bpx, bpa, bpb])
                k = 0
                for j in range(2):
                    for b in range(NB):
                        pp = pps[k % 2]; bpp = bpps[k % 2]; k += 1
                        sl = slice(b * 512, (b + 1) * 512)
                        for kt in range(8):
                            P.op("pe", lambda E: E.matmul(pp[:], pw[:, kt, j * 128:(j + 1) * 128], hT[:, kt, sl],
                                                          start=(kt == 0), stop=(kt == 7)),
                                 reads=[bpw, bh[kt]], writes=[bpp])
                        P.op("act", lambda E: E.copy(px[:, 16 + b * 512:16 + (b + 1) * 512], pp[:]),
                             reads=[bpp], writes=[bpx])

                    def shift_add(dst, src, sh, bd, bs):
                        P.op("dve", lambda E: E.tensor_tensor(dst[:, 16:16 + T], src[:, 16:16 + T],
                                                              src[:, 16 - sh:16 - sh + T], ALU.add),
                             reads=[bs], writes=[bd])

                    def delta(src, bs, r, w):
                        rows = slice(64 * r, 64 * r + 64)
                        P.op("dve", lambda E: E.scalar_tensor_tensor(pdl[rows, :], src[rows, 16:16 + T], 1.0 / w,
                                                                     px[rows, 16:16 + T], ALU.mult, ALU.subtract),
                             reads=[bs, bpx], writes=[bpdl])
                        P.op("dve", lambda E: E.tensor_tensor(ptmp[rows, :], src[rows, 16:32],
                                                              cst[rows, C_INV16 + 16 * j:C_INV16 + 16 * j + 16],
                                                              ALU.mult), reads=[bs, bcst], writes=[bptmp])
                        P.op("dve", lambda E: E.tensor_tensor(pdl[rows, 0:16], ptmp[rows, :], px[rows, 16:32],
                                                              ALU.subtract), reads=[bptmp, bpx, bpdl], writes=[bpdl])

                    shift_add(pa, px, 1, bpa, bpx)
                    if j == 0:
                        delta(pa, bpa, 0, 2)
                    shift_add(pb, pa, 2, bpb, bpa)
                    if j == 0:
                        delta(pb, bpb, 1, 4)
                    else:
                        shift_add(pa, pb, 4, bpa, bpb)
                        delta(pa, bpa, 0, 8)
                        shift_add(pb, pa, 8, bpb, bpa)
                        delta(pb, bpb, 1, 16)
                    for b in range(NB):
                        pp = pps[k % 2]; bpp = bpps[k % 2]; k += 1
                        sl = slice(b * 512, (b + 1) * 512)
                        P.op("pe", lambda E: E.matmul(pp[:], pbd[:, j, :], pdl[:, sl], start=True, stop=True),
                             reads=[bpbd, bpdl], writes=[bpp])
                        P.op("act", lambda E: E.activation(mixT[:, 6 + j, sl], pp[:], AF.Identity, scale=col(l, K_PS + j)),
                             reads=[bpp, bcols], writes=[bmix[6 + j]])
            P.barrier()

        def mla_stage(l, s):
            with contextlib.ExitStack() as _S:
                wA = _S.enter_context(sb("wA", [128, 8, 416], BF16))
                wuq = _S.enter_context(sb("wuq", [128, 2, 768], BF16))
                wuqr = _S.enter_context(sb("wuqr", [128, 2, 8, 32], BF16))
                wukv = _S.enter_context(sb("wukv", [128, 1024], BF16))
                wkr = _S.enter_context(sb("wkr", [128, 8, 32], BF16))
                cqn = _S.enter_context(sb("cqn", [128, 2, T], BF16))
                ckvn = _S.enter_context(sb("ckvn", [128, T], BF16))
                krope = _S.enter_context(sb("krope", [32, T], BF16))
                rp0 = _S.enter_context(sb("rp0", [32, 2, 512]))
                rp1 = _S.enter_context(sb("rp1", [32, 2, 512]))
                QT = _S.enter_context(sb("QT", [64, T], BF16))
                qrp = _S.enter_context(sb("qr", [32, T], BF16))
                KT = _S.enter_context(sb("KT", [64, T], BF16))
                Vh = _S.enter_context(sb("Vh", [128, NT, 64], BF16))
                csb = _S.enter_context(sb("csb", [128, 3, 512]))
                sq = _S.enter_context(sb("sq", [128, 3, 512], BF16))
                rs = _S.enter_context(sb("rs", [128, 2, 512]))
                rt = _S.enter_context(sb("rt", [32, 2, 512]))
                pt0 = _S.enter_context(sb("pt0", [128, 512], BF16))
                pt1 = _S.enter_context(sb("pt1", [128, 512], BF16))
                pt2 = _S.enter_context(sb("pt2", [128, 512], BF16))
                rden = _S.enter_context(sb("rden", [64, 512]))
                bwA, bwuq, bwuqr, bwukv, bwkr, bcqn, bckvn, bkrope, brope = [Buf() for _ in range(9)]
                bQT, bqr, bKT, bVh, bcsb, bsq, brs, brt, brden = [Buf() for _ in range(9)]
                pts = [pt0, pt1, pt2]; bpts = [Buf(), Buf(), Buf()]
                load_w_in(wA, bwA, l, 0, 416)
                P.dma("pool", wuq[:], dr["mla_w_uq"][l, :, :].rearrange("(j p) n -> p j n", p=128), writes=[bwuq])
                P.dma("pool", wukv[:], dr["mla_w_ukv"][l, :, :], writes=[bwukv])
                rps = [rp0, rp1]; brps = [Buf(), Buf()]; rpi = [0]

                def load_rope(sl):
                    i = rpi[0] % 2; rpi[0] += 1
                    P.dma("sp", rps[i][:], dr["rope"][:, :, sl], writes=[brps[i]])
                    return rps[i], brps[i]
                P.op("pool", lambda E: E.tensor_scalar_mul(wkr[:, :, 0:16], wA[:, :, 400:416], -1.0),
                     reads=[bwA], writes=[bwkr])
                P.op("pool", lambda E: E.tensor_copy(wkr[:, :, 16:32], wA[:, :, 384:400]), reads=[bwA], writes=[bwkr])
                wq4 = wuq[:].rearrange("p j (h d) -> p j h d", d=96)
                P.op("pool", lambda E: E.tensor_scalar_mul(wuqr[:, :, :, 0:16], wq4[:, :, :, 80:96], -1.0),
                     reads=[bwuq], writes=[bwuqr])
                P.op("pool", lambda E: E.tensor_copy(wuqr[:, :, :, 16:32], wq4[:, :, :, 64:80]),
                     reads=[bwuq], writes=[bwuqr])
                scale = 96.0 ** -0.5
                with contextlib.ExitStack() as _S:
                    m_a = _S.enter_context(ps("m_a", [128, 3, 512]))
                    m_s = _S.enter_context(ps("m_s", [128, 2, 512]))
                    m_r = _S.enter_context(ps("m_r", [32, 2, 512]))
                    bma, bms, bmr = PB(), PB(), PB()
                    for b in range(NB):
                        sl = slice(b * 512, (b + 1) * 512)
                        for j in range(3):
                            for kt in range(8):
                                P.op("pe", lambda E: E.matmul(m_a[:, j, :], wA[:, kt, j * 128:(j + 1) * 128],
                                                              hT[:, kt, sl], start=(kt == 0), stop=(kt == 7)),
                                     reads=[bwA, bh[kt]], writes=[bma])
                        P.op("act", lambda E: E.copy(csb[:], m_a[:]), reads=[bma], writes=[bcsb])
                        P.op("act", lambda E: E.activation(sq[:], m_a[:], AF.Square), reads=[bma], writes=[bsq])
                        P.op("pe", lambda E: E.matmul(m_s[:, 0, :], oneb[:], sq[:, 0, :], start=True, stop=False),
                             reads=[bsq, bcst], writes=[bms])
                        P.op("pe", lambda E: E.matmul(m_s[:, 0, :], oneb[:], sq[:, 1, :], start=False, stop=True),
                             reads=[bsq, bcst], writes=[bms])
                        P.op("pe", lambda E: E.matmul(m_s[:, 1, :], oneb[:], sq[:, 2, :], start=True, stop=True),
                             reads=[bsq, bcst], writes=[bms])
                        P.op("act", lambda E: E.activation(rs[:, 0, :], m_s[:, 0, :], AF.Sqrt, bias=RMS_EPS,
                                                           scale=1.0 / 256), reads=[bms], writes=[brs])
                        P.op("act", lambda E: E.activation(rs[:, 1, :], m_s[:, 1, :], AF.Sqrt, bias=RMS_EPS,
                                                           scale=1.0 / 128), reads=[bms], writes=[brs])
                        P.op("dve", lambda E: E.reciprocal(rs[:], rs[:]), reads=[brs], writes=[brs])
                        for j in range(2):
                            P.op("dve", lambda E: E.scalar_tensor_tensor(cqn[:, j, sl], csb[:, j, :],
                                                                         col(l, K_QN + j), rs[:, 0, :], ALU.mult,
                                                                         ALU.mult),
                                 reads=[bcsb, brs, bcols], writes=[bcqn])
                        P.op("dve", lambda E: E.scalar_tensor_tensor(ckvn[:, sl], csb[:, 2, :], col(l, K_KVN),
                                                                     rs[:, 1, :], ALU.mult, ALU.mult),
                             reads=[bcsb, brs, bcols], writes=[bckvn])
                        for kt in range(8):
                            P.op("pe", lambda E: E.matmul(m_r[:, 0, :], wA[:, kt, 384:416], hT[:, kt, sl],
                                                          start=(kt == 0), stop=(kt == 7)),
                                 reads=[bwA, bh[kt]], writes=[bmr])
                        for kt in range(8):
                            P.op("pe", lambda E: E.matmul(m_r[:, 1, :], wkr[:, kt, :], hT[:, kt, sl],
                                                          start=(kt == 0), stop=(kt == 7)),
                                 reads=[bwkr, bh[kt]], writes=[bmr])
                        rpt, brp = load_rope(sl)
                        P.op("dve", lambda E: E.tensor_tensor(rt[:], m_r[:], rpt[:], ALU.mult),
                             reads=[bmr, brp], writes=[brt])
                        P.op("dve", lambda E: E.tensor_tensor(krope[:, sl], rt[:, 0, :], rt[:, 1, :], ALU.add),
                             reads=[brt], writes=[bkrope])
                    P.barrier()
                with contextlib.ExitStack() as _S:
                    h_q_full = _S.enter_context(ps("h_q", [128, 512]))
                    h_q = h_q_full[0:64, :]
                    h_r = _S.enter_context(ps("h_r", [32, 2, 512]))
                    h_v = _S.enter_context(ps("h_v", [128, 4, 128]))
                    a_s0 = _S.enter_context(ps("a_s0", [128, 512]))
                    a_s1 = _S.enter_context(ps("a_s1", [128, 512]))
                    a_o = _S.enter_context(ps("a_ob", [64, 512]))
                    a_d = _S.enter_context(ps("a_db", [64, 512]))
                    h_k = h_q
                    bhq, bhr, bhv, bao, bad = [PB() for _ in range(5)]
                    bhk = bhq
                    a_s = [a_s0, a_s1, h_q_full]; bas = [PB(), PB(), bhq]
                    pi = 0
                    for h in range(8):
                        for b in range(NB):
                            sl = slice(b * 512, (b + 1) * 512)
                            for j in range(2):
                                P.op("pe", lambda E: E.matmul(h_q[:], wuq[:, j, 96 * h:96 * h + 64], cqn[:, j, sl],
                                                              start=(j == 0), stop=(j == 1)),
                                     reads=[bwuq, bcqn], writes=[bhq])
                            P.op("act", lambda E: E.copy(QT[:, sl], h_q[:]), reads=[bhq], writes=[bQT])
                            for j in range(2):
                                P.op("pe", lambda E: E.matmul(h_r[:, 0, :], wuq[:, j, 96 * h + 64:96 * h + 96],
                                                              cqn[:, j, sl], start=(j == 0), stop=(j == 1)),
                                     reads=[bwuq, bcqn], writes=[bhr])
                            for j in range(2):
                                P.op("pe", lambda E: E.matmul(h_r[:, 1, :], wuqr[:, j, h, :], cqn[:, j, sl],
                                                              start=(j == 0), stop=(j == 1)),
                                     reads=[bwuqr, bcqn], writes=[bhr])
                            rpt, brp = load_rope(sl)
                            P.op("dve", lambda E: E.tensor_tensor(rt[:], h_r[:], rpt[:], ALU.mult),
                                 reads=[bhr, brp], writes=[brt])
                            P.op("dve", lambda E: E.tensor_tensor(qrp[:, sl], rt[:, 0, :], rt[:, 1, :], ALU.add),
                                 reads=[brt], writes=[bqr])
                            P.op("pe", lambda E: E.matmul(h_k[:], wukv[:, 128 * h:128 * h + 64], ckvn[:, sl],
                                                          start=True, stop=True), reads=[bwukv, bckvn], writes=[bhk])
                            P.op("act", lambda E: E.copy(KT[:, sl], h_k[:]), reads=[bhk], writes=[bKT])
                            for tq in range(4):
                                tt_ = b * 4 + tq
                                P.op("pe", lambda E: E.matmul(h_v[:, tq, :], ckvn[:, tt_ * 128:(tt_ + 1) * 128],
                                                              wukv[:, 128 * h:128 * h + 128], start=True, stop=True),
                                     reads=[bwukv, bckvn], writes=[bhv])
                            P.op("act", lambda E: E.copy(Vh[:, b * 4:(b + 1) * 4, :], h_v[:, :, 64:128]),
                                 reads=[bhv], writes=[bVh])
                        r = h % 2
                        steps = []
                        for g in range(NB):
                            for kb in range(4 * g + 4):
                                steps.append((g, kb, kb == 0, kb == 4 * g + 3))

                        def issue_S(i):
                            g, kb, first, last = steps[i]
                            sps = a_s[i % 3]; bsp = bas[i % 3]
                            qs = slice(g * 512, (g + 1) * 512); ks = slice(kb * 128, (kb + 1) * 128)
                            P.op("pe", lambda E: E.matmul(sps, KT[:, ks], QT[:, qs], start=True, stop=False),
                                 reads=[bKT, bQT], writes=[bsp])
                            P.op("pe", lambda E: E.matmul(sps, krope[:, ks], qrp[:, qs], start=False, stop=True),
                                 reads=[bkrope, bqr], writes=[bsp])

                        def issue_rest(i):
                            g, kb, first, last = steps[i]
                            sps = a_s[i % 3]; bsp = bas[i % 3]
                            pt = pts[i % 3]; bpt = bpts[i % 3]
                            qs = slice(g * 512, (g + 1) * 512)
                            ii = max(kb - 4 * g, 0)
                            c0 = ii * 128
                            if not (dbg and dbg.get("x_b")):
                                P.op("act", lambda E: E.activation(pt[:, c0:512], sps[:, c0:512], AF.Exp, scale=scale),
                                     reads=[bsp], writes=[bpt])
                            if kb - 4 * g >= 0:
                                P.op("dve", lambda E: E.memset(pt[64:128, c0:c0 + 64], 0.0), writes=[bpt])
                            P.op("pe", lambda E: E.matmul(a_o[:, c0:512], Vh[:, kb, :], pt[:, c0:512], start=first,
                                                          stop=last), reads=[bVh, bpt], writes=[bao])
                            P.op("pe", lambda E: E.matmul(a_d[:, c0:512], oneb[:, 0:64], pt[:, c0:512], start=first,
                                                          stop=last), reads=[bpt, bcst], writes=[bad])
                            if last:
                                P.op("dve", lambda E: E.reciprocal(rden[:], a_d), reads=[bad], writes=[brden])
                                P.op("dve", lambda E: E.tensor_tensor(mixT[64 * r:64 * r + 64, h // 2, qs], a_o,
                                                                      rden[:], ALU.mult),
                                     reads=[bao, brden], writes=[bmix[h // 2]])

                        if dbg and dbg.get("x_a"):
                            steps = []
                        else:
                            issue_S(0)
                            issue_S(1)
                        for i in range(len(steps)):
                            if i + 2 < len(steps):
                                issue_S(i + 2)
                            issue_rest(i)
            P.barrier()

        def gdn_stage(l, s):
            with contextlib.ExitStack() as _S:
                gw = _S.enter_context(sb("gw", [128, 8, 256], BF16))
                gwab = _S.enter_context(sb("gwab", [128, 8, 8], BF16))
                gqT = _S.enter_context(sb("gqT", [128, 2, T], BF16))
                gkT = _S.enter_context(sb("gkT", [128, 2, T], BF16))
                gvT = _S.enter_context(sb("gvT", [128, 2, T], BF16))
                gzT = _S.enter_context(sb("gzT", [128, 2, T], BF16))
                gpre = _S.enter_context(sb("gpre", [128, 3 + T]))
                gacc = _S.enter_context(sb("gacc", [128, 512]))
                gsig = _S.enter_context(sb("gsig", [128, 512]))
                grn = _S.enter_context(sb("grn", [128, 512]))
                gpar = _S.enter_context(sb("gpar", [64, 12]))
                bgw, bgwab, bgq, bgk, bgv, bgz, bgpre, bgacc, bgsig, bgrn, bgpar = [Buf() for _ in range(11)]
                load_w_in(gwab, bgwab, l, 1440, 1448)
                P.dma("sp", gpar[:, 0:4], dr["gdn_a_log"][l:l + 1, :].partition_broadcast(64), writes=[bgpar])
                P.dma("sp", gpar[:, 4:8], dr["gdn_dt_bias"][l:l + 1, :].partition_broadcast(64), writes=[bgpar])
                P.op("act", lambda E: E.activation(gpar[:, 8:12], gpar[:, 0:4], AF.Exp), reads=[bgpar], writes=[bgpar])
                P.op("dve", lambda E: E.tensor_scalar_mul(gpar[:, 8:12], gpar[:, 8:12], -1.0), reads=[bgpar],
                     writes=[bgpar])
                P.op("pool", lambda E: E.memset(gpre[:, 0:3], 0.0), writes=[bgpre])
                bd = cst[:, C_BD:C_BD + 128]
                with contextlib.ExitStack() as _S:
                    g_p0 = _S.enter_context(ps("g_p0", [128, 512]))
                    g_p1 = _S.enter_context(ps("g_p1", [128, 512]))
                    g_ss = _S.enter_context(ps("g_ss", [128, 512]))
                    gps_ = [g_p0, g_p1]; bgps = [PB(), PB()]; bgss = PB()
                    k = 0
                    for gi, (c0, dst, bdst) in enumerate(((416, gqT, bgq), (672, gkT, bgk), (928, gvT, bgv),
                                                           (1184, gzT, bgz))):
                        load_w_in(gw, bgw, l, c0, c0 + 256)
                        for j in range(2):
                            if gi == 3:
                                for b in range(NB):
                                    sl = slice(b * 512, (b + 1) * 512)
                                    pp = gps_[k % 2]; bpp = bgps[k % 2]; k += 1
                                    for kt in range(8):
                                        P.op("pe", lambda E: E.matmul(pp[:], gw[:, kt, j * 128:(j + 1) * 128],
                                                                      hT[:, kt, sl], start=(kt == 0), stop=(kt == 7)),
                                             reads=[bgw, bh[kt]], writes=[bpp])
                                    P.op("act", lambda E: E.activation(gsig[:], pp[:], AF.Sigmoid), reads=[bpp],
                                         writes=[bgsig])
                                    P.op("dve", lambda E: E.tensor_tensor(dst[:, j, sl], pp[:], gsig[:], ALU.mult),
                                         reads=[bpp, bgsig], writes=[bdst])
                                continue
                            for b in range(NB):
                                sl = slice(b * 512, (b + 1) * 512)
                                pp = gps_[k % 2]; bpp = bgps[k % 2]; k += 1
                                for kt in range(8):
                                    P.op("pe", lambda E: E.matmul(pp[:], gw[:, kt, j * 128:(j + 1) * 128],
                                                                  hT[:, kt, sl], start=(kt == 0), stop=(kt == 7)),
                                         reads=[bgw, bh[kt]], writes=[bpp])
                                P.op("act", lambda E: E.copy(gpre[:, 3 + b * 512:3 + (b + 1) * 512], pp[:]),
                                     reads=[bpp], writes=[bgpre])
                            ct = gi * 2 + j
                            for b in range(NB):
                                sl = slice(b * 512, (b + 1) * 512)
                                P.op("dve", lambda E: E.tensor_scalar(gacc[:], gpre[:, b * 512:b * 512 + 512],
                                                                      col(l, K_CONV + ct * 4), None, ALU.mult),
                                     reads=[bgpre, bcols], writes=[bgacc])
                                for tap in range(1, 4):
                                    P.op("dve", lambda E: E.scalar_tensor_tensor(
                                        gacc[:], gpre[:, b * 512 + tap:b * 512 + tap + 512],
                                        col(l, K_CONV + ct * 4 + tap), gacc[:], ALU.mult, ALU.add),
                                        reads=[bgpre, bcols, bgacc], writes=[bgacc])
                                P.op("act", lambda E: E.activation(gsig[:], gacc[:], AF.Sigmoid), reads=[bgacc],
                                     writes=[bgsig])
                                if gi == 2:
                                    P.op("dve", lambda E: E.tensor_tensor(dst[:, j, sl], gacc[:], gsig[:], ALU.mult),
                                         reads=[bgacc, bgsig], writes=[bdst])
                                    continue
                                P.op("dve", lambda E: E.tensor_tensor(gacc[:], gacc[:], gsig[:], ALU.mult),
                                     reads=[bgacc, bgsig], writes=[bgacc])
                                P.op("act", lambda E: E.activation(gsig[:], gacc[:], AF.Square), reads=[bgacc],
                                     writes=[bgsig])
                                P.op("pe", lambda E: E.matmul(g_ss[:], bd, gsig[:], start=True, stop=True),
                                     reads=[bgsig, bcst], writes=[bgss])
                                P.op("act", lambda E: E.activation(grn[:], g_ss[:], AF.Sqrt, bias=RMS_EPS, scale=1.0),
                                     reads=[bgss], writes=[bgrn])
                                P.op("dve", lambda E: E.reciprocal(grn[:], grn[:]), reads=[bgrn], writes=[bgrn])
                                P.op("dve", lambda E: E.scalar_tensor_tensor(dst[:, j, sl], gacc[:],
                                                                             0.125 if gi == 0 else 1.0, grn[:],
                                                                             ALU.mult, ALU.mult),
                                     reads=[bgacc, bgrn], writes=[bdst])
                P.barrier()
                U = cst[0:64, C_U:C_U + 64]
                MLO = cst[0:64, C_MLO:C_MLO + 64]
                MUP = cst[0:64, C_MUP:C_MUP + 64]
                with contextlib.ExitStack() as _S:
                    sm = _S.enter_context(sb("c_sm", [64, 64]))
                    Gd = _S.enter_context(sb("c_Gd", [64, 4, 64]))
                    Dd = _S.enter_context(sb("c_D", [64, 4, 64]))
                    Ee = _S.enter_context(sb("c_E", [64, 4, 64]))
                    Et = _S.enter_context(sb("c_Et", [64, 4, 64]))
                    EGB = _S.enter_context(sb("c_EGB", [128, 256]))
                    tA = _S.enter_context(sb("c_tA", [64, 4, 64]))
                    A0 = _S.enter_context(sb("c_A0", [64, 4, 64], BF16))
                    A1 = _S.enter_context(sb("c_A1", [64, 4, 64], BF16))
                    B0 = _S.enter_context(sb("c_B0", [64, 4, 64], BF16))
                    B1 = _S.enter_context(sb("c_B1", [64, 4, 64], BF16))
                    Pm = _S.enter_context(sb("c_P", [64, 4, 64], BF16))
                    qkT = _S.enter_context(sb("c_qk", [64, 4, 64], BF16))
                    vtm = _S.enter_context(sb("c_vtm", [64, 4, 64]))
                    KDP = _S.enter_context(sb("c_KDP", [64, 4, 128], BF16))
                    VNP = _S.enter_context(sb("c_VNP", [64, 4, 128], BF16))
                    Rb = _S.enter_context(sb("c_R", [64, 4, 64], BF16))
                    t1 = _S.enter_context(sb("c_t1", [64, 4, 64]))
                    qdT = _S.enter_context(sb("c_qd", [128, 2, 64], BF16))
                    SP = _S.enter_context(sb("c_SP", [128, 2, 128]))
                    SPb = _S.enter_context(sb("c_SPb", [128, 2, 128], BF16))
                    glc = _S.enter_context(sb("c_gl", [128, 2]))
                    osq = _S.enter_context(sb("c_osq", [128, 2, 64]))
                    ors = _S.enter_context(sb("c_ors", [128, 2, 64]))
                    otm = _S.enter_context(sb("c_otm", [128, 2, 64]))
                    kmisc = _S.enter_context(ps("k_misc", [128, 512]))
                    kgcb = _S.enter_context(ps("k_gcb", [128, 256]))
                    kgk = _S.enter_context(ps("k_gk", [64, 2, 4, 64]))
                    ksq = _S.enter_context(ps("k_sq", [64, 2, 4, 64]))
                    kpv = _S.enter_context(ps("k_pv", [64, 2, 4, 64]))
                    ktr = _S.enter_context(ps("k_tr", [64, 3, 256], BF16))
                    kps1 = _S.enter_context(ps("k_ps1", [64, 4, 64]))
                    kU = _S.enter_context(ps("k_U", [128, 2, 128]))
                    (bsm, bGd, bD, bE, bEt, bEGB, btA, bP, bqk, bvtm, bKDP, bVNP, bR, bt1, bqd, bSP, bSPb, bgl, bosq,
                     bors, botm) = [Buf() for _ in range(21)]
                    bA = [Buf(), Buf()]; bB = [Buf(), Buf()]
                    b_ab = PB(); b_gcc = b_ab; b_ss = b_ab; b_o = b_ab
                    b_gcb = PB(); b_G = PB(); b_KQ = b_G; b_sq = PB(); b_pu = PB(); b_vn = b_pu
                    b_trB = PB(); b_trv = b_trB; b_trk = b_trB; b_ps1 = PB(); b_U = PB()
                    As = [A0, A1]; Bs = [B0, B1]
                    ab_ps = kmisc[0:64, 0:8]; gcc_ps = kmisc[0:64, 8:12]
                    ss_ps = kmisc[:, 64:192].rearrange("p (j c) -> p j c", j=2)
                    o_ps = kmisc[:, 256:384].rearrange("p (j c) -> p j c", j=2)
                    idb64 = idb[0:64, 0:64]
                    kz = _S.enter_context(sb("c_kz", [128, 2, 4, 64], BF16)); bkz = Buf()
                    P.op("pool", lambda E: E.memset(kz[:], 0.0), writes=[bkz])
                    P.op("pool", lambda E: E.memset(KDP[:], 0.0), writes=[bKDP])
                    P.op("pool", lambda E: E.memset(VNP[:], 0.0), writes=[bVNP])
                    P.op("pool", lambda E: E.memset(SP[:], 0.0), writes=[bSP])
                    P.op("pool", lambda E: E.memset(SPb[:], 0.0), writes=[bSPb])
                    g4, bt4, gcc, egc, ekd, tm4, sp4 = [sm[:, 4 * i:4 * i + 4] for i in range(7)]

                    def bc_h(ap4):
                        return ap4.unsqueeze(2).to_broadcast([64, 4, 64])

                    def bc_m(ap):
                        return ap.unsqueeze(1).to_broadcast([64, 4, 64])

                    opc = [0]
                    maxops = dbg.get("maxops", 10 ** 9) if dbg else 10 ** 9

                    def GOP(eng, fn, rd, wrt):
                        opc[0] += 1
                        if opc[0] <= maxops:
                            P.op(eng, fn, reads=rd, writes=wrt)

                    V = lambda fn, rd, wrt: GOP("dve", fn, rd, wrt)
                    AC = lambda fn, rd, wrt: GOP("act", fn, rd, wrt)
                    PE = lambda fn, rd, wrt: GOP("pe", fn, rd, wrt)
                    for c in range(NCH if not (dbg and 'nch' in dbg) else dbg['nch']):
                        cs_ = slice(c * 64, (c + 1) * 64)
                        for kt in range(8):
                            PE(lambda E: E.matmul(ab_ps, hT[:, kt, cs_], gwab[:, kt, :], start=(kt == 0),
                                                  stop=(kt == 7)), [bh[kt], bgwab], [b_ab])
                        V(lambda E: E.tensor_tensor(sp4, ab_ps[:, 0:4], gpar[:, 4:8], ALU.add), [b_ab, bgpar], [bsm])
                        AC(lambda E: E.activation(sp4, sp4, AF.Exp), [bsm], [bsm])
                        AC(lambda E: E.activation(sp4, sp4, AF.Ln, bias=1.0, scale=1.0), [bsm], [bsm])
                        V(lambda E: E.tensor_tensor(g4, sp4, gpar[:, 8:12], ALU.mult), [bsm, bgpar], [bsm])
                        AC(lambda E: E.activation(bt4, ab_ps[:, 4:8], AF.Exp, scale=-1.0), [b_ab], [bsm])
                        V(lambda E: E.tensor_scalar_add(bt4, bt4, 1.0), [bsm], [bsm])
                        V(lambda E: E.reciprocal(bt4, bt4), [bsm], [bsm])
                        PE(lambda E: E.matmul(gcc_ps, U, g4, start=True, stop=True), [bsm, bcst], [b_gcc])
                        V(lambda E: E.tensor_copy(gcc, gcc_ps), [b_gcc], [bsm])
                        V(lambda E: E.tensor_tensor(Gd[:], bc_m(U), bc_h(g4), ALU.mult), [bsm, bcst], [bGd])
                        PE(lambda E: E.matmul(kgcb[:], cst[0:64, C_ONE:C_ONE + 128],
                                              Gd[:].rearrange("p h j -> p (h j)"), start=True, stop=True),
                           [bGd, bcst], [b_gcb])
                        gcb3 = kgcb[0:64, :].rearrange("p (h j) -> p h j", h=4)
                        V(lambda E: E.tensor_tensor(Dd[:], bc_h(gcc), gcb3, ALU.subtract), [bsm, b_gcb], [bD])
                        V(lambda E: E.tensor_tensor(Ee[:], Dd[:], bc_m(MLO), ALU.add), [bD, bcst], [bE])
                        V(lambda E: E.tensor_tensor(Et[:], bc_m(MUP), Dd[:], ALU.subtract), [bD, bcst], [bEt])
                        AC(lambda E: E.activation(Ee[:], Ee[:], AF.Exp), [bE], [bE])
                        AC(lambda E: E.activation(Et[:], Et[:], AF.Exp), [bEt], [bEt])
                        AC(lambda E: E.activation(EGB[:], kgcb[:], AF.Exp), [b_gcb], [bEGB])
                        AC(lambda E: E.activation(egc, gcc, AF.Exp), [bsm], [bsm])
                        V(lambda E: E.tensor_tensor(tm4, gcb3[:, :, 63], gcc, ALU.subtract), [b_gcb, bsm], [bsm])
                        AC(lambda E: E.activation(ekd, tm4, AF.Exp), [bsm], [bsm])
                        EG4 = EGB[:].rearrange("p (j r c) -> p j r c", j=2, r=2)
                        V(lambda E: E.tensor_copy(glc[0:64, :], EG4[0:64, :, 0, 63]), [bEGB], [bgl])
                        V(lambda E: E.tensor_copy(glc[64:128, :], EG4[64:128, :, 1, 63]), [bEGB], [bgl])
                        for h in range(4):
                            j, r = h // 2, h % 2
                            rows = slice(64 * r, 64 * r + 64)
                            GOP("pool", lambda E: E.tensor_copy(kz[rows, 0, h, :], gkT[rows, j, cs_]), [bgk], [bkz])
                            GOP("pool", lambda E: E.tensor_copy(kz[rows, 1, h, :], gqT[rows, j, cs_]), [bgq], [bkz])
                        for h in range(4):
                            j, r = h // 2, h % 2
                            PE(lambda E: E.matmul(kgk[:, 0, h, :], gkT[:, j, cs_], kz[:, 0, h, :], start=True,
                                                  stop=True), [bgk, bkz], [b_G])
                            PE(lambda E: E.matmul(kgk[:, 1, h, :], gkT[:, j, cs_], kz[:, 1, h, :], start=True,
                                                  stop=True), [bgk, bkz], [b_KQ])
                        a = 0
                        V(lambda E: E.tensor_tensor(tA[:], kgk[:, 0, :, :], Ee[:], ALU.mult), [b_G, bE], [btA])
                        V(lambda E: E.tensor_tensor(As[a][:], tA[:], bc_h(bt4), ALU.mult), [btA, bsm], [bA[a]])
                        V(lambda E: E.tensor_tensor(qkT[:], kgk[:, 1, :, :], Et[:], ALU.mult), [b_KQ, bEt], [bqk])
                        trB = ktr[:, 0, :].rearrange("p (h i) -> p h i", h=4)
                        for h in range(4):
                            PE(lambda E: E.transpose(trB[:, h, :], As[a][:, h, :], idb64), [bA[a], bcst], [b_trB])
                        V(lambda E: E.tensor_copy(Bs[a][:], trB), [b_trB], [bB[a]])
                        V(lambda E: E.tensor_tensor(Pm[:], bc_m(idb64), Bs[a][:], ALU.subtract), [bB[a], bcst], [bP])
                        for lev in range(5):
                            n = 1 - a
                            for h in range(4):
                                PE(lambda E: E.matmul(ksq[:, 0, h, :], Bs[a][:, h, :], As[a][:, h, :], start=True,
                                                      stop=True), [bA[a], bB[a]], [b_sq])
                                PE(lambda E: E.matmul(ksq[:, 1, h, :], As[a][:, h, :], Bs[a][:, h, :], start=True,
                                                      stop=True), [bA[a], bB[a]], [b_sq])
                            AC(lambda E: E.copy(As[n][:], ksq[:, 0, :, :]), [b_sq], [bA[n]])
                            V(lambda E: E.tensor_copy(Bs[n][:], ksq[:, 1, :, :]), [b_sq], [bB[n]])
                            a = n
                            for h in range(4):
                                PE(lambda E: E.matmul(kpv[:, 0, h, :], As[a][:, h, :], Pm[:, h, :], start=True,
                                                      stop=True), [bA[a], bP], [b_pu])
                            V(lambda E: E.tensor_tensor(Pm[:], Pm[:], kpv[:, 0, :, :], ALU.add), [bP, b_pu], [bP])
                        for j in range(2):
                            PE(lambda E: E.transpose(ktr[:, 1, j * 128:(j + 1) * 128], gvT[:, j, cs_], idb[:]),
                               [bgv, bcst], [b_trv])
                            PE(lambda E: E.transpose(ktr[:, 2, j * 128:(j + 1) * 128], gkT[:, j, cs_], idb[:]),
                               [bgk, bcst], [b_trk])
                        AC(lambda E: E.copy(vtm[:].rearrange("p h d -> p (h d)"), ktr[:, 1, :]), [b_trv], [bvtm])
                        k4 = ktr[:, 2, :].rearrange("p (j r d) -> p j r d", j=2, r=2)
                        KD5 = KDP[:].rearrange("p (j r) m -> p j r m", j=2)
                        ekd3 = ekd.rearrange("p (j r) -> p j r", j=2)
                        for r in range(2):
                            V(lambda E: E.tensor_tensor(KD5[:, :, r, 64 * r:64 * r + 64], k4[:, :, r, :],
                                                        ekd3[:, :, r].unsqueeze(2).to_broadcast([64, 2, 64]),
                                                        ALU.mult), [b_trk, bsm], [bKDP])
                        for h in range(4):
                            j, r = h // 2, h % 2
                            rows = slice(64 * r, 64 * r + 64)
                            GOP("pool", lambda E: E.tensor_tensor(qdT[rows, j, :], gqT[rows, j, cs_],
                                                                  EGB[rows, h * 64:(h + 1) * 64], ALU.mult),
                                [bgq, bEGB], [bqd])
                        for j in range(2):
                            PE(lambda E: E.matmul(kps1[:, 2 * j:2 * j + 2, :].rearrange("p a b -> p (a b)"),
                                                  gkT[:, j, cs_], SPb[:, j, :], start=True, stop=True),
                               [bgk, bSPb], [b_ps1])
                        V(lambda E: E.tensor_tensor(t1[:], kps1[:], bc_h(egc), ALU.mult), [b_ps1, bsm], [bt1])
                        V(lambda E: E.tensor_tensor(t1[:], vtm[:], t1[:], ALU.subtract), [bvtm, bt1], [bt1])
                        V(lambda E: E.tensor_tensor(Rb[:], t1[:], bc_h(bt4), ALU.mult), [bt1, bsm], [bR])
                        for h in range(4):
                            PE(lambda E: E.matmul(kpv[:, 1, h, :], Pm[:, h, :], Rb[:, h, :], start=True, stop=True),
                               [bP, bR], [b_vn])
                        VN5 = VNP[:].rearrange("p (j r) m -> p j r m", j=2)
                        vn4 = kpv[:, 1, :, :].rearrange("p (j r) d -> p j r d", j=2)
                        for r in range(2):
                            AC(lambda E: E.copy(VN5[:, :, r, 64 * r:64 * r + 64], vn4[:, :, r, :]), [b_vn], [bVNP])
                        for j in range(2):
                            PE(lambda E: E.matmul(o_ps[:, j, :], SPb[:, j, :], qdT[:, j, :], start=True, stop=False),
                               [bSPb, bqd], [b_o])
                            PE(lambda E: E.matmul(o_ps[:, j, :], VNP[:, 2 * j, :], qkT[:, 2 * j, :], start=False,
                                                  stop=False), [bVNP, bqk], [b_o])
                            PE(lambda E: E.matmul(o_ps[:, j, :], VNP[:, 2 * j + 1, :], qkT[:, 2 * j + 1, :],
                                                  start=False, stop=True), [bVNP, bqk], [b_o])
                        AC(lambda E: E.activation(osq[:], o_ps, AF.Square), [b_o], [bosq])
                        PE(lambda E: E.matmul(ss_ps.rearrange("p j c -> p (j c)"), bd,
                                              osq[:].rearrange("p j c -> p (j c)"), start=True, stop=True),
                           [bosq, bcst], [b_ss])
                        AC(lambda E: E.activation(ors[:], ss_ps, AF.Sqrt, bias=RMS_EPS, scale=1.0 / 64), [b_ss], [bors])
                        V(lambda E: E.reciprocal(ors[:], ors[:]), [bors], [bors])
                        V(lambda E: E.scalar_tensor_tensor(otm[:], o_ps, col(l, K_GO), ors[:], ALU.mult, ALU.mult),
                          [b_o, bors, bcols], [botm])
                        for j in range(2):
                            V(lambda E: E.tensor_tensor(mixT[:, 4 + j, cs_], otm[:, j, :], gzT[:, j, cs_], ALU.mult),
                              [botm, bgz], [bmix[4 + j]])
                        for j in range(2):
                            PE(lambda E: E.matmul(kU[:, j, :], KDP[:, 2 * j, :], VNP[:, 2 * j, :], start=True,
                                                  stop=False), [bKDP, bVNP], [b_U])
                            PE(lambda E: E.matmul(kU[:, j, :], KDP[:, 2 * j + 1, :], VNP[:, 2 * j + 1, :],
                                                  start=False, stop=True), [bKDP, bVNP], [b_U])
                        for j in range(2):
                            V(lambda E: E.scalar_tensor_tensor(SP[:, j, :], SP[:, j, :], glc[:, j:j + 1],
                                                               kU[:, j, :], ALU.mult, ALU.add),
                              [bSP, bgl, b_U], [bSP])
                        AC(lambda E: E.copy(SPb[:], SP[:]), [bSP], [bSPb])
            P.barrier()

        def out_proj(l, s):
            with contextlib.ExitStack() as _S:
                wo = _S.enter_context(sb("wo", [128, 8, D], BF16))
                o0 = _S.enter_context(ps("ops0", [128, 512]))
                o1 = _S.enter_context(ps("ops1", [128, 512]))
                bwo = Buf(); ops_ = [o0, o1]; bops = [PB(), PB()]
                for kt in range(8):
                    P.dma("pool", wo[:, kt, :], dr["w_out"][l, kt * 128:(kt + 1) * 128, :], writes=[bwo])
                for kt in range(8):
                    P.op("act", lambda E: E.mul(xT[:, kt, :], xT[:, kt, :], ALPHA), reads=[bx[kt]], writes=[bx[kt]])
                k = 0
                for b in range(NB):
                    sl = slice(b * 512, (b + 1) * 512)
                    for m in range(8):
                        pp = ops_[k % 2]; bpp = bops[k % 2]; k += 1
                        for kt in range(8):
                            P.op("pe", lambda E: E.matmul(pp[:], wo[:, kt, m * 128:(m + 1) * 128], mixT[:, kt, sl],
                                                          start=(kt == 0), stop=(kt == 7)),
                                 reads=[bwo, bmix[kt]], writes=[bpp])
                        P.op("dve", lambda E: E.scalar_tensor_tensor(xT[:, m, sl], pp[:], modc(l, 2, m, s),
                                                                     xT[:, m, sl], ALU.mult, ALU.add),
                             reads=[bpp, bmod, bx[m]], writes=[bx[m]])
            P.barrier()

        def moe_stage(l, s):
            with contextlib.ExitStack() as _S:
                wr = _S.enter_context(sb("wr", [128, 8, 36]))
                brb = _S.enter_context(sb("brb", [128, 36]))
                h2f = _S.enter_context(sb("h2f", [128, 8, 512]))
                Lg = _S.enter_context(sb("Lg", [128, 36]))
                sm = _S.enter_context(sb("sm", [128, 16]))
                msk = _S.enter_context(sb("msk", [128, 3, 32]))
                Wt = _S.enter_context(sb("Wt", [128, 32]))
                WtT = _S.enter_context(sb("WtT", [32, T]))
                r_l = _S.enter_context(ps("r_l", [128, 36]))
                r_t = _S.enter_context(ps("r_t", [32, 128]))
                bwr, bbrb, bh2f, bLg, bsm, bmsk, bWt, bWtT = [Buf() for _ in range(8)]
                brl, brt_ = PB(), PB()
                P.dma("sp", wr[:], dr["w_r"][l, :, :].rearrange("(k p) n -> p k n", p=128), writes=[bwr])
                P.dma("sp", brb[:], dr["b_r"][l:l + 1, :].partition_broadcast(128), writes=[bbrb])
                for b in range(NB):
                    sl = slice(b * 512, (b + 1) * 512)
                    for kt in range(8):
                        P.op("act", lambda E: E.activation(h2f[:, kt, :], xT[:, kt, sl], AF.Identity,
                                                           bias=modc(l, 3, kt, s), scale=modc(l, 4, kt, s)),
                             reads=[bx[kt], bmod], writes=[bh2f])
                        P.op("pool", lambda E: E.tensor_copy(hT[:, kt, sl], h2f[:, kt, :]), reads=[bh2f],
                             writes=[bh[kt]])
                    for tq in range(4):
                        t0 = b * 512 + tq * 128
                        for kt in range(8):
                            P.op("pe", lambda E: E.matmul(r_l[:], h2f[:, kt, tq * 128:(tq + 1) * 128], wr[:, kt, :],
                                                          start=(kt == 0), stop=(kt == 7)),
                                 reads=[bh2f, bwr], writes=[brl])
                        V = lambda fn, rd, wrt: P.op("dve", fn, reads=rd, writes=wrt)
                        V(lambda E: E.tensor_tensor(Lg[:], r_l[:], brb[:], ALU.add), [brl, bbrb], [bLg])
                        V(lambda E: E.reduce_max(sm[:, 0:1], Lg[:, 0:4], AX.X), [bLg], [bsm])
                        V(lambda E: E.tensor_scalar_mul(sm[:, 1:2], sm[:, 0:1], -1.0), [bsm], [bsm])
                        V(lambda E: E.tensor_scalar(msk[:, 0, 0:4], Lg[:, 0:4], sm[:, 0:1], None, ALU.is_equal),
                          [bLg, bsm], [bmsk])
                        P.op("act", lambda E: E.activation(msk[:, 0, 8:12], Lg[:, 0:4], AF.Exp, bias=sm[:, 1:2],
                                                           scale=1.0, accum_out=sm[:, 2:3]),
                             reads=[bLg, bsm], writes=[bmsk, bsm])
                        V(lambda E: E.reciprocal(sm[:, 3:4], sm[:, 2:3]), [bsm], [bsm])
                        V(lambda E: E.tensor_scalar(msk[:, 0, 4:8], msk[:, 0, 0:4], -NEG, NEG, ALU.mult, ALU.add),
                          [bmsk], [bmsk])
                        V(lambda E: E.tensor_tensor(msk[:, 1, :].rearrange("p (g e) -> p g e", g=4),
                                                    Lg[:, 4:36].rearrange("p (g e) -> p g e", g=4),
                                                    msk[:, 0, 4:8].unsqueeze(2).to_broadcast([128, 4, 8]), ALU.add),
                          [bLg, bmsk], [bmsk])
                        V(lambda E: E.reduce_max(sm[:, 4:5], msk[:, 1, :], AX.X), [bmsk], [bsm])
                        V(lambda E: E.tensor_scalar(msk[:, 2, :], msk[:, 1, :], sm[:, 4:5], None, ALU.is_equal),
                          [bmsk, bsm], [bmsk])
                        V(lambda E: E.scalar_tensor_tensor(msk[:, 1, :], msk[:, 2, :], NEG, msk[:, 1, :], ALU.mult,
                                                           ALU.add), [bmsk], [bmsk])
                        V(lambda E: E.reduce_max(sm[:, 5:6], msk[:, 1, :], AX.X), [bmsk], [bsm])
                        V(lambda E: E.tensor_scalar(msk[:, 1, :], msk[:, 1, :], sm[:, 5:6], None, ALU.is_equal),
                          [bmsk, bsm], [bmsk])
                        V(lambda E: E.tensor_tensor(sm[:, 6:7], sm[:, 5:6], sm[:, 4:5], ALU.subtract), [bsm], [bsm])
                        P.op("act", lambda E: E.activation(sm[:, 7:8], sm[:, 6:7], AF.Exp), reads=[bsm], writes=[bsm])
                        V(lambda E: E.tensor_scalar_add(sm[:, 8:9], sm[:, 7:8], 1.0), [bsm], [bsm])
                        V(lambda E: E.reciprocal(sm[:, 8:9], sm[:, 8:9]), [bsm], [bsm])
                        V(lambda E: E.tensor_tensor(sm[:, 9:10], sm[:, 3:4], sm[:, 8:9], ALU.mult), [bsm], [bsm])
                        V(lambda E: E.tensor_tensor(sm[:, 10:11], sm[:, 9:10], sm[:, 7:8], ALU.mult), [bsm], [bsm])
                        V(lambda E: E.tensor_scalar(Wt[:], msk[:, 2, :], sm[:, 9:10], None, ALU.mult),
                          [bmsk, bsm], [bWt])
                        V(lambda E: E.scalar_tensor_tensor(Wt[:], msk[:, 1, :], sm[:, 10:11], Wt[:], ALU.mult,
                                                           ALU.add), [bmsk, bsm, bWt], [bWt])
                        P.op("pe", lambda E: E.transpose(r_t[:], Wt[:], ident), reads=[bWt, bcst], writes=[brt_])
                        P.op("act", lambda E: E.copy(WtT[:, t0:t0 + 128], r_t[:]), reads=[brt_], writes=[bWtT])
                P.dma("sp", scr[:, :], WtT[:], reads=[bWtT], writes=[bscr])
            P.barrier()
            for kt in range(8):
                P.op("act", lambda E: E.mul(xT[:, kt, :], xT[:, kt, :], ALPHA), reads=[bx[kt]], writes=[bx[kt]])
            TB = 256
            NTB = T // TB
            with contextlib.ExitStack() as _S:
                A_ = lambda nm, shp, dt=F32: _S.enter_context(sb(nm, shp, dt))
                stg_g, stg_u, stg_d = A_("stg_g", [128, 8, 256]), A_("stg_u", [128, 8, 256]), A_("stg_d", [128, 2, D])
                wg = [A_("wg0", [128, 8, 256], BF16), A_("wg1", [128, 8, 256], BF16)]
                wu = [A_("wu0", [128, 8, 256], BF16), A_("wu1", [128, 8, 256], BF16)]
                wd = [A_("wd0", [128, 2, D], BF16), A_("wd1", [128, 2, D], BF16)]
                wb = [A_("wb0", [128, T]), A_("wb1", [128, T])]
                ea = [A_("ea0", [128, 2, TB]), A_("ea1", [128, 2, TB])]
                eb = [A_("eb0", [128, 2, TB], BF16), A_("eb1", [128, 2, TB], BF16)]
                eg = [_S.enter_context(ps("eg0", [128, 2, TB])), _S.enter_context(ps("eg1", [128, 2, TB]))]
                eu = [_S.enter_context(ps("eu0", [128, 2, TB])), _S.enter_context(ps("eu1", [128, 2, TB]))]
                ey = _S.enter_context(ps("ey", [128, 8, TB]))
                bsg, bsu, bsd = Buf(), Buf(), Buf()
                bwg, bwu, bwd, bwb, bea, beb = [[Buf(), Buf()] for _ in range(6)]
                beg, beu = [[PB(), PB()] for _ in range(2)]
                beyb = [PB() for _ in range(4)]
                NE = NEXP if not (dbg and 'nexp' in dbg) else dbg['nexp']

                def dma_expert(e):
                    if e >= NE:
                        return
                    P.dma("sp", stg_g[:], dr["moe_w_gate"][l, e, :, :].rearrange("(k p) n -> p k n", p=128),
                          writes=[bsg])
                    P.dma("sp", stg_u[:], dr["moe_w_up"][l, e, :, :].rearrange("(k p) n -> p k n", p=128),
                          writes=[bsu])
                    P.dma("sp", stg_d[:], dr["moe_w_down"][l, e, :, :].rearrange("(f p) n -> p f n", p=128),
                          writes=[bsd])

                def cast_expert(e):
                    if e >= NE:
                        return
                    w = e % 2
                    P.op("act", lambda E: E.copy(wg[w][:], stg_g[:]), reads=[bsg], writes=[bwg[w]])
                    P.op("pool", lambda E: E.tensor_copy(wu[w][:], stg_u[:]), reads=[bsu], writes=[bwu[w]])
                    P.op("pool", lambda E: E.tensor_copy(wd[w][:], stg_d[:]), reads=[bsd], writes=[bwd[w]])
                    P.dma("sp", wb[w][:], scr[e:e + 1, :].partition_broadcast(128), reads=[bscr], writes=[bwb[w]])

                units = [(e, tb) for e in range(NE) for tb in range(NTB)]

                def gu(i):
                    e, tb = units[i]
                    w = e % 2; q = i % 2
                    sl = slice(tb * TB, (tb + 1) * TB)
                    for f in range(2):
                        for kt in range(8):
                            P.op("pe", lambda E: E.matmul(eg[q][:, f, :], wg[w][:, kt, f * 128:(f + 1) * 128],
                                                          hT[:, kt, sl], start=(kt == 0), stop=(kt == 7)),
                                 reads=[bwg[w], bh[kt]], writes=[beg[q]])
                    for f in range(2):
                        for kt in range(8):
                            P.op("pe", lambda E: E.matmul(eu[q][:, f, :], wu[w][:, kt, f * 128:(f + 1) * 128],
                                                          hT[:, kt, sl], start=(kt == 0), stop=(kt == 7)),
                                 reads=[bwu[w], bh[kt]], writes=[beu[q]])

                def rest(i):
                    e, tb = units[i]
                    w = e % 2; q = i % 2
                    sl = slice(tb * TB, (tb + 1) * TB)
                    P.op("act", lambda E: E.activation(ea[q][:], eg[q][:], AF.Silu), reads=[beg[q]], writes=[bea[q]])
                    P.op("dve", lambda E: E.tensor_tensor(ea[q][:], ea[q][:], eu[q][:], ALU.mult),
                         reads=[bea[q], beu[q]], writes=[bea[q]])
                    P.op("dve", lambda E: E.tensor_tensor(eb[q][:], ea[q][:],
                                                           wb[w][:, sl].unsqueeze(1).to_broadcast([128, 2, TB]),
                                                           ALU.mult), reads=[bea[q], bwb[w]], writes=[beb[q]])
                    for m in range(8):
                        for f in range(2):
                            P.op("pe", lambda E: E.matmul(ey[:, m, :], wd[w][:, f, m * 128:(m + 1) * 128],
                                                          eb[q][:, f, :], start=(f == 0), stop=(f == 1)),
                                 reads=[bwd[w], beb[q]], writes=[beyb[m // 2]])
                    for m in range(8 if not (dbg and dbg.get("x_noacc")) else 1):
                        P.op("dve", lambda E: E.scalar_tensor_tensor(xT[:, m, sl], ey[:, m, :], modc(l, 5, m, s),
                                                                     xT[:, m, sl], ALU.mult, ALU.add),
                             reads=[beyb[m // 2], bmod, bx[m]], writes=[bx[m]])

                dma_expert(0); cast_expert(0); dma_expert(1); cast_expert(1); dma_expert(2)
                if units:
                    gu(0)
                for i in range(len(units)):
                    if i + 1 < len(units):
                        gu(i + 1)
                    rest(i)
                    e, tb = units[i]
                    if tb == NTB - 1:
                        cast_expert(e + 2)
                        dma_expert(e + 3)
            P.barrier()

        STAGES = dbg.get("stages", "mgpoLME") if dbg else "mgpoLME"
        for s in range(NSEQ):
            for kt in range(8):
                P.dma("sp", xT[:, kt, :], dr["xT"][s, kt * 128:(kt + 1) * 128, :], writes=[bx[kt]])
            for l in range(DEPTH):
                modulate(l, s, 0, 1)
                if "z" in STAGES:
                    for kt in range(8):
                        P.op("pool", lambda E: E.memset(mixT[:, kt, :], 0.0), writes=[bmix[kt]])
                if "m" in STAGES:
                    mla_stage(l, s)
                if "g" in STAGES:
                    gdn_stage(l, s)
                if "p" in STAGES:
                    pool_stage(l, s)
                if dbg and dbg.get("dump") == "mix" and s == dbg.get("s", 0) and l == dbg.get("l", 0):
                    with contextlib.ExitStack() as _S:
                        dmp = _S.enter_context(sb("dmp", [128, 8, T]))
                        bd = Buf()
                        for kt in range(8):
                            P.op("dve", lambda E: E.tensor_copy(dmp[:, kt, :], (hT if dbg.get("src") == "h" else mixT)[:, kt, :]), reads=[bmix[kt], bh[kt]],
                                 writes=[bd])
                        P.dma("sp", dbg_out.rearrange("(k p) t -> p k t", p=128), dmp[:], reads=[bd])
                        P.barrier()
                if "o" in STAGES:
                    out_proj(l, s)
                if "L" in STAGES:
                    layer_norm(l, K_LN1G, K_LN1B)
                if "M" in STAGES:
                    moe_stage(l, s)
                if "E" in STAGES:
                    layer_norm(l, K_LN2G, K_LN2B)
            for kt in range(8):
                P.dma("sp", yT[s, kt * 128:(kt + 1) * 128, :], xT[:, kt, :], reads=[bx[kt]])
        P.barrier()
    P.es.close()
    return nc


def make_consts():
    c = np.zeros((128, NCON), np.float32)
    c[:, C_ID:C_ID + 128] = np.eye(128, dtype=np.float32)
    c[:, C_ONE:C_ONE + 128] = 1.0
    i = np.arange(64)
    c[:64, C_U:C_U + 64] = (i[:, None] <= i[None, :]).astype(np.float32)
    c[:64, C_MLO:C_MLO + 64] = np.where(i[:, None] > i[None, :], 0.0, NEG)
    c[:64, C_MUP:C_MUP + 64] = np.where(i[None, :] >= i[:, None], 0.0, NEG)
    t = np.arange(16)
    for j in range(2):
        for p in range(128):
            w = 2 ** (2 * j + p // 64 + 1)
            c[p, C_INV16 + 16 * j:C_INV16 + 16 * j + 16] = 1.0 / np.minimum(t + 1, w)
    p = np.arange(128)
    c[:, C_BD:C_BD + 128] = (p[:, None] // 64 == p[None, :] // 64).astype(np.float32)
    return c


def make_rope(T):
    inv_freq = np.power(np.float32(10000.0), -np.arange(0, 32, 2, dtype=np.float32) / np.float32(32)).astype(np.float32)
    ang = (np.arange(T, dtype=np.float32)[:, None] * inv_freq[None, :]).astype(np.float32)
    cos, sin = np.cos(ang).astype(np.float32).T, np.sin(ang).astype(np.float32).T
    r = np.zeros((32, 2, T), np.float32)
    r[:16, 0], r[16:, 0], r[:16, 1], r[16:, 1] = cos, cos, sin, sin
    return r


def make_cols(inp):
    L = inp["w_in"].shape[0]
    cols = np.zeros((L, 128, NCOLS), np.float32)

    def put(k, vec, n):
        cols[:, :, k:k + n] = vec.reshape(L, n, 128).transpose(0, 2, 1)

    put(K_QN, inp["mla_q_norm"], 2); put(K_KVN, inp["mla_kv_norm"], 1)
    put(K_LN1G, inp["ln1_g"], 8); put(K_LN1B, inp["ln1_b"], 8); put(K_LN2G, inp["ln2_g"], 8); put(K_LN2B, inp["ln2_b"], 8)
    put(K_PS, inp["pool_scale"], 2)
    cols[:, :, K_GO] = np.concatenate([inp["gdn_out_norm"], inp["gdn_out_norm"]], axis=1)
    cv = inp["gdn_conv"].reshape(L, 4, 6, 128)
    cols[:, :, K_CONV:K_CONV + 24] = cv.transpose(0, 3, 2, 1).reshape(L, 128, 24)
    put(K_BMOD, inp["b_mod"], 48)
    return cols


_NC_CACHE = {}


def run(inp, T, DEPTH, NSEQ, ncores, dbg=None):
    key = (T, DEPTH, NSEQ, repr(dbg))
    if key not in _NC_CACHE:
        _NC_CACHE[key] = build(T, DEPTH, NSEQ, dbg)
    nc = _NC_CACHE[key]
    f = lambda a: np.ascontiguousarray(np.asarray(a, dtype=np.float32))
    shared = {
        "consts": make_consts(), "rope": make_rope(T), "cols": f(make_cols(inp)),
        "w_in": f(inp["w_in"]), "mla_w_uq": f(inp["mla_w_uq"]), "mla_w_ukv": f(inp["mla_w_ukv"]),
        "gdn_a_log": f(inp["gdn_a_log"]), "gdn_dt_bias": f(inp["gdn_dt_bias"]), "pool_w": f(inp["pool_w"]),
        "w_out": f(inp["w_out"]), "w_mod": f(inp["w_mod"]),
        "w_r": f(np.concatenate([inp["router_w_group"], inp["router_w_expert"]], axis=2)),
        "b_r": f(np.concatenate([inp["router_b_group"], inp["router_b_expert"]], axis=1)),
        "moe_w_gate": f(inp["moe_w_gate"]), "moe_w_up": f(inp["moe_w_up"]), "moe_w_down": f(inp["moe_w_down"]),
    }
    x = np.asarray(inp["x"], np.float32); c = np.asarray(inp["c"], np.float32)
    in_maps = []
    for i in range(ncores):
        m = dict(shared)
        m["xT"] = f(x[i * NSEQ:(i + 1) * NSEQ].transpose(0, 2, 1))
        m["cT"] = f(c[i * NSEQ:(i + 1) * NSEQ].T)
        in_maps.append(m)
    res = run_bass_kernel_spmd(nc, in_maps, core_ids=list(range(ncores)))
    out = np.concatenate([r["yT"].transpose(0, 2, 1) for r in res.results], axis=0)
    return out, res


def kernel(**inputs):
    out, _ = run(inputs, 2048, 4, 2, 8)
    return out.astype(np.float32)
```

```python
import contextlib
import numpy as np
import concourse.bass as bass
import concourse.mybir as mybir
from concourse.bass_utils import run_bass_kernel_spmd

F32 = mybir.dt.float32
BF16 = mybir.dt.bfloat16
AF = mybir.ActivationFunctionType
ALU = mybir.AluOpType
AX = mybir.AxisListType

SEG = 28000
ENGS = ("pe", "dve", "act", "pool", "sp")
NDMA = 8

D = 1024
NEXP = 32
ALPHA = 8.0 ** 0.25
LN_EPS = 1e-5
RMS_EPS = 1e-6
NEG = -30000.0

C_ID, C_ONE, C_U, C_MLO, C_MUP, C_INV16, C_BD, NCON = 0, 128, 256, 320, 384, 448, 480, 608
K_QN, K_KVN, K_LN1G, K_LN1B, K_LN2G, K_LN2B, K_PS, K_GO, K_CONV, K_BMOD, NCOLS = 0, 2, 3, 11, 19, 27, 35, 37, 38, 62, 110


class Buf:
    __slots__ = ("name", "lw", "rd", "excl")

    def __init__(self, name="", excl=False):
        self.name = name
        self.lw = None
        self.rd = {}
        self.excl = excl


def PB():
    return Buf("psum", True)


class Prog:
    def __init__(self, nc, same_engine_sync=True):
        self.nc = nc
        self.es = contextlib.ExitStack()
        self.cnt = {e: 0 for e in ENGS}
        self.seen = {e: {} for e in ENGS}
        self.sems = {}
        self.dma_i = {e: 0 for e in ENGS}
        self.same = same_engine_sync
        self.E = {"pe": nc.tensor, "dve": nc.vector, "act": nc.scalar, "pool": nc.gpsimd, "sp": nc.sync}
        self.last_dma = {}

    def sem(self, key):
        if key not in self.sems:
            nm = "s_" + "_".join(str(k) for k in key)
            self.sems[key] = self.es.enter_context(self.nc.semaphore(nm))
        return self.sems[key]

    def _wait(self, eng, tok):
        key, val = tok
        if self.seen[eng].get(key, 0) >= val:
            return
        if key[0] == eng and (eng == "pe" or not self.same):
            return
        self.seen[eng][key] = val
        self.E[eng].wait_ge(self.sem(key), val)

    def _deps(self, eng, reads, writes):
        for b in reads:
            if b.lw is not None:
                self._wait(eng, b.lw)
        for b in writes:
            if b.excl:
                if b.lw is not None and b.lw[0][0] != eng:
                    self._wait(eng, b.lw)
                for k, v in b.rd.items():
                    if k[0] != eng:
                        self._wait(eng, (k, v))
                continue
            if b.lw is not None:
                self._wait(eng, b.lw)
            for k, v in b.rd.items():
                if k[0] != eng:
                    self._wait(eng, (k, v))

    def _mark(self, tok, reads, writes):
        k, v = tok
        for b in reads:
            if b.rd.get(k, 0) < v:
                b.rd[k] = v
        for b in writes:
            if b.excl:
                if b.lw is not None and b.lw[0][0] != k[0]:
                    pk, pv = b.lw
                    if b.rd.get(pk, 0) < pv:
                        b.rd[pk] = pv
                b.rd = {kk: vv for kk, vv in b.rd.items() if kk[0] != k[0]}
                b.lw = tok
                continue
            b.lw = tok
            b.rd = {}

    def op(self, eng, fn, reads=(), writes=()):
        if any(b.excl for b in reads):
            writes = list(writes) + [b for b in reads if b.excl]
            reads = [b for b in reads if not b.excl]
        self._deps(eng, reads, writes)
        n = self.cnt[eng]
        key = (eng, n // SEG)
        val = n % SEG + 1
        fn(self.E[eng]).then_inc(self.sem(key), 1)
        self.cnt[eng] = n + 1
        self._mark((key, val), reads, writes)

    def dma(self, eng, out, in_, reads=(), writes=(), **kw):
        i = self.dma_i[eng]
        self.dma_i[eng] = i + 1
        key = ("d" + eng, i % NDMA)
        val = (i // NDMA + 1) * 16
        if val > 16:
            self._wait(eng, (key, val - 16))
        self._deps(eng, reads, writes)
        self.E[eng].dma_start(out=out, in_=in_, **kw).then_inc(self.sem(key), 16)
        self.last_dma[key] = val
        self._mark((key, val), reads, writes)

    def barrier(self):
        toks = []
        for e in ENGS:
            n = self.cnt[e]
            if n:
                toks.append(((e, (n - 1) // SEG), (n - 1) % SEG + 1))
        toks += list(self.last_dma.items())
        for e in ENGS:
            for t in toks:
                self._wait(e, t)


def build(T, DEPTH, NSEQ, dbg=None):
    nc = bass.Bass("TRN2", target_bir_lowering=False)
    P = Prog(nc)
    NB = T // 512
    NT = T // 128
    NCH = T // 64
    dr = {}

    def din(name, shape):
        dr[name] = nc.dram_tensor(name, list(shape), F32, kind="ExternalInput").ap()

    din("xT", [NSEQ, D, T]); din("cT", [D, NSEQ]); din("consts", [128, NCON]); din("rope", [32, 2, T])
    din("cols", [4, 128, NCOLS]); din("w_in", [4, D, 1704]); din("mla_w_uq", [4, 256, 768])
    din("mla_w_ukv", [4, 128, 1024]); din("gdn_a_log", [4, 4]); din("gdn_dt_bias", [4, 4])
    din("pool_w", [4, 4, 64, 64]); din("w_out", [4, D, D]); din("w_mod", [4, D, 6 * D])
    din("w_r", [4, D, 36]); din("b_r", [4, 36])
    din("moe_w_gate", [4, NEXP, D, 256]); din("moe_w_up", [4, NEXP, D, 256]); din("moe_w_down", [4, NEXP, 256, D])
    yT = nc.dram_tensor("yT", [NSEQ, D, T], F32, kind="ExternalOutput").ap()
    scr = nc.dram_tensor("scr_wt", [NEXP, T], F32).ap()
    bscr = Buf("scr")
    wbf = {"w_in": nc.dram_tensor("bf_w_in", [4, D, 1704], BF16).ap(),
           "w_out": nc.dram_tensor("bf_w_out", [4, D, D], BF16).ap(),
           "mla_w_uq": nc.dram_tensor("bf_w_uq", [4, 256, 768], BF16).ap(),
           "mla_w_ukv": nc.dram_tensor("bf_w_ukv", [4, 128, 1024], BF16).ap()}
    bwbf = {k: [Buf() for _ in range(4)] for k in wbf}
    if dbg:
        dbg_out = nc.dram_tensor("dbg", [D, T], F32, kind="ExternalOutput").ap()

    uid = [0]

    def sb(name, shape, dt=F32):
        uid[0] += 1
        return nc.sbuf_tensor("%s_%d" % (name, uid[0]), list(shape), dt)

    @contextlib.contextmanager
    def ps(name, shape, dt=F32):
        uid[0] += 1
        isz = 4 if dt == F32 else 2
        per = 2048 // isz
        n = 1
        for d_ in shape[1:]:
            n *= d_
        nb = (n + per - 1) // per
        with nc.psum_tensor("%s_%d" % (name, uid[0]), [128, nb * per], dt) as t:
            v = t[0:shape[0], 0:n]
            if len(shape) == 3:
                v = v.rearrange("p (a b) -> p a b", a=shape[1])
            elif len(shape) == 4:
                v = v.rearrange("p (a b c) -> p a b c", a=shape[1], b=shape[2])
            yield v

    es = contextlib.ExitStack()
    with es:
        cst = es.enter_context(sb("cst", [128, NCON])); bcst = Buf()
        idb = es.enter_context(sb("idb", [128, 128], BF16))
        oneb = es.enter_context(sb("oneb", [128, 128], BF16))
        colsT = es.enter_context(sb("colsT", [128, DEPTH, NCOLS])); bcols = Buf()
        modT = es.enter_context(sb("modT", [128, DEPTH, 48, NSEQ])); bmod = Buf()
        xT = es.enter_context(sb("xT_sb", [128, 8, T])); bx = [Buf() for _ in range(8)]
        hT = es.enter_context(sb("hT_sb", [128, 8, T], BF16)); bh = [Buf() for _ in range(8)]
        mixT = es.enter_context(sb("mixT_sb", [128, 8, T], BF16)); bmix = [Buf() for _ in range(8)]

        P.dma("sp", cst[:], dr["consts"][:, :], writes=[bcst])
        for l in range(DEPTH):
            P.dma("sp", colsT[:, l, :], dr["cols"][l, :, :], writes=[bcols])
        P.op("dve", lambda E: E.tensor_copy(idb[:], cst[:, C_ID:C_ID + 128]), reads=[bcst], writes=[bcst])
        P.op("dve", lambda E: E.tensor_copy(oneb[:], cst[:, C_ONE:C_ONE + 128]), reads=[bcst], writes=[bcst])
        for l in range(DEPTH):
            for k_ in ("w_in", "mla_w_uq", "mla_w_ukv", "w_out"):
                rows = dr[k_].shape[1]
                for r0 in range(0, rows, 512):
                    r1 = min(rows, r0 + 512)
                    P.dma("pool", wbf[k_][l, r0:r1, :], dr[k_][l, r0:r1, :], writes=[bwbf[k_][l]])
        ident = cst[:, C_ID:C_ID + 128]
        ones = cst[:, C_ONE:C_ONE + 128]

        def col(l, k):
            return colsT[:, l, k:k + 1]

        with contextlib.ExitStack() as _S:
            scT = _S.enter_context(sb("scT", [128, 8, NSEQ]))
            sgT = _S.enter_context(sb("sgT", [128, 8, NSEQ]))
            wm0 = _S.enter_context(sb("wm0", [128, 8, 768]))
            wm1 = _S.enter_context(sb("wm1", [128, 8, 768]))
            modps = _S.enter_context(ps("modps", [128, 48, NSEQ]))
            bsc = Buf(); bwm = [Buf(), Buf()]; bmp = PB()
            wms = [wm0, wm1]
            P.dma("sp", scT[:], dr["cT"].rearrange("(k p) s -> p k s", p=128), writes=[bsc])
            P.op("act", lambda E: E.activation(sgT[:], scT[:], AF.Sigmoid), reads=[bsc], writes=[bsc])
            P.op("dve", lambda E: E.tensor_tensor(scT[:], scT[:], sgT[:], ALU.mult), reads=[bsc], writes=[bsc])
            ci = 0
            for l in range(DEPTH):
                for c in range(8):
                    wm = wms[ci % 2]; bw = bwm[ci % 2]; ci += 1
                    P.dma("sp", wm[:], dr["w_mod"][l, :, c * 768:(c + 1) * 768].rearrange("(k p) n -> p k n", p=128),
                          writes=[bw])
                    for mm in range(6):
                        m = c * 6 + mm
                        for kt in range(8):
                            P.op("pe", lambda E: E.matmul(modps[:, m, :], wm[:, kt, mm * 128:(mm + 1) * 128],
                                                          scT[:, kt, :], start=(kt == 0), stop=(kt == 7)),
                                 reads=[bw, bsc], writes=[bmp])
                P.op("dve", lambda E: E.tensor_tensor(
                    modT[:, l, :, :], modps[:],
                    colsT[:, l, K_BMOD:K_BMOD + 48].unsqueeze(2).to_broadcast([128, 48, NSEQ]), ALU.add),
                    reads=[bmp, bcols], writes=[bmod])
                for a in (8, 32):
                    P.op("dve", lambda E: E.tensor_scalar_add(modT[:, l, a:a + 16, :], modT[:, l, a:a + 16, :], 1.0),
                         reads=[bmod], writes=[bmod])
        P.barrier()

        def modc(l, chunk, kt, s):
            return modT[:, l, chunk * 8 + kt, s:s + 1]

        def layer_norm(l, kg, kb):
            with contextlib.ExitStack() as _S:
                p_s = _S.enter_context(ps("ln_s", [128, 512]))
                p_q = _S.enter_context(ps("ln_q", [128, 512]))
                sq = _S.enter_context(sb("ln_sq", [128, 2, 512]))
                mean = _S.enter_context(sb("ln_mean", [128, 512]))
                rstd = _S.enter_context(sb("ln_rstd", [128, 512]))
                tt = _S.enter_context(sb("ln_t", [128, 2, 512]))
                bps, bpq, bsq, bmean, brstd, btt = PB(), PB(), [Buf(), Buf()], Buf(), Buf(), [Buf(), Buf()]
                for b in range(NB):
                    sl = slice(b * 512, (b + 1) * 512)
                    for kt in range(8):
                        P.op("pe", lambda E: E.matmul(p_s[:], ones, xT[:, kt, sl], start=(kt == 0), stop=(kt == 7)),
                             reads=[bx[kt], bcst], writes=[bps])
                    for kt in range(8):
                        j = kt % 2
                        P.op("act", lambda E: E.activation(sq[:, j, :], xT[:, kt, sl], AF.Square),
                             reads=[bx[kt]], writes=[bsq[j]])
                        P.op("pe", lambda E: E.matmul(p_q[:], ones, sq[:, j, :], start=(kt == 0), stop=(kt == 7)),
                             reads=[bsq[j], bcst], writes=[bpq])
                    P.op("act", lambda E: E.mul(mean[:], p_s[:], 1.0 / D), reads=[bps], writes=[bmean])
                    P.op("dve", lambda E: E.tensor_tensor(rstd[:], mean[:], mean[:], ALU.mult),
                         reads=[bmean], writes=[brstd])
                    P.op("dve", lambda E: E.scalar_tensor_tensor(rstd[:], p_q[:], 1.0 / D, rstd[:], ALU.mult,
                                                                 ALU.subtract), reads=[bpq, brstd], writes=[brstd])
                    P.op("act", lambda E: E.activation(rstd[:], rstd[:], AF.Sqrt, bias=LN_EPS, scale=1.0),
                         reads=[brstd], writes=[brstd])
                    P.op("dve", lambda E: E.reciprocal(rstd[:], rstd[:]), reads=[brstd], writes=[brstd])
                    for kt in range(8):
                        j = kt % 2
                        P.op("pool", lambda E: E.tensor_tensor(tt[:, j, :], xT[:, kt, sl], mean[:], ALU.subtract),
                             reads=[bx[kt], bmean], writes=[btt[j]])
                        P.op("dve", lambda E: E.tensor_tensor(tt[:, j, :], tt[:, j, :], rstd[:], ALU.mult),
                             reads=[btt[j], brstd], writes=[btt[j]])
                        P.op("dve", lambda E: E.tensor_scalar(xT[:, kt, sl], tt[:, j, :], col(l, kg + kt),
                                                              col(l, kb + kt), ALU.mult, ALU.add),
                             reads=[btt[j], bcols], writes=[bx[kt]])
            P.barrier()

        def modulate(l, s, sh_chunk, sc_chunk):
            for kt in range(8):
                P.op("act", lambda E: E.activation(hT[:, kt, :], xT[:, kt, :], AF.Identity,
                                                   bias=modc(l, sh_chunk, kt, s), scale=modc(l, sc_chunk, kt, s)),
                     reads=[bx[kt], bmod], writes=[bh[kt]])

        def load_w_in(wt, bw, l, c0, c1):
            P.dma("sp", wt[:], wbf["w_in"][l, :, c0:c1].rearrange("(k p) n -> p k n", p=128),
                  reads=[bwbf["w_in"][l]], writes=[bw])

        def pool_stage(l, s):
            with contextlib.ExitStack() as _S:
                pw = _S.enter_context(sb("pw", [128, 8, 256], BF16))
                pbd = _S.enter_context(sb("pbd", [128, 2, 128], BF16))
                px = _S.enter_context(sb("px", [128, 16 + T]))
                pa = _S.enter_context(sb("pa", [128, 16 + T]))
                pb = _S.enter_context(sb("pb", [128, 16 + T]))
                pdl = _S.enter_context(sb("pdl", [128, T], BF16))
                ptmp = _S.enter_context(sb("ptmp", [128, 16]))
                pps0 = _S.enter_context(ps("pps0", [128, 512]))
                pps1 = _S.enter_context(ps("pps1", [128, 512]))
                bpw, bpbd, bpx, bpa, bpb, bpdl, bptmp = Buf(), Buf(), Buf(), Buf(), Buf(), Buf(), Buf()
                pps = [pps0, pps1]; bpps = [PB(), PB()]
                load_w_in(pw, bpw, l, 1448, 1704)
                P.op("pool", lambda E: E.memset(pbd[:], 0.0), writes=[bpbd])
                for g in range(4):
                    j, r = g // 2, g % 2
                    P.dma("pool", pbd[64 * r:64 * r + 64, j, 64 * r:64 * r + 64], dr["pool_w"][l, g, :, :],
                          writes=[bpbd])
                for t in (px, pa, pb):
                    P.op("pool", lambda E: E.memset(t[:, 0:16], 0.0), writes=[bpx, bpa, bpb])
                k = 0
                for j in range(2):
                    for b in range(NB):
                        pp = pps[k % 2]; bpp = bpps[k % 2]; k += 1
                        sl = slice(b * 512, (b + 1) * 512)
                        for kt in range(8):
                            P.op("pe", lambda E: E.matmul(pp[:], pw[:, kt, j * 128:(j + 1) * 128], hT[:, kt, sl],
                                                          start=(kt == 0), stop=(kt == 7)),
                                 reads=[bpw, bh[kt]], writes=[bpp])
                        P.op("act", lambda E: E.copy(px[:, 16 + b * 512:16 + (b + 1) * 512], pp[:]),
                             reads=[bpp], writes=[bpx])

                    def shift_add(dst, src, sh, bd, bs):
                        P.op("dve", lambda E: E.tensor_tensor(dst[:, 16:16 + T], src[:, 16:16 + T],
                                                              src[:, 16 - sh:16 - sh + T], ALU.add),
                             reads=[bs], writes=[bd])

                    def delta(src, bs, r, w):
                        rows = slice(64 * r, 64 * r + 64)
                        P.op("dve", lambda E: E.scalar_tensor_tensor(pdl[rows, :], src[rows, 16:16 + T], 1.0 / w,
                                                                     px[rows, 16:16 + T], ALU.mult, ALU.subtract),
                             reads=[bs, bpx], writes=[bpdl])
                        P.op("dve", lambda E: E.tensor_tensor(ptmp[rows, :], src[rows, 16:32],
                                                              cst[rows, C_INV16 + 16 * j:C_INV16 + 16 * j + 16],
                                                              ALU.mult), reads=[bs, bcst], writes=[bptmp])
                        P.op("dve", lambda E: E.tensor_tensor(pdl[rows, 0:16], ptmp[rows, :], px[rows, 16:32],
                                                              ALU.subtract), reads=[bptmp, bpx, bpdl], writes=[bpdl])

                    shift_add(pa, px, 1, bpa, bpx)
                    if j == 0:
                        delta(pa, bpa, 0, 2)
                    shift_add(pb, pa, 2, bpb, bpa)
                    if j == 0:
                        delta(pb, bpb, 1, 4)
                    else:
                        shift_add(pa, pb, 4, bpa, bpb)
                        delta(pa, bpa, 0, 8)
                        shift_add(pb, pa, 8, bpb, bpa)
                        delta(pb, bpb, 1, 16)
                    for b in range(NB):
                        pp = pps[k % 2]; bpp = bpps[k % 2]; k += 1
                        sl = slice(b * 512, (b + 1) * 512)
                        P.op("pe", lambda E: E.matmul(pp[:], pbd[:, j, :], pdl[:, sl], start=True, stop=True),
                             reads=[bpbd, bpdl], writes=[bpp])
                        P.op("act", lambda E: E.activation(mixT[:, 6 + j, sl], pp[:], AF.Identity, scale=col(l, K_PS + j)),
                             reads=[bpp, bcols], writes=[bmix[6 + j]])
            P.barrier()

        def mla_stage(l, s):
            with contextlib.ExitStack() as _S:
                wA = _S.enter_context(sb("wA", [128, 8, 416], BF16))
                wuq = _S.enter_context(sb("wuq", [128, 2, 768], BF16))
                wuqr = _S.enter_context(sb("wuqr", [128, 2, 8, 32], BF16))
                wukv = _S.enter_context(sb("wukv", [128, 1024], BF16))
                wkr = _S.enter_context(sb("wkr", [128, 8, 32], BF16))
                cqn = _S.enter_context(sb("cqn", [128, 2, T], BF16))
                ckvn = _S.enter_context(sb("ckvn", [128, T], BF16))
                krope = _S.enter_context(sb("krope", [32, T], BF16))
                rp0 = _S.enter_context(sb("rp0", [32, 2, 512]))
                rp1 = _S.enter_context(sb("rp1", [32, 2, 512]))
                QT = _S.enter_context(sb("QT", [64, T], BF16))
                qrp = _S.enter_context(sb("qr", [32, T], BF16))
                KT = _S.enter_context(sb("KT", [64, T], BF16))
                Vh = _S.enter_context(sb("Vh", [128, NT, 64], BF16))
                csb = _S.enter_context(sb("csb", [128, 3, 512]))
                sq = _S.enter_context(sb("sq", [128, 3, 512], BF16))
                rs = _S.enter_context(sb("rs", [128, 2, 512]))
                rt = _S.enter_context(sb("rt", [32, 2, 512]))
                pt0 = _S.enter_context(sb("pt0", [128, 512], BF16))
                pt1 = _S.enter_context(sb("pt1", [128, 512], BF16))
                pt2 = _S.enter_context(sb("pt2", [128, 512], BF16))
                rden = _S.enter_context(sb("rden", [64, 512]))
                bwA, bwuq, bwuqr, bwukv, bwkr, bcqn, bckvn, bkrope, brope = [Buf() for _ in range(9)]
                bQT, bqr, bKT, bVh, bcsb, bsq, brs, brt, brden = [Buf() for _ in range(9)]
                pts = [pt0, pt1, pt2]; bpts = [Buf(), Buf(), Buf()]
                load_w_in(wA, bwA, l, 0, 416)
                P.dma("sp", wuq[:], wbf["mla_w_uq"][l, :, :].rearrange("(j p) n -> p j n", p=128),
                      reads=[bwbf["mla_w_uq"][l]], writes=[bwuq])
                P.dma("sp", wukv[:], wbf["mla_w_ukv"][l, :, :], reads=[bwbf["mla_w_ukv"][l]], writes=[bwukv])
                rps = [rp0, rp1]; brps = [Buf(), Buf()]; rpi = [0]

                def load_rope(sl):
                    i = rpi[0] % 2; rpi[0] += 1
                    P.dma("sp", rps[i][:], dr["rope"][:, :, sl], writes=[brps[i]])
                    return rps[i], brps[i]
                P.op("pool", lambda E: E.tensor_scalar_mul(wkr[:, :, 0:16], wA[:, :, 400:416], -1.0),
                     reads=[bwA], writes=[bwkr])
                P.op("pool", lambda E: E.tensor_copy(wkr[:, :, 16:32], wA[:, :, 384:400]), reads=[bwA], writes=[bwkr])
                wq4 = wuq[:].rearrange("p j (h d) -> p j h d", d=96)
                P.op("pool", lambda E: E.tensor_scalar_mul(wuqr[:, :, :, 0:16], wq4[:, :, :, 80:96], -1.0),
                     reads=[bwuq], writes=[bwuqr])
                P.op("pool", lambda E: E.tensor_copy(wuqr[:, :, :, 16:32], wq4[:, :, :, 64:80]),
                     reads=[bwuq], writes=[bwuqr])
                scale = 96.0 ** -0.5
                with contextlib.ExitStack() as _S:
                    m_a = _S.enter_context(ps("m_a", [128, 3, 512]))
                    m_s = _S.enter_context(ps("m_s", [128, 2, 512]))
                    m_r = _S.enter_context(ps("m_r", [32, 2, 512]))
                    bma, bms, bmr = PB(), PB(), PB()
                    for b in range(NB):
                        sl = slice(b * 512, (b + 1) * 512)
                        for j in range(3):
                            for kt in range(8):
                                P.op("pe", lambda E: E.matmul(m_a[:, j, :], wA[:, kt, j * 128:(j + 1) * 128],
                                                              hT[:, kt, sl], start=(kt == 0), stop=(kt == 7)),
                                     reads=[bwA, bh[kt]], writes=[bma])
                        P.op("act", lambda E: E.copy(csb[:], m_a[:]), reads=[bma], writes=[bcsb])
                        P.op("act", lambda E: E.activation(sq[:], m_a[:], AF.Square), reads=[bma], writes=[bsq])
                        P.op("pe", lambda E: E.matmul(m_s[:, 0, :], oneb[:], sq[:, 0, :], start=True, stop=False),
                             reads=[bsq, bcst], writes=[bms])
                        P.op("pe", lambda E: E.matmul(m_s[:, 0, :], oneb[:], sq[:, 1, :], start=False, stop=True),
                             reads=[bsq, bcst], writes=[bms])
                        P.op("pe", lambda E: E.matmul(m_s[:, 1, :], oneb[:], sq[:, 2, :], start=True, stop=True),
                             reads=[bsq, bcst], writes=[bms])
                        P.op("act", lambda E: E.activation(rs[:, 0, :], m_s[:, 0, :], AF.Sqrt, bias=RMS_EPS,
                                                           scale=1.0 / 256), reads=[bms], writes=[brs])
                        P.op("act", lambda E: E.activation(rs[:, 1, :], m_s[:, 1, :], AF.Sqrt, bias=RMS_EPS,
                                                           scale=1.0 / 128), reads=[bms], writes=[brs])
                        P.op("dve", lambda E: E.reciprocal(rs[:], rs[:]), reads=[brs], writes=[brs])
                        for j in range(2):
                            P.op("dve", lambda E: E.scalar_tensor_tensor(cqn[:, j, sl], csb[:, j, :],
                                                                         col(l, K_QN + j), rs[:, 0, :], ALU.mult,
                                                                         ALU.mult),
                                 reads=[bcsb, brs, bcols], writes=[bcqn])
                        P.op("dve", lambda E: E.scalar_tensor_tensor(ckvn[:, sl], csb[:, 2, :], col(l, K_KVN),
                                                                     rs[:, 1, :], ALU.mult, ALU.mult),
                             reads=[bcsb, brs, bcols], writes=[bckvn])
                        for kt in range(8):
                            P.op("pe", lambda E: E.matmul(m_r[:, 0, :], wA[:, kt, 384:416], hT[:, kt, sl],
                                                          start=(kt == 0), stop=(kt == 7)),
                                 reads=[bwA, bh[kt]], writes=[bmr])
                        for kt in range(8):
                            P.op("pe", lambda E: E.matmul(m_r[:, 1, :], wkr[:, kt, :], hT[:, kt, sl],
                                                          start=(kt == 0), stop=(kt == 7)),
                                 reads=[bwkr, bh[kt]], writes=[bmr])
                        rpt, brp = load_rope(sl)
                        P.op("dve", lambda E: E.tensor_tensor(rt[:], m_r[:], rpt[:], ALU.mult),
                             reads=[bmr, brp], writes=[brt])
                        P.op("dve", lambda E: E.tensor_tensor(krope[:, sl], rt[:, 0, :], rt[:, 1, :], ALU.add),
                             reads=[brt], writes=[bkrope])
                    P.barrier()
                with contextlib.ExitStack() as _S:
                    h_q_full = _S.enter_context(ps("h_q", [128, 512]))
                    h_q = h_q_full[0:64, :]
                    h_r = _S.enter_context(ps("h_r", [32, 2, 512]))
                    h_v = _S.enter_context(ps("h_v", [128, 4, 128]))
                    a_s0 = _S.enter_context(ps("a_s0", [128, 512]))
                    a_s1 = _S.enter_context(ps("a_s1", [128, 512]))
                    a_o = _S.enter_context(ps("a_ob", [64, 512]))
                    a_d = _S.enter_context(ps("a_db", [64, 512]))
                    h_k = h_q
                    bhq, bhr, bhv, bao, bad = [PB() for _ in range(5)]
                    bhk = bhq
                    a_s = [a_s0, a_s1, h_q_full]; bas = [PB(), PB(), bhq]
                    pi = 0
                    for h in range(8):
                        for b in range(NB):
                            sl = slice(b * 512, (b + 1) * 512)
                            for j in range(2):
                                P.op("pe", lambda E: E.matmul(h_q[:], wuq[:, j, 96 * h:96 * h + 64], cqn[:, j, sl],
                                                              start=(j == 0), stop=(j == 1)),
                                     reads=[bwuq, bcqn], writes=[bhq])
                            P.op("act", lambda E: E.copy(QT[:, sl], h_q[:]), reads=[bhq], writes=[bQT])
                            for j in range(2):
                                P.op("pe", lambda E: E.matmul(h_r[:, 0, :], wuq[:, j, 96 * h + 64:96 * h + 96],
                                                              cqn[:, j, sl], start=(j == 0), stop=(j == 1)),
                                     reads=[bwuq, bcqn], writes=[bhr])
                            for j in range(2):
                                P.op("pe", lambda E: E.matmul(h_r[:, 1, :], wuqr[:, j, h, :], cqn[:, j, sl],
                                                              start=(j == 0), stop=(j == 1)),
                                     reads=[bwuqr, bcqn], writes=[bhr])
                            rpt, brp = load_rope(sl)
                            P.op("dve", lambda E: E.tensor_tensor(rt[:], h_r[:], rpt[:], ALU.mult),
                                 reads=[bhr, brp], writes=[brt])
                            P.op("dve", lambda E: E.tensor_tensor(qrp[:, sl], rt[:, 0, :], rt[:, 1, :], ALU.add),
                                 reads=[brt], writes=[bqr])
                            P.op("pe", lambda E: E.matmul(h_k[:], wukv[:, 128 * h:128 * h + 64], ckvn[:, sl],
                                                          start=True, stop=True), reads=[bwukv, bckvn], writes=[bhk])
                            P.op("act", lambda E: E.copy(KT[:, sl], h_k[:]), reads=[bhk], writes=[bKT])
                            for tq in range(4):
                                tt_ = b * 4 + tq
                                P.op("pe", lambda E: E.matmul(h_v[:, tq, :], ckvn[:, tt_ * 128:(tt_ + 1) * 128],
                                                              wukv[:, 128 * h:128 * h + 128], start=True, stop=True),
                                     reads=[bwukv, bckvn], writes=[bhv])
                            P.op("act", lambda E: E.copy(Vh[:, b * 4:(b + 1) * 4, :], h_v[:, :, 64:128]),
                                 reads=[bhv], writes=[bVh])
                        r = h % 2
                        steps = []
                        for g in range(NB):
                            for kb in range(4 * g + 4):
                                steps.append((g, kb, kb == 0, kb == 4 * g + 3))

                        def issue_S(i):
                            g, kb, first, last = steps[i]
                            sps = a_s[i % 3]; bsp = bas[i % 3]
                            qs = slice(g * 512, (g + 1) * 512); ks = slice(kb * 128, (kb + 1) * 128)
                            P.op("pe", lambda E: E.matmul(sps, KT[:, ks], QT[:, qs], start=True, stop=False),
                                 reads=[bKT, bQT], writes=[bsp])
                            P.op("pe", lambda E: E.matmul(sps, krope[:, ks], qrp[:, qs], start=False, stop=True),
                                 reads=[bkrope, bqr], writes=[bsp])

                        def issue_rest(i):
                            g, kb, first, last = steps[i]
                            sps = a_s[i % 3]; bsp = bas[i % 3]
                            pt = pts[i % 3]; bpt = bpts[i % 3]
                            qs = slice(g * 512, (g + 1) * 512)
                            ii = max(kb - 4 * g, 0)
                            c0 = ii * 128
                            if not (dbg and dbg.get("x_b")):
                                P.op("act", lambda E: E.activation(pt[:, c0:512], sps[:, c0:512], AF.Exp, scale=scale),
                                     reads=[bsp], writes=[bpt])
                            if kb - 4 * g >= 0:
                                P.op("dve", lambda E: E.memset(pt[64:128, c0:c0 + 64], 0.0), writes=[bpt])
                            P.op("pe", lambda E: E.matmul(a_o[:, c0:512], Vh[:, kb, :], pt[:, c0:512], start=first,
                                                          stop=last), reads=[bVh, bpt], writes=[bao])
                            P.op("pe", lambda E: E.matmul(a_d[:, c0:512], oneb[:, 0:64], pt[:, c0:512], start=first,
                                                          stop=last), reads=[bpt, bcst], writes=[bad])
                            if last:
                                P.op("dve", lambda E: E.reciprocal(rden[:], a_d), reads=[bad], writes=[brden])
                                P.op("dve", lambda E: E.tensor_tensor(mixT[64 * r:64 * r + 64, h // 2, qs], a_o,
                                                                      rden[:], ALU.mult),
                                     reads=[bao, brden], writes=[bmix[h // 2]])

                        if dbg and dbg.get("x_a"):
                            steps = []
                        else:
                            issue_S(0)
                            issue_S(1)
                        for i in range(len(steps)):
                            if i + 2 < len(steps):
                                issue_S(i + 2)
                            issue_rest(i)
            P.barrier()

        def gdn_stage(l, s):
            with contextlib.ExitStack() as _S:
                gw = _S.enter_context(sb("gw", [128, 8, 256], BF16))
                gwab = _S.enter_context(sb("gwab", [128, 8, 8], BF16))
                gqT = _S.enter_context(sb("gqT", [128, 2, T], BF16))
                gkT = _S.enter_context(sb("gkT", [128, 2, T], BF16))
                gvT = _S.enter_context(sb("gvT", [128, 2, T], BF16))
                gzT = _S.enter_context(sb("gzT", [128, 2, T], BF16))
                gpre = _S.enter_context(sb("gpre", [128, 3 + T]))
                gacc = _S.enter_context(sb("gacc", [128, 512]))
                gsig = _S.enter_context(sb("gsig", [128, 512]))
                grn = _S.enter_context(sb("grn", [128, 512]))
                gpar = _S.enter_context(sb("gpar", [64, 12]))
                bgw, bgwab, bgq, bgk, bgv, bgz, bgpre, bgacc, bgsig, bgrn, bgpar = [Buf() for _ in range(11)]
                load_w_in(gwab, bgwab, l, 1440, 1448)
                P.dma("sp", gpar[:, 0:4], dr["gdn_a_log"][l:l + 1, :].partition_broadcast(64), writes=[bgpar])
                P.dma("sp", gpar[:, 4:8], dr["gdn_dt_bias"][l:l + 1, :].partition_broadcast(64), writes=[bgpar])
                P.op("act", lambda E: E.activation(gpar[:, 8:12], gpar[:, 0:4], AF.Exp), reads=[bgpar], writes=[bgpar])
                P.op("dve", lambda E: E.tensor_scalar_mul(gpar[:, 8:12], gpar[:, 8:12], -1.0), reads=[bgpar],
                     writes=[bgpar])
                P.op("pool", lambda E: E.memset(gpre[:, 0:3], 0.0), writes=[bgpre])
                bd = cst[:, C_BD:C_BD + 128]
                with contextlib.ExitStack() as _S:
                    g_p0 = _S.enter_context(ps("g_p0", [128, 512]))
                    g_p1 = _S.enter_context(ps("g_p1", [128, 512]))
                    g_ss = _S.enter_context(ps("g_ss", [128, 512]))
                    gps_ = [g_p0, g_p1]; bgps = [PB(), PB()]; bgss = PB()
                    k = 0
                    for gi, (c0, dst, bdst) in enumerate(((416, gqT, bgq), (672, gkT, bgk), (928, gvT, bgv),
                                                           (1184, gzT, bgz))):
                        load_w_in(gw, bgw, l, c0, c0 + 256)
                        for j in range(2):
                            if gi == 3:
                                for b in range(NB):
                                    sl = slice(b * 512, (b + 1) * 512)
                                    pp = gps_[k % 2]; bpp = bgps[k % 2]; k += 1
                                    for kt in range(8):
                                        P.op("pe", lambda E: E.matmul(pp[:], gw[:, kt, j * 128:(j + 1) * 128],
                                                                      hT[:, kt, sl], start=(kt == 0), stop=(kt == 7)),
                                             reads=[bgw, bh[kt]], writes=[bpp])
                                    P.op("act", lambda E: E.activation(gsig[:], pp[:], AF.Sigmoid), reads=[bpp],
                                         writes=[bgsig])
                                    P.op("dve", lambda E: E.tensor_tensor(dst[:, j, sl], pp[:], gsig[:], ALU.mult),
                                         reads=[bpp, bgsig], writes=[bdst])
                                continue
                            for b in range(NB):
                                sl = slice(b * 512, (b + 1) * 512)
                                pp = gps_[k % 2]; bpp = bgps[k % 2]; k += 1
                                for kt in range(8):
                                    P.op("pe", lambda E: E.matmul(pp[:], gw[:, kt, j * 128:(j + 1) * 128],
                                                                  hT[:, kt, sl], start=(kt == 0), stop=(kt == 7)),
                                         reads=[bgw, bh[kt]], writes=[bpp])
                                P.op("act", lambda E: E.copy(gpre[:, 3 + b * 512:3 + (b + 1) * 512], pp[:]),
                                     reads=[bpp], writes=[bgpre])
                            ct = gi * 2 + j
                            for b in range(NB):
                                sl = slice(b * 512, (b + 1) * 512)
                                P.op("dve", lambda E: E.tensor_scalar(gacc[:], gpre[:, b * 512:b * 512 + 512],
                                                                      col(l, K_CONV + ct * 4), None, ALU.mult),
                                     reads=[bgpre, bcols], writes=[bgacc])
                                for tap in range(1, 4):
                                    P.op("dve", lambda E: E.scalar_tensor_tensor(
                                        gacc[:], gpre[:, b * 512 + tap:b * 512 + tap + 512],
                                        col(l, K_CONV + ct * 4 + tap), gacc[:], ALU.mult, ALU.add),
                                        reads=[bgpre, bcols, bgacc], writes=[bgacc])
                                P.op("act", lambda E: E.activation(gsig[:], gacc[:], AF.Sigmoid), reads=[bgacc],
                                     writes=[bgsig])
                                if gi == 2:
                                    P.op("dve", lambda E: E.tensor_tensor(dst[:, j, sl], gacc[:], gsig[:], ALU.mult),
                                         reads=[bgacc, bgsig], writes=[bdst])
                                    continue
                                P.op("dve", lambda E: E.tensor_tensor(gacc[:], gacc[:], gsig[:], ALU.mult),
                                     reads=[bgacc, bgsig], writes=[bgacc])
                                P.op("act", lambda E: E.activation(gsig[:], gacc[:], AF.Square), reads=[bgacc],
                                     writes=[bgsig])
                                P.op("pe", lambda E: E.matmul(g_ss[:], bd, gsig[:], start=True, stop=True),
                                     reads=[bgsig, bcst], writes=[bgss])
                                P.op("act", lambda E: E.activation(grn[:], g_ss[:], AF.Sqrt, bias=RMS_EPS, scale=1.0),
                                     reads=[bgss], writes=[bgrn])
                                P.op("dve", lambda E: E.reciprocal(grn[:], grn[:]), reads=[bgrn], writes=[bgrn])
                                P.op("dve", lambda E: E.scalar_tensor_tensor(dst[:, j, sl], gacc[:],
                                                                             0.125 if gi == 0 else 1.0, grn[:],
                                                                             ALU.mult, ALU.mult),
                                     reads=[bgacc, bgrn], writes=[bdst])
                P.barrier()
                U = cst[0:64, C_U:C_U + 64]
                MLO = cst[0:64, C_MLO:C_MLO + 64]
                MUP = cst[0:64, C_MUP:C_MUP + 64]
                with contextlib.ExitStack() as _S:
                    sm = _S.enter_context(sb("c_sm", [64, 64]))
                    sm_b = _S.enter_context(sb("d_sm", [64, 64]))
                    Gd = _S.enter_context(sb("c_Gd", [64, 4, 64]))
                    Dd = _S.enter_context(sb("c_D", [64, 4, 64]))
                    Ee = _S.enter_context(sb("c_E", [64, 4, 64]))
                    Et = _S.enter_context(sb("c_Et", [64, 4, 64]))
                    EGB = _S.enter_context(sb("c_EGB", [128, 256]))
                    tA = _S.enter_context(sb("c_tA", [64, 4, 64]))
                    A0 = _S.enter_context(sb("c_A0", [64, 4, 64], BF16))
                    A1 = _S.enter_context(sb("c_A1", [64, 4, 64], BF16))
                    B0 = _S.enter_context(sb("c_B0", [64, 4, 64], BF16))
                    B1 = _S.enter_context(sb("c_B1", [64, 4, 64], BF16))
                    Pm = _S.enter_context(sb("c_P", [64, 4, 64], BF16))
                    Pm_b = _S.enter_context(sb("d_P", [64, 4, 64], BF16))
                    qkT = _S.enter_context(sb("c_qk", [64, 4, 64], BF16))
                    qkT_b = _S.enter_context(sb("d_qk", [64, 4, 64], BF16))
                    vtm = _S.enter_context(sb("c_vtm", [64, 4, 64]))
                    vtm_b = _S.enter_context(sb("d_vtm", [64, 4, 64]))
                    KDP = _S.enter_context(sb("c_KDP", [64, 4, 128], BF16))
                    KDP_b = _S.enter_context(sb("d_KDP", [64, 4, 128], BF16))
                    VNP = _S.enter_context(sb("c_VNP", [64, 4, 128], BF16))
                    Rb = _S.enter_context(sb("c_R", [64, 4, 64], BF16))
                    t1 = _S.enter_context(sb("c_t1", [64, 4, 64]))
                    qdT = _S.enter_context(sb("c_qd", [128, 2, 64], BF16))
                    qdT_b = _S.enter_context(sb("d_qd", [128, 2, 64], BF16))
                    SP = _S.enter_context(sb("c_SP", [128, 2, 128]))
                    SPb = _S.enter_context(sb("c_SPb", [128, 2, 128], BF16))
                    glc = _S.enter_context(sb("c_gl", [128, 2]))
                    glc_b = _S.enter_context(sb("d_gl", [128, 2]))
                    osq = _S.enter_context(sb("c_osq", [128, 2, 64]))
                    ors = _S.enter_context(sb("c_ors", [128, 2, 64]))
                    otm = _S.enter_context(sb("c_otm", [128, 2, 64]))
                    kmisc = _S.enter_context(ps("k_misc", [128, 512]))
                    kgcb = _S.enter_context(ps("k_gcb", [128, 256]))
                    kgk = _S.enter_context(ps("k_gk", [64, 2, 4, 64]))
                    kpv = _S.enter_context(ps("k_pv", [64, 2, 4, 64]))
                    ktr = _S.enter_context(ps("k_tr", [64, 3, 256], BF16))
                    kps1v = _S.enter_context(ps("k_ps1", [64, 2, 4, 64]))
                    kps1 = kps1v[:, 0, :, :]
                    ko = _S.enter_context(ps("k_o", [128, 512]))
                    ksq = kgk
                    kU = _S.enter_context(ps("k_U", [128, 2, 128]))
                    (bsm, bGd, bD, bE, bEt, bEGB, btA, bP, bqk, bvtm, bKDP, bVNP, bR, bt1, bqd, bSP, bSPb, bgl, bosq,
                     bors, botm) = [Buf() for _ in range(21)]
                    bA = [Buf(), Buf()]; bB = [Buf(), Buf()]
                    b_ab = PB(); b_gcc = b_ab; b_o = PB(); b_ss = b_o
                    b_gcb = PB(); b_G = PB(); b_KQ = b_G; b_sq = b_G; b_pu = PB()
                    b_trB = PB(); b_trv = b_trB; b_trk = b_trB; b_ps1 = PB(); b_vn = b_ps1; b_U = PB()
                    As = [A0, A1]; Bs = [B0, B1]
                    ab_ps = kmisc[0:64, 0:8]; gcc_ps = kmisc[0:64, 8:12]
                    ss_ps = ko[:, 64:192].rearrange("p (j c) -> p j c", j=2)
                    o_ps = ko[:, 256:384].rearrange("p (j c) -> p j c", j=2)
                    idb64 = idb[0:64, 0:64]
                    kz = _S.enter_context(sb("c_kz", [128, 2, 4, 64], BF16)); bkz = Buf()
                    P.op("pool", lambda E: E.memset(kz[:], 0.0), writes=[bkz])
                    sm2 = [sm, sm_b]; Pm2 = [Pm, Pm_b]; qk2 = [qkT, qkT_b]; vtm2 = [vtm, vtm_b]; KDP2 = [KDP, KDP_b]
                    qd2 = [qdT, qdT_b]; gl2 = [glc, glc_b]
                    bsm2, bP2, bqk2, bvtm2, bKDP2, bqd2, bgl2 = [[Buf(), Buf()] for _ in range(7)]
                    for i_ in range(2):
                        P.op("pool", lambda E: E.memset(KDP2[i_][:], 0.0), writes=[bKDP2[i_]])
                    P.op("pool", lambda E: E.memset(VNP[:], 0.0), writes=[bVNP])
                    P.op("pool", lambda E: E.memset(SP[:], 0.0), writes=[bSP])
                    P.op("pool", lambda E: E.memset(SPb[:], 0.0), writes=[bSPb])
                    g4, bt4, gcc, egc, ekd, tm4, sp4 = [sm[:, 4 * i:4 * i + 4] for i in range(7)]

                    def bc_h(ap4):
                        return ap4.unsqueeze(2).to_broadcast([64, 4, 64])

                    def bc_m(ap):
                        return ap.unsqueeze(1).to_broadcast([64, 4, 64])

                    opc = [0]
                    maxops = dbg.get("maxops", 10 ** 9) if dbg else 10 ** 9

                    def GOP(eng, fn, rd, wrt):
                        opc[0] += 1
                        if opc[0] <= maxops:
                            P.op(eng, fn, reads=rd, writes=wrt)

                    V = lambda fn, rd, wrt: GOP("dve", fn, rd, wrt)
                    AC = lambda fn, rd, wrt: GOP("act", fn, rd, wrt)
                    PE = lambda fn, rd, wrt: GOP("pe", fn, rd, wrt)
                    def prep(c):
                        par = c % 2
                        cs_ = slice(c * 64, (c + 1) * 64)
                        sm = sm2[par]; bsm = bsm2[par]; Pm = Pm2[par]; bP = bP2[par]; qkT = qk2[par]; bqk = bqk2[par]
                        vtm = vtm2[par]; bvtm = bvtm2[par]; KDP = KDP2[par]; bKDP = bKDP2[par]
                        qdT = qd2[par]; bqd = bqd2[par]; glc = gl2[par]; bgl = bgl2[par]
                        g4, bt4, gcc, egc, ekd, tm4, sp4 = [sm[:, 4 * i:4 * i + 4] for i in range(7)]
                        for kt in range(8):
                            yield PE(lambda E: E.matmul(ab_ps, hT[:, kt, cs_], gwab[:, kt, :], start=(kt == 0),
                                                  stop=(kt == 7)), [bh[kt], bgwab], [b_ab])
                        yield V(lambda E: E.tensor_tensor(sp4, ab_ps[:, 0:4], gpar[:, 4:8], ALU.add), [b_ab, bgpar], [bsm])
                        yield AC(lambda E: E.activation(sp4, sp4, AF.Exp), [bsm], [bsm])
                        yield AC(lambda E: E.activation(sp4, sp4, AF.Ln, bias=1.0, scale=1.0), [bsm], [bsm])
                        yield V(lambda E: E.tensor_tensor(g4, sp4, gpar[:, 8:12], ALU.mult), [bsm, bgpar], [bsm])
                        yield AC(lambda E: E.activation(bt4, ab_ps[:, 4:8], AF.Exp, scale=-1.0), [b_ab], [bsm])
                        yield V(lambda E: E.tensor_scalar_add(bt4, bt4, 1.0), [bsm], [bsm])
                        yield V(lambda E: E.reciprocal(bt4, bt4), [bsm], [bsm])
                        yield PE(lambda E: E.matmul(gcc_ps, U, g4, start=True, stop=True), [bsm, bcst], [b_gcc])
                        yield V(lambda E: E.tensor_copy(gcc, gcc_ps), [b_gcc], [bsm])
                        yield V(lambda E: E.tensor_tensor(Gd[:], bc_m(U), bc_h(g4), ALU.mult), [bsm, bcst], [bGd])
                        yield PE(lambda E: E.matmul(kgcb[:], cst[0:64, C_ONE:C_ONE + 128],
                                              Gd[:].rearrange("p h j -> p (h j)"), start=True, stop=True),
                           [bGd, bcst], [b_gcb])
                        gcb3 = kgcb[0:64, :].rearrange("p (h j) -> p h j", h=4)
                        yield V(lambda E: E.tensor_tensor(Dd[:], bc_h(gcc), gcb3, ALU.subtract), [bsm, b_gcb], [bD])
                        yield V(lambda E: E.tensor_tensor(Ee[:], Dd[:], bc_m(MLO), ALU.add), [bD, bcst], [bE])
                        yield V(lambda E: E.tensor_tensor(Et[:], bc_m(MUP), Dd[:], ALU.subtract), [bD, bcst], [bEt])
                        yield AC(lambda E: E.activation(Ee[:], Ee[:], AF.Exp), [bE], [bE])
                        yield AC(lambda E: E.activation(Et[:], Et[:], AF.Exp), [bEt], [bEt])
                        yield AC(lambda E: E.activation(EGB[:], kgcb[:], AF.Exp), [b_gcb], [bEGB])
                        yield AC(lambda E: E.activation(egc, gcc, AF.Exp), [bsm], [bsm])
                        yield V(lambda E: E.tensor_tensor(tm4, gcb3[:, :, 63], gcc, ALU.subtract), [b_gcb, bsm], [bsm])
                        yield AC(lambda E: E.activation(ekd, tm4, AF.Exp), [bsm], [bsm])
                        EG4 = EGB[:].rearrange("p (j r c) -> p j r c", j=2, r=2)
                        yield V(lambda E: E.tensor_copy(glc[0:64, :], EG4[0:64, :, 0, 63]), [bEGB], [bgl])
                        yield V(lambda E: E.tensor_copy(glc[64:128, :], EG4[64:128, :, 1, 63]), [bEGB], [bgl])
                        for h in range(4):
                            j, r = h // 2, h % 2
                            rows = slice(64 * r, 64 * r + 64)
                            yield GOP("pool", lambda E: E.tensor_copy(kz[rows, 0, h, :], gkT[rows, j, cs_]), [bgk], [bkz])
                            yield GOP("pool", lambda E: E.tensor_copy(kz[rows, 1, h, :], gqT[rows, j, cs_]), [bgq], [bkz])
                        for h in range(4):
                            j, r = h // 2, h % 2
                            yield PE(lambda E: E.matmul(kgk[:, 0, h, :], gkT[:, j, cs_], kz[:, 0, h, :], start=True,
                                                  stop=True), [bgk, bkz], [b_G])
                            yield PE(lambda E: E.matmul(kgk[:, 1, h, :], gkT[:, j, cs_], kz[:, 1, h, :], start=True,
                                                  stop=True), [bgk, bkz], [b_KQ])
                        a = 0
                        yield V(lambda E: E.tensor_tensor(tA[:], kgk[:, 0, :, :], Ee[:], ALU.mult), [b_G, bE], [btA])
                        yield V(lambda E: E.tensor_tensor(As[a][:], tA[:], bc_h(bt4), ALU.mult), [btA, bsm], [bA[a]])
                        yield V(lambda E: E.tensor_tensor(qkT[:], kgk[:, 1, :, :], Et[:], ALU.mult), [b_KQ, bEt], [bqk])
                        trB = ktr[:, 0, :].rearrange("p (h i) -> p h i", h=4)
                        for h in range(4):
                            yield PE(lambda E: E.transpose(trB[:, h, :], As[a][:, h, :], idb64), [bA[a], bcst], [b_trB])
                        yield V(lambda E: E.tensor_copy(Bs[a][:], trB), [b_trB], [bB[a]])
                        yield V(lambda E: E.tensor_tensor(Pm[:], bc_m(idb64), Bs[a][:], ALU.subtract), [bB[a], bcst], [bP])
                        for lev in range(5):
                            n = 1 - a
                            for h in range(4):
                                yield PE(lambda E: E.matmul(ksq[:, 0, h, :], Bs[a][:, h, :], As[a][:, h, :], start=True,
                                                      stop=True), [bA[a], bB[a]], [b_sq])
                                yield PE(lambda E: E.matmul(ksq[:, 1, h, :], As[a][:, h, :], Bs[a][:, h, :], start=True,
                                                      stop=True), [bA[a], bB[a]], [b_sq])
                            yield AC(lambda E: E.copy(As[n][:], ksq[:, 0, :, :]), [b_sq], [bA[n]])
                            yield V(lambda E: E.tensor_copy(Bs[n][:], ksq[:, 1, :, :]), [b_sq], [bB[n]])
                            a = n
                            for h in range(4):
                                yield PE(lambda E: E.matmul(kpv[:, 0, h, :], As[a][:, h, :], Pm[:, h, :], start=True,
                                                      stop=True), [bA[a], bP], [b_pu])
                            yield V(lambda E: E.tensor_tensor(Pm[:], Pm[:], kpv[:, 0, :, :], ALU.add), [bP, b_pu], [bP])
                        for j in range(2):
                            yield PE(lambda E: E.transpose(ktr[:, 1, j * 128:(j + 1) * 128], gvT[:, j, cs_], idb[:]),
                               [bgv, bcst], [b_trv])
                            yield PE(lambda E: E.transpose(ktr[:, 2, j * 128:(j + 1) * 128], gkT[:, j, cs_], idb[:]),
                               [bgk, bcst], [b_trk])
                        yield AC(lambda E: E.copy(vtm[:].rearrange("p h d -> p (h d)"), ktr[:, 1, :]), [b_trv], [bvtm])
                        k4 = ktr[:, 2, :].rearrange("p (j r d) -> p j r d", j=2, r=2)
                        KD5 = KDP[:].rearrange("p (j r) m -> p j r m", j=2)
                        ekd3 = ekd.rearrange("p (j r) -> p j r", j=2)
                        for r in range(2):
                            yield V(lambda E: E.tensor_tensor(KD5[:, :, r, 64 * r:64 * r + 64], k4[:, :, r, :],
                                                        ekd3[:, :, r].unsqueeze(2).to_broadcast([64, 2, 64]),
                                                        ALU.mult), [b_trk, bsm], [bKDP])
                        for h in range(4):
                            j, r = h // 2, h % 2
                            rows = slice(64 * r, 64 * r + 64)
                            yield GOP("pool", lambda E: E.tensor_tensor(qdT[rows, j, :], gqT[rows, j, cs_],
                                                                  EGB[rows, h * 64:(h + 1) * 64], ALU.mult),
                                [bgq, bEGB], [bqd])

                    def scan(c):
                        par = c % 2
                        cs_ = slice(c * 64, (c + 1) * 64)
                        sm = sm2[par]; bsm = bsm2[par]; Pm = Pm2[par]; bP = bP2[par]; qkT = qk2[par]; bqk = bqk2[par]
                        vtm = vtm2[par]; bvtm = bvtm2[par]; KDP = KDP2[par]; bKDP = bKDP2[par]
                        qdT = qd2[par]; bqd = bqd2[par]; glc = gl2[par]; bgl = bgl2[par]
                        g4, bt4, gcc, egc, ekd, tm4, sp4 = [sm[:, 4 * i:4 * i + 4] for i in range(7)]
                        for j in range(2):
                            yield PE(lambda E: E.matmul(kps1[:, 2 * j:2 * j + 2, :].rearrange("p a b -> p (a b)"),
                                                  gkT[:, j, cs_], SPb[:, j, :], start=True, stop=True),
                               [bgk, bSPb], [b_ps1])
                        yield V(lambda E: E.tensor_tensor(t1[:], kps1[:], bc_h(egc), ALU.mult), [b_ps1, bsm], [bt1])
                        yield V(lambda E: E.tensor_tensor(t1[:], vtm[:], t1[:], ALU.subtract), [bvtm, bt1], [bt1])
                        yield V(lambda E: E.tensor_tensor(Rb[:], t1[:], bc_h(bt4), ALU.mult), [bt1, bsm], [bR])
                        for h in range(4):
                            yield PE(lambda E: E.matmul(kps1v[:, 1, h, :], Pm[:, h, :], Rb[:, h, :], start=True, stop=True),
                               [bP, bR], [b_vn])
                        VN5 = VNP[:].rearrange("p (j r) m -> p j r m", j=2)
                        vn4 = kps1v[:, 1, :, :].rearrange("p (j r) d -> p j r d", j=2)
                        for r in range(2):
                            yield AC(lambda E: E.copy(VN5[:, :, r, 64 * r:64 * r + 64], vn4[:, :, r, :]), [b_vn], [bVNP])
                        for j in range(2):
                            yield PE(lambda E: E.matmul(o_ps[:, j, :], SPb[:, j, :], qdT[:, j, :], start=True, stop=False),
                               [bSPb, bqd], [b_o])
                            yield PE(lambda E: E.matmul(o_ps[:, j, :], VNP[:, 2 * j, :], qkT[:, 2 * j, :], start=False,
                                                  stop=False), [bVNP, bqk], [b_o])
                            yield PE(lambda E: E.matmul(o_ps[:, j, :], VNP[:, 2 * j + 1, :], qkT[:, 2 * j + 1, :],
                                                  start=False, stop=True), [bVNP, bqk], [b_o])
                        yield AC(lambda E: E.activation(osq[:], o_ps, AF.Square), [b_o], [bosq])
                        yield PE(lambda E: E.matmul(ss_ps.rearrange("p j c -> p (j c)"), bd,
                                              osq[:].rearrange("p j c -> p (j c)"), start=True, stop=True),
                           [bosq, bcst], [b_ss])
                        yield AC(lambda E: E.activation(ors[:], ss_ps, AF.Sqrt, bias=RMS_EPS, scale=1.0 / 64), [b_ss], [bors])
                        yield V(lambda E: E.reciprocal(ors[:], ors[:]), [bors], [bors])
                        yield V(lambda E: E.scalar_tensor_tensor(otm[:], o_ps, col(l, K_GO), ors[:], ALU.mult, ALU.mult),
                          [b_o, bors, bcols], [botm])
                        for j in range(2):
                            yield V(lambda E: E.tensor_tensor(mixT[:, 4 + j, cs_], otm[:, j, :], gzT[:, j, cs_], ALU.mult),
                              [botm, bgz], [bmix[4 + j]])
                        for j in range(2):
                            yield PE(lambda E: E.matmul(kU[:, j, :], KDP[:, 2 * j, :], VNP[:, 2 * j, :], start=True,
                                                  stop=False), [bKDP, bVNP], [b_U])
                            yield PE(lambda E: E.matmul(kU[:, j, :], KDP[:, 2 * j + 1, :], VNP[:, 2 * j + 1, :],
                                                  start=False, stop=True), [bKDP, bVNP], [b_U])
                        for j in range(2):
                            yield V(lambda E: E.scalar_tensor_tensor(SP[:, j, :], SP[:, j, :], glc[:, j:j + 1],
                                                               kU[:, j, :], ALU.mult, ALU.add),
                              [bSP, bgl, b_U], [bSP])
                        yield AC(lambda E: E.copy(SPb[:], SP[:]), [bSP], [bSPb])

                    def run_il(g1, g2, r1=3, r2=1):
                        d1 = g1 is None
                        d2 = g2 is None
                        while not (d1 and d2):
                            for _ in range(r1):
                                if not d1:
                                    try:
                                        next(g1)
                                    except StopIteration:
                                        d1 = True
                            for _ in range(r2):
                                if not d2:
                                    try:
                                        next(g2)
                                    except StopIteration:
                                        d2 = True

                    NCHX = NCH if not (dbg and 'nch' in dbg) else dbg['nch']
                    if NCHX:
                        run_il(prep(0), None)
                    for c in range(NCHX):
                        run_il(prep(c + 1) if c + 1 < NCHX else None, scan(c))
            P.barrier()

        def out_proj(l, s):
            with contextlib.ExitStack() as _S:
                wo = _S.enter_context(sb("wo", [128, 8, D], BF16))
                o0 = _S.enter_context(ps("ops0", [128, 512]))
                o1 = _S.enter_context(ps("ops1", [128, 512]))
                bwo = Buf(); ops_ = [o0, o1]; bops = [PB(), PB()]
                P.dma("sp", wo[:], wbf["w_out"][l, :, :].rearrange("(k p) n -> p k n", p=128),
                      reads=[bwbf["w_out"][l]], writes=[bwo])
                for kt in range(8):
                    P.op("act", lambda E: E.mul(xT[:, kt, :], xT[:, kt, :], ALPHA), reads=[bx[kt]], writes=[bx[kt]])
                k = 0
                for b in range(NB):
                    sl = slice(b * 512, (b + 1) * 512)
                    for m in range(8):
                        pp = ops_[k % 2]; bpp = bops[k % 2]; k += 1
                        for kt in range(8):
                            P.op("pe", lambda E: E.matmul(pp[:], wo[:, kt, m * 128:(m + 1) * 128], mixT[:, kt, sl],
                                                          start=(kt == 0), stop=(kt == 7)),
                                 reads=[bwo, bmix[kt]], writes=[bpp])
                        P.op("dve", lambda E: E.scalar_tensor_tensor(xT[:, m, sl], pp[:], modc(l, 2, m, s),
                                                                     xT[:, m, sl], ALU.mult, ALU.add),
                             reads=[bpp, bmod, bx[m]], writes=[bx[m]])
            P.barrier()

        def moe_stage(l, s):
            with contextlib.ExitStack() as _S:
                wr = _S.enter_context(sb("wr", [128, 8, 36]))
                brb = _S.enter_context(sb("brb", [128, 36]))
                h2f = _S.enter_context(sb("h2f", [128, 8, 512]))
                Lg = _S.enter_context(sb("Lg", [128, 36]))
                sm = _S.enter_context(sb("sm", [128, 16]))
                msk = _S.enter_context(sb("msk", [128, 3, 32]))
                Wt = _S.enter_context(sb("Wt", [128, 32]))
                WtT = _S.enter_context(sb("WtT", [32, T]))
                r_l = _S.enter_context(ps("r_l", [128, 36]))
                r_t = _S.enter_context(ps("r_t", [32, 128]))
                bwr, bbrb, bh2f, bLg, bsm, bmsk, bWt, bWtT = [Buf() for _ in range(8)]
                brl, brt_ = PB(), PB()
                P.dma("sp", wr[:], dr["w_r"][l, :, :].rearrange("(k p) n -> p k n", p=128), writes=[bwr])
                P.dma("sp", brb[:], dr["b_r"][l:l + 1, :].partition_broadcast(128), writes=[bbrb])
                for b in range(NB):
                    sl = slice(b * 512, (b + 1) * 512)
                    for kt in range(8):
                        P.op("act", lambda E: E.activation(h2f[:, kt, :], xT[:, kt, sl], AF.Identity,
                                                           bias=modc(l, 3, kt, s), scale=modc(l, 4, kt, s)),
                             reads=[bx[kt], bmod], writes=[bh2f])
                        P.op("pool", lambda E: E.tensor_copy(hT[:, kt, sl], h2f[:, kt, :]), reads=[bh2f],
                             writes=[bh[kt]])
                    for tq in range(4):
                        t0 = b * 512 + tq * 128
                        for kt in range(8):
                            P.op("pe", lambda E: E.matmul(r_l[:], h2f[:, kt, tq * 128:(tq + 1) * 128], wr[:, kt, :],
                                                          start=(kt == 0), stop=(kt == 7)),
                                 reads=[bh2f, bwr], writes=[brl])
                        V = lambda fn, rd, wrt: P.op("dve", fn, reads=rd, writes=wrt)
                        V(lambda E: E.tensor_tensor(Lg[:], r_l[:], brb[:], ALU.add), [brl, bbrb], [bLg])
                        V(lambda E: E.reduce_max(sm[:, 0:1], Lg[:, 0:4], AX.X), [bLg], [bsm])
                        V(lambda E: E.tensor_scalar_mul(sm[:, 1:2], sm[:, 0:1], -1.0), [bsm], [bsm])
                        V(lambda E: E.tensor_scalar(msk[:, 0, 0:4], Lg[:, 0:4], sm[:, 0:1], None, ALU.is_equal),
                          [bLg, bsm], [bmsk])
                        P.op("act", lambda E: E.activation(msk[:, 0, 8:12], Lg[:, 0:4], AF.Exp, bias=sm[:, 1:2],
                                                           scale=1.0, accum_out=sm[:, 2:3]),
                             reads=[bLg, bsm], writes=[bmsk, bsm])
                        V(lambda E: E.reciprocal(sm[:, 3:4], sm[:, 2:3]), [bsm], [bsm])
                        V(lambda E: E.tensor_scalar(msk[:, 0, 4:8], msk[:, 0, 0:4], -NEG, NEG, ALU.mult, ALU.add),
                          [bmsk], [bmsk])
                        V(lambda E: E.tensor_tensor(msk[:, 1, :].rearrange("p (g e) -> p g e", g=4),
                                                    Lg[:, 4:36].rearrange("p (g e) -> p g e", g=4),
                                                    msk[:, 0, 4:8].unsqueeze(2).to_broadcast([128, 4, 8]), ALU.add),
                          [bLg, bmsk], [bmsk])
                        V(lambda E: E.reduce_max(sm[:, 4:5], msk[:, 1, :], AX.X), [bmsk], [bsm])
                        V(lambda E: E.tensor_scalar(msk[:, 2, :], msk[:, 1, :], sm[:, 4:5], None, ALU.is_equal),
                          [bmsk, bsm], [bmsk])
                        V(lambda E: E.scalar_tensor_tensor(msk[:, 1, :], msk[:, 2, :], NEG, msk[:, 1, :], ALU.mult,
                                                           ALU.add), [bmsk], [bmsk])
                        V(lambda E: E.reduce_max(sm[:, 5:6], msk[:, 1, :], AX.X), [bmsk], [bsm])
                        V(lambda E: E.tensor_scalar(msk[:, 1, :], msk[:, 1, :], sm[:, 5:6], None, ALU.is_equal),
                          [bmsk, bsm], [bmsk])
                        V(lambda E: E.tensor_tensor(sm[:, 6:7], sm[:, 5:6], sm[:, 4:5], ALU.subtract), [bsm], [bsm])
                        P.op("act", lambda E: E.activation(sm[:, 7:8], sm[:, 6:7], AF.Exp), reads=[bsm], writes=[bsm])
                        V(lambda E: E.tensor_scalar_add(sm[:, 8:9], sm[:, 7:8], 1.0), [bsm], [bsm])
                        V(lambda E: E.reciprocal(sm[:, 8:9], sm[:, 8:9]), [bsm], [bsm])
                        V(lambda E: E.tensor_tensor(sm[:, 9:10], sm[:, 3:4], sm[:, 8:9], ALU.mult), [bsm], [bsm])
                        V(lambda E: E.tensor_tensor(sm[:, 10:11], sm[:, 9:10], sm[:, 7:8], ALU.mult), [bsm], [bsm])
                        V(lambda E: E.tensor_scalar(Wt[:], msk[:, 2, :], sm[:, 9:10], None, ALU.mult),
                          [bmsk, bsm], [bWt])
                        V(lambda E: E.scalar_tensor_tensor(Wt[:], msk[:, 1, :], sm[:, 10:11], Wt[:], ALU.mult,
                                                           ALU.add), [bmsk, bsm, bWt], [bWt])
                        P.op("pe", lambda E: E.transpose(r_t[:], Wt[:], ident), reads=[bWt, bcst], writes=[brt_])
                        P.op("act", lambda E: E.copy(WtT[:, t0:t0 + 128], r_t[:]), reads=[brt_], writes=[bWtT])
                P.dma("sp", scr[:, :], WtT[:], reads=[bWtT], writes=[bscr])
            P.barrier()
            for kt in range(8):
                P.op("act", lambda E: E.mul(xT[:, kt, :], xT[:, kt, :], ALPHA), reads=[bx[kt]], writes=[bx[kt]])
            TB = 256
            NTB = T // TB
            with contextlib.ExitStack() as _S:
                A_ = lambda nm, shp, dt=F32: _S.enter_context(sb(nm, shp, dt))
                stg_g, stg_u, stg_d = A_("stg_g", [128, 8, 256]), A_("stg_u", [128, 8, 256]), A_("stg_d", [128, 2, D])
                wg = [A_("wg0", [128, 8, 256], BF16), A_("wg1", [128, 8, 256], BF16)]
                wu = [A_("wu0", [128, 8, 256], BF16), A_("wu1", [128, 8, 256], BF16)]
                wd = [A_("wd0", [128, 2, D], BF16), A_("wd1", [128, 2, D], BF16)]
                wb = [A_("wb0", [128, T]), A_("wb1", [128, T])]
                ea = [A_("ea0", [128, 2, TB]), A_("ea1", [128, 2, TB])]
                eb = [A_("eb0", [128, 2, TB], BF16), A_("eb1", [128, 2, TB], BF16)]
                eg = [_S.enter_context(ps("eg0", [128, 2, TB])), _S.enter_context(ps("eg1", [128, 2, TB]))]
                eu = [_S.enter_context(ps("eu0", [128, 2, TB])), _S.enter_context(ps("eu1", [128, 2, TB]))]
                ey = _S.enter_context(ps("ey", [128, 8, TB]))
                bsg, bsu, bsd = Buf(), Buf(), Buf()
                bwg, bwu, bwd, bwb, bea, beb = [[Buf(), Buf()] for _ in range(6)]
                beg, beu = [[PB(), PB()] for _ in range(2)]
                beyb = [PB() for _ in range(4)]
                NE = NEXP if not (dbg and 'nexp' in dbg) else dbg['nexp']

                def dma_expert(e):
                    if e >= NE:
                        return
                    P.dma("sp", stg_g[:], dr["moe_w_gate"][l, e, :, :].rearrange("(k p) n -> p k n", p=128),
                          writes=[bsg])
                    P.dma("sp", stg_u[:], dr["moe_w_up"][l, e, :, :].rearrange("(k p) n -> p k n", p=128),
                          writes=[bsu])
                    P.dma("sp", stg_d[:], dr["moe_w_down"][l, e, :, :].rearrange("(f p) n -> p f n", p=128),
                          writes=[bsd])

                def cast_expert(e):
                    if e >= NE:
                        return
                    w = e % 2
                    P.op("act", lambda E: E.copy(wg[w][:], stg_g[:]), reads=[bsg], writes=[bwg[w]])
                    P.op("pool", lambda E: E.tensor_copy(wu[w][:], stg_u[:]), reads=[bsu], writes=[bwu[w]])
                    P.op("pool", lambda E: E.tensor_copy(wd[w][:], stg_d[:]), reads=[bsd], writes=[bwd[w]])
                    P.dma("sp", wb[w][:], scr[e:e + 1, :].partition_broadcast(128), reads=[bscr], writes=[bwb[w]])

                units = [(e, tb) for e in range(NE) for tb in range(NTB)]

                def gu(i):
                    e, tb = units[i]
                    w = e % 2; q = i % 2
                    sl = slice(tb * TB, (tb + 1) * TB)
                    for f in range(2):
                        for kt in range(8):
                            P.op("pe", lambda E: E.matmul(eg[q][:, f, :], wg[w][:, kt, f * 128:(f + 1) * 128],
                                                          hT[:, kt, sl], start=(kt == 0), stop=(kt == 7)),
                                 reads=[bwg[w], bh[kt]], writes=[beg[q]])
                    for f in range(2):
                        for kt in range(8):
                            P.op("pe", lambda E: E.matmul(eu[q][:, f, :], wu[w][:, kt, f * 128:(f + 1) * 128],
                                                          hT[:, kt, sl], start=(kt == 0), stop=(kt == 7)),
                                 reads=[bwu[w], bh[kt]], writes=[beu[q]])

                def acc(i):
                    e, tb = units[i]
                    sl = slice(tb * TB, (tb + 1) * TB)
                    for m in range(8):
                        P.op("dve", lambda E: E.scalar_tensor_tensor(xT[:, m, sl], ey[:, m, :], modc(l, 5, m, s),
                                                                     xT[:, m, sl], ALU.mult, ALU.add),
                             reads=[beyb[m // 2], bmod, bx[m]], writes=[bx[m]])

                def elem(i):
                    e, tb = units[i]
                    w = e % 2; q = i % 2
                    sl = slice(tb * TB, (tb + 1) * TB)
                    P.op("act", lambda E: E.activation(ea[q][:], eg[q][:], AF.Silu), reads=[beg[q]], writes=[bea[q]])
                    P.op("dve", lambda E: E.tensor_tensor(ea[q][:], ea[q][:], eu[q][:], ALU.mult),
                         reads=[bea[q], beu[q]], writes=[bea[q]])
                    P.op("dve", lambda E: E.tensor_tensor(eb[q][:], ea[q][:],
                                                          wb[w][:, sl].unsqueeze(1).to_broadcast([128, 2, TB]),
                                                          ALU.mult), reads=[bea[q], bwb[w]], writes=[beb[q]])

                def down(i):
                    e, tb = units[i]
                    w = e % 2; q = i % 2
                    for m in range(8):
                        for f in range(2):
                            P.op("pe", lambda E: E.matmul(ey[:, m, :], wd[w][:, f, m * 128:(m + 1) * 128],
                                                          eb[q][:, f, :], start=(f == 0), stop=(f == 1)),
                                 reads=[bwd[w], beb[q]], writes=[beyb[m // 2]])

                dma_expert(0); cast_expert(0); dma_expert(1); cast_expert(1); dma_expert(2)
                if units:
                    gu(0)
                    elem(0)
                for i in range(len(units)):
                    if i + 1 < len(units):
                        gu(i + 1)
                    down(i)
                    if i + 1 < len(units):
                        elem(i + 1)
                    acc(i)
                    e, tb = units[i]
                    if tb == NTB - 1:
                        cast_expert(e + 2)
                        dma_expert(e + 3)
            P.barrier()

        STAGES = dbg.get("stages", "mgpoLME") if dbg else "mgpoLME"
        for s in range(NSEQ):
            for kt in range(8):
                P.dma("sp", xT[:, kt, :], dr["xT"][s, kt * 128:(kt + 1) * 128, :], writes=[bx[kt]])
            for l in range(DEPTH):
                modulate(l, s, 0, 1)
                if "z" in STAGES:
                    for kt in range(8):
                        P.op("pool", lambda E: E.memset(mixT[:, kt, :], 0.0), writes=[bmix[kt]])
                if "m" in STAGES:
                    mla_stage(l, s)
                if "g" in STAGES:
                    gdn_stage(l, s)
                if "p" in STAGES:
                    pool_stage(l, s)
                if dbg and dbg.get("dump") == "mix" and s == dbg.get("s", 0) and l == dbg.get("l", 0):
                    with contextlib.ExitStack() as _S:
                        dmp = _S.enter_context(sb("dmp", [128, 8, T]))
                        bd = Buf()
                        for kt in range(8):
                            P.op("dve", lambda E: E.tensor_copy(dmp[:, kt, :], (hT if dbg.get("src") == "h" else mixT)[:, kt, :]), reads=[bmix[kt], bh[kt]],
                                 writes=[bd])
                        P.dma("sp", dbg_out.rearrange("(k p) t -> p k t", p=128), dmp[:], reads=[bd])
                        P.barrier()
                if "o" in STAGES:
                    out_proj(l, s)
                if "L" in STAGES:
                    layer_norm(l, K_LN1G, K_LN1B)
                if "M" in STAGES:
                    moe_stage(l, s)
                if "E" in STAGES:
                    layer_norm(l, K_LN2G, K_LN2B)
            for kt in range(8):
                P.dma("sp", yT[s, kt * 128:(kt + 1) * 128, :], xT[:, kt, :], reads=[bx[kt]])
        P.barrier()
    P.es.close()
    return nc


def make_consts():
    c = np.zeros((128, NCON), np.float32)
    c[:, C_ID:C_ID + 128] = np.eye(128, dtype=np.float32)
    c[:, C_ONE:C_ONE + 128] = 1.0
    i = np.arange(64)
    c[:64, C_U:C_U + 64] = (i[:, None] <= i[None, :]).astype(np.float32)
    c[:64, C_MLO:C_MLO + 64] = np.where(i[:, None] > i[None, :], 0.0, NEG)
    c[:64, C_MUP:C_MUP + 64] = np.where(i[None, :] >= i[:, None], 0.0, NEG)
    t = np.arange(16)
    for j in range(2):
        for p in range(128):
            w = 2 ** (2 * j + p // 64 + 1)
            c[p, C_INV16 + 16 * j:C_INV16 + 16 * j + 16] = 1.0 / np.minimum(t + 1, w)
    p = np.arange(128)
    c[:, C_BD:C_BD + 128] = (p[:, None] // 64 == p[None, :] // 64).astype(np.float32)
    return c


def make_rope(T):
    inv_freq = np.power(np.float32(10000.0), -np.arange(0, 32, 2, dtype=np.float32) / np.float32(32)).astype(np.float32)
    ang = (np.arange(T, dtype=np.float32)[:, None] * inv_freq[None, :]).astype(np.float32)
    cos, sin = np.cos(ang).astype(np.float32).T, np.sin(ang).astype(np.float32).T
    r = np.zeros((32, 2, T), np.float32)
    r[:16, 0], r[16:, 0], r[:16, 1], r[16:, 1] = cos, cos, sin, sin
    return r


def make_cols(inp):
    L = inp["w_in"].shape[0]
    cols = np.zeros((L, 128, NCOLS), np.float32)

    def put(k, vec, n):
        cols[:, :, k:k + n] = vec.reshape(L, n, 128).transpose(0, 2, 1)

    put(K_QN, inp["mla_q_norm"], 2); put(K_KVN, inp["mla_kv_norm"], 1)
    put(K_LN1G, inp["ln1_g"], 8); put(K_LN1B, inp["ln1_b"], 8); put(K_LN2G, inp["ln2_g"], 8); put(K_LN2B, inp["ln2_b"], 8)
    put(K_PS, inp["pool_scale"], 2)
    cols[:, :, K_GO] = np.concatenate([inp["gdn_out_norm"], inp["gdn_out_norm"]], axis=1)
    cv = inp["gdn_conv"].reshape(L, 4, 6, 128)
    cols[:, :, K_CONV:K_CONV + 24] = cv.transpose(0, 3, 2, 1).reshape(L, 128, 24)
    put(K_BMOD, inp["b_mod"], 48)
    return cols


_NC_CACHE = {}


def run(inp, T, DEPTH, NSEQ, ncores, dbg=None):
    key = (T, DEPTH, NSEQ, repr(dbg))
    if key not in _NC_CACHE:
        _NC_CACHE[key] = build(T, DEPTH, NSEQ, dbg)
    nc = _NC_CACHE[key]
    f = lambda a: np.ascontiguousarray(np.asarray(a, dtype=np.float32))
    shared = {
        "consts": make_consts(), "rope": make_rope(T), "cols": f(make_cols(inp)),
        "w_in": f(inp["w_in"]), "mla_w_uq": f(inp["mla_w_uq"]), "mla_w_ukv": f(inp["mla_w_ukv"]),
        "gdn_a_log": f(inp["gdn_a_log"]), "gdn_dt_bias": f(inp["gdn_dt_bias"]), "pool_w": f(inp["pool_w"]),
        "w_out": f(inp["w_out"]), "w_mod": f(inp["w_mod"]),
        "w_r": f(np.concatenate([inp["router_w_group"], inp["router_w_expert"]], axis=2)),
        "b_r": f(np.concatenate([inp["router_b_group"], inp["router_b_expert"]], axis=1)),
        "moe_w_gate": f(inp["moe_w_gate"]), "moe_w_up": f(inp["moe_w_up"]), "moe_w_down": f(inp["moe_w_down"]),
    }
    x = np.asarray(inp["x"], np.float32); c = np.asarray(inp["c"], np.float32)
    in_maps = []
    for i in range(ncores):
        m = dict(shared)
        m["xT"] = f(x[i * NSEQ:(i + 1) * NSEQ].transpose(0, 2, 1))
        m["cT"] = f(c[i * NSEQ:(i + 1) * NSEQ].T)
        in_maps.append(m)
    res = run_bass_kernel_spmd(nc, in_maps, core_ids=list(range(ncores)))
    out = np.concatenate([r["yT"].transpose(0, 2, 1) for r in res.results], axis=0)
    return out, res


def kernel(**inputs):
    out, _ = run(inputs, 2048, 4, 2, 8)
    return out.astype(np.float32)
```

```python
import contextlib
import numpy as np
import concourse.bass as bass
import concourse.mybir as mybir
from concourse.bass_utils import run_bass_kernel_spmd

F32 = mybir.dt.float32
BF16 = mybir.dt.bfloat16
AF = mybir.ActivationFunctionType
ALU = mybir.AluOpType
AX = mybir.AxisListType

SEG = 28000
ENGS = ("pe", "dve", "act", "pool", "sp")
NDMA = 8

D = 1024
NEXP = 32
ALPHA = 8.0 ** 0.25
LN_EPS = 1e-5
RMS_EPS = 1e-6
NEG = -30000.0

C_ID, C_ONE, C_U, C_MLO, C_MUP, C_INV16, C_BD, NCON = 0, 128, 256, 320, 384, 448, 480, 608
K_QN, K_KVN, K_LN1G, K_LN1B, K_LN2G, K_LN2B, K_PS, K_GO, K_CONV, K_BMOD, NCOLS = 0, 2, 3, 11, 19, 27, 35, 37, 38, 62, 110


class Buf:
    __slots__ = ("name", "lw", "rd", "excl")

    def __init__(self, name="", excl=False):
        self.name = name
        self.lw = None
        self.rd = {}
        self.excl = excl


def PB():
    return Buf("psum", True)


class Prog:
    def __init__(self, nc, same_engine_sync=True):
        self.nc = nc
        self.es = contextlib.ExitStack()
        self.cnt = {e: 0 for e in ENGS}
        self.seen = {e: {} for e in ENGS}
        self.sems = {}
        self.dma_i = {e: 0 for e in ENGS}
        self.same = same_engine_sync
        self.E = {"pe": nc.tensor, "dve": nc.vector, "act": nc.scalar, "pool": nc.gpsimd, "sp": nc.sync}
        self.last_dma = {}

    def sem(self, key):
        if key not in self.sems:
            nm = "s_" + "_".join(str(k) for k in key)
            self.sems[key] = self.es.enter_context(self.nc.semaphore(nm))
        return self.sems[key]

    def _wait(self, eng, tok):
        key, val = tok
        if self.seen[eng].get(key, 0) >= val:
            return
        if key[0] == eng and (eng == "pe" or not self.same):
            return
        self.seen[eng][key] = val
        self.E[eng].wait_ge(self.sem(key), val)

    def _deps(self, eng, reads, writes):
        for b in reads:
            if b.lw is not None:
                self._wait(eng, b.lw)
        for b in writes:
            if b.excl:
                if b.lw is not None and b.lw[0][0] != eng:
                    self._wait(eng, b.lw)
                for k, v in b.rd.items():
                    if k[0] != eng:
                        self._wait(eng, (k, v))
                continue
            if b.lw is not None:
                self._wait(eng, b.lw)
            for k, v in b.rd.items():
                if k[0] != eng:
                    self._wait(eng, (k, v))

    def _mark(self, tok, reads, writes):
        k, v = tok
        for b in reads:
            if b.rd.get(k, 0) < v:
                b.rd[k] = v
        for b in writes:
            if b.excl:
                if b.lw is not None and b.lw[0][0] != k[0]:
                    pk, pv = b.lw
                    if b.rd.get(pk, 0) < pv:
                        b.rd[pk] = pv
                b.rd = {kk: vv for kk, vv in b.rd.items() if kk[0] != k[0]}
                b.lw = tok
                continue
            b.lw = tok
            b.rd = {}

    def op(self, eng, fn, reads=(), writes=()):
        if any(b.excl for b in reads):
            writes = list(writes) + [b for b in reads if b.excl]
            reads = [b for b in reads if not b.excl]
        self._deps(eng, reads, writes)
        n = self.cnt[eng]
        key = (eng, n // SEG)
        val = n % SEG + 1
        fn(self.E[eng]).then_inc(self.sem(key), 1)
        self.cnt[eng] = n + 1
        self._mark((key, val), reads, writes)

    def dma(self, eng, out, in_, reads=(), writes=(), **kw):
        i = self.dma_i[eng]
        self.dma_i[eng] = i + 1
        key = ("d" + eng, i % NDMA)
        val = (i // NDMA + 1) * 16
        if val > 16:
            self._wait(eng, (key, val - 16))
        self._deps(eng, reads, writes)
        self.E[eng].dma_start(out=out, in_=in_, **kw).then_inc(self.sem(key), 16)
        self.last_dma[key] = val
        self._mark((key, val), reads, writes)

    def barrier(self):
        toks = []
        for e in ENGS:
            n = self.cnt[e]
            if n:
                toks.append(((e, (n - 1) // SEG), (n - 1) % SEG + 1))
        toks += list(self.last_dma.items())
        for e in ENGS:
            for t in toks:
                self._wait(e, t)


def build(T, DEPTH, NSEQ, dbg=None):
    nc = bass.Bass("TRN2", target_bir_lowering=False)
    P = Prog(nc)
    NB = T // 512
    NT = T // 128
    NCH = T // 64
    dr = {}

    def din(name, shape):
        dr[name] = nc.dram_tensor(name, list(shape), F32, kind="ExternalInput").ap()

    din("xT", [NSEQ, D, T]); din("cT", [D, NSEQ]); din("consts", [128, NCON]); din("rope", [32, 2, T])
    din("cols", [4, 128, NCOLS]); din("w_in", [4, D, 1704]); din("mla_w_uq", [4, 256, 768])
    din("mla_w_ukv", [4, 128, 1024]); din("gdn_a_log", [4, 4]); din("gdn_dt_bias", [4, 4])
    din("pool_w", [4, 4, 64, 64]); din("w_out", [4, D, D]); din("w_mod", [4, D, 6 * D])
    din("w_r", [4, D, 36]); din("b_r", [4, 36])
    din("moe_w_gate", [4, NEXP, D, 256]); din("moe_w_up", [4, NEXP, D, 256]); din("moe_w_down", [4, NEXP, 256, D])
    yT = nc.dram_tensor("yT", [NSEQ, D, T], F32, kind="ExternalOutput").ap()
    scr = nc.dram_tensor("scr_wt", [NEXP, T], F32).ap()
    bscr = Buf("scr")
    wbf = {"w_in": nc.dram_tensor("bf_w_in", [4, D, 1704], BF16).ap(),
           "w_out": nc.dram_tensor("bf_w_out", [4, D, D], BF16).ap(),
           "mla_w_uq": nc.dram_tensor("bf_w_uq", [4, 256, 768], BF16).ap(),
           "mla_w_ukv": nc.dram_tensor("bf_w_ukv", [4, 128, 1024], BF16).ap()}
    bwbf = {k: [Buf() for _ in range(4)] for k in wbf}
    if dbg:
        dbg_out = nc.dram_tensor("dbg", [D, T], F32, kind="ExternalOutput").ap()

    uid = [0]

    def sb(name, shape, dt=F32):
        uid[0] += 1
        return nc.sbuf_tensor("%s_%d" % (name, uid[0]), list(shape), dt)

    @contextlib.contextmanager
    def ps(name, shape, dt=F32):
        uid[0] += 1
        isz = 4 if dt == F32 else 2
        per = 2048 // isz
        n = 1
        for d_ in shape[1:]:
            n *= d_
        nb = (n + per - 1) // per
        with nc.psum_tensor("%s_%d" % (name, uid[0]), [128, nb * per], dt) as t:
            v = t[0:shape[0], 0:n]
            if len(shape) == 3:
                v = v.rearrange("p (a b) -> p a b", a=shape[1])
            elif len(shape) == 4:
                v = v.rearrange("p (a b c) -> p a b c", a=shape[1], b=shape[2])
            yield v

    es = contextlib.ExitStack()
    with es:
        cst = es.enter_context(sb("cst", [128, NCON])); bcst = Buf()
        idb = es.enter_context(sb("idb", [128, 128], BF16))
        oneb = es.enter_context(sb("oneb", [128, 128], BF16))
        colsT = es.enter_context(sb("colsT", [128, DEPTH, NCOLS])); bcols = Buf()
        modT = es.enter_context(sb("modT", [128, DEPTH, 48, NSEQ])); bmod = Buf()
        xT = es.enter_context(sb("xT_sb", [128, 8, T])); bx = [Buf() for _ in range(8)]
        hT = es.enter_context(sb("hT_sb", [128, 8, T], BF16)); bh = [Buf() for _ in range(8)]
        mixT = es.enter_context(sb("mixT_sb", [128, 8, T], BF16)); bmix = [Buf() for _ in range(8)]

        P.dma("sp", cst[:], dr["consts"][:, :], writes=[bcst])
        for l in range(DEPTH):
            P.dma("sp", colsT[:, l, :], dr["cols"][l, :, :], writes=[bcols])
        P.op("dve", lambda E: E.tensor_copy(idb[:], cst[:, C_ID:C_ID + 128]), reads=[bcst], writes=[bcst])
        P.op("dve", lambda E: E.tensor_copy(oneb[:], cst[:, C_ONE:C_ONE + 128]), reads=[bcst], writes=[bcst])
        for l in range(DEPTH):
            for k_ in ("w_in", "mla_w_uq", "mla_w_ukv", "w_out"):
                rows = dr[k_].shape[1]
                for r0 in range(0, rows, 512):
                    r1 = min(rows, r0 + 512)
                    P.dma("pool", wbf[k_][l, r0:r1, :], dr[k_][l, r0:r1, :], writes=[bwbf[k_][l]])
        ident = cst[:, C_ID:C_ID + 128]
        ones = cst[:, C_ONE:C_ONE + 128]

        def col(l, k):
            return colsT[:, l, k:k + 1]

        with contextlib.ExitStack() as _S:
            scT = _S.enter_context(sb("scT", [128, 8, NSEQ]))
            sgT = _S.enter_context(sb("sgT", [128, 8, NSEQ]))
            wm0 = _S.enter_context(sb("wm0", [128, 8, 768]))
            wm1 = _S.enter_context(sb("wm1", [128, 8, 768]))
            modps = _S.enter_context(ps("modps", [128, 48, NSEQ]))
            bsc = Buf(); bwm = [Buf(), Buf()]; bmp = PB()
            wms = [wm0, wm1]
            P.dma("sp", scT[:], dr["cT"].rearrange("(k p) s -> p k s", p=128), writes=[bsc])
            P.op("act", lambda E: E.activation(sgT[:], scT[:], AF.Sigmoid), reads=[bsc], writes=[bsc])
            P.op("dve", lambda E: E.tensor_tensor(scT[:], scT[:], sgT[:], ALU.mult), reads=[bsc], writes=[bsc])
            ci = 0
            for l in range(DEPTH):
                for c in range(8):
                    wm = wms[ci % 2]; bw = bwm[ci % 2]; ci += 1
                    P.dma("sp", wm[:], dr["w_mod"][l, :, c * 768:(c + 1) * 768].rearrange("(k p) n -> p k n", p=128),
                          writes=[bw])
                    for mm in range(6):
                        m = c * 6 + mm
                        for kt in range(8):
                            P.op("pe", lambda E: E.matmul(modps[:, m, :], wm[:, kt, mm * 128:(mm + 1) * 128],
                                                          scT[:, kt, :], start=(kt == 0), stop=(kt == 7)),
                                 reads=[bw, bsc], writes=[bmp])
                P.op("dve", lambda E: E.tensor_tensor(
                    modT[:, l, :, :], modps[:],
                    colsT[:, l, K_BMOD:K_BMOD + 48].unsqueeze(2).to_broadcast([128, 48, NSEQ]), ALU.add),
                    reads=[bmp, bcols], writes=[bmod])
                for a in (8, 32):
                    P.op("dve", lambda E: E.tensor_scalar_add(modT[:, l, a:a + 16, :], modT[:, l, a:a + 16, :], 1.0),
                         reads=[bmod], writes=[bmod])
        P.barrier()

        def modc(l, chunk, kt, s):
            return modT[:, l, chunk * 8 + kt, s:s + 1]

        def layer_norm(l, kg, kb):
            with contextlib.ExitStack() as _S:
                p_s = _S.enter_context(ps("ln_s", [128, 512]))
                p_q = _S.enter_context(ps("ln_q", [128, 512]))
                sq = _S.enter_context(sb("ln_sq", [128, 2, 512]))
                mean = _S.enter_context(sb("ln_mean", [128, 512]))
                rstd = _S.enter_context(sb("ln_rstd", [128, 512]))
                tt = _S.enter_context(sb("ln_t", [128, 2, 512]))
                bps, bpq, bsq, bmean, brstd, btt = PB(), PB(), [Buf(), Buf()], Buf(), Buf(), [Buf(), Buf()]
                for b in range(NB):
                    sl = slice(b * 512, (b + 1) * 512)
                    for kt in range(8):
                        P.op("pe", lambda E: E.matmul(p_s[:], ones, xT[:, kt, sl], start=(kt == 0), stop=(kt == 7)),
                             reads=[bx[kt], bcst], writes=[bps])
                    for kt in range(8):
                        j = kt % 2
                        P.op("act", lambda E: E.activation(sq[:, j, :], xT[:, kt, sl], AF.Square),
                             reads=[bx[kt]], writes=[bsq[j]])
                        P.op("pe", lambda E: E.matmul(p_q[:], ones, sq[:, j, :], start=(kt == 0), stop=(kt == 7)),
                             reads=[bsq[j], bcst], writes=[bpq])
                    P.op("act", lambda E: E.mul(mean[:], p_s[:], 1.0 / D), reads=[bps], writes=[bmean])
                    P.op("dve", lambda E: E.tensor_tensor(rstd[:], mean[:], mean[:], ALU.mult),
                         reads=[bmean], writes=[brstd])
                    P.op("dve", lambda E: E.scalar_tensor_tensor(rstd[:], p_q[:], 1.0 / D, rstd[:], ALU.mult,
                                                                 ALU.subtract), reads=[bpq, brstd], writes=[brstd])
                    P.op("act", lambda E: E.activation(rstd[:], rstd[:], AF.Sqrt, bias=LN_EPS, scale=1.0),
                         reads=[brstd], writes=[brstd])
                    P.op("dve", lambda E: E.reciprocal(rstd[:], rstd[:]), reads=[brstd], writes=[brstd])
                    for kt in range(8):
                        j = kt % 2
                        P.op("pool", lambda E: E.tensor_tensor(tt[:, j, :], xT[:, kt, sl], mean[:], ALU.subtract),
                             reads=[bx[kt], bmean], writes=[btt[j]])
                        P.op("dve", lambda E: E.tensor_tensor(tt[:, j, :], tt[:, j, :], rstd[:], ALU.mult),
                             reads=[btt[j], brstd], writes=[btt[j]])
                        P.op("dve", lambda E: E.tensor_scalar(xT[:, kt, sl], tt[:, j, :], col(l, kg + kt),
                                                              col(l, kb + kt), ALU.mult, ALU.add),
                             reads=[btt[j], bcols], writes=[bx[kt]])
            P.barrier()

        def modulate(l, s, sh_chunk, sc_chunk):
            for kt in range(8):
                P.op("act", lambda E: E.activation(hT[:, kt, :], xT[:, kt, :], AF.Identity,
                                                   bias=modc(l, sh_chunk, kt, s), scale=modc(l, sc_chunk, kt, s)),
                     reads=[bx[kt], bmod], writes=[bh[kt]])

        def load_w_in(wt, bw, l, c0, c1):
            P.dma("sp", wt[:], wbf["w_in"][l, :, c0:c1].rearrange("(k p) n -> p k n", p=128),
                  reads=[bwbf["w_in"][l]], writes=[bw])

        def pool_stage(l, s):
            with contextlib.ExitStack() as _S:
                pw = _S.enter_context(sb("pw", [128, 8, 256], BF16))
                pbd = _S.enter_context(sb("pbd", [128, 2, 128], BF16))
                px = _S.enter_context(sb("px", [128, 16 + T]))
                pa = _S.enter_context(sb("pa", [128, 16 + T]))
                pb = _S.enter_context(sb("pb", [128, 16 + T]))
                pdl = _S.enter_context(sb("pdl", [128, T], BF16))
                ptmp = _S.enter_context(sb("ptmp", [128, 16]))
                pps0 = _S.enter_context(ps("pps0", [128, 512]))
                pps1 = _S.enter_context(ps("pps1", [128, 512]))
                bpw, bpbd, bpx, bpa, bpb, bpdl, bptmp = Buf(), Buf(), Buf(), Buf(), Buf(), Buf(), Buf()
                pps = [pps0, pps1]; bpps = [PB(), PB()]
                load_w_in(pw, bpw, l, 1448, 1704)
                P.op("pool", lambda E: E.memset(pbd[:], 0.0), writes=[bpbd])
                for g in range(4):
                    j, r = g // 2, g % 2
                    P.dma("pool", pbd[64 * r:64 * r + 64, j, 64 * r:64 * r + 64], dr["pool_w"][l, g, :, :],
                          writes=[bpbd])
                for t in (px, pa, pb):
                    P.op("pool", lambda E: E.memset(t[:, 0:16], 0.0), writes=[bpx, bpa, bpb])
                k = 0
                for j in range(2):
                    for b in range(NB):
                        pp = pps[k % 2]; bpp = bpps[k % 2]; k += 1
                        sl = slice(b * 512, (b + 1) * 512)
                        for kt in range(8):
                            P.op("pe", lambda E: E.matmul(pp[:], pw[:, kt, j * 128:(j + 1) * 128], hT[:, kt, sl],
                                                          start=(kt == 0), stop=(kt == 7)),
                                 reads=[bpw, bh[kt]], writes=[bpp])
                        P.op("act", lambda E: E.copy(px[:, 16 + b * 512:16 + (b + 1) * 512], pp[:]),
                             reads=[bpp], writes=[bpx])

                    def shift_add(dst, src, sh, bd, bs):
                        P.op("dve", lambda E: E.tensor_tensor(dst[:, 16:16 + T], src[:, 16:16 + T],
                                                              src[:, 16 - sh:16 - sh + T], ALU.add),
                             reads=[bs], writes=[bd])

                    def delta(src, bs, r, w):
                        rows = slice(64 * r, 64 * r + 64)
                        P.op("dve", lambda E: E.scalar_tensor_tensor(pdl[rows, :], src[rows, 16:16 + T], 1.0 / w,
                                                                     px[rows, 16:16 + T], ALU.mult, ALU.subtract),
                             reads=[bs, bpx], writes=[bpdl])
                        P.op("dve", lambda E: E.tensor_tensor(ptmp[rows, :], src[rows, 16:32],
                                                              cst[rows, C_INV16 + 16 * j:C_INV16 + 16 * j + 16],
                                                              ALU.mult), reads=[bs, bcst], writes=[bptmp])
                        P.op("dve", lambda E: E.tensor_tensor(pdl[rows, 0:16], ptmp[rows, :], px[rows, 16:32],
                                                              ALU.subtract), reads=[bptmp, bpx, bpdl], writes=[bpdl])

                    shift_add(pa, px, 1, bpa, bpx)
                    if j == 0:
                        delta(pa, bpa, 0, 2)
                    shift_add(pb, pa, 2, bpb, bpa)
                    if j == 0:
                        delta(pb, bpb, 1, 4)
                    else:
                        shift_add(pa, pb, 4, bpa, bpb)
                        delta(pa, bpa, 0, 8)
                        shift_add(pb, pa, 8, bpb, bpa)
                        delta(pb, bpb, 1, 16)
                    for b in range(NB):
                        pp = pps[k % 2]; bpp = bpps[k % 2]; k += 1
                        sl = slice(b * 512, (b + 1) * 512)
                        P.op("pe", lambda E: E.matmul(pp[:], pbd[:, j, :], pdl[:, sl], start=True, stop=True),
                             reads=[bpbd, bpdl], writes=[bpp])
                        P.op("act", lambda E: E.activation(mixT[:, 6 + j, sl], pp[:], AF.Identity, scale=col(l, K_PS + j)),
                             reads=[bpp, bcols], writes=[bmix[6 + j]])
            P.barrier()

        def mla_stage(l, s):
            with contextlib.ExitStack() as _S:
                wA = _S.enter_context(sb("wA", [128, 8, 416], BF16))
                wuq = _S.enter_context(sb("wuq", [128, 2, 768], BF16))
                wuqr = _S.enter_context(sb("wuqr", [128, 2, 8, 32], BF16))
                wukv = _S.enter_context(sb("wukv", [128, 1024], BF16))
                wkr = _S.enter_context(sb("wkr", [128, 8, 32], BF16))
                cqn = _S.enter_context(sb("cqn", [128, 2, T], BF16))
                ckvn = _S.enter_context(sb("ckvn", [128, T], BF16))
                krope = _S.enter_context(sb("krope", [32, T], BF16))
                rp0 = _S.enter_context(sb("rp0", [32, 2, 512]))
                rp1 = _S.enter_context(sb("rp1", [32, 2, 512]))
                QT = _S.enter_context(sb("QT", [96, T], BF16))
                qrp = _S.enter_context(sb("qr", [32, 512], BF16))
                KT = _S.enter_context(sb("KT", [96, T], BF16))
                Vh = _S.enter_context(sb("Vh", [128, NT, 128], BF16))
                csb = _S.enter_context(sb("csb", [128, 3, 512]))
                sq = _S.enter_context(sb("sq", [128, 3, 512], BF16))
                rs = _S.enter_context(sb("rs", [128, 2, 512]))
                rt = _S.enter_context(sb("rt", [32, 2, 512]))
                pt0 = _S.enter_context(sb("pt0", [128, 512], BF16))
                pt1 = _S.enter_context(sb("pt1", [128, 512], BF16))
                pt2 = _S.enter_context(sb("pt2", [128, 512], BF16))
                rden = _S.enter_context(sb("rden", [64, 512]))
                bwA, bwuq, bwuqr, bwukv, bwkr, bcqn, bckvn, bkrope, brope = [Buf() for _ in range(9)]
                bQT, bqr, bKT, bVh, bcsb, bsq, brs, brt, brden = [Buf() for _ in range(9)]
                pts = [pt0, pt1, pt2]; bpts = [Buf(), Buf(), Buf()]
                load_w_in(wA, bwA, l, 0, 416)
                P.dma("sp", wuq[:], wbf["mla_w_uq"][l, :, :].rearrange("(j p) n -> p j n", p=128),
                      reads=[bwbf["mla_w_uq"][l]], writes=[bwuq])
                P.dma("sp", wukv[:], wbf["mla_w_ukv"][l, :, :], reads=[bwbf["mla_w_ukv"][l]], writes=[bwukv])
                rps = [rp0, rp1]; brps = [Buf(), Buf()]; rpi = [0]

                def load_rope(sl):
                    i = rpi[0] % 2; rpi[0] += 1
                    P.dma("sp", rps[i][:], dr["rope"][:, :, sl], writes=[brps[i]])
                    return rps[i], brps[i]
                P.op("pool", lambda E: E.tensor_scalar_mul(wkr[:, :, 0:16], wA[:, :, 400:416], -1.0),
                     reads=[bwA], writes=[bwkr])
                P.op("pool", lambda E: E.tensor_copy(wkr[:, :, 16:32], wA[:, :, 384:400]), reads=[bwA], writes=[bwkr])
                wq4 = wuq[:].rearrange("p j (h d) -> p j h d", d=96)
                P.op("pool", lambda E: E.tensor_scalar_mul(wuqr[:, :, :, 0:16], wq4[:, :, :, 80:96], -1.0),
                     reads=[bwuq], writes=[bwuqr])
                P.op("pool", lambda E: E.tensor_copy(wuqr[:, :, :, 16:32], wq4[:, :, :, 64:80]),
                     reads=[bwuq], writes=[bwuqr])
                P.op("pool", lambda E: E.memset(Vh[:, :, 64:128], 1.0), writes=[bVh])
                scale = 96.0 ** -0.5
                with contextlib.ExitStack() as _S:
                    m_a = _S.enter_context(ps("m_a", [128, 3, 512]))
                    m_s = _S.enter_context(ps("m_s", [128, 2, 512]))
                    m_r = _S.enter_context(ps("m_r", [32, 2, 512]))
                    bma, bms, bmr = PB(), PB(), PB()
                    for b in range(NB):
                        sl = slice(b * 512, (b + 1) * 512)
                        for j in range(3):
                            for kt in range(8):
                                P.op("pe", lambda E: E.matmul(m_a[:, j, :], wA[:, kt, j * 128:(j + 1) * 128],
                                                              hT[:, kt, sl], start=(kt == 0), stop=(kt == 7)),
                                     reads=[bwA, bh[kt]], writes=[bma])
                        P.op("act", lambda E: E.copy(csb[:], m_a[:]), reads=[bma], writes=[bcsb])
                        P.op("act", lambda E: E.activation(sq[:], m_a[:], AF.Square), reads=[bma], writes=[bsq])
                        P.op("pe", lambda E: E.matmul(m_s[:, 0, :], oneb[:], sq[:, 0, :], start=True, stop=False),
                             reads=[bsq, bcst], writes=[bms])
                        P.op("pe", lambda E: E.matmul(m_s[:, 0, :], oneb[:], sq[:, 1, :], start=False, stop=True),
                             reads=[bsq, bcst], writes=[bms])
                        P.op("pe", lambda E: E.matmul(m_s[:, 1, :], oneb[:], sq[:, 2, :], start=True, stop=True),
                             reads=[bsq, bcst], writes=[bms])
                        P.op("act", lambda E: E.activation(rs[:, 0, :], m_s[:, 0, :], AF.Sqrt, bias=RMS_EPS,
                                                           scale=1.0 / 256), reads=[bms], writes=[brs])
                        P.op("act", lambda E: E.activation(rs[:, 1, :], m_s[:, 1, :], AF.Sqrt, bias=RMS_EPS,
                                                           scale=1.0 / 128), reads=[bms], writes=[brs])
                        P.op("dve", lambda E: E.reciprocal(rs[:], rs[:]), reads=[brs], writes=[brs])
                        for j in range(2):
                            P.op("dve", lambda E: E.scalar_tensor_tensor(cqn[:, j, sl], csb[:, j, :],
                                                                         col(l, K_QN + j), rs[:, 0, :], ALU.mult,
                                                                         ALU.mult),
                                 reads=[bcsb, brs, bcols], writes=[bcqn])
                        P.op("dve", lambda E: E.scalar_tensor_tensor(ckvn[:, sl], csb[:, 2, :], col(l, K_KVN),
                                                                     rs[:, 1, :], ALU.mult, ALU.mult),
                             reads=[bcsb, brs, bcols], writes=[bckvn])
                        for kt in range(8):
                            P.op("pe", lambda E: E.matmul(m_r[:, 0, :], wA[:, kt, 384:416], hT[:, kt, sl],
                                                          start=(kt == 0), stop=(kt == 7)),
                                 reads=[bwA, bh[kt]], writes=[bmr])
                        for kt in range(8):
                            P.op("pe", lambda E: E.matmul(m_r[:, 1, :], wkr[:, kt, :], hT[:, kt, sl],
                                                          start=(kt == 0), stop=(kt == 7)),
                                 reads=[bwkr, bh[kt]], writes=[bmr])
                        rpt, brp = load_rope(sl)
                        P.op("dve", lambda E: E.tensor_tensor(rt[:], m_r[:], rpt[:], ALU.mult),
                             reads=[bmr, brp], writes=[brt])
                        P.op("dve", lambda E: E.tensor_tensor(krope[:, sl], rt[:, 0, :], rt[:, 1, :], ALU.add),
                             reads=[brt], writes=[bkrope])
                        P.op("act", lambda E: E.copy(KT[64:96, sl], krope[:, sl]), reads=[bkrope], writes=[bKT])
                    P.barrier()
                with contextlib.ExitStack() as _S:
                    h_q_full = _S.enter_context(ps("h_q", [128, 512]))
                    h_q = h_q_full[0:64, :]
                    h_r = _S.enter_context(ps("h_r", [32, 2, 512]))
                    h_v = _S.enter_context(ps("h_v", [128, 4, 128]))
                    a_s0 = _S.enter_context(ps("a_s0", [128, 512]))
                    a_s1 = _S.enter_context(ps("a_s1", [128, 512]))
                    a_of = _S.enter_context(ps("a_ob", [128, 512]))
                    a_o = a_of[0:64, :]
                    h_k = h_q
                    bhq, bhr, bhv, bao, bad = [PB() for _ in range(5)]
                    bhk = bhq
                    a_s = [a_s0, a_s1, h_q_full]; bas = [PB(), PB(), bhq]
                    pi = 0
                    for h in range(8):
                        for b in range(NB):
                            sl = slice(b * 512, (b + 1) * 512)
                            for j in range(2):
                                P.op("pe", lambda E: E.matmul(h_q[:], wuq[:, j, 96 * h:96 * h + 64], cqn[:, j, sl],
                                                              start=(j == 0), stop=(j == 1)),
                                     reads=[bwuq, bcqn], writes=[bhq])
                            P.op("act", lambda E: E.copy(QT[0:64, sl], h_q[:]), reads=[bhq], writes=[bQT])
                            for j in range(2):
                                P.op("pe", lambda E: E.matmul(h_r[:, 0, :], wuq[:, j, 96 * h + 64:96 * h + 96],
                                                              cqn[:, j, sl], start=(j == 0), stop=(j == 1)),
                                     reads=[bwuq, bcqn], writes=[bhr])
                            for j in range(2):
                                P.op("pe", lambda E: E.matmul(h_r[:, 1, :], wuqr[:, j, h, :], cqn[:, j, sl],
                                                              start=(j == 0), stop=(j == 1)),
                                     reads=[bwuqr, bcqn], writes=[bhr])
                            rpt, brp = load_rope(sl)
                            P.op("dve", lambda E: E.tensor_tensor(rt[:], h_r[:], rpt[:], ALU.mult),
                                 reads=[bhr, brp], writes=[brt])
                            P.op("dve", lambda E: E.tensor_tensor(qrp[:], rt[:, 0, :], rt[:, 1, :], ALU.add),
                                 reads=[brt], writes=[bqr])
                            P.op("act", lambda E: E.copy(QT[64:96, sl], qrp[:]), reads=[bqr], writes=[bQT])
                            P.op("pe", lambda E: E.matmul(h_k[:], wukv[:, 128 * h:128 * h + 64], ckvn[:, sl],
                                                          start=True, stop=True), reads=[bwukv, bckvn], writes=[bhk])
                            P.op("act", lambda E: E.copy(KT[0:64, sl], h_k[:]), reads=[bhk], writes=[bKT])
                            for tq in range(4):
                                tt_ = b * 4 + tq
                                P.op("pe", lambda E: E.matmul(h_v[:, tq, :], ckvn[:, tt_ * 128:(tt_ + 1) * 128],
                                                              wukv[:, 128 * h:128 * h + 128], start=True, stop=True),
                                     reads=[bwukv, bckvn], writes=[bhv])
                            P.op("act", lambda E: E.copy(Vh[:, b * 4:(b + 1) * 4, 0:64], h_v[:, :, 64:128]),
                                 reads=[bhv], writes=[bVh])
                        r = h % 2
                        steps = []
                        for g in range(NB):
                            for kb in range(4 * g + 4):
                                steps.append((g, kb, kb == 0, kb == 4 * g + 3))

                        def issue_S(i):
                            g, kb, first, last = steps[i]
                            sps = a_s[i % 3]; bsp = bas[i % 3]
                            qs = slice(g * 512, (g + 1) * 512); ks = slice(kb * 128, (kb + 1) * 128)
                            P.op("pe", lambda E: E.matmul(sps, KT[:, ks], QT[:, qs], start=True, stop=True),
                                 reads=[bKT, bQT], writes=[bsp])

                        def issue_rest(i):
                            g, kb, first, last = steps[i]
                            sps = a_s[i % 3]; bsp = bas[i % 3]
                            pt = pts[i % 3]; bpt = bpts[i % 3]
                            qs = slice(g * 512, (g + 1) * 512)
                            ii = max(kb - 4 * g, 0)
                            c0 = ii * 128
                            if not (dbg and dbg.get("x_b")):
                                P.op("act", lambda E: E.activation(pt[:, c0:512], sps[:, c0:512], AF.Exp, scale=scale),
                                     reads=[bsp], writes=[bpt])
                            if kb - 4 * g >= 0:
                                P.op("dve", lambda E: E.memset(pt[64:128, c0:c0 + 64], 0.0), writes=[bpt])
                            P.op("pe", lambda E: E.matmul(a_of[:, c0:512], Vh[:, kb, :], pt[:, c0:512], start=first,
                                                          stop=last), reads=[bVh, bpt], writes=[bao])
                            if last:
                                P.op("dve", lambda E: E.reciprocal(rden[:], a_of[64:128, :]), reads=[bao], writes=[brden])
                                P.op("dve", lambda E: E.tensor_tensor(mixT[64 * r:64 * r + 64, h // 2, qs], a_o,
                                                                      rden[:], ALU.mult),
                                     reads=[bao, brden], writes=[bmix[h // 2]])

                        if dbg and dbg.get("x_a"):
                            steps = []
                        else:
                            issue_S(0)
                            issue_S(1)
                        for i in range(len(steps)):
                            if i + 2 < len(steps):
                                issue_S(i + 2)
                            issue_rest(i)
            P.barrier()

        def gdn_stage(l, s):
            with contextlib.ExitStack() as _S:
                gw = _S.enter_context(sb("gw", [128, 8, 256], BF16))
                gwab = _S.enter_context(sb("gwab", [128, 8, 8], BF16))
                gqT = _S.enter_context(sb("gqT", [128, 2, T], BF16))
                gkT = _S.enter_context(sb("gkT", [128, 2, T], BF16))
                gvT = _S.enter_context(sb("gvT", [128, 2, T], BF16))
                gzT = _S.enter_context(sb("gzT", [128, 2, T], BF16))
                gpre = _S.enter_context(sb("gpre", [128, 3 + T]))
                gacc = _S.enter_context(sb("gacc", [128, 512]))
                gsig = _S.enter_context(sb("gsig", [128, 512]))
                grn = _S.enter_context(sb("grn", [128, 512]))
                gpar = _S.enter_context(sb("gpar", [64, 12]))
                bgw, bgwab, bgq, bgk, bgv, bgz, bgpre, bgacc, bgsig, bgrn, bgpar = [Buf() for _ in range(11)]
                load_w_in(gwab, bgwab, l, 1440, 1448)
                P.dma("sp", gpar[:, 0:4], dr["gdn_a_log"][l:l + 1, :].partition_broadcast(64), writes=[bgpar])
                P.dma("sp", gpar[:, 4:8], dr["gdn_dt_bias"][l:l + 1, :].partition_broadcast(64), writes=[bgpar])
                P.op("act", lambda E: E.activation(gpar[:, 8:12], gpar[:, 0:4], AF.Exp), reads=[bgpar], writes=[bgpar])
                P.op("dve", lambda E: E.tensor_scalar_mul(gpar[:, 8:12], gpar[:, 8:12], -1.0), reads=[bgpar],
                     writes=[bgpar])
                P.op("pool", lambda E: E.memset(gpre[:, 0:3], 0.0), writes=[bgpre])
                bd = cst[:, C_BD:C_BD + 128]
                with contextlib.ExitStack() as _S:
                    g_p0 = _S.enter_context(ps("g_p0", [128, 512]))
                    g_p1 = _S.enter_context(ps("g_p1", [128, 512]))
                    g_ss = _S.enter_context(ps("g_ss", [128, 512]))
                    gps_ = [g_p0, g_p1]; bgps = [PB(), PB()]; bgss = PB()
                    k = 0
                    for gi, (c0, dst, bdst) in enumerate(((416, gqT, bgq), (672, gkT, bgk), (928, gvT, bgv),
                                                           (1184, gzT, bgz))):
                        load_w_in(gw, bgw, l, c0, c0 + 256)
                        for j in range(2):
                            if gi == 3:
                                for b in range(NB):
                                    sl = slice(b * 512, (b + 1) * 512)
                                    pp = gps_[k % 2]; bpp = bgps[k % 2]; k += 1
                                    for kt in range(8):
                                        P.op("pe", lambda E: E.matmul(pp[:], gw[:, kt, j * 128:(j + 1) * 128],
                                                                      hT[:, kt, sl], start=(kt == 0), stop=(kt == 7)),
                                             reads=[bgw, bh[kt]], writes=[bpp])
                                    P.op("act", lambda E: E.activation(gsig[:], pp[:], AF.Sigmoid), reads=[bpp],
                                         writes=[bgsig])
                                    P.op("dve", lambda E: E.tensor_tensor(dst[:, j, sl], pp[:], gsig[:], ALU.mult),
                                         reads=[bpp, bgsig], writes=[bdst])
                                continue
                            for b in range(NB):
                                sl = slice(b * 512, (b + 1) * 512)
                                pp = gps_[k % 2]; bpp = bgps[k % 2]; k += 1
                                for kt in range(8):
                                    P.op("pe", lambda E: E.matmul(pp[:], gw[:, kt, j * 128:(j + 1) * 128],
                                                                  hT[:, kt, sl], start=(kt == 0), stop=(kt == 7)),
                                         reads=[bgw, bh[kt]], writes=[bpp])
                                P.op("act", lambda E: E.copy(gpre[:, 3 + b * 512:3 + (b + 1) * 512], pp[:]),
                                     reads=[bpp], writes=[bgpre])
                            ct = gi * 2 + j
                            for b in range(NB):
                                sl = slice(b * 512, (b + 1) * 512)
                                P.op("dve", lambda E: E.tensor_scalar(gacc[:], gpre[:, b * 512:b * 512 + 512],
                                                                      col(l, K_CONV + ct * 4), None, ALU.mult),
                                     reads=[bgpre, bcols], writes=[bgacc])
                                for tap in range(1, 4):
                                    P.op("dve", lambda E: E.scalar_tensor_tensor(
                                        gacc[:], gpre[:, b * 512 + tap:b * 512 + tap + 512],
                                        col(l, K_CONV + ct * 4 + tap), gacc[:], ALU.mult, ALU.add),
                                        reads=[bgpre, bcols, bgacc], writes=[bgacc])
                                P.op("act", lambda E: E.activation(gsig[:], gacc[:], AF.Sigmoid), reads=[bgacc],
                                     writes=[bgsig])
                                if gi == 2:
                                    P.op("dve", lambda E: E.tensor_tensor(dst[:, j, sl], gacc[:], gsig[:], ALU.mult),
                                         reads=[bgacc, bgsig], writes=[bdst])
                                    continue
                                P.op("dve", lambda E: E.tensor_tensor(gacc[:], gacc[:], gsig[:], ALU.mult),
                                     reads=[bgacc, bgsig], writes=[bgacc])
                                P.op("act", lambda E: E.activation(gsig[:], gacc[:], AF.Square), reads=[bgacc],
                                     writes=[bgsig])
                                P.op("pe", lambda E: E.matmul(g_ss[:], bd, gsig[:], start=True, stop=True),
                                     reads=[bgsig, bcst], writes=[bgss])
                                P.op("act", lambda E: E.activation(grn[:], g_ss[:], AF.Sqrt, bias=RMS_EPS, scale=1.0),
                                     reads=[bgss], writes=[bgrn])
                                P.op("dve", lambda E: E.reciprocal(grn[:], grn[:]), reads=[bgrn], writes=[bgrn])
                                P.op("dve", lambda E: E.scalar_tensor_tensor(dst[:, j, sl], gacc[:],
                                                                             0.125 if gi == 0 else 1.0, grn[:],
                                                                             ALU.mult, ALU.mult),
                                     reads=[bgacc, bgrn], writes=[bdst])
                P.barrier()
                U = cst[0:64, C_U:C_U + 64]
                MLO = cst[0:64, C_MLO:C_MLO + 64]
                MUP = cst[0:64, C_MUP:C_MUP + 64]
                with contextlib.ExitStack() as _S:
                    sm = _S.enter_context(sb("c_sm", [64, 64]))
                    sm_b = _S.enter_context(sb("d_sm", [64, 64]))
                    Gd = _S.enter_context(sb("c_Gd", [64, 4, 64]))
                    Dd = _S.enter_context(sb("c_D", [64, 4, 64]))
                    Ee = _S.enter_context(sb("c_E", [64, 4, 64]))
                    Et = _S.enter_context(sb("c_Et", [64, 4, 64]))
                    EGB = _S.enter_context(sb("c_EGB", [128, 256]))
                    tA = _S.enter_context(sb("c_tA", [64, 4, 64]))
                    A0 = _S.enter_context(sb("c_A0", [64, 4, 64], BF16))
                    A1 = _S.enter_context(sb("c_A1", [64, 4, 64], BF16))
                    B0 = _S.enter_context(sb("c_B0", [64, 4, 64], BF16))
                    B1 = _S.enter_context(sb("c_B1", [64, 4, 64], BF16))
                    Pm = _S.enter_context(sb("c_P", [64, 4, 64], BF16))
                    Pm_b = _S.enter_context(sb("d_P", [64, 4, 64], BF16))
                    qkT = _S.enter_context(sb("c_qk", [64, 4, 64], BF16))
                    qkT_b = _S.enter_context(sb("d_qk", [64, 4, 64], BF16))
                    vtm = _S.enter_context(sb("c_vtm", [64, 4, 64]))
                    vtm_b = _S.enter_context(sb("d_vtm", [64, 4, 64]))
                    KDP = _S.enter_context(sb("c_KDP", [64, 4, 128], BF16))
                    KDP_b = _S.enter_context(sb("d_KDP", [64, 4, 128], BF16))
                    VNP = _S.enter_context(sb("c_VNP", [64, 4, 128], BF16))
                    Rb = _S.enter_context(sb("c_R", [64, 4, 64], BF16))
                    t1 = _S.enter_context(sb("c_t1", [64, 4, 64]))
                    qdT = _S.enter_context(sb("c_qd", [128, 2, 64], BF16))
                    qdT_b = _S.enter_context(sb("d_qd", [128, 2, 64], BF16))
                    SP = _S.enter_context(sb("c_SP", [128, 2, 128]))
                    SPb = _S.enter_context(sb("c_SPb", [128, 2, 128], BF16))
                    glc = _S.enter_context(sb("c_gl", [128, 2]))
                    glc_b = _S.enter_context(sb("d_gl", [128, 2]))
                    osq = _S.enter_context(sb("c_osq", [128, 2, 64]))
                    ors = _S.enter_context(sb("c_ors", [128, 2, 64]))
                    otm = _S.enter_context(sb("c_otm", [128, 2, 64]))
                    kmisc = _S.enter_context(ps("k_misc", [128, 512]))
                    kgcb = _S.enter_context(ps("k_gcb", [128, 256]))
                    kgk = _S.enter_context(ps("k_gk", [64, 2, 4, 64]))
                    kpv = _S.enter_context(ps("k_pv", [64, 2, 4, 64]))
                    ktr = _S.enter_context(ps("k_tr", [64, 3, 256], BF16))
                    kps1v = _S.enter_context(ps("k_ps1", [64, 2, 4, 64]))
                    kps1 = kps1v[:, 0, :, :]
                    ko = _S.enter_context(ps("k_o", [128, 512]))
                    ksq = kgk
                    kU = _S.enter_context(ps("k_U", [128, 2, 128]))
                    (bsm, bGd, bD, bE, bEt, bEGB, btA, bP, bqk, bvtm, bKDP, bVNP, bR, bt1, bqd, bSP, bSPb, bgl, bosq,
                     bors, botm) = [Buf() for _ in range(21)]
                    bA = [Buf(), Buf()]; bB = [Buf(), Buf()]
                    b_ab = PB(); b_gcc = b_ab; b_o = PB(); b_ss = b_o
                    b_gcb = PB(); b_G = PB(); b_KQ = b_G; b_sq = b_G; b_pu = PB()
                    b_trB = PB(); b_trv = b_trB; b_trk = b_trB; b_ps1 = PB(); b_vn = b_ps1; b_U = PB()
                    As = [A0, A1]; Bs = [B0, B1]
                    ab_ps = kmisc[0:64, 0:8]; gcc_ps = kmisc[0:64, 8:12]
                    ss_ps = ko[:, 64:192].rearrange("p (j c) -> p j c", j=2)
                    o_ps = ko[:, 256:384].rearrange("p (j c) -> p j c", j=2)
                    idb64 = idb[0:64, 0:64]
                    kz = _S.enter_context(sb("c_kz", [128, 2, 4, 64], BF16)); bkz = Buf()
                    P.op("pool", lambda E: E.memset(kz[:], 0.0), writes=[bkz])
                    sm2 = [sm, sm_b]; Pm2 = [Pm, Pm_b]; qk2 = [qkT, qkT_b]; vtm2 = [vtm, vtm_b]; KDP2 = [KDP, KDP_b]
                    qd2 = [qdT, qdT_b]; gl2 = [glc, glc_b]
                    bsm2, bP2, bqk2, bvtm2, bKDP2, bqd2, bgl2 = [[Buf(), Buf()] for _ in range(7)]
                    for i_ in range(2):
                        P.op("pool", lambda E: E.memset(KDP2[i_][:], 0.0), writes=[bKDP2[i_]])
                    P.op("pool", lambda E: E.memset(VNP[:], 0.0), writes=[bVNP])
                    P.op("pool", lambda E: E.memset(SP[:], 0.0), writes=[bSP])
                    P.op("pool", lambda E: E.memset(SPb[:], 0.0), writes=[bSPb])
                    g4, bt4, gcc, egc, ekd, tm4, sp4 = [sm[:, 4 * i:4 * i + 4] for i in range(7)]

                    def bc_h(ap4):
                        return ap4.unsqueeze(2).to_broadcast([64, 4, 64])

                    def bc_m(ap):
                        return ap.unsqueeze(1).to_broadcast([64, 4, 64])

                    opc = [0]
                    maxops = dbg.get("maxops", 10 ** 9) if dbg else 10 ** 9

                    def GOP(eng, fn, rd, wrt):
                        opc[0] += 1
                        if opc[0] <= maxops:
                            P.op(eng, fn, reads=rd, writes=wrt)

                    V = lambda fn, rd, wrt: GOP("dve", fn, rd, wrt)
                    AC = lambda fn, rd, wrt: GOP("act", fn, rd, wrt)
                    PE = lambda fn, rd, wrt: GOP("pe", fn, rd, wrt)
                    def prep(c):
                        par = c % 2
                        cs_ = slice(c * 64, (c + 1) * 64)
                        sm = sm2[par]; bsm = bsm2[par]; Pm = Pm2[par]; bP = bP2[par]; qkT = qk2[par]; bqk = bqk2[par]
                        vtm = vtm2[par]; bvtm = bvtm2[par]; KDP = KDP2[par]; bKDP = bKDP2[par]
                        qdT = qd2[par]; bqd = bqd2[par]; glc = gl2[par]; bgl = bgl2[par]
                        g4, bt4, gcc, egc, ekd, tm4, sp4 = [sm[:, 4 * i:4 * i + 4] for i in range(7)]
                        for kt in range(8):
                            yield PE(lambda E: E.matmul(ab_ps, hT[:, kt, cs_], gwab[:, kt, :], start=(kt == 0),
                                                  stop=(kt == 7)), [bh[kt], bgwab], [b_ab])
                        yield V(lambda E: E.tensor_tensor(sp4, ab_ps[:, 0:4], gpar[:, 4:8], ALU.add), [b_ab, bgpar], [bsm])
                        yield AC(lambda E: E.activation(sp4, sp4, AF.Exp), [bsm], [bsm])
                        yield AC(lambda E: E.activation(sp4, sp4, AF.Ln, bias=1.0, scale=1.0), [bsm], [bsm])
                        yield V(lambda E: E.tensor_tensor(g4, sp4, gpar[:, 8:12], ALU.mult), [bsm, bgpar], [bsm])
                        yield AC(lambda E: E.activation(bt4, ab_ps[:, 4:8], AF.Exp, scale=-1.0), [b_ab], [bsm])
                        yield V(lambda E: E.tensor_scalar_add(bt4, bt4, 1.0), [bsm], [bsm])
                        yield V(lambda E: E.reciprocal(bt4, bt4), [bsm], [bsm])
                        yield PE(lambda E: E.matmul(gcc_ps, U, g4, start=True, stop=True), [bsm, bcst], [b_gcc])
                        yield V(lambda E: E.tensor_copy(gcc, gcc_ps), [b_gcc], [bsm])
                        yield V(lambda E: E.tensor_tensor(Gd[:], bc_m(U), bc_h(g4), ALU.mult), [bsm, bcst], [bGd])
                        yield PE(lambda E: E.matmul(kgcb[:], cst[0:64, C_ONE:C_ONE + 128],
                                              Gd[:].rearrange("p h j -> p (h j)"), start=True, stop=True),
                           [bGd, bcst], [b_gcb])
                        gcb3 = kgcb[0:64, :].rearrange("p (h j) -> p h j", h=4)
                        yield V(lambda E: E.tensor_tensor(Dd[:], bc_h(gcc), gcb3, ALU.subtract), [bsm, b_gcb], [bD])
                        yield V(lambda E: E.tensor_tensor(Ee[:], Dd[:], bc_m(MLO), ALU.add), [bD, bcst], [bE])
                        yield V(lambda E: E.tensor_tensor(Et[:], bc_m(MUP), Dd[:], ALU.subtract), [bD, bcst], [bEt])
                        yield AC(lambda E: E.activation(Ee[:], Ee[:], AF.Exp), [bE], [bE])
                        yield AC(lambda E: E.activation(Et[:], Et[:], AF.Exp), [bEt], [bEt])
                        yield AC(lambda E: E.activation(EGB[:], kgcb[:], AF.Exp), [b_gcb], [bEGB])
                        yield AC(lambda E: E.activation(egc, gcc, AF.Exp), [bsm], [bsm])
                        yield V(lambda E: E.tensor_tensor(tm4, gcb3[:, :, 63], gcc, ALU.subtract), [b_gcb, bsm], [bsm])
                        yield AC(lambda E: E.activation(ekd, tm4, AF.Exp), [bsm], [bsm])
                        EG4 = EGB[:].rearrange("p (j r c) -> p j r c", j=2, r=2)
                        yield V(lambda E: E.tensor_copy(glc[0:64, :], EG4[0:64, :, 0, 63]), [bEGB], [bgl])
                        yield V(lambda E: E.tensor_copy(glc[64:128, :], EG4[64:128, :, 1, 63]), [bEGB], [bgl])
                        for h in range(4):
                            j, r = h // 2, h % 2
                            rows = slice(64 * r, 64 * r + 64)
                            yield GOP("pool", lambda E: E.tensor_copy(kz[rows, 0, h, :], gkT[rows, j, cs_]), [bgk], [bkz])
                            yield GOP("pool", lambda E: E.tensor_copy(kz[rows, 1, h, :], gqT[rows, j, cs_]), [bgq], [bkz])
                        for h in range(4):
                            j, r = h // 2, h % 2
                            yield PE(lambda E: E.matmul(kgk[:, 0, h, :], gkT[:, j, cs_], kz[:, 0, h, :], start=True,
                                                  stop=True), [bgk, bkz], [b_G])
                            yield PE(lambda E: E.matmul(kgk[:, 1, h, :], gkT[:, j, cs_], kz[:, 1, h, :], start=True,
                                                  stop=True), [bgk, bkz], [b_KQ])
                        a = 0
                        yield V(lambda E: E.tensor_tensor(tA[:], kgk[:, 0, :, :], Ee[:], ALU.mult), [b_G, bE], [btA])
                        yield V(lambda E: E.tensor_tensor(As[a][:], tA[:], bc_h(bt4), ALU.mult), [btA, bsm], [bA[a]])
                        yield V(lambda E: E.tensor_tensor(qkT[:], kgk[:, 1, :, :], Et[:], ALU.mult), [b_KQ, bEt], [bqk])
                        trB = ktr[:, 0, :].rearrange("p (h i) -> p h i", h=4)
                        for h in range(4):
                            yield PE(lambda E: E.transpose(trB[:, h, :], As[a][:, h, :], idb64), [bA[a], bcst], [b_trB])
                        yield V(lambda E: E.tensor_copy(Bs[a][:], trB), [b_trB], [bB[a]])
                        yield V(lambda E: E.tensor_tensor(Pm[:], bc_m(idb64), Bs[a][:], ALU.subtract), [bB[a], bcst], [bP])
                        for lev in range(5):
                            n = 1 - a
                            for h in range(4):
                                yield PE(lambda E: E.matmul(ksq[:, 0, h, :], Bs[a][:, h, :], As[a][:, h, :], start=True,
                                                      stop=True), [bA[a], bB[a]], [b_sq])
                                yield PE(lambda E: E.matmul(ksq[:, 1, h, :], As[a][:, h, :], Bs[a][:, h, :], start=True,
                                                      stop=True), [bA[a], bB[a]], [b_sq])
                            yield AC(lambda E: E.copy(As[n][:], ksq[:, 0, :, :]), [b_sq], [bA[n]])
                            yield V(lambda E: E.tensor_copy(Bs[n][:], ksq[:, 1, :, :]), [b_sq], [bB[n]])
                            a = n
                            for h in range(4):
                                yield PE(lambda E: E.matmul(kpv[:, 0, h, :], As[a][:, h, :], Pm[:, h, :], start=True,
                                                      stop=True), [bA[a], bP], [b_pu])
                            yield V(lambda E: E.tensor_tensor(Pm[:], Pm[:], kpv[:, 0, :, :], ALU.add), [bP, b_pu], [bP])
                        for j in range(2):
                            yield PE(lambda E: E.transpose(ktr[:, 1, j * 128:(j + 1) * 128], gvT[:, j, cs_], idb[:]),
                               [bgv, bcst], [b_trv])
                            yield PE(lambda E: E.transpose(ktr[:, 2, j * 128:(j + 1) * 128], gkT[:, j, cs_], idb[:]),
                               [bgk, bcst], [b_trk])
                        yield AC(lambda E: E.copy(vtm[:].rearrange("p h d -> p (h d)"), ktr[:, 1, :]), [b_trv], [bvtm])
                        k4 = ktr[:, 2, :].rearrange("p (j r d) -> p j r d", j=2, r=2)
                        KD5 = KDP[:].rearrange("p (j r) m -> p j r m", j=2)
                        ekd3 = ekd.rearrange("p (j r) -> p j r", j=2)
                        for r in range(2):
                            yield V(lambda E: E.tensor_tensor(KD5[:, :, r, 64 * r:64 * r + 64], k4[:, :, r, :],
                                                        ekd3[:, :, r].unsqueeze(2).to_broadcast([64, 2, 64]),
                                                        ALU.mult), [b_trk, bsm], [bKDP])
                        for h in range(4):
                            j, r = h // 2, h % 2
                            rows = slice(64 * r, 64 * r + 64)
                            yield GOP("pool", lambda E: E.tensor_tensor(qdT[rows, j, :], gqT[rows, j, cs_],
                                                                  EGB[rows, h * 64:(h + 1) * 64], ALU.mult),
                                [bgq, bEGB], [bqd])

                    def scan(c):
                        par = c % 2
                        cs_ = slice(c * 64, (c + 1) * 64)
                        sm = sm2[par]; bsm = bsm2[par]; Pm = Pm2[par]; bP = bP2[par]; qkT = qk2[par]; bqk = bqk2[par]
                        vtm = vtm2[par]; bvtm = bvtm2[par]; KDP = KDP2[par]; bKDP = bKDP2[par]
                        qdT = qd2[par]; bqd = bqd2[par]; glc = gl2[par]; bgl = bgl2[par]
                        g4, bt4, gcc, egc, ekd, tm4, sp4 = [sm[:, 4 * i:4 * i + 4] for i in range(7)]
                        for j in range(2):
                            yield PE(lambda E: E.matmul(kps1[:, 2 * j:2 * j + 2, :].rearrange("p a b -> p (a b)"),
                                                  gkT[:, j, cs_], SPb[:, j, :], start=True, stop=True),
                               [bgk, bSPb], [b_ps1])
                        yield V(lambda E: E.tensor_tensor(t1[:], kps1[:], bc_h(egc), ALU.mult), [b_ps1, bsm], [bt1])
                        yield V(lambda E: E.tensor_tensor(t1[:], vtm[:], t1[:], ALU.subtract), [bvtm, bt1], [bt1])
                        yield V(lambda E: E.tensor_tensor(Rb[:], t1[:], bc_h(bt4), ALU.mult), [bt1, bsm], [bR])
                        for h in range(4):
                            yield PE(lambda E: E.matmul(kps1v[:, 1, h, :], Pm[:, h, :], Rb[:, h, :], start=True, stop=True),
                               [bP, bR], [b_vn])
                        VN5 = VNP[:].rearrange("p (j r) m -> p j r m", j=2)
                        vn4 = kps1v[:, 1, :, :].rearrange("p (j r) d -> p j r d", j=2)
                        for r in range(2):
                            yield AC(lambda E: E.copy(VN5[:, :, r, 64 * r:64 * r + 64], vn4[:, :, r, :]), [b_vn], [bVNP])
                        for j in range(2):
                            yield PE(lambda E: E.matmul(o_ps[:, j, :], SPb[:, j, :], qdT[:, j, :], start=True, stop=False),
                               [bSPb, bqd], [b_o])
                            yield PE(lambda E: E.matmul(o_ps[:, j, :], VNP[:, 2 * j, :], qkT[:, 2 * j, :], start=False,
                                                  stop=False), [bVNP, bqk], [b_o])
                            yield PE(lambda E: E.matmul(o_ps[:, j, :], VNP[:, 2 * j + 1, :], qkT[:, 2 * j + 1, :],
                                                  start=False, stop=True), [bVNP, bqk], [b_o])
                        yield AC(lambda E: E.activation(osq[:], o_ps, AF.Square), [b_o], [bosq])
                        yield PE(lambda E: E.matmul(ss_ps.rearrange("p j c -> p (j c)"), bd,
                                              osq[:].rearrange("p j c -> p (j c)"), start=True, stop=True),
                           [bosq, bcst], [b_ss])
                        yield AC(lambda E: E.activation(ors[:], ss_ps, AF.Sqrt, bias=RMS_EPS, scale=1.0 / 64), [b_ss], [bors])
                        yield V(lambda E: E.reciprocal(ors[:], ors[:]), [bors], [bors])
                        yield V(lambda E: E.scalar_tensor_tensor(otm[:], o_ps, col(l, K_GO), ors[:], ALU.mult, ALU.mult),
                          [b_o, bors, bcols], [botm])
                        for j in range(2):
                            yield V(lambda E: E.tensor_tensor(mixT[:, 4 + j, cs_], otm[:, j, :], gzT[:, j, cs_], ALU.mult),
                              [botm, bgz], [bmix[4 + j]])
                        for j in range(2):
                            yield PE(lambda E: E.matmul(kU[:, j, :], KDP[:, 2 * j, :], VNP[:, 2 * j, :], start=True,
                                                  stop=False), [bKDP, bVNP], [b_U])
                            yield PE(lambda E: E.matmul(kU[:, j, :], KDP[:, 2 * j + 1, :], VNP[:, 2 * j + 1, :],
                                                  start=False, stop=True), [bKDP, bVNP], [b_U])
                        for j in range(2):
                            yield V(lambda E: E.scalar_tensor_tensor(SP[:, j, :], SP[:, j, :], glc[:, j:j + 1],
                                                               kU[:, j, :], ALU.mult, ALU.add),
                              [bSP, bgl, b_U], [bSP])
                        yield AC(lambda E: E.copy(SPb[:], SP[:]), [bSP], [bSPb])

                    def run_il(g1, g2, r1=3, r2=1):
                        d1 = g1 is None
                        d2 = g2 is None
                        while not (d1 and d2):
                            for _ in range(r1):
                                if not d1:
                                    try:
                                        next(g1)
                                    except StopIteration:
                                        d1 = True
                            for _ in range(r2):
                                if not d2:
                                    try:
                                        next(g2)
                                    except StopIteration:
                                        d2 = True

                    NCHX = NCH if not (dbg and 'nch' in dbg) else dbg['nch']
                    if NCHX:
                        run_il(prep(0), None)
                    for c in range(NCHX):
                        run_il(prep(c + 1) if c + 1 < NCHX else None, scan(c))
            P.barrier()

        def out_proj(l, s):
            with contextlib.ExitStack() as _S:
                wo = _S.enter_context(sb("wo", [128, 8, D], BF16))
                o0 = _S.enter_context(ps("ops0", [128, 512]))
                o1 = _S.enter_context(ps("ops1", [128, 512]))
                bwo = Buf(); ops_ = [o0, o1]; bops = [PB(), PB()]
                P.dma("sp", wo[:], wbf["w_out"][l, :, :].rearrange("(k p) n -> p k n", p=128),
                      reads=[bwbf["w_out"][l]], writes=[bwo])
                for kt in range(8):
                    P.op("act", lambda E: E.mul(xT[:, kt, :], xT[:, kt, :], ALPHA), reads=[bx[kt]], writes=[bx[kt]])
                k = 0
                for b in range(NB):
                    sl = slice(b * 512, (b + 1) * 512)
                    for m in range(8):
                        pp = ops_[k % 2]; bpp = bops[k % 2]; k += 1
                        for kt in range(8):
                            P.op("pe", lambda E: E.matmul(pp[:], wo[:, kt, m * 128:(m + 1) * 128], mixT[:, kt, sl],
                                                          start=(kt == 0), stop=(kt == 7)),
                                 reads=[bwo, bmix[kt]], writes=[bpp])
                        P.op("dve", lambda E: E.scalar_tensor_tensor(xT[:, m, sl], pp[:], modc(l, 2, m, s),
                                                                     xT[:, m, sl], ALU.mult, ALU.add),
                             reads=[bpp, bmod, bx[m]], writes=[bx[m]])
            P.barrier()

        def moe_stage(l, s):
            with contextlib.ExitStack() as _S:
                wr = _S.enter_context(sb("wr", [128, 8, 36]))
                brb = _S.enter_context(sb("brb", [128, 36]))
                h2f = _S.enter_context(sb("h2f", [128, 8, 512]))
                Lg = _S.enter_context(sb("Lg", [128, 36]))
                sm = _S.enter_context(sb("sm", [128, 16]))
                msk = _S.enter_context(sb("msk", [128, 3, 32]))
                Wt = _S.enter_context(sb("Wt", [128, 32]))
                WtT = _S.enter_context(sb("WtT", [32, T]))
                r_l = _S.enter_context(ps("r_l", [128, 36]))
                r_t = _S.enter_context(ps("r_t", [32, 128]))
                bwr, bbrb, bh2f, bLg, bsm, bmsk, bWt, bWtT = [Buf() for _ in range(8)]
                brl, brt_ = PB(), PB()
                P.dma("sp", wr[:], dr["w_r"][l, :, :].rearrange("(k p) n -> p k n", p=128), writes=[bwr])
                P.dma("sp", brb[:], dr["b_r"][l:l + 1, :].partition_broadcast(128), writes=[bbrb])
                for b in range(NB):
                    sl = slice(b * 512, (b + 1) * 512)
                    for kt in range(8):
                        P.op("act", lambda E: E.activation(h2f[:, kt, :], xT[:, kt, sl], AF.Identity,
                                                           bias=modc(l, 3, kt, s), scale=modc(l, 4, kt, s)),
                             reads=[bx[kt], bmod], writes=[bh2f])
                        P.op("pool", lambda E: E.tensor_copy(hT[:, kt, sl], h2f[:, kt, :]), reads=[bh2f],
                             writes=[bh[kt]])
                    for tq in range(4):
                        t0 = b * 512 + tq * 128
                        for kt in range(8):
                            P.op("pe", lambda E: E.matmul(r_l[:], h2f[:, kt, tq * 128:(tq + 1) * 128], wr[:, kt, :],
                                                          start=(kt == 0), stop=(kt == 7)),
                                 reads=[bh2f, bwr], writes=[brl])
                        V = lambda fn, rd, wrt: P.op("dve", fn, reads=rd, writes=wrt)
                        V(lambda E: E.tensor_tensor(Lg[:], r_l[:], brb[:], ALU.add), [brl, bbrb], [bLg])
                        V(lambda E: E.reduce_max(sm[:, 0:1], Lg[:, 0:4], AX.X), [bLg], [bsm])
                        V(lambda E: E.tensor_scalar_mul(sm[:, 1:2], sm[:, 0:1], -1.0), [bsm], [bsm])
                        V(lambda E: E.tensor_scalar(msk[:, 0, 0:4], Lg[:, 0:4], sm[:, 0:1], None, ALU.is_equal),
                          [bLg, bsm], [bmsk])
                        P.op("act", lambda E: E.activation(msk[:, 0, 8:12], Lg[:, 0:4], AF.Exp, bias=sm[:, 1:2],
                                                           scale=1.0, accum_out=sm[:, 2:3]),
                             reads=[bLg, bsm], writes=[bmsk, bsm])
                        V(lambda E: E.reciprocal(sm[:, 3:4], sm[:, 2:3]), [bsm], [bsm])
                        V(lambda E: E.tensor_scalar(msk[:, 0, 4:8], msk[:, 0, 0:4], -NEG, NEG, ALU.mult, ALU.add),
                          [bmsk], [bmsk])
                        V(lambda E: E.tensor_tensor(msk[:, 1, :].rearrange("p (g e) -> p g e", g=4),
                                                    Lg[:, 4:36].rearrange("p (g e) -> p g e", g=4),
                                                    msk[:, 0, 4:8].unsqueeze(2).to_broadcast([128, 4, 8]), ALU.add),
                          [bLg, bmsk], [bmsk])
                        V(lambda E: E.reduce_max(sm[:, 4:5], msk[:, 1, :], AX.X), [bmsk], [bsm])
                        V(lambda E: E.tensor_scalar(msk[:, 2, :], msk[:, 1, :], sm[:, 4:5], None, ALU.is_equal),
                          [bmsk, bsm], [bmsk])
                        V(lambda E: E.scalar_tensor_tensor(msk[:, 1, :], msk[:, 2, :], NEG, msk[:, 1, :], ALU.mult,
                                                           ALU.add), [bmsk], [bmsk])
                        V(lambda E: E.reduce_max(sm[:, 5:6], msk[:, 1, :], AX.X), [bmsk], [bsm])
                        V(lambda E: E.tensor_scalar(msk[:, 1, :], msk[:, 1, :], sm[:, 5:6], None, ALU.is_equal),
                          [bmsk, bsm], [bmsk])
                        V(lambda E: E.tensor_tensor(sm[:, 6:7], sm[:, 5:6], sm[:, 4:5], ALU.subtract), [bsm], [bsm])
                        P.op("act", lambda E: E.activation(sm[:, 7:8], sm[:, 6:7], AF.Exp), reads=[bsm], writes=[bsm])
                        V(lambda E: E.tensor_scalar_add(sm[:, 8:9], sm[:, 7:8], 1.0), [bsm], [bsm])
                        V(lambda E: E.reciprocal(sm[:, 8:9], sm[:, 8:9]), [bsm], [bsm])
                        V(lambda E: E.tensor_tensor(sm[:, 9:10], sm[:, 3:4], sm[:, 8:9], ALU.mult), [bsm], [bsm])
                        V(lambda E: E.tensor_tensor(sm[:, 10:11], sm[:, 9:10], sm[:, 7:8], ALU.mult), [bsm], [bsm])
                        V(lambda E: E.tensor_scalar(Wt[:], msk[:, 2, :], sm[:, 9:10], None, ALU.mult),
                          [bmsk, bsm], [bWt])
                        V(lambda E: E.scalar_tensor_tensor(Wt[:], msk[:, 1, :], sm[:, 10:11], Wt[:], ALU.mult,
                                                           ALU.add), [bmsk, bsm, bWt], [bWt])
                        P.op("pe", lambda E: E.transpose(r_t[:], Wt[:], ident), reads=[bWt, bcst], writes=[brt_])
                        P.op("act", lambda E: E.copy(WtT[:, t0:t0 + 128], r_t[:]), reads=[brt_], writes=[bWtT])
                P.dma("sp", scr[:, :], WtT[:], reads=[bWtT], writes=[bscr])
            P.barrier()
            for kt in range(8):
                P.op("act", lambda E: E.mul(xT[:, kt, :], xT[:, kt, :], ALPHA), reads=[bx[kt]], writes=[bx[kt]])
            TB = 256
            NTB = T // TB
            with contextlib.ExitStack() as _S:
                A_ = lambda nm, shp, dt=F32: _S.enter_context(sb(nm, shp, dt))
                stg_g, stg_u, stg_d = A_("stg_g", [128, 8, 256]), A_("stg_u", [128, 8, 256]), A_("stg_d", [128, 2, D])
                wg = [A_("wg0", [128, 8, 256], BF16), A_("wg1", [128, 8, 256], BF16)]
                wu = [A_("wu0", [128, 8, 256], BF16), A_("wu1", [128, 8, 256], BF16)]
                wd = [A_("wd0", [128, 2, D], BF16), A_("wd1", [128, 2, D], BF16)]
                wb = [A_("wb0", [128, T]), A_("wb1", [128, T])]
                ea = [A_("ea0", [128, 2, TB]), A_("ea1", [128, 2, TB])]
                eb = [A_("eb0", [128, 2, TB], BF16), A_("eb1", [128, 2, TB], BF16)]
                eg = [_S.enter_context(ps("eg0", [128, 2, TB])), _S.enter_context(ps("eg1", [128, 2, TB]))]
                eu = [_S.enter_context(ps("eu0", [128, 2, TB])), _S.enter_context(ps("eu1", [128, 2, TB]))]
                ey = _S.enter_context(ps("ey", [128, 8, TB]))
                bsg, bsu, bsd = Buf(), Buf(), Buf()
                bwg, bwu, bwd, bwb, bea, beb = [[Buf(), Buf()] for _ in range(6)]
                beg, beu = [[PB(), PB()] for _ in range(2)]
                beyb = [PB() for _ in range(4)]
                NE = NEXP if not (dbg and 'nexp' in dbg) else dbg['nexp']

                def dma_expert(e):
                    if e >= NE:
                        return
                    P.dma("sp", stg_g[:], dr["moe_w_gate"][l, e, :, :].rearrange("(k p) n -> p k n", p=128),
                          writes=[bsg])
                    P.dma("sp", stg_u[:], dr["moe_w_up"][l, e, :, :].rearrange("(k p) n -> p k n", p=128),
                          writes=[bsu])
                    P.dma("sp", stg_d[:], dr["moe_w_down"][l, e, :, :].rearrange("(f p) n -> p f n", p=128),
                          writes=[bsd])

                def cast_expert(e):
                    if e >= NE:
                        return
                    w = e % 2
                    P.op("act", lambda E: E.copy(wg[w][:], stg_g[:]), reads=[bsg], writes=[bwg[w]])
                    P.op("pool", lambda E: E.tensor_copy(wu[w][:], stg_u[:]), reads=[bsu], writes=[bwu[w]])
                    P.op("pool", lambda E: E.tensor_copy(wd[w][:], stg_d[:]), reads=[bsd], writes=[bwd[w]])
                    P.dma("sp", wb[w][:], scr[e:e + 1, :].partition_broadcast(128), reads=[bscr], writes=[bwb[w]])

                units = [(e, tb) for e in range(NE) for tb in range(NTB)]

                def gu(i):
                    e, tb = units[i]
                    w = e % 2; q = i % 2
                    sl = slice(tb * TB, (tb + 1) * TB)
                    for f in range(2):
                        for kt in range(8):
                            P.op("pe", lambda E: E.matmul(eg[q][:, f, :], wg[w][:, kt, f * 128:(f + 1) * 128],
                                                          hT[:, kt, sl], start=(kt == 0), stop=(kt == 7)),
                                 reads=[bwg[w], bh[kt]], writes=[beg[q]])
                    for f in range(2):
                        for kt in range(8):
                            P.op("pe", lambda E: E.matmul(eu[q][:, f, :], wu[w][:, kt, f * 128:(f + 1) * 128],
                                                          hT[:, kt, sl], start=(kt == 0), stop=(kt == 7)),
                                 reads=[bwu[w], bh[kt]], writes=[beu[q]])

                def acc(i):
                    e, tb = units[i]
                    sl = slice(tb * TB, (tb + 1) * TB)
                    for m in range(8):
                        P.op("dve", lambda E: E.scalar_tensor_tensor(xT[:, m, sl], ey[:, m, :], modc(l, 5, m, s),
                                                                     xT[:, m, sl], ALU.mult, ALU.add),
                             reads=[beyb[m // 2], bmod, bx[m]], writes=[bx[m]])

                def elem(i):
                    e, tb = units[i]
                    w = e % 2; q = i % 2
                    sl = slice(tb * TB, (tb + 1) * TB)
                    P.op("act", lambda E: E.activation(ea[q][:], eg[q][:], AF.Silu), reads=[beg[q]], writes=[bea[q]])
                    P.op("dve", lambda E: E.tensor_tensor(ea[q][:], ea[q][:], eu[q][:], ALU.mult),
                         reads=[bea[q], beu[q]], writes=[bea[q]])
                    P.op("dve", lambda E: E.tensor_tensor(eb[q][:], ea[q][:],
                                                          wb[w][:, sl].unsqueeze(1).to_broadcast([128, 2, TB]),
                                                          ALU.mult), reads=[bea[q], bwb[w]], writes=[beb[q]])

                def down(i):
                    e, tb = units[i]
                    w = e % 2; q = i % 2
                    for m in range(8):
                        for f in range(2):
                            P.op("pe", lambda E: E.matmul(ey[:, m, :], wd[w][:, f, m * 128:(m + 1) * 128],
                                                          eb[q][:, f, :], start=(f == 0), stop=(f == 1)),
                                 reads=[bwd[w], beb[q]], writes=[beyb[m // 2]])

                dma_expert(0); cast_expert(0); dma_expert(1); cast_expert(1); dma_expert(2)
                if units:
                    gu(0)
                    elem(0)
                for i in range(len(units)):
                    if i + 1 < len(units):
                        gu(i + 1)
                    down(i)
                    if i + 1 < len(units):
                        elem(i + 1)
                    acc(i)
                    e, tb = units[i]
                    if tb == NTB - 1:
                        cast_expert(e + 2)
                        dma_expert(e + 3)
            P.barrier()

        STAGES = dbg.get("stages", "mgpoLME") if dbg else "mgpoLME"
        for s in range(NSEQ):
            for kt in range(8):
                P.dma("sp", xT[:, kt, :], dr["xT"][s, kt * 128:(kt + 1) * 128, :], writes=[bx[kt]])
            for l in range(DEPTH):
                modulate(l, s, 0, 1)
                if "z" in STAGES:
                    for kt in range(8):
                        P.op("pool", lambda E: E.memset(mixT[:, kt, :], 0.0), writes=[bmix[kt]])
                if "m" in STAGES:
                    mla_stage(l, s)
                if "g" in STAGES:
                    gdn_stage(l, s)
                if "p" in STAGES:
                    pool_stage(l, s)
                if dbg and dbg.get("dump") == "mix" and s == dbg.get("s", 0) and l == dbg.get("l", 0):
                    with contextlib.ExitStack() as _S:
                        dmp = _S.enter_context(sb("dmp", [128, 8, T]))
                        bd = Buf()
                        for kt in range(8):
                            P.op("dve", lambda E: E.tensor_copy(dmp[:, kt, :], (hT if dbg.get("src") == "h" else mixT)[:, kt, :]), reads=[bmix[kt], bh[kt]],
                                 writes=[bd])
                        P.dma("sp", dbg_out.rearrange("(k p) t -> p k t", p=128), dmp[:], reads=[bd])
                        P.barrier()
                if "o" in STAGES:
                    out_proj(l, s)
                if "L" in STAGES:
                    layer_norm(l, K_LN1G, K_LN1B)
                if "M" in STAGES:
                    moe_stage(l, s)
                if "E" in STAGES:
                    layer_norm(l, K_LN2G, K_LN2B)
            for kt in range(8):
                P.dma("sp", yT[s, kt * 128:(kt + 1) * 128, :], xT[:, kt, :], reads=[bx[kt]])
        P.barrier()
    P.es.close()
    return nc


def make_consts():
    c = np.zeros((128, NCON), np.float32)
    c[:, C_ID:C_ID + 128] = np.eye(128, dtype=np.float32)
    c[:, C_ONE:C_ONE + 128] = 1.0
    i = np.arange(64)
    c[:64, C_U:C_U + 64] = (i[:, None] <= i[None, :]).astype(np.float32)
    c[:64, C_MLO:C_MLO + 64] = np.where(i[:, None] > i[None, :], 0.0, NEG)
    c[:64, C_MUP:C_MUP + 64] = np.where(i[None, :] >= i[:, None], 0.0, NEG)
    t = np.arange(16)
    for j in range(2):
        for p in range(128):
            w = 2 ** (2 * j + p // 64 + 1)
            c[p, C_INV16 + 16 * j:C_INV16 + 16 * j + 16] = 1.0 / np.minimum(t + 1, w)
    p = np.arange(128)
    c[:, C_BD:C_BD + 128] = (p[:, None] // 64 == p[None, :] // 64).astype(np.float32)
    return c


def make_rope(T):
    inv_freq = np.power(np.float32(10000.0), -np.arange(0, 32, 2, dtype=np.float32) / np.float32(32)).astype(np.float32)
    ang = (np.arange(T, dtype=np.float32)[:, None] * inv_freq[None, :]).astype(np.float32)
    cos, sin = np.cos(ang).astype(np.float32).T, np.sin(ang).astype(np.float32).T
    r = np.zeros((32, 2, T), np.float32)
    r[:16, 0], r[16:, 0], r[:16, 1], r[16:, 1] = cos, cos, sin, sin
    return r


def make_cols(inp):
    L = inp["w_in"].shape[0]
    cols = np.zeros((L, 128, NCOLS), np.float32)

    def put(k, vec, n):
        cols[:, :, k:k + n] = vec.reshape(L, n, 128).transpose(0, 2, 1)

    put(K_QN, inp["mla_q_norm"], 2); put(K_KVN, inp["mla_kv_norm"], 1)
    put(K_LN1G, inp["ln1_g"], 8); put(K_LN1B, inp["ln1_b"], 8); put(K_LN2G, inp["ln2_g"], 8); put(K_LN2B, inp["ln2_b"], 8)
    put(K_PS, inp["pool_scale"], 2)
    cols[:, :, K_GO] = np.concatenate([inp["gdn_out_norm"], inp["gdn_out_norm"]], axis=1)
    cv = inp["gdn_conv"].reshape(L, 4, 6, 128)
    cols[:, :, K_CONV:K_CONV + 24] = cv.transpose(0, 3, 2, 1).reshape(L, 128, 24)
    put(K_BMOD, inp["b_mod"], 48)
    return cols


_NC_CACHE = {}


def run(inp, T, DEPTH, NSEQ, ncores, dbg=None):
    key = (T, DEPTH, NSEQ, repr(dbg))
    if key not in _NC_CACHE:
        _NC_CACHE[key] = build(T, DEPTH, NSEQ, dbg)
    nc = _NC_CACHE[key]
    f = lambda a: np.ascontiguousarray(np.asarray(a, dtype=np.float32))
    shared = {
        "consts": make_consts(), "rope": make_rope(T), "cols": f(make_cols(inp)),
        "w_in": f(inp["w_in"]), "mla_w_uq": f(inp["mla_w_uq"]), "mla_w_ukv": f(inp["mla_w_ukv"]),
        "gdn_a_log": f(inp["gdn_a_log"]), "gdn_dt_bias": f(inp["gdn_dt_bias"]), "pool_w": f(inp["pool_w"]),
        "w_out": f(inp["w_out"]), "w_mod": f(inp["w_mod"]),
        "w_r": f(np.concatenate([inp["router_w_group"], inp["router_w_expert"]], axis=2)),
        "b_r": f(np.concatenate([inp["router_b_group"], inp["router_b_expert"]], axis=1)),
        "moe_w_gate": f(inp["moe_w_gate"]), "moe_w_up": f(inp["moe_w_up"]), "moe_w_down": f(inp["moe_w_down"]),
    }
    x = np.asarray(inp["x"], np.float32); c = np.asarray(inp["c"], np.float32)
    in_maps = []
    for i in range(ncores):
        m = dict(shared)
        m["xT"] = f(x[i * NSEQ:(i + 1) * NSEQ].transpose(0, 2, 1))
        m["cT"] = f(c[i * NSEQ:(i + 1) * NSEQ].T)
        in_maps.append(m)
    res = run_bass_kernel_spmd(nc, in_maps, core_ids=list(range(ncores)))
    out = np.concatenate([r["yT"].transpose(0, 2, 1) for r in res.results], axis=0)
    return out, res


def kernel(**inputs):
    out, _ = run(inputs, 2048, 4, 2, 8)
    return out.astype(np.float32)
```

```python
import contextlib
import numpy as np
import concourse.bass as bass
import concourse.mybir as mybir
from concourse.bass_utils import run_bass_kernel_spmd

F32 = mybir.dt.float32
BF16 = mybir.dt.bfloat16
AF = mybir.ActivationFunctionType
ALU = mybir.AluOpType
AX = mybir.AxisListType

SEG = 28000
ENGS = ("pe", "dve", "act", "pool", "sp")
NDMA = 8

D = 1024
NEXP = 32
ALPHA = 8.0 ** 0.25
LN_EPS = 1e-5
RMS_EPS = 1e-6
NEG = -30000.0

C_ID, C_ONE, C_U, C_MLO, C_MUP, C_INV16, C_BD, NCON = 0, 128, 256, 320, 384, 448, 480, 608
K_QN, K_KVN, K_LN1G, K_LN1B, K_LN2G, K_LN2B, K_PS, K_GO, K_CONV, K_BMOD, NCOLS = 0, 2, 3, 11, 19, 27, 35, 37, 38, 62, 110


class Buf:
    __slots__ = ("name", "lw", "rd", "excl")

    def __init__(self, name="", excl=False):
        self.name = name
        self.lw = None
        self.rd = {}
        self.excl = excl


def PB():
    return Buf("psum", True)


class Prog:
    def __init__(self, nc, same_engine_sync=True):
        self.nc = nc
        self.es = contextlib.ExitStack()
        self.cnt = {e: 0 for e in ENGS}
        self.seen = {e: {} for e in ENGS}
        self.sems = {}
        self.dma_i = {e: 0 for e in ENGS}
        self.same = same_engine_sync
        self.E = {"pe": nc.tensor, "dve": nc.vector, "act": nc.scalar, "pool": nc.gpsimd, "sp": nc.sync}
        self.last_dma = {}

    def sem(self, key):
        if key not in self.sems:
            nm = "s_" + "_".join(str(k) for k in key)
            self.sems[key] = self.es.enter_context(self.nc.semaphore(nm))
        return self.sems[key]

    def _wait(self, eng, tok):
        key, val = tok
        if self.seen[eng].get(key, 0) >= val:
            return
        if key[0] == eng and (eng == "pe" or not self.same):
            return
        self.seen[eng][key] = val
        self.E[eng].wait_ge(self.sem(key), val)

    def _deps(self, eng, reads, writes):
        for b in reads:
            if b.lw is not None:
                self._wait(eng, b.lw)
        for b in writes:
            if b.excl:
                if b.lw is not None and b.lw[0][0] != eng:
                    self._wait(eng, b.lw)
                for k, v in b.rd.items():
                    if k[0] != eng:
                        self._wait(eng, (k, v))
                continue
            if b.lw is not None:
                self._wait(eng, b.lw)
            for k, v in b.rd.items():
                if k[0] != eng:
                    self._wait(eng, (k, v))

    def _mark(self, tok, reads, writes):
        k, v = tok
        for b in reads:
            if b.rd.get(k, 0) < v:
                b.rd[k] = v
        for b in writes:
            if b.excl:
                if b.lw is not None and b.lw[0][0] != k[0]:
                    pk, pv = b.lw
                    if b.rd.get(pk, 0) < pv:
                        b.rd[pk] = pv
                b.rd = {kk: vv for kk, vv in b.rd.items() if kk[0] != k[0]}
                b.lw = tok
                continue
            b.lw = tok
            b.rd = {}

    def op(self, eng, fn, reads=(), writes=()):
        if any(b.excl for b in reads):
            writes = list(writes) + [b for b in reads if b.excl]
            reads = [b for b in reads if not b.excl]
        self._deps(eng, reads, writes)
        n = self.cnt[eng]
        key = (eng, n // SEG)
        val = n % SEG + 1
        fn(self.E[eng]).then_inc(self.sem(key), 1)
        self.cnt[eng] = n + 1
        self._mark((key, val), reads, writes)

    def dma(self, eng, out, in_, reads=(), writes=(), **kw):
        i = self.dma_i[eng]
        self.dma_i[eng] = i + 1
        key = ("d" + eng, i % NDMA)
        val = (i // NDMA + 1) * 16
        if val > 16:
            self._wait(eng, (key, val - 16))
        self._deps(eng, reads, writes)
        self.E[eng].dma_start(out=out, in_=in_, **kw).then_inc(self.sem(key), 16)
        self.last_dma[key] = val
        self._mark((key, val), reads, writes)

    def barrier(self):
        toks = []
        for e in ENGS:
            n = self.cnt[e]
            if n:
                toks.append(((e, (n - 1) // SEG), (n - 1) % SEG + 1))
        toks += list(self.last_dma.items())
        for e in ENGS:
            for t in toks:
                self._wait(e, t)


def build(T, DEPTH, NSEQ, dbg=None):
    nc = bass.Bass("TRN2", target_bir_lowering=False)
    P = Prog(nc)
    NB = T // 512
    NT = T // 128
    NCH = T // 64
    dr = {}

    def din(name, shape):
        dr[name] = nc.dram_tensor(name, list(shape), F32, kind="ExternalInput").ap()

    din("xT", [NSEQ, D, T]); din("cT", [D, NSEQ]); din("consts", [128, NCON]); din("rope", [32, 2, T])
    din("cols", [4, 128, NCOLS]); din("w_in", [4, D, 1704]); din("mla_w_uq", [4, 256, 768])
    din("mla_w_ukv", [4, 128, 1024]); din("gdn_a_log", [4, 4]); din("gdn_dt_bias", [4, 4])
    din("pool_w", [4, 4, 64, 64]); din("w_out", [4, D, D]); din("w_mod", [4, D, 6 * D])
    din("w_r", [4, D, 36]); din("b_r", [4, 36])
    din("moe_w_gate", [4, NEXP, D, 256]); din("moe_w_up", [4, NEXP, D, 256]); din("moe_w_down", [4, NEXP, 256, D])
    yT = nc.dram_tensor("yT", [NSEQ, D, T], F32, kind="ExternalOutput").ap()
    scr = nc.dram_tensor("scr_wt", [NEXP, T], F32).ap()
    bscr = Buf("scr")
    wbf = {"w_in": nc.dram_tensor("bf_w_in", [4, D, 1704], BF16).ap(),
           "w_out": nc.dram_tensor("bf_w_out", [4, D, D], BF16).ap(),
           "mla_w_uq": nc.dram_tensor("bf_w_uq", [4, 256, 768], BF16).ap(),
           "mla_w_ukv": nc.dram_tensor("bf_w_ukv", [4, 128, 1024], BF16).ap()}
    bwbf = {k: [Buf() for _ in range(4)] for k in wbf}
    if dbg:
        dbg_out = nc.dram_tensor("dbg", [D, T], F32, kind="ExternalOutput").ap()

    uid = [0]

    def sb(name, shape, dt=F32):
        uid[0] += 1
        return nc.sbuf_tensor("%s_%d" % (name, uid[0]), list(shape), dt)

    @contextlib.contextmanager
    def ps(name, shape, dt=F32):
        uid[0] += 1
        isz = 4 if dt == F32 else 2
        per = 2048 // isz
        n = 1
        for d_ in shape[1:]:
            n *= d_
        nb = (n + per - 1) // per
        with nc.psum_tensor("%s_%d" % (name, uid[0]), [128, nb * per], dt) as t:
            v = t[0:shape[0], 0:n]
            if len(shape) == 3:
                v = v.rearrange("p (a b) -> p a b", a=shape[1])
            elif len(shape) == 4:
                v = v.rearrange("p (a b c) -> p a b c", a=shape[1], b=shape[2])
            yield v

    es = contextlib.ExitStack()
    with es:
        cst = es.enter_context(sb("cst", [128, NCON])); bcst = Buf()
        idb = es.enter_context(sb("idb", [128, 128], BF16))
        oneb = es.enter_context(sb("oneb", [128, 128], BF16))
        colsT = es.enter_context(sb("colsT", [128, DEPTH, NCOLS])); bcols = Buf()
        modT = es.enter_context(sb("modT", [128, DEPTH, 48, NSEQ])); bmod = Buf()
        xT = es.enter_context(sb("xT_sb", [128, 8, T])); bx = [Buf() for _ in range(8)]
        hT = es.enter_context(sb("hT_sb", [128, 8, T], BF16)); bh = [Buf() for _ in range(8)]
        mixT = es.enter_context(sb("mixT_sb", [128, 8, T], BF16)); bmix = [Buf() for _ in range(8)]

        P.dma("sp", cst[:], dr["consts"][:, :], writes=[bcst])
        for l in range(DEPTH):
            P.dma("sp", colsT[:, l, :], dr["cols"][l, :, :], writes=[bcols])
        P.op("dve", lambda E: E.tensor_copy(idb[:], cst[:, C_ID:C_ID + 128]), reads=[bcst], writes=[bcst])
        P.op("dve", lambda E: E.tensor_copy(oneb[:], cst[:, C_ONE:C_ONE + 128]), reads=[bcst], writes=[bcst])
        for l in range(DEPTH):
            for k_ in ("w_in", "mla_w_uq", "mla_w_ukv", "w_out"):
                rows = dr[k_].shape[1]
                for r0 in range(0, rows, 512):
                    r1 = min(rows, r0 + 512)
                    P.dma("pool", wbf[k_][l, r0:r1, :], dr[k_][l, r0:r1, :], writes=[bwbf[k_][l]])
        ident = cst[:, C_ID:C_ID + 128]
        ones = cst[:, C_ONE:C_ONE + 128]

        def col(l, k):
            return colsT[:, l, k:k + 1]

        with contextlib.ExitStack() as _S:
            scT = _S.enter_context(sb("scT", [128, 8, NSEQ]))
            sgT = _S.enter_context(sb("sgT", [128, 8, NSEQ]))
            wm0 = _S.enter_context(sb("wm0", [128, 8, 768]))
            wm1 = _S.enter_context(sb("wm1", [128, 8, 768]))
            modps = _S.enter_context(ps("modps", [128, 48, NSEQ]))
            bsc = Buf(); bwm = [Buf(), Buf()]; bmp = PB()
            wms = [wm0, wm1]
            P.dma("sp", scT[:], dr["cT"].rearrange("(k p) s -> p k s", p=128), writes=[bsc])
            P.op("act", lambda E: E.activation(sgT[:], scT[:], AF.Sigmoid), reads=[bsc], writes=[bsc])
            P.op("dve", lambda E: E.tensor_tensor(scT[:], scT[:], sgT[:], ALU.mult), reads=[bsc], writes=[bsc])
            ci = 0
            for l in range(DEPTH):
                for c in range(8):
                    wm = wms[ci % 2]; bw = bwm[ci % 2]; ci += 1
                    P.dma("sp", wm[:], dr["w_mod"][l, :, c * 768:(c + 1) * 768].rearrange("(k p) n -> p k n", p=128),
                          writes=[bw])
                    for mm in range(6):
                        m = c * 6 + mm
                        for kt in range(8):
                            P.op("pe", lambda E: E.matmul(modps[:, m, :], wm[:, kt, mm * 128:(mm + 1) * 128],
                                                          scT[:, kt, :], start=(kt == 0), stop=(kt == 7)),
                                 reads=[bw, bsc], writes=[bmp])
                P.op("dve", lambda E: E.tensor_tensor(
                    modT[:, l, :, :], modps[:],
                    colsT[:, l, K_BMOD:K_BMOD + 48].unsqueeze(2).to_broadcast([128, 48, NSEQ]), ALU.add),
                    reads=[bmp, bcols], writes=[bmod])
                for a in (8, 32):
                    P.op("dve", lambda E: E.tensor_scalar_add(modT[:, l, a:a + 16, :], modT[:, l, a:a + 16, :], 1.0),
                         reads=[bmod], writes=[bmod])
        P.barrier()

        def modc(l, chunk, kt, s):
            return modT[:, l, chunk * 8 + kt, s:s + 1]

        def layer_norm(l, kg, kb):
            with contextlib.ExitStack() as _S:
                p_s = _S.enter_context(ps("ln_s", [128, 512]))
                p_q = _S.enter_context(ps("ln_q", [128, 512]))
                sq = _S.enter_context(sb("ln_sq", [128, 2, 512]))
                mean = _S.enter_context(sb("ln_mean", [128, 512]))
                rstd = _S.enter_context(sb("ln_rstd", [128, 512]))
                tt = _S.enter_context(sb("ln_t", [128, 2, 512]))
                bps, bpq, bsq, bmean, brstd, btt = PB(), PB(), [Buf(), Buf()], Buf(), Buf(), [Buf(), Buf()]
                for b in range(NB):
                    sl = slice(b * 512, (b + 1) * 512)
                    for kt in range(8):
                        P.op("pe", lambda E: E.matmul(p_s[:], ones, xT[:, kt, sl], start=(kt == 0), stop=(kt == 7)),
                             reads=[bx[kt], bcst], writes=[bps])
                    for kt in range(8):
                        j = kt % 2
                        P.op("act", lambda E: E.activation(sq[:, j, :], xT[:, kt, sl], AF.Square),
                             reads=[bx[kt]], writes=[bsq[j]])
                        P.op("pe", lambda E: E.matmul(p_q[:], ones, sq[:, j, :], start=(kt == 0), stop=(kt == 7)),
                             reads=[bsq[j], bcst], writes=[bpq])
                    P.op("act", lambda E: E.mul(mean[:], p_s[:], 1.0 / D), reads=[bps], writes=[bmean])
                    P.op("dve", lambda E: E.tensor_tensor(rstd[:], mean[:], mean[:], ALU.mult),
                         reads=[bmean], writes=[brstd])
                    P.op("dve", lambda E: E.scalar_tensor_tensor(rstd[:], p_q[:], 1.0 / D, rstd[:], ALU.mult,
                                                                 ALU.subtract), reads=[bpq, brstd], writes=[brstd])
                    P.op("act", lambda E: E.activation(rstd[:], rstd[:], AF.Sqrt, bias=LN_EPS, scale=1.0),
                         reads=[brstd], writes=[brstd])
                    P.op("dve", lambda E: E.reciprocal(rstd[:], rstd[:]), reads=[brstd], writes=[brstd])
                    for kt in range(8):
                        j = kt % 2
                        P.op("pool", lambda E: E.tensor_tensor(tt[:, j, :], xT[:, kt, sl], mean[:], ALU.subtract),
                             reads=[bx[kt], bmean], writes=[btt[j]])
                        P.op("dve", lambda E: E.tensor_tensor(tt[:, j, :], tt[:, j, :], rstd[:], ALU.mult),
                             reads=[btt[j], brstd], writes=[btt[j]])
                        P.op("dve", lambda E: E.tensor_scalar(xT[:, kt, sl], tt[:, j, :], col(l, kg + kt),
                                                              col(l, kb + kt), ALU.mult, ALU.add),
                             reads=[btt[j], bcols], writes=[bx[kt]])
            P.barrier()

        def modulate(l, s, sh_chunk, sc_chunk):
            for kt in range(8):
                P.op("act", lambda E: E.activation(hT[:, kt, :], xT[:, kt, :], AF.Identity,
                                                   bias=modc(l, sh_chunk, kt, s), scale=modc(l, sc_chunk, kt, s)),
                     reads=[bx[kt], bmod], writes=[bh[kt]])

        def load_w_in(wt, bw, l, c0, c1):
            P.dma("sp", wt[:], wbf["w_in"][l, :, c0:c1].rearrange("(k p) n -> p k n", p=128),
                  reads=[bwbf["w_in"][l]], writes=[bw])

        def pool_stage(l, s):
            with contextlib.ExitStack() as _S:
                pw = _S.enter_context(sb("pw", [128, 8, 256], BF16))
                pbd = _S.enter_context(sb("pbd", [128, 2, 128], BF16))
                px = _S.enter_context(sb("px", [128, 16 + T]))
                pa = _S.enter_context(sb("pa", [128, 16 + T]))
                pb = _S.enter_context(sb("pb", [128, 16 + T]))
                pdl = _S.enter_context(sb("pdl", [128, T], BF16))
                ptmp = _S.enter_context(sb("ptmp", [128, 16]))
                pps0 = _S.enter_context(ps("pps0", [128, 512]))
                pps1 = _S.enter_context(ps("pps1", [128, 512]))
                bpw, bpbd, bpx, bpa, bpb, bpdl, bptmp = Buf(), Buf(), Buf(), Buf(), Buf(), Buf(), Buf()
                pps = [pps0, pps1]; bpps = [PB(), PB()]
                load_w_in(pw, bpw, l, 1448, 1704)
                P.op("pool", lambda E: E.memset(pbd[:], 0.0), writes=[bpbd])
                for g in range(4):
                    j, r = g // 2, g % 2
                    P.dma("pool", pbd[64 * r:64 * r + 64, j, 64 * r:64 * r + 64], dr["pool_w"][l, g, :, :],
                          writes=[bpbd])
                for t in (px, pa, pb):
                    P.op("pool", lambda E: E.memset(t[:, 0:16], 0.0), writes=[bpx, bpa, bpb])
                k = 0
                for j in range(2):
                    for b in range(NB):
                        pp = pps[k % 2]; bpp = bpps[k % 2]; k += 1
                        sl = slice(b * 512, (b + 1) * 512)
                        for kt in range(8):
                            P.op("pe", lambda E: E.matmul(pp[:], pw[:, kt, j * 128:(j + 1) * 128], hT[:, kt, sl],
                                                          start=(kt == 0), stop=(kt == 7)),
                                 reads=[bpw, bh[kt]], writes=[bpp])
                        P.op("act", lambda E: E.copy(px[:, 16 + b * 512:16 + (b + 1) * 512], pp[:]),
                             reads=[bpp], writes=[bpx])

                    def shift_add(dst, src, sh, bd, bs):
                        P.op("dve", lambda E: E.tensor_tensor(dst[:, 16:16 + T], src[:, 16:16 + T],
                                                              src[:, 16 - sh:16 - sh + T], ALU.add),
                             reads=[bs], writes=[bd])

                    def delta(src, bs, r, w):
                        rows = slice(64 * r, 64 * r + 64)
                        P.op("dve", lambda E: E.scalar_tensor_tensor(pdl[rows, :], src[rows, 16:16 + T], 1.0 / w,
                                                                     px[rows, 16:16 + T], ALU.mult, ALU.subtract),
                             reads=[bs, bpx], writes=[bpdl])
                        P.op("dve", lambda E: E.tensor_tensor(ptmp[rows, :], src[rows, 16:32],
                                                              cst[rows, C_INV16 + 16 * j:C_INV16 + 16 * j + 16],
                                                              ALU.mult), reads=[bs, bcst], writes=[bptmp])
                        P.op("dve", lambda E: E.tensor_tensor(pdl[rows, 0:16], ptmp[rows, :], px[rows, 16:32],
                                                              ALU.subtract), reads=[bptmp, bpx, bpdl], writes=[bpdl])

                    shift_add(pa, px, 1, bpa, bpx)
                    if j == 0:
                        delta(pa, bpa, 0, 2)
                    shift_add(pb, pa, 2, bpb, bpa)
                    if j == 0:
                        delta(pb, bpb, 1, 4)
                    else:
                        shift_add(pa, pb, 4, bpa, bpb)
                        delta(pa, bpa, 0, 8)
                        shift_add(pb, pa, 8, bpb, bpa)
                        delta(pb, bpb, 1, 16)
                    for b in range(NB):
                        pp = pps[k % 2]; bpp = bpps[k % 2]; k += 1
                        sl = slice(b * 512, (b + 1) * 512)
                        P.op("pe", lambda E: E.matmul(pp[:], pbd[:, j, :], pdl[:, sl], start=True, stop=True),
                             reads=[bpbd, bpdl], writes=[bpp])
                        P.op("act", lambda E: E.activation(mixT[:, 6 + j, sl], pp[:], AF.Identity, scale=col(l, K_PS + j)),
                             reads=[bpp, bcols], writes=[bmix[6 + j]])
            P.barrier()

        def mla_stage(l, s):
            with contextlib.ExitStack() as _S:
                wA = _S.enter_context(sb("wA", [128, 8, 416], BF16))
                wuq = _S.enter_context(sb("wuq", [128, 2, 768], BF16))
                wuqr = _S.enter_context(sb("wuqr", [128, 2, 8, 32], BF16))
                wukv = _S.enter_context(sb("wukv", [128, 1024], BF16))
                wkr = _S.enter_context(sb("wkr", [128, 8, 32], BF16))
                cqn = _S.enter_context(sb("cqn", [128, 2, T], BF16))
                ckvn = _S.enter_context(sb("ckvn", [128, T], BF16))
                krope = _S.enter_context(sb("krope", [32, T], BF16))
                rp0 = _S.enter_context(sb("rp0", [32, 2, 512]))
                rp1 = _S.enter_context(sb("rp1", [32, 2, 512]))
                QT = _S.enter_context(sb("QT", [96, T], BF16))
                qrp = _S.enter_context(sb("qr", [32, 512], BF16))
                KT = _S.enter_context(sb("KT", [96, T], BF16))
                Vh = _S.enter_context(sb("Vh", [128, NT, 128], BF16))
                csb = _S.enter_context(sb("csb", [128, 3, 512]))
                sq = _S.enter_context(sb("sq", [128, 3, 512], BF16))
                rs = _S.enter_context(sb("rs", [128, 2, 512]))
                rt = _S.enter_context(sb("rt", [32, 2, 512]))
                pt0 = _S.enter_context(sb("pt0", [128, 512], BF16))
                pt1 = _S.enter_context(sb("pt1", [128, 512], BF16))
                pt2 = _S.enter_context(sb("pt2", [128, 512], BF16))
                rden = _S.enter_context(sb("rden", [64, 512]))
                bwA, bwuq, bwuqr, bwukv, bwkr, bcqn, bckvn, bkrope, brope = [Buf() for _ in range(9)]
                bQT, bqr, bKT, bVh, bcsb, bsq, brs, brt, brden = [Buf() for _ in range(9)]
                pts = [pt0, pt1, pt2]; bpts = [Buf(), Buf(), Buf()]
                load_w_in(wA, bwA, l, 0, 416)
                P.dma("sp", wuq[:], wbf["mla_w_uq"][l, :, :].rearrange("(j p) n -> p j n", p=128),
                      reads=[bwbf["mla_w_uq"][l]], writes=[bwuq])
                P.dma("sp", wukv[:], wbf["mla_w_ukv"][l, :, :], reads=[bwbf["mla_w_ukv"][l]], writes=[bwukv])
                rps = [rp0, rp1]; brps = [Buf(), Buf()]; rpi = [0]

                def load_rope(sl):
                    i = rpi[0] % 2; rpi[0] += 1
                    P.dma("sp", rps[i][:], dr["rope"][:, :, sl], writes=[brps[i]])
                    return rps[i], brps[i]
                P.op("pool", lambda E: E.tensor_scalar_mul(wkr[:, :, 0:16], wA[:, :, 400:416], -1.0),
                     reads=[bwA], writes=[bwkr])
                P.op("pool", lambda E: E.tensor_copy(wkr[:, :, 16:32], wA[:, :, 384:400]), reads=[bwA], writes=[bwkr])
                wq4 = wuq[:].rearrange("p j (h d) -> p j h d", d=96)
                P.op("pool", lambda E: E.tensor_scalar_mul(wuqr[:, :, :, 0:16], wq4[:, :, :, 80:96], -1.0),
                     reads=[bwuq], writes=[bwuqr])
                P.op("pool", lambda E: E.tensor_copy(wuqr[:, :, :, 16:32], wq4[:, :, :, 64:80]),
                     reads=[bwuq], writes=[bwuqr])
                P.op("pool", lambda E: E.memset(Vh[:, :, 64:128], 1.0), writes=[bVh])
                scale = 96.0 ** -0.5
                with contextlib.ExitStack() as _S:
                    m_a = _S.enter_context(ps("m_a", [128, 3, 512]))
                    m_s = _S.enter_context(ps("m_s", [128, 2, 512]))
                    m_r = _S.enter_context(ps("m_r", [32, 2, 512]))
                    bma, bms, bmr = PB(), PB(), PB()
                    for b in range(NB):
                        sl = slice(b * 512, (b + 1) * 512)
                        for j in range(3):
                            for kt in range(8):
                                P.op("pe", lambda E: E.matmul(m_a[:, j, :], wA[:, kt, j * 128:(j + 1) * 128],
                                                              hT[:, kt, sl], start=(kt == 0), stop=(kt == 7)),
                                     reads=[bwA, bh[kt]], writes=[bma])
                        P.op("act", lambda E: E.copy(csb[:], m_a[:]), reads=[bma], writes=[bcsb])
                        P.op("act", lambda E: E.activation(sq[:], m_a[:], AF.Square), reads=[bma], writes=[bsq])
                        P.op("pe", lambda E: E.matmul(m_s[:, 0, :], oneb[:], sq[:, 0, :], start=True, stop=False),
                             reads=[bsq, bcst], writes=[bms])
                        P.op("pe", lambda E: E.matmul(m_s[:, 0, :], oneb[:], sq[:, 1, :], start=False, stop=True),
                             reads=[bsq, bcst], writes=[bms])
                        P.op("pe", lambda E: E.matmul(m_s[:, 1, :], oneb[:], sq[:, 2, :], start=True, stop=True),
                             reads=[bsq, bcst], writes=[bms])
                        P.op("act", lambda E: E.activation(rs[:, 0, :], m_s[:, 0, :], AF.Sqrt, bias=RMS_EPS,
                                                           scale=1.0 / 256), reads=[bms], writes=[brs])
                        P.op("act", lambda E: E.activation(rs[:, 1, :], m_s[:, 1, :], AF.Sqrt, bias=RMS_EPS,
                                                           scale=1.0 / 128), reads=[bms], writes=[brs])
                        P.op("dve", lambda E: E.reciprocal(rs[:], rs[:]), reads=[brs], writes=[brs])
                        for j in range(2):
                            P.op("dve", lambda E: E.scalar_tensor_tensor(cqn[:, j, sl], csb[:, j, :],
                                                                         col(l, K_QN + j), rs[:, 0, :], ALU.mult,
                                                                         ALU.mult),
                                 reads=[bcsb, brs, bcols], writes=[bcqn])
                        P.op("dve", lambda E: E.scalar_tensor_tensor(ckvn[:, sl], csb[:, 2, :], col(l, K_KVN),
                                                                     rs[:, 1, :], ALU.mult, ALU.mult),
                             reads=[bcsb, brs, bcols], writes=[bckvn])
                        for kt in range(8):
                            P.op("pe", lambda E: E.matmul(m_r[:, 0, :], wA[:, kt, 384:416], hT[:, kt, sl],
                                                          start=(kt == 0), stop=(kt == 7)),
                                 reads=[bwA, bh[kt]], writes=[bmr])
                        for kt in range(8):
                            P.op("pe", lambda E: E.matmul(m_r[:, 1, :], wkr[:, kt, :], hT[:, kt, sl],
                                                          start=(kt == 0), stop=(kt == 7)),
                                 reads=[bwkr, bh[kt]], writes=[bmr])
                        rpt, brp = load_rope(sl)
                        P.op("dve", lambda E: E.tensor_tensor(rt[:], m_r[:], rpt[:], ALU.mult),
                             reads=[bmr, brp], writes=[brt])
                        P.op("dve", lambda E: E.tensor_tensor(krope[:, sl], rt[:, 0, :], rt[:, 1, :], ALU.add),
                             reads=[brt], writes=[bkrope])
                        P.op("act", lambda E: E.copy(KT[64:96, sl], krope[:, sl]), reads=[bkrope], writes=[bKT])
                    P.barrier()
                with contextlib.ExitStack() as _S:
                    h_q_full = _S.enter_context(ps("h_q", [128, 512]))
                    h_q = h_q_full[0:64, :]
                    h_r = _S.enter_context(ps("h_r", [32, 2, 512]))
                    h_v = _S.enter_context(ps("h_v", [128, 4, 128]))
                    a_s0 = _S.enter_context(ps("a_s0", [128, 512]))
                    a_s1 = _S.enter_context(ps("a_s1", [128, 512]))
                    a_of = _S.enter_context(ps("a_ob", [128, 512]))
                    a_o = a_of[0:64, :]
                    h_k = h_q
                    bhq, bhr, bhv, bao, bad = [PB() for _ in range(5)]
                    bhk = bhq
                    a_s = [a_s0, a_s1, h_q_full]; bas = [PB(), PB(), bhq]
                    pi = 0
                    for h in range(8):
                        for b in range(NB):
                            sl = slice(b * 512, (b + 1) * 512)
                            for j in range(2):
                                P.op("pe", lambda E: E.matmul(h_q[:], wuq[:, j, 96 * h:96 * h + 64], cqn[:, j, sl],
                                                              start=(j == 0), stop=(j == 1)),
                                     reads=[bwuq, bcqn], writes=[bhq])
                            P.op("act", lambda E: E.copy(QT[0:64, sl], h_q[:]), reads=[bhq], writes=[bQT])
                            for j in range(2):
                                P.op("pe", lambda E: E.matmul(h_r[:, 0, :], wuq[:, j, 96 * h + 64:96 * h + 96],
                                                              cqn[:, j, sl], start=(j == 0), stop=(j == 1)),
                                     reads=[bwuq, bcqn], writes=[bhr])
                            for j in range(2):
                                P.op("pe", lambda E: E.matmul(h_r[:, 1, :], wuqr[:, j, h, :], cqn[:, j, sl],
                                                              start=(j == 0), stop=(j == 1)),
                                     reads=[bwuqr, bcqn], writes=[bhr])
                            rpt, brp = load_rope(sl)
                            P.op("dve", lambda E: E.tensor_tensor(rt[:], h_r[:], rpt[:], ALU.mult),
                                 reads=[bhr, brp], writes=[brt])
                            P.op("dve", lambda E: E.tensor_tensor(qrp[:], rt[:, 0, :], rt[:, 1, :], ALU.add),
                                 reads=[brt], writes=[bqr])
                            P.op("act", lambda E: E.copy(QT[64:96, sl], qrp[:]), reads=[bqr], writes=[bQT])
                            P.op("pe", lambda E: E.matmul(h_k[:], wukv[:, 128 * h:128 * h + 64], ckvn[:, sl],
                                                          start=True, stop=True), reads=[bwukv, bckvn], writes=[bhk])
                            P.op("act", lambda E: E.copy(KT[0:64, sl], h_k[:]), reads=[bhk], writes=[bKT])
                            for tq in range(4):
                                tt_ = b * 4 + tq
                                P.op("pe", lambda E: E.matmul(h_v[:, tq, :], ckvn[:, tt_ * 128:(tt_ + 1) * 128],
                                                              wukv[:, 128 * h:128 * h + 128], start=True, stop=True),
                                     reads=[bwukv, bckvn], writes=[bhv])
                            P.op("act", lambda E: E.copy(Vh[:, b * 4:(b + 1) * 4, 0:64], h_v[:, :, 64:128]),
                                 reads=[bhv], writes=[bVh])
                        r = h % 2
                        steps = []
                        for g in range(NB):
                            for kb in range(4 * g + 4):
                                steps.append((g, kb, kb == 0, kb == 4 * g + 3))

                        def issue_S(i):
                            g, kb, first, last = steps[i]
                            sps = a_s[i % 3]; bsp = bas[i % 3]
                            qs = slice(g * 512, (g + 1) * 512); ks = slice(kb * 128, (kb + 1) * 128)
                            P.op("pe", lambda E: E.matmul(sps, KT[:, ks], QT[:, qs], start=True, stop=True),
                                 reads=[bKT, bQT], writes=[bsp])

                        def issue_rest(i):
                            g, kb, first, last = steps[i]
                            sps = a_s[i % 3]; bsp = bas[i % 3]
                            pt = pts[i % 3]; bpt = bpts[i % 3]
                            qs = slice(g * 512, (g + 1) * 512)
                            ii = max(kb - 4 * g, 0)
                            c0 = ii * 128
                            if not (dbg and dbg.get("x_b")):
                                P.op("act", lambda E: E.activation(pt[:, c0:512], sps[:, c0:512], AF.Exp, scale=scale),
                                     reads=[bsp], writes=[bpt])
                            if kb - 4 * g >= 0:
                                P.op("dve", lambda E: E.memset(pt[64:128, c0:c0 + 64], 0.0), writes=[bpt])
                            P.op("pe", lambda E: E.matmul(a_of[:, c0:512], Vh[:, kb, :], pt[:, c0:512], start=first,
                                                          stop=last), reads=[bVh, bpt], writes=[bao])
                            if last:
                                P.op("dve", lambda E: E.reciprocal(rden[:], a_of[64:128, :]), reads=[bao], writes=[brden])
                                P.op("dve", lambda E: E.tensor_tensor(mixT[64 * r:64 * r + 64, h // 2, qs], a_o,
                                                                      rden[:], ALU.mult),
                                     reads=[bao, brden], writes=[bmix[h // 2]])

                        if dbg and dbg.get("x_a"):
                            steps = []
                        else:
                            issue_S(0)
                            issue_S(1)
                        for i in range(len(steps)):
                            if i + 2 < len(steps):
                                issue_S(i + 2)
                            issue_rest(i)
            P.barrier()

        def gdn_stage(l, s):
            with contextlib.ExitStack() as _S:
                gw = _S.enter_context(sb("gw", [128, 8, 256], BF16))
                gwab = _S.enter_context(sb("gwab", [128, 8, 8], BF16))
                gqT = _S.enter_context(sb("gqT", [128, 2, T], BF16))
                gkT = _S.enter_context(sb("gkT", [128, 2, T], BF16))
                gvT = _S.enter_context(sb("gvT", [128, 2, T], BF16))
                gzT = _S.enter_context(sb("gzT", [128, 2, T], BF16))
                gpre = _S.enter_context(sb("gpre", [128, 3 + T]))
                gacc = _S.enter_context(sb("gacc", [128, 512]))
                gsig = _S.enter_context(sb("gsig", [128, 512]))
                grn = _S.enter_context(sb("grn", [128, 512]))
                gpar = _S.enter_context(sb("gpar", [64, 12]))
                bgw, bgwab, bgq, bgk, bgv, bgz, bgpre, bgacc, bgsig, bgrn, bgpar = [Buf() for _ in range(11)]
                load_w_in(gwab, bgwab, l, 1440, 1448)
                P.dma("sp", gpar[:, 0:4], dr["gdn_a_log"][l:l + 1, :].partition_broadcast(64), writes=[bgpar])
                P.dma("sp", gpar[:, 4:8], dr["gdn_dt_bias"][l:l + 1, :].partition_broadcast(64), writes=[bgpar])
                P.op("act", lambda E: E.activation(gpar[:, 8:12], gpar[:, 0:4], AF.Exp), reads=[bgpar], writes=[bgpar])
                P.op("dve", lambda E: E.tensor_scalar_mul(gpar[:, 8:12], gpar[:, 8:12], -1.0), reads=[bgpar],
                     writes=[bgpar])
                P.op("pool", lambda E: E.memset(gpre[:, 0:3], 0.0), writes=[bgpre])
                bd = cst[:, C_BD:C_BD + 128]
                with contextlib.ExitStack() as _S:
                    g_p0 = _S.enter_context(ps("g_p0", [128, 512]))
                    g_p1 = _S.enter_context(ps("g_p1", [128, 512]))
                    g_ss = _S.enter_context(ps("g_ss", [128, 512]))
                    gps_ = [g_p0, g_p1]; bgps = [PB(), PB()]; bgss = PB()
                    k = 0
                    for gi, (c0, dst, bdst) in enumerate(((416, gqT, bgq), (672, gkT, bgk), (928, gvT, bgv),
                                                           (1184, gzT, bgz))):
                        load_w_in(gw, bgw, l, c0, c0 + 256)
                        for j in range(2):
                            if gi == 3:
                                for b in range(NB):
                                    sl = slice(b * 512, (b + 1) * 512)
                                    pp = gps_[k % 2]; bpp = bgps[k % 2]; k += 1
                                    for kt in range(8):
                                        P.op("pe", lambda E: E.matmul(pp[:], gw[:, kt, j * 128:(j + 1) * 128],
                                                                      hT[:, kt, sl], start=(kt == 0), stop=(kt == 7)),
                                             reads=[bgw, bh[kt]], writes=[bpp])
                                    P.op("act", lambda E: E.activation(gsig[:], pp[:], AF.Sigmoid), reads=[bpp],
                                         writes=[bgsig])
                                    P.op("dve", lambda E: E.tensor_tensor(dst[:, j, sl], pp[:], gsig[:], ALU.mult),
                                         reads=[bpp, bgsig], writes=[bdst])
                                continue
                            for b in range(NB):
                                sl = slice(b * 512, (b + 1) * 512)
                                pp = gps_[k % 2]; bpp = bgps[k % 2]; k += 1
                                for kt in range(8):
                                    P.op("pe", lambda E: E.matmul(pp[:], gw[:, kt, j * 128:(j + 1) * 128],
                                                                  hT[:, kt, sl], start=(kt == 0), stop=(kt == 7)),
                                         reads=[bgw, bh[kt]], writes=[bpp])
                                P.op("act", lambda E: E.copy(gpre[:, 3 + b * 512:3 + (b + 1) * 512], pp[:]),
                                     reads=[bpp], writes=[bgpre])
                            ct = gi * 2 + j
                            for b in range(NB):
                                sl = slice(b * 512, (b + 1) * 512)
                                P.op("dve", lambda E: E.tensor_scalar(gacc[:], gpre[:, b * 512:b * 512 + 512],
                                                                      col(l, K_CONV + ct * 4), None, ALU.mult),
                                     reads=[bgpre, bcols], writes=[bgacc])
                                for tap in range(1, 4):
                                    P.op("dve", lambda E: E.scalar_tensor_tensor(
                                        gacc[:], gpre[:, b * 512 + tap:b * 512 + tap + 512],
                                        col(l, K_CONV + ct * 4 + tap), gacc[:], ALU.mult, ALU.add),
                                        reads=[bgpre, bcols, bgacc], writes=[bgacc])
                                P.op("act", lambda E: E.activation(gsig[:], gacc[:], AF.Sigmoid), reads=[bgacc],
                                     writes=[bgsig])
                                if gi == 2:
                                    P.op("dve", lambda E: E.tensor_tensor(dst[:, j, sl], gacc[:], gsig[:], ALU.mult),
                                         reads=[bgacc, bgsig], writes=[bdst])
                                    continue
                                P.op("dve", lambda E: E.tensor_tensor(gacc[:], gacc[:], gsig[:], ALU.mult),
                                     reads=[bgacc, bgsig], writes=[bgacc])
                                P.op("act", lambda E: E.activation(gsig[:], gacc[:], AF.Square), reads=[bgacc],
                                     writes=[bgsig])
                                P.op("pe", lambda E: E.matmul(g_ss[:], bd, gsig[:], start=True, stop=True),
                                     reads=[bgsig, bcst], writes=[bgss])
                                P.op("act", lambda E: E.activation(grn[:], g_ss[:], AF.Sqrt, bias=RMS_EPS, scale=1.0),
                                     reads=[bgss], writes=[bgrn])
                                P.op("dve", lambda E: E.reciprocal(grn[:], grn[:]), reads=[bgrn], writes=[bgrn])
                                P.op("dve", lambda E: E.scalar_tensor_tensor(dst[:, j, sl], gacc[:],
                                                                             0.125 if gi == 0 else 1.0, grn[:],
                                                                             ALU.mult, ALU.mult),
                                     reads=[bgacc, bgrn], writes=[bdst])
                P.barrier()
                U = cst[0:64, C_U:C_U + 64]
                MLO = cst[0:64, C_MLO:C_MLO + 64]
                MUP = cst[0:64, C_MUP:C_MUP + 64]
                with contextlib.ExitStack() as _S:
                    sm = _S.enter_context(sb("c_sm", [64, 64]))
                    sm_b = _S.enter_context(sb("d_sm", [64, 64]))
                    Gd = _S.enter_context(sb("c_Gd", [64, 4, 64]))
                    Dd = _S.enter_context(sb("c_D", [64, 4, 64]))
                    Ee = _S.enter_context(sb("c_E", [64, 4, 64]))
                    Et = _S.enter_context(sb("c_Et", [64, 4, 64]))
                    EGB = _S.enter_context(sb("c_EGB", [128, 256]))
                    tA = _S.enter_context(sb("c_tA", [64, 4, 64]))
                    A0 = _S.enter_context(sb("c_A0", [64, 4, 64], BF16))
                    A1 = _S.enter_context(sb("c_A1", [64, 4, 64], BF16))
                    B0 = _S.enter_context(sb("c_B0", [64, 4, 64], BF16))
                    B1 = _S.enter_context(sb("c_B1", [64, 4, 64], BF16))
                    Pm = _S.enter_context(sb("c_P", [64, 4, 64], BF16))
                    Pm_b = _S.enter_context(sb("d_P", [64, 4, 64], BF16))
                    qkT = _S.enter_context(sb("c_qk", [64, 4, 64], BF16))
                    qkT_b = _S.enter_context(sb("d_qk", [64, 4, 64], BF16))
                    vtm = _S.enter_context(sb("c_vtm", [64, 4, 64]))
                    vtm_b = _S.enter_context(sb("d_vtm", [64, 4, 64]))
                    KDP = _S.enter_context(sb("c_KDP", [64, 4, 128], BF16))
                    KDP_b = _S.enter_context(sb("d_KDP", [64, 4, 128], BF16))
                    VNP = _S.enter_context(sb("c_VNP", [64, 4, 128], BF16))
                    Rb = _S.enter_context(sb("c_R", [64, 4, 64], BF16))
                    t1 = _S.enter_context(sb("c_t1", [64, 4, 64]))
                    qdT = _S.enter_context(sb("c_qd", [128, 2, 64], BF16))
                    qdT_b = _S.enter_context(sb("d_qd", [128, 2, 64], BF16))
                    SP = _S.enter_context(sb("c_SP", [128, 2, 128]))
                    SPb = _S.enter_context(sb("c_SPb", [128, 2, 128], BF16))
                    glc = _S.enter_context(sb("c_gl", [128, 2]))
                    glc_b = _S.enter_context(sb("d_gl", [128, 2]))
                    osq = _S.enter_context(sb("c_osq", [128, 2, 64]))
                    ors = _S.enter_context(sb("c_ors", [128, 2, 64]))
                    otm = _S.enter_context(sb("c_otm", [128, 2, 64]))
                    kmisc = _S.enter_context(ps("k_misc", [128, 512]))
                    kgcb = _S.enter_context(ps("k_gcb", [128, 256]))
                    kgk = _S.enter_context(ps("k_gk", [64, 2, 4, 64]))
                    kpv = _S.enter_context(ps("k_pv", [64, 2, 4, 64]))
                    ktr = _S.enter_context(ps("k_tr", [64, 3, 256], BF16))
                    kps1v = _S.enter_context(ps("k_ps1", [64, 2, 4, 64]))
                    kps1 = kps1v[:, 0, :, :]
                    ko = _S.enter_context(ps("k_o", [128, 512]))
                    ksq = kgk
                    kU = _S.enter_context(ps("k_U", [128, 2, 128]))
                    (bsm, bGd, bD, bE, bEt, bEGB, btA, bP, bqk, bvtm, bKDP, bVNP, bR, bt1, bqd, bSP, bSPb, bgl, bosq,
                     bors, botm) = [Buf() for _ in range(21)]
                    bA = [Buf(), Buf()]; bB = [Buf(), Buf()]
                    b_ab = PB(); b_gcc = b_ab; b_o = PB(); b_ss = b_o
                    b_gcb = PB(); b_G = PB(); b_KQ = b_G; b_sq = b_G; b_pu = PB()
                    b_trB = PB(); b_trv = b_trB; b_trk = b_trB; b_ps1 = PB(); b_vn = b_ps1; b_U = PB()
                    As = [A0, A1]; Bs = [B0, B1]
                    ab_ps = kmisc[0:64, 0:8]; gcc_ps = kmisc[0:64, 8:12]
                    ss_ps = ko[:, 64:192].rearrange("p (j c) -> p j c", j=2)
                    o_ps = ko[:, 256:384].rearrange("p (j c) -> p j c", j=2)
                    idb64 = idb[0:64, 0:64]
                    kz = _S.enter_context(sb("c_kz", [128, 2, 4, 64], BF16)); bkz = Buf()
                    P.op("pool", lambda E: E.memset(kz[:], 0.0), writes=[bkz])
                    sm2 = [sm, sm_b]; Pm2 = [Pm, Pm_b]; qk2 = [qkT, qkT_b]; vtm2 = [vtm, vtm_b]; KDP2 = [KDP, KDP_b]
                    qd2 = [qdT, qdT_b]; gl2 = [glc, glc_b]
                    bsm2, bP2, bqk2, bvtm2, bKDP2, bqd2, bgl2 = [[Buf(), Buf()] for _ in range(7)]
                    for i_ in range(2):
                        P.op("pool", lambda E: E.memset(KDP2[i_][:], 0.0), writes=[bKDP2[i_]])
                    P.op("pool", lambda E: E.memset(VNP[:], 0.0), writes=[bVNP])
                    P.op("pool", lambda E: E.memset(SP[:], 0.0), writes=[bSP])
                    P.op("pool", lambda E: E.memset(SPb[:], 0.0), writes=[bSPb])
                    g4, bt4, gcc, egc, ekd, tm4, sp4 = [sm[:, 4 * i:4 * i + 4] for i in range(7)]

                    def bc_h(ap4):
                        return ap4.unsqueeze(2).to_broadcast([64, 4, 64])

                    def bc_m(ap):
                        return ap.unsqueeze(1).to_broadcast([64, 4, 64])

                    opc = [0]
                    maxops = dbg.get("maxops", 10 ** 9) if dbg else 10 ** 9

                    def GOP(eng, fn, rd, wrt):
                        opc[0] += 1
                        if opc[0] <= maxops:
                            P.op(eng, fn, reads=rd, writes=wrt)

                    V = lambda fn, rd, wrt: GOP("dve", fn, rd, wrt)
                    AC = lambda fn, rd, wrt: GOP("act", fn, rd, wrt)
                    PE = lambda fn, rd, wrt: GOP("pe", fn, rd, wrt)
                    def prep(c):
                        par = c % 2
                        cs_ = slice(c * 64, (c + 1) * 64)
                        sm = sm2[par]; bsm = bsm2[par]; Pm = Pm2[par]; bP = bP2[par]; qkT = qk2[par]; bqk = bqk2[par]
                        vtm = vtm2[par]; bvtm = bvtm2[par]; KDP = KDP2[par]; bKDP = bKDP2[par]
                        qdT = qd2[par]; bqd = bqd2[par]; glc = gl2[par]; bgl = bgl2[par]
                        g4, bt4, gcc, egc, ekd, tm4, sp4 = [sm[:, 4 * i:4 * i + 4] for i in range(7)]
                        for kt in range(8):
                            yield PE(lambda E: E.matmul(ab_ps, hT[:, kt, cs_], gwab[:, kt, :], start=(kt == 0),
                                                  stop=(kt == 7)), [bh[kt], bgwab], [b_ab])
                        yield V(lambda E: E.tensor_tensor(sp4, ab_ps[:, 0:4], gpar[:, 4:8], ALU.add), [b_ab, bgpar], [bsm])
                        yield AC(lambda E: E.activation(sp4, sp4, AF.Exp), [bsm], [bsm])
                        yield AC(lambda E: E.activation(sp4, sp4, AF.Ln, bias=1.0, scale=1.0), [bsm], [bsm])
                        yield V(lambda E: E.tensor_tensor(g4, sp4, gpar[:, 8:12], ALU.mult), [bsm, bgpar], [bsm])
                        yield AC(lambda E: E.activation(bt4, ab_ps[:, 4:8], AF.Exp, scale=-1.0), [b_ab], [bsm])
                        yield V(lambda E: E.tensor_scalar_add(bt4, bt4, 1.0), [bsm], [bsm])
                        yield V(lambda E: E.reciprocal(bt4, bt4), [bsm], [bsm])
                        yield PE(lambda E: E.matmul(gcc_ps, U, g4, start=True, stop=True), [bsm, bcst], [b_gcc])
                        yield V(lambda E: E.tensor_copy(gcc, gcc_ps), [b_gcc], [bsm])
                        yield V(lambda E: E.tensor_tensor(Gd[:], bc_m(U), bc_h(g4), ALU.mult), [bsm, bcst], [bGd])
                        yield PE(lambda E: E.matmul(kgcb[:], cst[0:64, C_ONE:C_ONE + 128],
                                              Gd[:].rearrange("p h j -> p (h j)"), start=True, stop=True),
                           [bGd, bcst], [b_gcb])
                        gcb3 = kgcb[0:64, :].rearrange("p (h j) -> p h j", h=4)
                        yield V(lambda E: E.tensor_tensor(Dd[:], bc_h(gcc), gcb3, ALU.subtract), [bsm, b_gcb], [bD])
                        yield V(lambda E: E.tensor_tensor(Ee[:], Dd[:], bc_m(MLO), ALU.add), [bD, bcst], [bE])
                        yield V(lambda E: E.tensor_tensor(Et[:], bc_m(MUP), Dd[:], ALU.subtract), [bD, bcst], [bEt])
                        yield AC(lambda E: E.activation(Ee[:], Ee[:], AF.Exp), [bE], [bE])
                        yield AC(lambda E: E.activation(Et[:], Et[:], AF.Exp), [bEt], [bEt])
                        yield AC(lambda E: E.activation(EGB[:], kgcb[:], AF.Exp), [b_gcb], [bEGB])
                        yield AC(lambda E: E.activation(egc, gcc, AF.Exp), [bsm], [bsm])
                        yield V(lambda E: E.tensor_tensor(tm4, gcb3[:, :, 63], gcc, ALU.subtract), [b_gcb, bsm], [bsm])
                        yield AC(lambda E: E.activation(ekd, tm4, AF.Exp), [bsm], [bsm])
                        EG4 = EGB[:].rearrange("p (j r c) -> p j r c", j=2, r=2)
                        yield V(lambda E: E.tensor_copy(glc[0:64, :], EG4[0:64, :, 0, 63]), [bEGB], [bgl])
                        yield V(lambda E: E.tensor_copy(glc[64:128, :], EG4[64:128, :, 1, 63]), [bEGB], [bgl])
                        for h in range(4):
                            j, r = h // 2, h % 2
                            rows = slice(64 * r, 64 * r + 64)
                            yield GOP("pool", lambda E: E.tensor_copy(kz[rows, 0, h, :], gkT[rows, j, cs_]), [bgk], [bkz])
                            yield GOP("pool", lambda E: E.tensor_copy(kz[rows, 1, h, :], gqT[rows, j, cs_]), [bgq], [bkz])
                        for h in range(4):
                            j, r = h // 2, h % 2
                            yield PE(lambda E: E.matmul(kgk[:, 0, h, :], gkT[:, j, cs_], kz[:, 0, h, :], start=True,
                                                  stop=True), [bgk, bkz], [b_G])
                            yield PE(lambda E: E.matmul(kgk[:, 1, h, :], gkT[:, j, cs_], kz[:, 1, h, :], start=True,
                                                  stop=True), [bgk, bkz], [b_KQ])
                        a = 0
                        yield V(lambda E: E.tensor_tensor(tA[:], kgk[:, 0, :, :], Ee[:], ALU.mult), [b_G, bE], [btA])
                        yield V(lambda E: E.tensor_tensor(As[a][:], tA[:], bc_h(bt4), ALU.mult), [btA, bsm], [bA[a]])
                        yield V(lambda E: E.tensor_tensor(qkT[:], kgk[:, 1, :, :], Et[:], ALU.mult), [b_KQ, bEt], [bqk])
                        trB = ktr[:, 0, :].rearrange("p (h i) -> p h i", h=4)
                        for h in range(4):
                            yield PE(lambda E: E.transpose(trB[:, h, :], As[a][:, h, :], idb64), [bA[a], bcst], [b_trB])
                        yield V(lambda E: E.tensor_copy(Bs[a][:], trB), [b_trB], [bB[a]])
                        yield V(lambda E: E.tensor_tensor(Pm[:], bc_m(idb64), Bs[a][:], ALU.subtract), [bB[a], bcst], [bP])
                        for lev in range(5):
                            n = 1 - a
                            for h in range(4):
                                yield PE(lambda E: E.matmul(ksq[:, 0, h, :], Bs[a][:, h, :], As[a][:, h, :], start=True,
                                                      stop=True), [bA[a], bB[a]], [b_sq])
                                yield PE(lambda E: E.matmul(ksq[:, 1, h, :], As[a][:, h, :], Bs[a][:, h, :], start=True,
                                                      stop=True), [bA[a], bB[a]], [b_sq])
                            yield AC(lambda E: E.copy(As[n][:], ksq[:, 0, :, :]), [b_sq], [bA[n]])
                            yield V(lambda E: E.tensor_copy(Bs[n][:], ksq[:, 1, :, :]), [b_sq], [bB[n]])
                            a = n
                            for h in range(4):
                                yield PE(lambda E: E.matmul(kpv[:, 0, h, :], As[a][:, h, :], Pm[:, h, :], start=True,
                                                      stop=True), [bA[a], bP], [b_pu])
                            yield V(lambda E: E.tensor_tensor(Pm[:], Pm[:], kpv[:, 0, :, :], ALU.add), [bP, b_pu], [bP])
                        for j in range(2):
                            yield PE(lambda E: E.transpose(ktr[:, 1, j * 128:(j + 1) * 128], gvT[:, j, cs_], idb[:]),
                               [bgv, bcst], [b_trv])
                            yield PE(lambda E: E.transpose(ktr[:, 2, j * 128:(j + 1) * 128], gkT[:, j, cs_], idb[:]),
                               [bgk, bcst], [b_trk])
                        yield AC(lambda E: E.copy(vtm[:].rearrange("p h d -> p (h d)"), ktr[:, 1, :]), [b_trv], [bvtm])
                        k4 = ktr[:, 2, :].rearrange("p (j r d) -> p j r d", j=2, r=2)
                        KD5 = KDP[:].rearrange("p (j r) m -> p j r m", j=2)
                        ekd3 = ekd.rearrange("p (j r) -> p j r", j=2)
                        for r in range(2):
                            yield V(lambda E: E.tensor_tensor(KD5[:, :, r, 64 * r:64 * r + 64], k4[:, :, r, :],
                                                        ekd3[:, :, r].unsqueeze(2).to_broadcast([64, 2, 64]),
                                                        ALU.mult), [b_trk, bsm], [bKDP])
                        for h in range(4):
                            j, r = h // 2, h % 2
                            rows = slice(64 * r, 64 * r + 64)
                            yield GOP("pool", lambda E: E.tensor_tensor(qdT[rows, j, :], gqT[rows, j, cs_],
                                                                  EGB[rows, h * 64:(h + 1) * 64], ALU.mult),
                                [bgq, bEGB], [bqd])

                    def scan(c):
                        par = c % 2
                        cs_ = slice(c * 64, (c + 1) * 64)
                        sm = sm2[par]; bsm = bsm2[par]; Pm = Pm2[par]; bP = bP2[par]; qkT = qk2[par]; bqk = bqk2[par]
                        vtm = vtm2[par]; bvtm = bvtm2[par]; KDP = KDP2[par]; bKDP = bKDP2[par]
                        qdT = qd2[par]; bqd = bqd2[par]; glc = gl2[par]; bgl = bgl2[par]
                        g4, bt4, gcc, egc, ekd, tm4, sp4 = [sm[:, 4 * i:4 * i + 4] for i in range(7)]
                        for j in range(2):
                            yield PE(lambda E: E.matmul(kps1[:, 2 * j:2 * j + 2, :].rearrange("p a b -> p (a b)"),
                                                  gkT[:, j, cs_], SPb[:, j, :], start=True, stop=True),
                               [bgk, bSPb], [b_ps1])
                        yield V(lambda E: E.tensor_tensor(t1[:], kps1[:], bc_h(egc), ALU.mult), [b_ps1, bsm], [bt1])
                        yield V(lambda E: E.tensor_tensor(t1[:], vtm[:], t1[:], ALU.subtract), [bvtm, bt1], [bt1])
                        yield V(lambda E: E.tensor_tensor(Rb[:], t1[:], bc_h(bt4), ALU.mult), [bt1, bsm], [bR])
                        for h in range(4):
                            yield PE(lambda E: E.matmul(kps1v[:, 1, h, :], Pm[:, h, :], Rb[:, h, :], start=True, stop=True),
                               [bP, bR], [b_vn])
                        VN5 = VNP[:].rearrange("p (j r) m -> p j r m", j=2)
                        vn4 = kps1v[:, 1, :, :].rearrange("p (j r) d -> p j r d", j=2)
                        for r in range(2):
                            yield AC(lambda E: E.copy(VN5[:, :, r, 64 * r:64 * r + 64], vn4[:, :, r, :]), [b_vn], [bVNP])
                        for j in range(2):
                            yield PE(lambda E: E.matmul(o_ps[:, j, :], SPb[:, j, :], qdT[:, j, :], start=True, stop=False),
                               [bSPb, bqd], [b_o])
                            yield PE(lambda E: E.matmul(o_ps[:, j, :], VNP[:, 2 * j, :], qkT[:, 2 * j, :], start=False,
                                                  stop=False), [bVNP, bqk], [b_o])
                            yield PE(lambda E: E.matmul(o_ps[:, j, :], VNP[:, 2 * j + 1, :], qkT[:, 2 * j + 1, :],
                                                  start=False, stop=True), [bVNP, bqk], [b_o])
                        yield AC(lambda E: E.activation(osq[:], o_ps, AF.Square), [b_o], [bosq])
                        yield PE(lambda E: E.matmul(ss_ps.rearrange("p j c -> p (j c)"), bd,
                                              osq[:].rearrange("p j c -> p (j c)"), start=True, stop=True),
                           [bosq, bcst], [b_ss])
                        yield AC(lambda E: E.activation(ors[:], ss_ps, AF.Sqrt, bias=RMS_EPS, scale=1.0 / 64), [b_ss], [bors])
                        yield V(lambda E: E.reciprocal(ors[:], ors[:]), [bors], [bors])
                        yield V(lambda E: E.scalar_tensor_tensor(otm[:], o_ps, col(l, K_GO), ors[:], ALU.mult, ALU.mult),
                          [b_o, bors, bcols], [botm])
                        for j in range(2):
                            yield V(lambda E: E.tensor_tensor(mixT[:, 4 + j, cs_], otm[:, j, :], gzT[:, j, cs_], ALU.mult),
                              [botm, bgz], [bmix[4 + j]])
                        for j in range(2):
                            yield PE(lambda E: E.matmul(kU[:, j, :], KDP[:, 2 * j, :], VNP[:, 2 * j, :], start=True,
                                                  stop=False), [bKDP, bVNP], [b_U])
                            yield PE(lambda E: E.matmul(kU[:, j, :], KDP[:, 2 * j + 1, :], VNP[:, 2 * j + 1, :],
                                                  start=False, stop=True), [bKDP, bVNP], [b_U])
                        for j in range(2):
                            yield V(lambda E: E.scalar_tensor_tensor(SP[:, j, :], SP[:, j, :], glc[:, j:j + 1],
                                                               kU[:, j, :], ALU.mult, ALU.add),
                              [bSP, bgl, b_U], [bSP])
                        yield AC(lambda E: E.copy(SPb[:], SP[:]), [bSP], [bSPb])

                    def run_il(g1, g2, r1=(dbg or {}).get('r1', 1), r2=(dbg or {}).get('r2', 1)):
                        d1 = g1 is None
                        d2 = g2 is None
                        while not (d1 and d2):
                            for _ in range(r1):
                                if not d1:
                                    try:
                                        next(g1)
                                    except StopIteration:
                                        d1 = True
                            for _ in range(r2):
                                if not d2:
                                    try:
                                        next(g2)
                                    except StopIteration:
                                        d2 = True

                    NCHX = NCH if not (dbg and 'nch' in dbg) else dbg['nch']
                    if NCHX:
                        run_il(prep(0), None)
                    for c in range(NCHX):
                        run_il(prep(c + 1) if c + 1 < NCHX else None, scan(c))
            P.barrier()

        def out_proj(l, s):
            with contextlib.ExitStack() as _S:
                wo = _S.enter_context(sb("wo", [128, 8, D], BF16))
                o0 = _S.enter_context(ps("ops0", [128, 512]))
                o1 = _S.enter_context(ps("ops1", [128, 512]))
                bwo = Buf(); ops_ = [o0, o1]; bops = [PB(), PB()]
                P.dma("sp", wo[:], wbf["w_out"][l, :, :].rearrange("(k p) n -> p k n", p=128),
                      reads=[bwbf["w_out"][l]], writes=[bwo])
                for kt in range(8):
                    P.op("act", lambda E: E.mul(xT[:, kt, :], xT[:, kt, :], ALPHA), reads=[bx[kt]], writes=[bx[kt]])
                k = 0
                for b in range(NB):
                    sl = slice(b * 512, (b + 1) * 512)
                    for m in range(8):
                        pp = ops_[k % 2]; bpp = bops[k % 2]; k += 1
                        for kt in range(8):
                            P.op("pe", lambda E: E.matmul(pp[:], wo[:, kt, m * 128:(m + 1) * 128], mixT[:, kt, sl],
                                                          start=(kt == 0), stop=(kt == 7)),
                                 reads=[bwo, bmix[kt]], writes=[bpp])
                        P.op("dve", lambda E: E.scalar_tensor_tensor(xT[:, m, sl], pp[:], modc(l, 2, m, s),
                                                                     xT[:, m, sl], ALU.mult, ALU.add),
                             reads=[bpp, bmod, bx[m]], writes=[bx[m]])
            P.barrier()

        def moe_stage(l, s):
            with contextlib.ExitStack() as _S:
                wr = _S.enter_context(sb("wr", [128, 8, 36]))
                brb = _S.enter_context(sb("brb", [128, 36]))
                h2f = _S.enter_context(sb("h2f", [128, 8, 512]))
                Lg4 = [_S.enter_context(sb("Lg", [128, 36])) for _ in range(4)]
                sm4 = [_S.enter_context(sb("sm", [128, 16])) for _ in range(4)]
                msk4 = [_S.enter_context(sb("msk", [128, 3, 32])) for _ in range(4)]
                Wt4 = [_S.enter_context(sb("Wt", [128, 32])) for _ in range(4)]
                WtT = _S.enter_context(sb("WtT", [32, T]))
                rl4 = [_S.enter_context(ps("r_l", [128, 36])) for _ in range(4)]
                rt4 = [_S.enter_context(ps("r_t", [32, 128])) for _ in range(4)]
                bLg4, bsm4, bmsk4, bWt4 = [[Buf() for _ in range(4)] for _ in range(4)]
                brl4 = [PB() for _ in range(4)]; brt4 = [PB() for _ in range(4)]
                bwr, bbrb, bh2f, bLg, bsm, bmsk, bWt, bWtT = [Buf() for _ in range(8)]
                brl, brt_ = PB(), PB()
                P.dma("sp", wr[:], dr["w_r"][l, :, :].rearrange("(k p) n -> p k n", p=128), writes=[bwr])
                P.dma("sp", brb[:], dr["b_r"][l:l + 1, :].partition_broadcast(128), writes=[bbrb])
                for b in range(NB):
                    sl = slice(b * 512, (b + 1) * 512)
                    for kt in range(8):
                        P.op("act", lambda E: E.activation(h2f[:, kt, :], xT[:, kt, sl], AF.Identity,
                                                           bias=modc(l, 3, kt, s), scale=modc(l, 4, kt, s)),
                             reads=[bx[kt], bmod], writes=[bh2f])
                        P.op("pool", lambda E: E.tensor_copy(hT[:, kt, sl], h2f[:, kt, :]), reads=[bh2f],
                             writes=[bh[kt]])
                    def rt_tile(tq):
                        Lg, sm, msk, Wt, r_l, r_t = Lg4[tq], sm4[tq], msk4[tq], Wt4[tq], rl4[tq], rt4[tq]
                        bLg, bsm, bmsk, bWt, brl, brt_ = bLg4[tq], bsm4[tq], bmsk4[tq], bWt4[tq], brl4[tq], brt4[tq]
                        t0 = b * 512 + tq * 128
                        for kt in range(8):
                            yield P.op("pe", lambda E: E.matmul(r_l[:], h2f[:, kt, tq * 128:(tq + 1) * 128], wr[:, kt, :],
                                                          start=(kt == 0), stop=(kt == 7)),
                                 reads=[bh2f, bwr], writes=[brl])
                        V = lambda fn, rd, wrt: P.op("dve", fn, reads=rd, writes=wrt)
                        yield V(lambda E: E.tensor_tensor(Lg[:], r_l[:], brb[:], ALU.add), [brl, bbrb], [bLg])
                        yield V(lambda E: E.reduce_max(sm[:, 0:1], Lg[:, 0:4], AX.X), [bLg], [bsm])
                        yield V(lambda E: E.tensor_scalar_mul(sm[:, 1:2], sm[:, 0:1], -1.0), [bsm], [bsm])
                        yield V(lambda E: E.tensor_scalar(msk[:, 0, 0:4], Lg[:, 0:4], sm[:, 0:1], None, ALU.is_equal),
                          [bLg, bsm], [bmsk])
                        yield P.op("act", lambda E: E.activation(msk[:, 0, 8:12], Lg[:, 0:4], AF.Exp, bias=sm[:, 1:2],
                                                           scale=1.0, accum_out=sm[:, 2:3]),
                             reads=[bLg, bsm], writes=[bmsk, bsm])
                        yield V(lambda E: E.reciprocal(sm[:, 3:4], sm[:, 2:3]), [bsm], [bsm])
                        yield V(lambda E: E.tensor_scalar(msk[:, 0, 4:8], msk[:, 0, 0:4], -NEG, NEG, ALU.mult, ALU.add),
                          [bmsk], [bmsk])
                        yield V(lambda E: E.tensor_tensor(msk[:, 1, :].rearrange("p (g e) -> p g e", g=4),
                                                    Lg[:, 4:36].rearrange("p (g e) -> p g e", g=4),
                                                    msk[:, 0, 4:8].unsqueeze(2).to_broadcast([128, 4, 8]), ALU.add),
                          [bLg, bmsk], [bmsk])
                        yield V(lambda E: E.reduce_max(sm[:, 4:5], msk[:, 1, :], AX.X), [bmsk], [bsm])
                        yield V(lambda E: E.tensor_scalar(msk[:, 2, :], msk[:, 1, :], sm[:, 4:5], None, ALU.is_equal),
                          [bmsk, bsm], [bmsk])
                        yield V(lambda E: E.scalar_tensor_tensor(msk[:, 1, :], msk[:, 2, :], NEG, msk[:, 1, :], ALU.mult,
                                                           ALU.add), [bmsk], [bmsk])
                        yield V(lambda E: E.reduce_max(sm[:, 5:6], msk[:, 1, :], AX.X), [bmsk], [bsm])
                        yield V(lambda E: E.tensor_scalar(msk[:, 1, :], msk[:, 1, :], sm[:, 5:6], None, ALU.is_equal),
                          [bmsk, bsm], [bmsk])
                        yield V(lambda E: E.tensor_tensor(sm[:, 6:7], sm[:, 5:6], sm[:, 4:5], ALU.subtract), [bsm], [bsm])
                        yield P.op("act", lambda E: E.activation(sm[:, 7:8], sm[:, 6:7], AF.Exp), reads=[bsm], writes=[bsm])
                        yield V(lambda E: E.tensor_scalar_add(sm[:, 8:9], sm[:, 7:8], 1.0), [bsm], [bsm])
                        yield V(lambda E: E.reciprocal(sm[:, 8:9], sm[:, 8:9]), [bsm], [bsm])
                        yield V(lambda E: E.tensor_tensor(sm[:, 9:10], sm[:, 3:4], sm[:, 8:9], ALU.mult), [bsm], [bsm])
                        yield V(lambda E: E.tensor_tensor(sm[:, 10:11], sm[:, 9:10], sm[:, 7:8], ALU.mult), [bsm], [bsm])
                        yield V(lambda E: E.tensor_scalar(Wt[:], msk[:, 2, :], sm[:, 9:10], None, ALU.mult),
                          [bmsk, bsm], [bWt])
                        yield V(lambda E: E.scalar_tensor_tensor(Wt[:], msk[:, 1, :], sm[:, 10:11], Wt[:], ALU.mult,
                                                           ALU.add), [bmsk, bsm, bWt], [bWt])
                        yield P.op("pe", lambda E: E.transpose(r_t[:], Wt[:], ident), reads=[bWt, bcst], writes=[brt_])
                        yield P.op("act", lambda E: E.copy(WtT[:, t0:t0 + 128], r_t[:]), reads=[brt_], writes=[bWtT])
                    gens = [rt_tile(tq) for tq in range(4)]
                    while gens:
                        for g_ in list(gens):
                            try:
                                next(g_)
                            except StopIteration:
                                gens.remove(g_)
                P.dma("sp", scr[:, :], WtT[:], reads=[bWtT], writes=[bscr])
            P.barrier()
            for kt in range(8):
                P.op("act", lambda E: E.mul(xT[:, kt, :], xT[:, kt, :], ALPHA), reads=[bx[kt]], writes=[bx[kt]])
            TB = 256
            NTB = T // TB
            with contextlib.ExitStack() as _S:
                A_ = lambda nm, shp, dt=F32: _S.enter_context(sb(nm, shp, dt))
                stg_g, stg_u, stg_d = A_("stg_g", [128, 8, 256]), A_("stg_u", [128, 8, 256]), A_("stg_d", [128, 2, D])
                wg = [A_("wg0", [128, 8, 256], BF16), A_("wg1", [128, 8, 256], BF16)]
                wu = [A_("wu0", [128, 8, 256], BF16), A_("wu1", [128, 8, 256], BF16)]
                wd = [A_("wd0", [128, 2, D], BF16), A_("wd1", [128, 2, D], BF16)]
                wb = [A_("wb0", [128, T]), A_("wb1", [128, T])]
                ea = [A_("ea0", [128, 2, TB]), A_("ea1", [128, 2, TB])]
                eb = [A_("eb0", [128, 2, TB], BF16), A_("eb1", [128, 2, TB], BF16)]
                eg = [_S.enter_context(ps("eg0", [128, 2, TB])), _S.enter_context(ps("eg1", [128, 2, TB]))]
                eu = [_S.enter_context(ps("eu0", [128, 2, TB])), _S.enter_context(ps("eu1", [128, 2, TB]))]
                ey = _S.enter_context(ps("ey", [128, 8, TB]))
                bsg, bsu, bsd = Buf(), Buf(), Buf()
                bwg, bwu, bwd, bwb, bea, beb = [[Buf(), Buf()] for _ in range(6)]
                beg, beu = [[PB(), PB()] for _ in range(2)]
                beyb = [PB() for _ in range(4)]
                NE = NEXP if not (dbg and 'nexp' in dbg) else dbg['nexp']

                def dma_expert(e):
                    if e >= NE:
                        return
                    P.dma("sp", stg_g[:], dr["moe_w_gate"][l, e, :, :].rearrange("(k p) n -> p k n", p=128),
                          writes=[bsg])
                    P.dma("sp", stg_u[:], dr["moe_w_up"][l, e, :, :].rearrange("(k p) n -> p k n", p=128),
                          writes=[bsu])
                    P.dma("sp", stg_d[:], dr["moe_w_down"][l, e, :, :].rearrange("(f p) n -> p f n", p=128),
                          writes=[bsd])

                def cast_expert(e):
                    if e >= NE:
                        return
                    w = e % 2
                    P.op("act", lambda E: E.copy(wg[w][:], stg_g[:]), reads=[bsg], writes=[bwg[w]])
                    P.op("pool", lambda E: E.tensor_copy(wu[w][:], stg_u[:]), reads=[bsu], writes=[bwu[w]])
                    P.op("pool", lambda E: E.tensor_copy(wd[w][:], stg_d[:]), reads=[bsd], writes=[bwd[w]])
                    P.dma("sp", wb[w][:], scr[e:e + 1, :].partition_broadcast(128), reads=[bscr], writes=[bwb[w]])

                units = [(e, tb) for e in range(NE) for tb in range(NTB)]

                def gu(i):
                    e, tb = units[i]
                    w = e % 2; q = i % 2
                    sl = slice(tb * TB, (tb + 1) * TB)
                    for f in range(2):
                        for kt in range(8):
                            P.op("pe", lambda E: E.matmul(eg[q][:, f, :], wg[w][:, kt, f * 128:(f + 1) * 128],
                                                          hT[:, kt, sl], start=(kt == 0), stop=(kt == 7)),
                                 reads=[bwg[w], bh[kt]], writes=[beg[q]])
                    for f in range(2):
                        for kt in range(8):
                            P.op("pe", lambda E: E.matmul(eu[q][:, f, :], wu[w][:, kt, f * 128:(f + 1) * 128],
                                                          hT[:, kt, sl], start=(kt == 0), stop=(kt == 7)),
                                 reads=[bwu[w], bh[kt]], writes=[beu[q]])

                def acc(i):
                    e, tb = units[i]
                    sl = slice(tb * TB, (tb + 1) * TB)
                    for m in range(8):
                        P.op("dve", lambda E: E.scalar_tensor_tensor(xT[:, m, sl], ey[:, m, :], modc(l, 5, m, s),
                                                                     xT[:, m, sl], ALU.mult, ALU.add),
                             reads=[beyb[m // 2], bmod, bx[m]], writes=[bx[m]])

                def elem(i):
                    e, tb = units[i]
                    w = e % 2; q = i % 2
                    sl = slice(tb * TB, (tb + 1) * TB)
                    P.op("act", lambda E: E.activation(ea[q][:], eg[q][:], AF.Silu), reads=[beg[q]], writes=[bea[q]])
                    P.op("dve", lambda E: E.tensor_tensor(ea[q][:], ea[q][:], eu[q][:], ALU.mult),
                         reads=[bea[q], beu[q]], writes=[bea[q]])
                    P.op("dve", lambda E: E.tensor_tensor(eb[q][:], ea[q][:],
                                                          wb[w][:, sl].unsqueeze(1).to_broadcast([128, 2, TB]),
                                                          ALU.mult), reads=[bea[q], bwb[w]], writes=[beb[q]])

                def down(i):
                    e, tb = units[i]
                    w = e % 2; q = i % 2
                    for m in range(8):
                        for f in range(2):
                            P.op("pe", lambda E: E.matmul(ey[:, m, :], wd[w][:, f, m * 128:(m + 1) * 128],
                                                          eb[q][:, f, :], start=(f == 0), stop=(f == 1)),
                                 reads=[bwd[w], beb[q]], writes=[beyb[m // 2]])

                dma_expert(0); cast_expert(0); dma_expert(1); cast_expert(1); dma_expert(2)
                if units:
                    gu(0)
                    elem(0)
                for i in range(len(units)):
                    if i + 1 < len(units):
                        gu(i + 1)
                    down(i)
                    if i + 1 < len(units):
                        elem(i + 1)
                    acc(i)
                    e, tb = units[i]
                    if tb == NTB - 1:
                        cast_expert(e + 2)
                        dma_expert(e + 3)
            P.barrier()

        STAGES = dbg.get("stages", "mgpoLME") if dbg else "mgpoLME"
        for s in range(NSEQ):
            for kt in range(8):
                P.dma("sp", xT[:, kt, :], dr["xT"][s, kt * 128:(kt + 1) * 128, :], writes=[bx[kt]])
            for l in range(DEPTH):
                modulate(l, s, 0, 1)
                if "z" in STAGES:
                    for kt in range(8):
                        P.op("pool", lambda E: E.memset(mixT[:, kt, :], 0.0), writes=[bmix[kt]])
                if "m" in STAGES:
                    mla_stage(l, s)
                if "g" in STAGES:
                    gdn_stage(l, s)
                if "p" in STAGES:
                    pool_stage(l, s)
                if dbg and dbg.get("dump") == "mix" and s == dbg.get("s", 0) and l == dbg.get("l", 0):
                    with contextlib.ExitStack() as _S:
                        dmp = _S.enter_context(sb("dmp", [128, 8, T]))
                        bd = Buf()
                        for kt in range(8):
                            P.op("dve", lambda E: E.tensor_copy(dmp[:, kt, :], (hT if dbg.get("src") == "h" else mixT)[:, kt, :]), reads=[bmix[kt], bh[kt]],
                                 writes=[bd])
                        P.dma("sp", dbg_out.rearrange("(k p) t -> p k t", p=128), dmp[:], reads=[bd])
                        P.barrier()
                if "o" in STAGES:
                    out_proj(l, s)
                if "L" in STAGES:
                    layer_norm(l, K_LN1G, K_LN1B)
                if "M" in STAGES:
                    moe_stage(l, s)
                if "E" in STAGES:
                    layer_norm(l, K_LN2G, K_LN2B)
            for kt in range(8):
                P.dma("sp", yT[s, kt * 128:(kt + 1) * 128, :], xT[:, kt, :], reads=[bx[kt]])
        P.barrier()
    P.es.close()
    return nc


def make_consts():
    c = np.zeros((128, NCON), np.float32)
    c[:, C_ID:C_ID + 128] = np.eye(128, dtype=np.float32)
    c[:, C_ONE:C_ONE + 128] = 1.0
    i = np.arange(64)
    c[:64, C_U:C_U + 64] = (i[:, None] <= i[None, :]).astype(np.float32)
    c[:64, C_MLO:C_MLO + 64] = np.where(i[:, None] > i[None, :], 0.0, NEG)
    c[:64, C_MUP:C_MUP + 64] = np.where(i[None, :] >= i[:, None], 0.0, NEG)
    t = np.arange(16)
    for j in range(2):
        for p in range(128):
            w = 2 ** (2 * j + p // 64 + 1)
            c[p, C_INV16 + 16 * j:C_INV16 + 16 * j + 16] = 1.0 / np.minimum(t + 1, w)
    p = np.arange(128)
    c[:, C_BD:C_BD + 128] = (p[:, None] // 64 == p[None, :] // 64).astype(np.float32)
    return c


def make_rope(T):
    inv_freq = np.power(np.float32(10000.0), -np.arange(0, 32, 2, dtype=np.float32) / np.float32(32)).astype(np.float32)
    ang = (np.arange(T, dtype=np.float32)[:, None] * inv_freq[None, :]).astype(np.float32)
    cos, sin = np.cos(ang).astype(np.float32).T, np.sin(ang).astype(np.float32).T
    r = np.zeros((32, 2, T), np.float32)
    r[:16, 0], r[16:, 0], r[:16, 1], r[16:, 1] = cos, cos, sin, sin
    return r


def make_cols(inp):
    L = inp["w_in"].shape[0]
    cols = np.zeros((L, 128, NCOLS), np.float32)

    def put(k, vec, n):
        cols[:, :, k:k + n] = vec.reshape(L, n, 128).transpose(0, 2, 1)

    put(K_QN, inp["mla_q_norm"], 2); put(K_KVN, inp["mla_kv_norm"], 1)
    put(K_LN1G, inp["ln1_g"], 8); put(K_LN1B, inp["ln1_b"], 8); put(K_LN2G, inp["ln2_g"], 8); put(K_LN2B, inp["ln2_b"], 8)
    put(K_PS, inp["pool_scale"], 2)
    cols[:, :, K_GO] = np.concatenate([inp["gdn_out_norm"], inp["gdn_out_norm"]], axis=1)
    cv = inp["gdn_conv"].reshape(L, 4, 6, 128)
    cols[:, :, K_CONV:K_CONV + 24] = cv.transpose(0, 3, 2, 1).reshape(L, 128, 24)
    put(K_BMOD, inp["b_mod"], 48)
    return cols


_NC_CACHE = {}


def run(inp, T, DEPTH, NSEQ, ncores, dbg=None):
    key = (T, DEPTH, NSEQ, repr(dbg))
    if key not in _NC_CACHE:
        _NC_CACHE[key] = build(T, DEPTH, NSEQ, dbg)
    nc = _NC_CACHE[key]
    f = lambda a: np.ascontiguousarray(np.asarray(a, dtype=np.float32))
    shared = {
        "consts": make_consts(), "rope": make_rope(T), "cols": f(make_cols(inp)),
        "w_in": f(inp["w_in"]), "mla_w_uq": f(inp["mla_w_uq"]), "mla_w_ukv": f(inp["mla_w_ukv"]),
        "gdn_a_log": f(inp["gdn_a_log"]), "gdn_dt_bias": f(inp["gdn_dt_bias"]), "pool_w": f(inp["pool_w"]),
        "w_out": f(inp["w_out"]), "w_mod": f(inp["w_mod"]),
        "w_r": f(np.concatenate([inp["router_w_group"], inp["router_w_expert"]], axis=2)),
        "b_r": f(np.concatenate([inp["router_b_group"], inp["router_b_expert"]], axis=1)),
        "moe_w_gate": f(inp["moe_w_gate"]), "moe_w_up": f(inp["moe_w_up"]), "moe_w_down": f(inp["moe_w_down"]),
    }
    x = np.asarray(inp["x"], np.float32); c = np.asarray(inp["c"], np.float32)
    in_maps = []
    for i in range(ncores):
        m = dict(shared)
        m["xT"] = f(x[i * NSEQ:(i + 1) * NSEQ].transpose(0, 2, 1))
        m["cT"] = f(c[i * NSEQ:(i + 1) * NSEQ].T)
        in_maps.append(m)
    res = run_bass_kernel_spmd(nc, in_maps, core_ids=list(range(ncores)))
    out = np.concatenate([r["yT"].transpose(0, 2, 1) for r in res.results], axis=0)
    return out, res


def kernel(**inputs):
    out, _ = run(inputs, 2048, 4, 2, 8)
    return out.astype(np.float32)
```
